# Optimizing a Trainium2 kernel written in Bass

```python
import math
import jax, jax.numpy as jnp
from jax import lax
import numpy as np

D_MODEL = 1024
BATCH = 8
SEQ = 4096
DEPTH = 2

GRID_W = 64
CTX_LEN = 256
CONV_W = 3

HY_WIDTH = 256
HY_BANDS = 16
HY_EMB = 1 + 2 * HY_BANDS
HY_HIDDEN = 64

SC_WIDTH = 256

HG_HEADS = 4
HG_DK = 64
HG_DV = 64
HG_KEY = HG_HEADS * HG_DK
HG_VAL = HG_HEADS * HG_DV
HG_CHUNK = 16

SSD_HEADS = 4
SSD_HEADDIM = 64
SSD_INNER = SSD_HEADS * SSD_HEADDIM
SSD_STATE = 64
SSD_GROUPS = 2
SSD_CHUNK = 64
SSD_XBC = SSD_INNER + 2 * SSD_GROUPS * SSD_STATE

N_BRANCH = 4
BRANCH_W = 256

HY_COLS = 3 * HY_WIDTH
SC_COLS = 3 * SC_WIDTH
HG_COLS = 3 * HG_KEY + 2 * HG_VAL
SSD_COLS = SSD_INNER + SSD_XBC + 2 * SSD_HEADS
IN_COLS = HY_COLS + SC_COLS + HG_COLS + SSD_COLS
IN_SPLITS = (HY_COLS, HY_COLS + SC_COLS, HY_COLS + SC_COLS + HG_COLS)

N_EXPERTS = 16
N_EXPERT_GROUPS = 4
TOP_K = 2
D_FF_EXPERT = 512

DEEPNORM_ALPHA = (2 * DEPTH) ** 0.25
DEEPNORM_BETA = (8 * DEPTH) ** -0.25
LN_EPS = 1e-5
RMS_EPS = 1e-6

kernel_name = 'hybrid_dit_hyena_conv_hgrn2_ssd_moe'


def _layernorm(x, g, b):
    xf = x.astype(jnp.float32)
    mu = jnp.mean(xf, -1, keepdims=True)
    var = jnp.mean(jnp.square(xf - mu), -1, keepdims=True)
    return ((xf - mu) * lax.rsqrt(var + LN_EPS) * g + b).astype(x.dtype)


def _rmsnorm(x, g):
    xf = x.astype(jnp.float32)
    return (xf * lax.rsqrt(jnp.mean(jnp.square(xf), -1, keepdims=True) + RMS_EPS) * g).astype(x.dtype)


def _dwconv(u, w, b=None):
    L = u.shape[1]
    pad = CONV_W // 2
    up = jnp.pad(u, ((0, 0), (pad, pad), (0, 0)))
    y = sum(up[:, k:k + L] * w[k] for k in range(CONV_W))
    return y if b is None else y + b


def _sincos_1d(pos, dim):
    omega = 1.0 / (10000.0 ** (jnp.arange(dim // 2, dtype=jnp.float32) / (dim // 2)))
    ang = pos.astype(jnp.float32)[:, None] * omega[None]
    return jnp.concatenate([jnp.sin(ang), jnp.cos(ang)], -1)


def _grid_pos_embed(rows):
    row = jnp.repeat(jnp.arange(rows), GRID_W)
    col = jnp.tile(jnp.arange(GRID_W), rows)
    return jnp.concatenate([_sincos_1d(row, D_MODEL // 2), _sincos_1d(col, D_MODEL // 2)], -1)


def _hyena_filter(L, lp):
    t = jnp.linspace(0.0, 1.0, L, dtype=jnp.float32)[:, None]
    bands = jnp.linspace(1e-4, HY_BANDS - 1, HY_BANDS, dtype=jnp.float32)
    ang = (2.0 * math.pi / L) * jnp.arange(L, dtype=jnp.float32)[:, None] * bands[None]
    feat = jnp.concatenate([t, jnp.cos(ang), -jnp.sin(ang)], -1)
    h = jnp.sin(lp['hy_freq1'] * (feat @ lp['hy_w1'] + lp['hy_b1']))
    h = jnp.sin(lp['hy_freq2'] * (h @ lp['hy_w2'] + lp['hy_b2']))
    h = (h @ lp['hy_w3']).astype(jnp.float32)
    window = jnp.exp(-t * jnp.abs(lp['hy_decay'].astype(jnp.float32)))
    h_fwd = h[:, :HY_WIDTH] * window
    h_bwd = h[:, HY_WIDTH:] * window
    filt = jnp.concatenate([h_fwd, jnp.zeros((1, HY_WIDTH), jnp.float32), h_bwd[:0:-1]], 0)
    return filt / jnp.sum(jnp.abs(filt), 0, keepdims=True)


def _fftconv(u, filt, bias):
    L = u.shape[1]
    uf = u.astype(jnp.float32)
    spec = jnp.fft.rfft(uf, n=2 * L, axis=1) * jnp.fft.rfft(filt, n=2 * L, axis=0)[None]
    y = jnp.fft.irfft(spec, n=2 * L, axis=1)[:, :L]
    return (y + uf * bias.astype(jnp.float32)).astype(u.dtype)


def _hyena_branch(u, lp):
    u = _dwconv(u, lp['hy_conv_w'], lp['hy_conv_b'])
    x0, x1, v = jnp.split(u, 3, -1)
    filt = _hyena_filter(u.shape[1], lp)
    return x0 * _fftconv(x1 * v, filt, lp['hy_bias'])


def _shortconv_branch(u, conv_w):
    bg, cg, xs = jnp.split(u, 3, -1)
    return bg * _dwconv(cg * xs, conv_w)


def _gla_chunked(q, k, v, logf, s0):
    Bsz, L, H, DK = q.shape
    DV = v.shape[-1]
    n = L // HG_CHUNK
    blk = lambda t: t.astype(jnp.float32).reshape(Bsz, n, HG_CHUNK, H, t.shape[-1])
    q, k, v, logf = blk(q), blk(k), blk(v), blk(logf)
    b = jnp.cumsum(logf, axis=2)
    b_last = b[:, :, -1]
    q_dec = q * jnp.exp(b)
    k_inv = k * jnp.exp(-b)
    k_end = k * jnp.exp(b_last[:, :, None] - b)
    causal = jnp.tril(jnp.ones((HG_CHUNK, HG_CHUNK), bool))
    scores = jnp.where(causal, jnp.einsum('bnthd,bnshd->bnhts', q_dec, k_inv), 0.0)
    o_intra = jnp.einsum('bnhts,bnshv->bnthv', scores, v)

    def step(S, inp):
        qd, ke, vc, bl = inp
        o = jnp.einsum('bthd,bhdv->bthv', qd, S)
        S = jnp.exp(bl)[..., None] * S + jnp.einsum('bshd,bshv->bhdv', ke, vc)
        return S, o

    mv = lambda t: jnp.moveaxis(t, 1, 0)
    s_final, o_inter = lax.scan(step, s0, (mv(q_dec), mv(k_end), mv(v), mv(b_last)))
    o = o_intra + jnp.moveaxis(o_inter, 0, 1)
    return o.reshape(Bsz, L, H, DV), s_final


def _hgrn_lower_bounds(logits):
    p = jax.nn.softmax(logits.astype(jnp.float32), axis=1)
    return jnp.cumsum(p, axis=1) - p[:, :1]


def _hgrn_prep(u, lb):
    Bsz, L = u.shape[:2]
    q, f_f, f_b, i, g = jnp.split(u, (HG_KEY, 2 * HG_KEY, 3 * HG_KEY, 3 * HG_KEY + HG_VAL), -1)
    heads = lambda t: t.reshape(Bsz, L, HG_HEADS, -1)
    logf = [heads(jnp.log(lb[d] + (1.0 - lb[d]) * jax.nn.sigmoid(f.astype(jnp.float32))))
            for d, f in enumerate((f_f, f_b))]
    return heads(q.astype(jnp.float32)) * HG_DK ** -0.5, logf, heads(i), g


def _hgrn_bidir(q, logf, v, s_f, s_b):
    flip = lambda t: jnp.flip(t, axis=1)
    o_f, s_f = _gla_chunked(q, -jnp.expm1(logf[0]), v, logf[0], s_f)
    o_b, s_b = _gla_chunked(flip(q), flip(-jnp.expm1(logf[1])), flip(v), flip(logf[1]), s_b)
    return o_f + flip(o_b), s_f, s_b


def _hgrn_readout(o, g, norm_g):
    o = _rmsnorm(o, norm_g).reshape(g.shape)
    return (o * jax.nn.silu(g.astype(jnp.float32))).astype(g.dtype)


def _hgrn_branch(u_ctx, u_lat, lb, norm_g, ctx_out):
    s0 = jnp.zeros((u_lat.shape[0], HG_HEADS, HG_DK, HG_DV), jnp.float32)
    q_c, lf_c, v_c, g_c = _hgrn_prep(u_ctx, lb)
    q_l, lf_l, v_l, g_l = _hgrn_prep(u_lat, lb)
    o_c, s_f, s_b = _hgrn_bidir(q_c, lf_c, v_c, s0, s0)
    o_l, _, _ = _hgrn_bidir(q_l, lf_l, v_l, s_f, s_b)
    y_l = _hgrn_readout(o_l, g_l, norm_g)
    y_c = _hgrn_readout(o_c, g_c, norm_g) if ctx_out else None
    return y_c, y_l


def _ssd_chunked(x, a, bm, cm, s0):
    Bsz, L, H, P = x.shape
    n = L // SSD_CHUNK
    blk = lambda t: t.astype(jnp.float32).reshape((Bsz, n, SSD_CHUNK) + t.shape[2:])
    x, a, bm, cm = blk(x), blk(a), blk(bm), blk(cm)
    cum = jnp.cumsum(a, axis=2)
    causal = jnp.tril(jnp.ones((SSD_CHUNK, SSD_CHUNK), bool))[:, :, None]
    decay = jnp.exp(jnp.where(causal, cum[:, :, :, None, :] - cum[:, :, None, :, :], -jnp.inf))
    scores = jnp.einsum('bcthn,bcshn->bctsh', cm, bm) * decay
    y_diag = jnp.einsum('bctsh,bcshp->bcthp', scores, x)
    states = jnp.einsum('bcshn,bcshp->bchpn', bm * jnp.exp(cum[:, :, -1:] - cum)[..., None], x)
    c_dec = cm * jnp.exp(cum)[..., None]
    total = jnp.exp(cum[:, :, -1])

    def step(S, inp):
        cd, st, tot = inp
        y = jnp.einsum('bthn,bhpn->bthp', cd, S)
        return tot[:, :, None, None] * S + st, y

    mv = lambda t: jnp.moveaxis(t, 1, 0)
    s_final, y_off = lax.scan(step, s0, (mv(c_dec), mv(states), mv(total)))
    y = y_diag + jnp.moveaxis(y_off, 0, 1)
    return y.reshape(Bsz, L, H, P), s_final


def _ssd_prep(u, lp):
    Bsz, L = u.shape[:2]
    z, xbc, dt = jnp.split(u, (SSD_INNER, SSD_INNER + SSD_XBC), -1)
    xbc = jax.nn.silu(_dwconv(xbc, lp['ssd_conv_w'], lp['ssd_conv_b']))
    xs, bm, cm = jnp.split(xbc, (SSD_INNER, SSD_INNER + SSD_GROUPS * SSD_STATE), -1)
    rep = SSD_HEADS // SSD_GROUPS
    xs = xs.reshape(Bsz, L, SSD_HEADS, SSD_HEADDIM)
    bm = jnp.repeat(bm.reshape(Bsz, L, SSD_GROUPS, SSD_STATE), rep, axis=2)
    cm = jnp.repeat(cm.reshape(Bsz, L, SSD_GROUPS, SSD_STATE), rep, axis=2)
    dt = jax.nn.softplus(dt.astype(jnp.float32).reshape(Bsz, L, 2, SSD_HEADS) + lp['ssd_dt_bias'])
    a = -jnp.exp(lp['ssd_a_log'].astype(jnp.float32)) * dt
    return z, xs, bm, cm, dt, a


def _ssd_bidir(xs, bm, cm, dt, a, d_skip, s_f, s_b):
    flip = lambda t: jnp.flip(t, axis=1)
    xf = xs.astype(jnp.float32)
    y_f, s_f = _ssd_chunked(xf * dt[:, :, 0, :, None], a[:, :, 0], bm, cm, s_f)
    y_b, s_b = _ssd_chunked(flip(xf * dt[:, :, 1, :, None]), flip(a[:, :, 1]), flip(bm), flip(cm), s_b)
    y = y_f + flip(y_b) + xf * d_skip.astype(jnp.float32)[:, None]
    return y, s_f, s_b


def _ssd_readout(y, z, norm_g):
    y = y.reshape(z.shape)
    return _rmsnorm(y * jax.nn.silu(z.astype(jnp.float32)), norm_g).astype(z.dtype)


def _ssd_branch(u_ctx, u_lat, lp, ctx_out):
    s0 = jnp.zeros((u_lat.shape[0], SSD_HEADS, SSD_HEADDIM, SSD_STATE), jnp.float32)
    z_c, *rest_c = _ssd_prep(u_ctx, lp)
    z_l, *rest_l = _ssd_prep(u_lat, lp)
    y_c, s_f, s_b = _ssd_bidir(*rest_c, lp['ssd_d'], s0, s0)
    y_l, _, _ = _ssd_bidir(*rest_l, lp['ssd_d'], s_f, s_b)
    out_l = _ssd_readout(y_l, z_l, lp['ssd_norm_g'])
    out_c = _ssd_readout(y_c, z_c, lp['ssd_norm_g']) if ctx_out else None
    return out_c, out_l


def _merge(h, branches, lp):
    gates = jnp.split(jax.nn.sigmoid(h @ lp['w_gate'] + lp['b_gate']), N_BRANCH, -1)
    y = sum(g * (br @ lp['w_br'][k]) for k, (g, br) in enumerate(zip(gates, branches)))
    return y @ lp['w_o']


def _token_mixer(h_lat, h_ctx, lp, lb, ctx_out):
    u_lat = h_lat @ lp['w_in']
    u_ctx = h_ctx @ lp['w_in']
    hy_l, sc_l, hg_l, ssd_l = jnp.split(u_lat, IN_SPLITS, -1)
    hy_c, sc_c, hg_c, ssd_c = jnp.split(u_ctx, IN_SPLITS, -1)
    hg_y_c, hg_y_l = _hgrn_branch(hg_c, hg_l, lb, lp['hg_norm_g'], ctx_out)
    ssd_y_c, ssd_y_l = _ssd_branch(ssd_c, ssd_l, lp, ctx_out)
    y_lat = _merge(h_lat, (_hyena_branch(hy_l, lp), _shortconv_branch(sc_l, lp['sc_conv_w']), hg_y_l, ssd_y_l), lp)
    y_ctx = None
    if ctx_out:
        y_ctx = _merge(h_ctx, (_hyena_branch(hy_c, lp), _shortconv_branch(sc_c, lp['sc_conv_w']), hg_y_c, ssd_y_c), lp)
    return y_lat, y_ctx


def _moe(h, w_router, b_router, w1, w3, w2):
    scores = jax.nn.softmax((h @ w_router).astype(jnp.float32), -1)
    sel = scores + b_router.astype(jnp.float32)
    grp = sel.reshape(sel.shape[:-1] + (N_EXPERT_GROUPS, N_EXPERTS // N_EXPERT_GROUPS))
    grp_score = jnp.sum(lax.top_k(grp, TOP_K)[0], -1)
    in_grp = jax.nn.one_hot(jnp.argmax(grp_score, -1), N_EXPERT_GROUPS, dtype=bool)[..., None]
    masked = jnp.where(in_grp, grp, -jnp.inf).reshape(sel.shape)
    _, idx = lax.top_k(masked, TOP_K)
    w = jnp.take_along_axis(scores, idx, -1)
    w = w / jnp.sum(w, -1, keepdims=True)
    gate = jnp.sum(jax.nn.one_hot(idx, N_EXPERTS, dtype=jnp.float32) * w[..., None], -2).astype(h.dtype)
    out = 0
    for e in range(N_EXPERTS):
        ye = (jax.nn.silu(h @ w1[e]) * (h @ w3[e])) @ w2[e]
        out = out + gate[..., e:e + 1] * ye
    return out


def setup_inputs(seed: int = 0) -> dict:
    key = jax.random.key(seed)
    ks = iter(jax.random.split(key, 48))
    nrm = lambda shape, s: s * jax.random.normal(next(ks), shape, jnp.float32)
    D = D_MODEL
    dt0 = jnp.exp(jax.random.uniform(next(ks), (DEPTH, 2, SSD_HEADS), jnp.float32, math.log(1e-3), math.log(1e-1)))
    decay0 = jnp.abs(jnp.linspace(math.log(1e-2) / 0.3, math.log(1e-2) / 1.5, HY_WIDTH, dtype=jnp.float32))
    return {
        'x': nrm((BATCH, SEQ, D), 1.0),
        'c': nrm((BATCH, D), 1.0),
        'ctx': nrm((BATCH, CTX_LEN, D), 1.0),
        'c_ctx': nrm((D,), 1.0),
        'w_ada': nrm((DEPTH, D, 6 * D), 0.5 * D ** -0.5),
        'b_ada': nrm((DEPTH, 6 * D), 0.02),
        'w_in': nrm((DEPTH, D, IN_COLS), D ** -0.5),
        'hy_conv_w': nrm((DEPTH, CONV_W, HY_COLS), CONV_W ** -0.5),
        'hy_conv_b': nrm((DEPTH, HY_COLS), 0.02),
        'hy_w1': nrm((DEPTH, HY_EMB, HY_HIDDEN), HY_EMB ** -0.5),
        'hy_b1': nrm((DEPTH, HY_HIDDEN), 0.1),
        'hy_freq1': 1.0 + nrm((DEPTH, HY_HIDDEN), 0.1),
        'hy_w2': nrm((DEPTH, HY_HIDDEN, HY_HIDDEN), HY_HIDDEN ** -0.5),
        'hy_b2': nrm((DEPTH, HY_HIDDEN), 0.1),
        'hy_freq2': 1.0 + nrm((DEPTH, HY_HIDDEN), 0.1),
        'hy_w3': nrm((DEPTH, HY_HIDDEN, 2 * HY_WIDTH), HY_HIDDEN ** -0.5),
        'hy_decay': decay0[None] * (1.0 + nrm((DEPTH, HY_WIDTH), 0.05)),
        'hy_bias': nrm((DEPTH, HY_WIDTH), 0.1),
        'sc_conv_w': nrm((DEPTH, CONV_W, SC_WIDTH), CONV_W ** -0.5),
        'hg_lb_logits': nrm((2, DEPTH, HG_KEY), 0.5),
        'hg_norm_g': 1.0 + nrm((DEPTH, HG_DV), 0.02),
        'ssd_conv_w': nrm((DEPTH, CONV_W, SSD_XBC), CONV_W ** -0.5),
        'ssd_conv_b': nrm((DEPTH, SSD_XBC), 0.02),
        'ssd_a_log': jnp.log(jax.random.uniform(next(ks), (DEPTH, 2, SSD_HEADS), jnp.float32, 1.0, 16.0)),
        'ssd_dt_bias': dt0 + jnp.log(-jnp.expm1(-dt0)),
        'ssd_d': 1.0 + nrm((DEPTH, SSD_HEADS), 0.1),
        'ssd_norm_g': 1.0 + nrm((DEPTH, SSD_INNER), 0.02),
        'w_gate': nrm((DEPTH, D, N_BRANCH * D), D ** -0.5),
        'b_gate': nrm((DEPTH, N_BRANCH * D), 0.02),
        'w_br': nrm((DEPTH, N_BRANCH, BRANCH_W, D), BRANCH_W ** -0.5),
        'w_o': nrm((DEPTH, D, D), DEEPNORM_BETA * D ** -0.5),
        'ln1_g': 1.0 + nrm((DEPTH, D), 0.02),
        'ln1_b': nrm((DEPTH, D), 0.02),
        'ln2_g': 1.0 + nrm((DEPTH, D), 0.02),
        'ln2_b': nrm((DEPTH, D), 0.02),
        'w_router': nrm((D, N_EXPERTS), D ** -0.5),
        'b_router': nrm((N_EXPERTS,), 0.01),
        'w_e1': nrm((DEPTH, N_EXPERTS, D, D_FF_EXPERT), D ** -0.5),
        'w_e3': nrm((DEPTH, N_EXPERTS, D, D_FF_EXPERT), D ** -0.5),
        'w_e2': nrm((DEPTH, N_EXPERTS, D_FF_EXPERT, D), DEEPNORM_BETA * D_FF_EXPERT ** -0.5),
    }


def reference(x, c, ctx, c_ctx, w_ada, b_ada, w_in, hy_conv_w, hy_conv_b, hy_w1, hy_b1, hy_freq1,
              hy_w2, hy_b2, hy_freq2, hy_w3, hy_decay, hy_bias, sc_conv_w, hg_lb_logits, hg_norm_g,
              ssd_conv_w, ssd_conv_b, ssd_a_log, ssd_dt_bias, ssd_d, ssd_norm_g, w_gate, b_gate, w_br,
              w_o, ln1_g, ln1_b, ln2_g, ln2_b, w_router, b_router, w_e1, w_e3, w_e2):
    ROWS = x.shape[1] // GRID_W
    lat = x + _grid_pos_embed(ROWS).astype(x.dtype)[None]
    cx = ctx
    lower_bounds = _hgrn_lower_bounds(hg_lb_logits)
    for l in range(DEPTH):
        ctx_out = l < DEPTH - 1
        lp = dict(w_in=w_in[l], hy_conv_w=hy_conv_w[l], hy_conv_b=hy_conv_b[l], hy_w1=hy_w1[l],
                  hy_b1=hy_b1[l], hy_freq1=hy_freq1[l], hy_w2=hy_w2[l], hy_b2=hy_b2[l],
                  hy_freq2=hy_freq2[l], hy_w3=hy_w3[l], hy_decay=hy_decay[l], hy_bias=hy_bias[l],
                  sc_conv_w=sc_conv_w[l], hg_norm_g=hg_norm_g[l], ssd_conv_w=ssd_conv_w[l],
                  ssd_conv_b=ssd_conv_b[l], ssd_a_log=ssd_a_log[l], ssd_dt_bias=ssd_dt_bias[l],
                  ssd_d=ssd_d[l], ssd_norm_g=ssd_norm_g[l], w_gate=w_gate[l], b_gate=b_gate[l],
                  w_br=w_br[l], w_o=w_o[l])
        mod = jax.nn.silu(c) @ w_ada[l] + b_ada[l]
        mod_c = jax.nn.silu(c_ctx) @ w_ada[l] + b_ada[l]
        sh1, sc1, g1, sh2, sc2, g2 = jnp.split(mod[:, None, :], 6, -1)
        csh1, csc1, cg1, csh2, csc2, cg2 = jnp.split(mod_c, 6, -1)
        y_lat, y_ctx = _token_mixer(lat * (1 + sc1) + sh1, cx * (1 + csc1) + csh1, lp, lower_bounds[:, l], ctx_out)
        moe = lambda h: _moe(h, w_router, b_router, w_e1[l], w_e3[l], w_e2[l])
        lat = _layernorm(DEEPNORM_ALPHA * lat + g1 * y_lat, ln1_g[l], ln1_b[l])
        lat = _layernorm(DEEPNORM_ALPHA * lat + g2 * moe(lat * (1 + sc2) + sh2), ln2_g[l], ln2_b[l])
        if ctx_out:
            cx = _layernorm(DEEPNORM_ALPHA * cx + cg1 * y_ctx, ln1_g[l], ln1_b[l])
            cx = _layernorm(DEEPNORM_ALPHA * cx + cg2 * moe(cx * (1 + csc2) + csh2), ln2_g[l], ln2_b[l])
    return lat
```

```python
import math
import numpy as np
import ml_dtypes
import concourse.bass as bass
import concourse.mybir as mybir
from concourse.bass_utils import run_bass_kernel_spmd

F32 = mybir.dt.float32
BF16 = mybir.dt.bfloat16
AF = mybir.ActivationFunctionType
ALU = mybir.AluOpType
AX = mybir.AxisListType

D = 1024
L = 4096
LC = 256
NT = L + LC
KC = 8
DEPTH = 2
IN_COLS = 3592
UROWS = 29 * 128
NEG = -30000.0
ALPHA = (2 * DEPTH) ** 0.25
LN_EPS = 1e-5
RMS_EPS = 1e-6
NE = 16
DFF = 512
SEM_LIMIT = 30000


class Buf:
    def __init__(self, name, t):
        self.name = name
        self.t = t
        self.st = {}

    def __getitem__(self, idx):
        return View(self, self.t[idx], None)

    def k(self, key):
        return View(self, self.t[:], key)


class View:
    def __init__(self, buf, ap, key):
        self.buf = buf
        self.ap = ap
        self.key = key

    def __getitem__(self, idx):
        return View(self.buf, self.ap[idx], self.key)

    def k(self, key):
        return View(self.buf, self.ap, key)

    def r(self, pat, **kw):
        return View(self.buf, self.ap.rearrange(pat, **kw), self.key)

    def bc(self, shape):
        return View(self.buf, self.ap.to_broadcast(list(shape)), self.key)

    def un(self, axis):
        return View(self.buf, self.ap.unsqueeze(axis), self.key)


def _ap(x):
    return x.ap if isinstance(x, View) else x


class FW:
    NDMA = 8
    LIMIT = 10 ** 9
    POOL_TO = "dve"

    def __init__(self, nc):
        self.nc = nc
        self.eng = {"pe": nc.tensor, "dve": nc.vector, "act": nc.scalar,
                    "pool": nc.gpsimd, "sp": nc.sync}
        self.sem = {}
        self.cnt = {}
        self.seen = {}
        self.last = {}
        self._guards = []
        self._semguards = []
        self.bufs = []
        self._nsem = 0
        for e in self.eng:
            self.sem[e] = self._newsem("s_" + e)
            self.cnt[e] = 0
            self.seen[e] = {}
            self.last[e] = []
        self.dsem = {}
        self.dcnt = {}
        self.dlast = {}
        self.drr = {}
        for q in ("sp", "act", "pool"):
            self.dsem[q] = [self._newsem("d_%s%d" % (q, i)) for i in range(self.NDMA)]
            self.dcnt[q] = [0] * self.NDMA
            self.dlast[q] = [None] * self.NDMA
            self.drr[q] = 0
        self.n_inst = 0
        self.n_wait = 0

    def _newsem(self, name):
        self._nsem += 1
        g = self.nc.semaphore("%s_%d" % (name, self._nsem))
        s = g.__enter__()
        self._semguards.append(g)
        return s

    def sb(self, name, shape, dt=F32):
        self._nalloc = getattr(self, "_nalloc", 0) + 1
        g = self.nc.sbuf_tensor("sb%d_%s" % (self._nalloc, name), list(shape), dt)
        t = g.__enter__()
        self._guards.append(g)
        b = Buf(name, t)
        self.bufs.append(b)
        return b

    def ps(self, name, shape, dt=F32):
        g = self.nc.psum_tensor("pp_" + name, list(shape), dt)
        t = g.__enter__()
        self._guards.append(g)
        b = Buf(name, t)
        self.bufs.append(b)
        return b

    def dram(self, name, shape, dt=F32, kind="Internal"):
        t = self.nc.dram_tensor(name, list(shape), dt, kind=kind)
        b = Buf(name, t.ap())
        self.bufs.append(b)
        return b

    def mark(self):
        return len(self._guards)

    def release(self, mark):
        self.barrier()
        while len(self._guards) > mark:
            g = self._guards.pop()
            g.__exit__(None, None, None)

    def close(self):
        while self._guards:
            g = self._guards.pop()
            g.__exit__(None, None, None)
        while self._semguards:
            g = self._semguards.pop()
            g.__exit__(None, None, None)

    def _deps(self, reads, writes):
        deps = []
        for a in reads:
            b, k = a.buf, a.key
            for kk, s in b.st.items():
                if k is None or kk is None or kk == k:
                    if s[0] is not None:
                        deps.append(s[0])
        for a in writes:
            b, k = a.buf, a.key
            for kk, s in b.st.items():
                if k is None or kk is None or kk == k:
                    if s[0] is not None:
                        deps.append(s[0])
                    deps.extend(s[1])
        return deps

    def _record(self, reads, writes, tok):
        for a in reads:
            s = a.buf.st.setdefault(a.key, [None, []])
            s[1].append(tok)
            if len(s[1]) > 48:
                m = {}
                for (sm, v) in s[1]:
                    if id(sm) not in m or m[id(sm)][1] < v:
                        m[id(sm)] = (sm, v)
                s[1] = list(m.values())
        for a in writes:
            if a.key is None:
                a.buf.st = {None: [tok, []]}
            else:
                a.buf.st[a.key] = [tok, []]

    def _emit_waits(self, e, deps, same_engine=True):
        engine = self.eng[e]
        seen = self.seen[e]
        need = {}
        for (sm, v) in deps:
            if (not same_engine) and sm is self.sem[e]:
                continue
            if seen.get(id(sm), 0) >= v:
                continue
            if id(sm) not in need or need[id(sm)][1] < v:
                need[id(sm)] = (sm, v)
        for (sm, v) in need.values():
            engine.wait_ge(sm, v)
            seen[id(sm)] = v
            self.n_wait += 1

    def op(self, e, fn, reads=(), writes=(), same_engine=None):
        if e == "pool":
            e = FW.POOL_TO
        if same_engine is None:
            same_engine = (e != "pe")
        if self.n_inst >= FW.LIMIT:
            return None
        reads = [r for r in reads if isinstance(r, View)]
        writes = [w for w in writes if isinstance(w, View)]
        deps = self._deps(reads, writes)
        self._emit_waits(e, deps, same_engine)
        if self.cnt[e] >= SEM_LIMIT:
            self.sem[e] = self._newsem("s_" + e)
            self.cnt[e] = 0
        ins = fn(self.eng[e])
        self.cnt[e] += 1
        ins.then_inc(self.sem[e], 1)
        tok = (self.sem[e], self.cnt[e])
        self.last[e] = [tok] + [t for t in self.last[e] if t[0] is not self.sem[e]][:1]
        self._record(reads, writes, tok)
        self.n_inst += 1
        return tok

    def dma(self, q, out, in_, **kw):
        if self.n_inst >= FW.LIMIT:
            return None
        reads, writes = [in_], [out]
        deps = self._deps(reads, writes)
        slot = self.drr[q]
        self.drr[q] = (slot + 1) % self.NDMA
        if self.dlast[q][slot] is not None:
            deps.append(self.dlast[q][slot])
        self._emit_waits(q, deps, True)
        if self.dcnt[q][slot] >= SEM_LIMIT:
            self.dsem[q][slot] = self._newsem("d_%s%d" % (q, slot))
            self.dcnt[q][slot] = 0
        sm = self.dsem[q][slot]
        ins = self.eng[q].dma_start(out=out.ap, in_=in_.ap, **kw)
        self.dcnt[q][slot] += 16
        ins.then_inc(sm, 16)
        tok = (sm, self.dcnt[q][slot])
        self.dlast[q][slot] = tok
        self._record(reads, writes, tok)
        self.n_inst += 1
        return tok

    def all_tokens(self):
        toks = []
        for e in self.eng:
            toks.extend(self.last[e])
        for q in self.dlast:
            toks.extend(t for t in self.dlast[q] if t is not None)
        return toks

    def barrier(self):
        toks = self.all_tokens()
        for e in self.eng:
            self._emit_waits(e, toks, True)
        for b in self.bufs:
            b.st = {}

    def mm(self, out, lhsT, rhs, start=True, stop=True):
        return self.op("pe", lambda e: e.matmul(out.ap, lhsT=lhsT.ap, rhs=rhs.ap, start=start, stop=stop),
                       [lhsT, rhs], [out])

    def tr(self, out, in_, ident):
        return self.op("pe", lambda e: e.transpose(out.ap, in_.ap, ident.ap), [in_, ident], [out])

    def act(self, out, in_, func, bias=0.0, scale=1.0, accum=None, eng="act"):
        kw = {}
        if accum is not None:
            kw["accum_out"] = accum.ap
        return self.op("act", lambda e: e.activation(out=out.ap, in_=in_.ap, func=func, bias=_ap(bias),
                                                     scale=_ap(scale), **kw),
                       [in_, bias, scale], [out, accum])

    def tt(self, out, a, b, op, eng="dve"):
        return self.op(eng, lambda e: e.tensor_tensor(out=out.ap, in0=a.ap, in1=b.ap, op=op), [a, b], [out])

    def ts(self, out, a, s1, op0, s2=None, op1=None, eng="dve", accum=None):
        kw = {}
        if op1 is not None:
            kw["op1"] = op1
        if accum is not None:
            kw["accum_out"] = accum.ap
        return self.op(eng, lambda e: e.tensor_scalar(out=out.ap, in0=a.ap, scalar1=_ap(s1), scalar2=_ap(s2),
                                                      op0=op0, **kw), [a, s1, s2], [out, accum])

    def stt(self, out, a, s, b, op0, op1, eng="dve"):
        return self.op("dve", lambda e: e.scalar_tensor_tensor(out=out.ap, in0=a.ap, scalar=_ap(s), in1=b.ap,
                                                             op0=op0, op1=op1), [a, s, b], [out])

    def cp(self, out, in_, eng="dve"):
        if eng == "act":
            return self.op("act", lambda e: e.copy(out=out.ap, in_=in_.ap), [in_], [out])
        return self.op(eng, lambda e: e.tensor_copy(out=out.ap, in_=in_.ap), [in_], [out])

    def memset(self, out, val, eng="pool"):
        return self.op("dve" if eng == "pool" else eng, lambda e: e.memset(out.ap, val), [], [out])

    def red(self, out, in_, op, axis=AX.X, eng="dve"):
        return self.op(eng, lambda e: e.tensor_reduce(out=out.ap, in_=in_.ap, axis=axis, op=op), [in_], [out])

    def scan(self, out, d0, d1, init, op0, op1):
        return self.op("dve", lambda e: e.tensor_tensor_scan(out=out.ap, data0=d0.ap, data1=d1.ap,
                                                             initial=_ap(init), op0=op0, op1=op1),
                       [d0, d1, init], [out])


C_IDENT, C_ONES, C_TRIF, C_TRIB, C_NEGF, C_NEGB, C_MIF, C_MIB, C_SCAN = [i * 128 for i in range(9)]
C_CMASK = 1152
C_SEL = 1160
C_NEGTL = C_SEL + 16 * 128
C_NEGTC = C_NEGTL + 32
C_CMASKR = C_NEGTC + 8
C_SCAN16 = C_CMASKR + 8
NCST = C_SCAN16 + 16

PP = {}
_off = 0
for _n, _w in [("hycw", 18), ("hycb", 6), ("sccw", 6), ("sdcw", 12), ("sdcb", 4), ("hybias", 2), ("lbl", 8),
               ("hgng", 1), ("bgate", 32), ("ln1g", 8), ("ln1b", 8), ("ln2g", 8), ("ln2b", 8), ("bada", 48),
               ("hyb1", 1), ("hyf1", 1), ("hyb2", 1), ("hyf2", 1), ("lbl64", 16)]:
    PP[_n] = _off
    _off += _w
NPP = _off
RB = {}
_off = 0
for _n, _w in [("hydecay", 256), ("ssdng", 256), ("dtbias", 8), ("alog", 8), ("ssdd", 4), ("brouter", 16)]:
    RB[_n] = _off
    _off += _w
NRB = _off

_CONST_CACHE = {}


def _sincos_1d(pos, dim):
    omega = 1.0 / (10000.0 ** (np.arange(dim // 2, dtype=np.float32) / np.float32(dim // 2)))
    ang = pos.astype(np.float32)[:, None] * omega[None].astype(np.float32)
    return np.concatenate([np.sin(ang), np.cos(ang)], -1).astype(np.float32)


def _hy_feat(Lx):
    t = np.linspace(0.0, 1.0, Lx, dtype=np.float32)[:, None]
    bands = np.linspace(1e-4, 15, 16, dtype=np.float32)
    ang = (np.float32(2.0 * math.pi / Lx)) * np.arange(Lx, dtype=np.float32)[:, None] * bands[None]
    feat = np.concatenate([t, np.cos(ang), -np.sin(ang)], -1).astype(np.float32)
    return feat, t[:, 0]


def _dft_tables(Lx, TB):
    N = 2 * Lx
    nT = Lx // 128
    t = np.arange(Lx, dtype=np.int64)
    k = np.arange(Lx, dtype=np.int64)
    m = ((2 * k[None, :] + 1) * t[:, None]) % (2 * N)
    ang = m.astype(np.float64) * (math.pi / N)
    tabs = [np.cos(ang), np.sin(ang)]
    tf = np.empty((2, nT, 128, nT, 128), dtype=ml_dtypes.bfloat16)
    ti = np.empty((2, Lx // TB, 128, nT, TB), dtype=ml_dtypes.bfloat16)
    for j, T in enumerate(tabs):
        T4 = T.reshape(nT, 128, nT, 128)
        tf[j] = T4.transpose(2, 1, 0, 3).astype(ml_dtypes.bfloat16)
        Tt = T.T.reshape(nT, 128, Lx // TB, TB)
        ti[j] = Tt.transpose(2, 1, 0, 3).astype(ml_dtypes.bfloat16)
    return tf.reshape(2, nT, 128, nT * 128), ti.reshape(2, Lx // TB, 128, nT * TB)


def _constants():
    if _CONST_CACHE:
        return _CONST_CACHE
    cst = np.zeros((128, NCST), np.float32)
    r = np.arange(128)
    cst[:, C_IDENT:C_IDENT + 128] = np.eye(128)
    cst[:, C_ONES:C_ONES + 128] = 1.0
    cst[:, C_TRIF:C_TRIF + 128] = (r[:, None] <= r[None, :])
    cst[:, C_TRIB:C_TRIB + 128] = (r[:, None] >= r[None, :])
    cst[:, C_NEGF:C_NEGF + 128] = np.where(r[:, None] > r[None, :], NEG, 0.0)
    cst[:, C_NEGB:C_NEGB + 128] = np.where(r[:, None] < r[None, :], NEG, 0.0)
    same = (r[:, None] // 16) == (r[None, :] // 16)
    cst[:, C_MIF:C_MIF + 128] = same & (r[:, None] <= r[None, :])
    cst[:, C_MIB:C_MIB + 128] = same & (r[:, None] >= r[None, :])
    cst[:, C_SCAN:C_SCAN + 128] = ((r % 16) != 0)[None, :]
    cst[:, C_CMASK:C_CMASK + 8] = (r[:, None] // 16) == np.arange(8)[None, :]
    cst[:, C_CMASKR:C_CMASKR + 8] = (r[:, None] // 16) == (7 - np.arange(8))[None, :]
    cst[:, C_SCAN16:C_SCAN16 + 16] = (np.arange(16) != 0)[None, :]
    sel = np.zeros((128, 16, 128), np.float32)
    for e in range(16):
        sel[e, e, :] = 1.0
    cst[:, C_SEL:C_SEL + 2048] = sel.reshape(128, 2048)
    featL, tL = _hy_feat(L)
    featC, tC = _hy_feat(LC)
    cst[:, C_NEGTL:C_NEGTL + 32] = -tL.reshape(32, 128).T
    cst[:, C_NEGTC:C_NEGTC + 2] = -tC.reshape(2, 128).T
    feat = np.concatenate([featC.T, featL.T], 1).astype(np.float32)
    rows = L // 64
    row = np.repeat(np.arange(rows), 64)
    col = np.tile(np.arange(64), rows)
    pos = np.concatenate([_sincos_1d(row, D // 2), _sincos_1d(col, D // 2)], -1).astype(np.float32)
    tfl, til = _dft_tables(L, 512)
    tfc, tic = _dft_tables(LC, 256)
    _CONST_CACHE.update(cst=cst, feat=np.ascontiguousarray(feat), pos=pos, tfl=tfl, til=til, tfc=tfc, tic=tic)
    return _CONST_CACHE


def _chunks(v, n):
    return np.ascontiguousarray(np.asarray(v, np.float32).reshape(n, 128).T)


def _shared_inputs(inp):
    pp = np.zeros((DEPTH, 128, NPP), np.float32)
    rb = np.zeros((DEPTH, 128, NRB), np.float32)
    for l in range(DEPTH):
        def put(name, arr):
            arr = np.asarray(arr, np.float32)
            pp[l, :arr.shape[0], PP[name]:PP[name] + arr.shape[1]] = arr
        cw = inp["hy_conv_w"][l]
        put("hycw", cw.reshape(3, 6, 128).transpose(2, 1, 0).reshape(128, 18))
        put("hycb", _chunks(inp["hy_conv_b"][l], 6))
        put("sccw", inp["sc_conv_w"][l].reshape(3, 2, 128).transpose(2, 1, 0).reshape(128, 6))
        put("sdcw", inp["ssd_conv_w"][l].reshape(3, 4, 128).transpose(2, 1, 0).reshape(128, 12))
        put("sdcb", _chunks(inp["ssd_conv_b"][l], 4))
        put("hybias", _chunks(inp["hy_bias"][l], 2))
        lbl = inp["hg_lb_logits"]
        put("lbl", lbl.reshape(2, DEPTH, 2, 128).transpose(3, 0, 1, 2).reshape(128, 8))
        put("lbl64", lbl.reshape(2, DEPTH, 4, 64).transpose(3, 0, 1, 2).reshape(64, 16))
        put("hgng", np.asarray(inp["hg_norm_g"][l]).reshape(64, 1))
        put("bgate", _chunks(inp["b_gate"][l], 32))
        put("ln1g", _chunks(inp["ln1_g"][l], 8))
        put("ln1b", _chunks(inp["ln1_b"][l], 8))
        put("ln2g", _chunks(inp["ln2_g"][l], 8))
        put("ln2b", _chunks(inp["ln2_b"][l], 8))
        put("bada", _chunks(inp["b_ada"][l], 48))
        put("hyb1", np.asarray(inp["hy_b1"][l]).reshape(64, 1))
        put("hyf1", np.asarray(inp["hy_freq1"][l]).reshape(64, 1))
        put("hyb2", np.asarray(inp["hy_b2"][l]).reshape(64, 1))
        put("hyf2", np.asarray(inp["hy_freq2"][l]).reshape(64, 1))

        def rput(name, row):
            row = np.asarray(row, np.float32).reshape(1, -1)
            rb[l, :, RB[name]:RB[name] + row.shape[1]] = np.broadcast_to(row, (128, row.shape[1]))
        rput("hydecay", inp["hy_decay"][l])
        rput("ssdng", inp["ssd_norm_g"][l])
        rput("dtbias", inp["ssd_dt_bias"][l])
        rput("alog", inp["ssd_a_log"][l])
        rput("ssdd", inp["ssd_d"][l])
        rput("brouter", inp["b_router"])
    sh = {"pp": pp, "rb": rb}
    for n in ("w_ada", "w_in", "hy_w1", "hy_w2", "hy_w3", "w_gate", "w_br", "w_o", "w_router",
              "w_e1", "w_e3", "w_e2"):
        sh[n] = np.ascontiguousarray(np.asarray(inp[n], np.float32))
    return sh


GROUPS = [(0, LC)] + [(LC + 512 * i, 512) for i in range(L // 512)]


class Prog:
    def __init__(self, dbg=(), stop_after=None, layers=DEPTH):
        self.dbg = set(dbg)
        self.stop_after = stop_after
        self.layers = layers
        nc = bass.Bass("TRN2", target_bir_lowering=False)
        self.nc = nc
        fw = FW(nc)
        self.fw = fw
        din = lambda name, shape, dt=F32: fw.dram(name, shape, dt, kind="ExternalInput")
        self.x_d = din("x", [L, D])
        self.ctx_d = din("ctx", [LC, D])
        self.cc_d = din("cc", [128, KC * 2])
        self.pos_d = din("pos", [L, D])
        self.cst_d = din("cst", [128, NCST])
        self.feat_d = din("feat", [33, NT])
        self.tfl_d = din("tfl", [2, 32, 128, 32 * 128], BF16)
        self.til_d = din("til", [2, 8, 128, 32 * 512], BF16)
        self.tfc_d = din("tfc", [2, 2, 128, 2 * 128], BF16)
        self.tic_d = din("tic", [2, 1, 128, 2 * 256], BF16)
        self.pp_d = din("pp", [DEPTH, 128, NPP])
        self.rb_d = din("rb", [DEPTH, 128, NRB])
        self.w_ada_d = din("w_ada", [DEPTH, D, 6 * D])
        self.w_in_d = din("w_in", [DEPTH, D, IN_COLS])
        self.hy_w1_d = din("hy_w1", [DEPTH, 33, 64])
        self.hy_w2_d = din("hy_w2", [DEPTH, 64, 64])
        self.hy_w3_d = din("hy_w3", [DEPTH, 64, 512])
        self.w_gate_d = din("w_gate", [DEPTH, D, 4 * D])
        self.w_br_d = din("w_br", [DEPTH, 4, 256, D])
        self.w_o_d = din("w_o", [DEPTH, D, D])
        self.w_router_d = din("w_router", [D, NE])
        self.w_e1_d = din("w_e1", [DEPTH, NE, D, DFF])
        self.w_e3_d = din("w_e3", [DEPTH, NE, D, DFF])
        self.w_e2_d = din("w_e2", [DEPTH, NE, DFF, D])
        self.out_d = fw.dram("out", [L, D], F32, kind="ExternalOutput")

        def scratch(name, shape, dt=F32):
            kind = "ExternalOutput" if name in self.dbg else "Internal"
            return fw.dram(name, shape, dt, kind=kind)
        self.resT = scratch("resT", [D, NT])
        self.uT = scratch("uT", [UROWS, NT])
        self.brT = scratch("brT", [D, NT], BF16)
        self.lat1T = scratch("lat1T", [D, NT])
        self.h2T = scratch("h2T", [D, NT], BF16)
        self.gateT = scratch("gateT", [NE, NT])
        self.hgo = scratch("hgo", [64, 4 * NT])
        self.hgo2 = scratch("hgo2", [64, 4 * NT])
        self.yf = scratch("yf", [NT, 256])
        self.dtT = scratch("dtT", [NT, 8])
        self.wgc = scratch("wgc", [8, 128, KC * 4 * 128], BF16)
        self.modd = scratch("modd", [128, 96])
        self.we1b = scratch("we1b", [NE, D, DFF], BF16)
        self.we3b = scratch("we3b", [NE, D, DFF], BF16)
        self.we2b = scratch("we2b", [NE, DFF, D], BF16)

        self.cst = fw.sb("cst", [128, NCST])
        fw.dma("sp", self.cst[:], self.cst_d[:])
        self.PS = [fw.ps("ps%d" % i, [128, 512]) for i in range(8)]
        self.pp = fw.sb("pp", [128, NPP])
        self.rb = fw.sb("rb", [128, NRB])
        self.modL = fw.sb("modL", [128, 48])
        self.modC = fw.sb("modC", [128, 48])
        self.build()
        fw.barrier()
        fw.close()

    def c(self, off, w=128, rows=128):
        return self.cst[0:rows, off:off + w]

    def build(self):
        fw = self.fw
        self.phase_init()
        if self.stop_after == "init":
            return
        for l in range(self.layers):
            self.l = l
            fw.dma("sp", self.pp[:], self.pp_d[l])
            fw.dma("sp", self.rb[:], self.rb_d[l])
            self.phase_mod(l)
            if self.stop_after in ("mod", "mod%d" % l):
                return
            self.phase_p1(l)
            if self.stop_after in ("p1", "p1%d" % l):
                return
            self.phase_wcast(l)
            self.phase_sc(l)
            if self.stop_after in ("sc", "sc%d" % l):
                return
            self.phase_hyena(l, ctx=False)
            if l < DEPTH - 1:
                self.phase_hyena(l, ctx=True)
            if self.stop_after in ("hy", "hy%d" % l):
                return
            self.phase_hgrn(l)
            if self.stop_after in ("hg", "hg%d" % l):
                return
            self.phase_ssd(l)
            if self.stop_after in ("ssd", "ssd%d" % l):
                return
            self.phase_merge(l)
            if self.stop_after in ("merge", "merge%d" % l):
                return
            self.phase_moe(l)
            if self.stop_after == "moe%d" % l:
                return

    def phase_init(self):
        fw = self.fw
        m = fw.mark()
        xt = [fw.sb("xt%d" % i, [128, D]) for i in range(2)]
        pt = [fw.sb("pt%d" % i, [128, D]) for i in range(2)]
        st = [fw.sb("st%d" % i, [128, KC, 512]) for i in range(2)]
        ident = self.c(C_IDENT)
        resv = self.resT[:].r("(kc p) t -> p kc t", p=128)
        ti = 0
        for gi, (tok0, TB) in enumerate(GROUPS):
            sg = st[gi % 2]
            for j in range(TB // 128):
                a = xt[ti % 2]
                if gi == 0:
                    fw.dma("sp", a[:], self.ctx_d[j * 128:(j + 1) * 128, :])
                else:
                    r0 = tok0 - LC + j * 128
                    fw.dma("sp", a[:], self.x_d[r0:r0 + 128, :])
                    p = pt[ti % 2]
                    fw.dma("pool", p[:], self.pos_d[r0:r0 + 128, :])
                    fw.tt(a[:], a[:], p[:], ALU.add)
                for half in range(2):
                    ps = self.PS[(ti * 2 + half) % 4]
                    for q in range(4):
                        kc = half * 4 + q
                        fw.tr(ps[:, q * 128:(q + 1) * 128], a[:, kc * 128:(kc + 1) * 128], ident)
                    dst = sg[:, half * 4:(half + 1) * 4, j * 128:(j + 1) * 128]
                    src = ps[:].r("p (q t) -> p q t", q=4)
                    if half == 0:
                        fw.cp(dst, src, eng="dve")
                    else:
                        fw.cp(dst, src, eng="act")
                ti += 1
            fw.dma("sp", resv[:, :, tok0:tok0 + TB].k(("g", gi)), sg[:, :, 0:TB])
        fw.release(m)

    def phase_mod(self, l):
        fw = self.fw
        m = fw.mark()
        cs = fw.sb("cs", [128, KC * 2])
        fw.dma("sp", cs[:], self.cc_d[:])
        csil = fw.sb("csil", [128, KC * 2])
        fw.act(csil[:], cs[:], AF.Silu)
        wa = [fw.sb("wa%d" % i, [128, KC, 1024]) for i in range(2)]
        ps = self.PS[0]
        wv = self.w_ada_d[l].r("(kc p) n -> p kc n", p=128)
        for blk in range(6):
            w = wa[blk % 2]
            fw.dma("sp" if blk % 2 == 0 else "pool", w[:], wv[:, :, blk * 1024:(blk + 1) * 1024])
            for jj in range(8):
                j = blk * 8 + jj
                for kc in range(KC):
                    fw.mm(ps[:, j * 2:j * 2 + 2], w[:, kc, jj * 128:(jj + 1) * 128], csil[:, kc * 2:kc * 2 + 2],
                          start=(kc == 0), stop=(kc == KC - 1))
        psv = ps[:, 0:96].r("p (j t) -> p j t", t=2)
        bada = self.pp[:, PP["bada"]:PP["bada"] + 48]
        fw.tt(self.modL[:], psv[:, :, 0], bada, ALU.add)
        fw.tt(self.modC[:], psv[:, :, 1], bada, ALU.add)
        for mm_ in (self.modL, self.modC):
            for o in (8, 32):
                fw.ts(mm_[:, o:o + 8], mm_[:, o:o + 8], 1.0, ALU.add)
        if "modd" in self.dbg:
            fw.dma("sp", self.modd[:, 0:48], self.modL[:])
            fw.dma("sp", self.modd[:, 48:96], self.modC[:])
        fw.release(m)

    def mod_for(self, gi):
        return self.modC if gi == 0 else self.modL

    def load_cast(self, dst, src_view, stage, n, i):
        fw = self.fw
        s = stage[i % len(stage)]
        fw.dma("sp" if i % 2 == 0 else "pool", s[:, 0:n], src_view)
        eng = ("dve", "act", "pool")[i % 3]
        fw.cp(dst, s[:, 0:n], eng=eng)

    def phase_p1(self, l):
        fw = self.fw
        m = fw.mark()
        wb = fw.sb("winb", [128, KC, UROWS], BF16)
        stage = [fw.sb("wst%d" % i, [128, IN_COLS]) for i in range(2)]
        fw.memset(wb[:, :, IN_COLS:UROWS], 0.0)
        wv = self.w_in_d[l].r("(kc p) n -> p kc n", p=128)
        for kc in range(KC):
            self.load_cast(wb[:, kc, 0:IN_COLS].k(kc), wv[:, kc, :], stage, IN_COLS, kc)
        rt = [fw.sb("rt%d" % i, [128, KC, 512]) for i in range(2)]
        ht = [fw.sb("ht%d" % i, [128, KC, 512], BF16) for i in range(2)]
        og = [fw.sb("og%d" % i, [128, 4, 512]) for i in range(3)]
        dts = [fw.sb("dts%d" % i, [128, 8]) for i in range(2)]
        resv = self.resT[:].r("(kc p) t -> p kc t", p=128)
        uv = self.uT[:].r("(c p) t -> p c t", p=128)
        oi = 0
        for gi, (tok0, TB) in enumerate(GROUPS):
            r = rt[gi % 2]
            h = ht[gi % 2]
            md = self.mod_for(gi)
            fw.dma("sp", r[:, :, 0:TB], resv[:, :, tok0:tok0 + TB])
            for kc in range(KC):
                fw.act(h[:, kc, 0:TB].k(kc), r[:, kc, 0:TB], AF.Identity, bias=md[:, kc:kc + 1], scale=md[:, 8 + kc:9 + kc])
            for c0 in range(0, 29, 4):
                nchunk = min(4, 29 - c0)
                o = og[oi % 3]
                for cj in range(nchunk):
                    cidx = c0 + cj
                    ps = self.PS[cidx % 4]
                    for kc in range(KC):
                        fw.mm(ps[:, 0:TB], wb[:, kc, cidx * 128:(cidx + 1) * 128], h[:, kc, 0:TB],
                              start=(kc == 0), stop=(kc == KC - 1))
                    fw.cp(o[:, cj, 0:TB].k(cj), ps[:, 0:TB], eng=("dve" if cidx % 2 == 0 else "act"))
                fw.dma("sp" if oi % 2 == 0 else "pool", uv[:, c0:c0 + nchunk, tok0:tok0 + TB].k(("g", gi, c0)),
                       o[:, 0:nchunk, 0:TB])
                oi += 1
            for jt in range(TB // 128):
                ps = self.PS[4 + jt % 2]
                for kc in range(KC):
                    fw.mm(ps[:, 0:8], h[:, kc, jt * 128:(jt + 1) * 128], wb[:, kc, 3584:3592],
                          start=(kc == 0), stop=(kc == KC - 1))
                dq = dts[jt % 2]
                fw.cp(dq[:], ps[:, 0:8], eng="act")
                fw.dma("pool", self.dtT[tok0 + jt * 128:tok0 + (jt + 1) * 128, :].k(("dt", tok0, jt)), dq[:])
        fw.release(m)

    def uv(self):
        return self.uT[:].r("(c p) t -> p c t", p=128)

    def brv(self):
        return self.brT[:].r("(c p) t -> p c t", p=128)

    def load_halo(self, dst, c0, nch, tok0, TB, q="sp"):
        fw = self.fw
        s0, s1 = (0, LC) if tok0 < LC else (LC, NT)
        lo = tok0 - 1
        hi = tok0 + TB + 1
        d0 = 0
        if lo < s0:
            fw.memset(dst[:, 0:nch, 0:1], 0.0)
            lo += 1
            d0 = 1
        if hi > s1:
            fw.memset(dst[:, 0:nch, TB + 1:TB + 2], 0.0)
            hi -= 1
        fw.dma(q, dst[:, 0:nch, d0:d0 + (hi - lo)], self.uv()[:, c0:c0 + nch, lo:hi])

    def conv3(self, out, src, wcol, TB, bias=None, eng="dve"):
        fw = self.fw
        w = lambda k: self.pp[:, wcol + k:wcol + k + 1]
        if bias is not None:
            fw.ts(out, src[:, 0:TB], w(0), ALU.mult, bias, ALU.add, eng=eng)
        else:
            fw.ts(out, src[:, 0:TB], w(0), ALU.mult, eng=eng)
        fw.stt(out, src[:, 1:TB + 1], w(1), out, ALU.mult, ALU.add)
        fw.stt(out, src[:, 2:TB + 2], w(2), out, ALU.mult, ALU.add)

    def phase_sc(self, l):
        fw = self.fw
        m = fw.mark()
        tin = [fw.sb("sct%d" % i, [128, 6, 514]) for i in range(2)]
        mm_ = [fw.sb("scm%d" % i, [128, 2, 514]) for i in range(2)]
        acc = [fw.sb("sca%d" % i, [128, 2, 512]) for i in range(2)]
        ob = [fw.sb("sco%d" % i, [128, 2, 512], BF16) for i in range(2)]
        for gi, (tok0, TB) in enumerate(GROUPS):
            if gi == 0 and l == DEPTH - 1:
                continue
            t = tin[gi % 2]
            self.load_halo(t, 6, 6, tok0, TB)
            mv = mm_[gi % 2]
            fw.tt(mv[:, :, 0:TB + 2], t[:, 2:4, 0:TB + 2], t[:, 4:6, 0:TB + 2], ALU.mult)
            a = acc[gi % 2]
            o = ob[gi % 2]
            for cc in range(2):
                self.conv3(a[:, cc, 0:TB], mv[:, cc, :], PP["sccw"] + cc * 3, TB, eng=("dve" if cc == 0 else "pool"))
                fw.tt(o[:, cc, 0:TB], a[:, cc, 0:TB], t[:, cc, 1:TB + 1], ALU.mult, eng=("dve" if cc == 0 else "pool"))
            fw.dma("pool", self.brv()[:, 2:4, tok0:tok0 + TB].k(("sc", gi)), o[:, :, 0:TB])
        fw.release(m)

    def phase_hyena(self, l, ctx):
        fw = self.fw
        m = fw.mark()
        Lx = LC if ctx else L
        base = 0 if ctx else LC
        TB = 256 if ctx else 512
        nT = Lx // 128
        nG = Lx // TB
        tpb = TB // 128
        tf_d = self.tfc_d if ctx else self.tfl_d
        ti_d = self.tic_d if ctx else self.til_d
        negt = self.c(C_NEGTC, 2) if ctx else self.c(C_NEGTL, 32)
        ident = self.c(C_IDENT)
        ones = self.c(C_ONES)
        AU = fw.sb("AU", [128, nT, 768], BF16)
        ZB = fw.sb("ZB", [128, nT, 512], BF16)
        X0 = fw.sb("X0", [128, 2, Lx], BF16)
        WB = fw.sb("WB", [128, 2, Lx], BF16)
        rn = fw.sb("rn", [128, 256])
        m2 = fw.mark()
        w1 = fw.sb("hw1", [33, 64])
        w2 = fw.sb("hw2", [64, 64])
        w3 = fw.sb("hw3", [64, 512])
        fw.dma("sp", w1[:], self.hy_w1_d[l])
        fw.dma("sp", w2[:], self.hy_w2_d[l])
        fw.dma("sp", w3[:], self.hy_w3_d[l])
        bf = fw.sb("hbf", [64, 2])
        ppc = lambda n: self.pp[0:64, PP[n]:PP[n] + 1]
        fw.tt(bf[:, 0:1], ppc("hyb1"), ppc("hyf1"), ALU.mult)
        fw.tt(bf[:, 1:2], ppc("hyb2"), ppc("hyf2"), ALU.mult)
        h1 = fw.sb("hh1", [64, Lx])
        h2 = fw.sb("hh2", [64, Lx])
        ft = [fw.sb("hft%d" % i, [33, 512]) for i in range(2)]
        tmp = [fw.sb("htmp%d" % i, [64, 512]) for i in range(2)]
        TF = min(512, Lx)
        tmpk = [fw.sb("htmpk%d" % i, [64, 512]) for i in range(2)]
        MAGIC = 12582912.0
        for layer in range(2):
            for g in range(Lx // TF):
                ps = self.PS[g % 2]
                if layer == 0:
                    f = ft[g % 2]
                    fw.dma("sp", f[:, 0:TF], self.feat_d[:, base + g * TF:base + (g + 1) * TF])
                    fw.mm(ps[0:64, 0:TF], w1[:, :], f[:, 0:TF])
                    fq, bq, dst = ppc("hyf1"), bf[:, 0:1], h1
                else:
                    fw.mm(ps[0:64, 0:TF], w2[:, :], h1[:, g * TF:(g + 1) * TF])
                    fq, bq, dst = ppc("hyf2"), bf[:, 1:2], h2
                t = tmp[g % 2]
                fw.ts(t[:, 0:TF], ps[0:64, 0:TF], fq, ALU.mult, bq, ALU.add)
                kq = tmpk[g % 2]
                fw.ts(kq[:, 0:TF], t[:, 0:TF], 1.0 / (2.0 * math.pi), ALU.mult, MAGIC, ALU.add)
                fw.ts(kq[:, 0:TF], kq[:, 0:TF], -MAGIC, ALU.add)
                fw.stt(t[:, 0:TF], kq[:, 0:TF], -2.0 * math.pi, t[:, 0:TF], ALU.mult, ALU.add)
                fw.ts(t[:, 0:TF], t[:, 0:TF], -3.141592, ALU.max, 3.141592, ALU.min)
                fw.act(dst[:, g * TF:(g + 1) * TF].k(g), t[:, 0:TF], AF.Sin)
        adec = fw.sb("adec", [128, 256])
        dv = self.rb[:, RB["hydecay"]:RB["hydecay"] + 256]
        fw.stt(adec[:], dv, -1.0, dv, ALU.mult, ALU.max)
        win = [fw.sb("hwin%d" % i, [128, 256]) for i in range(2)]
        hf = [fw.sb("hhf%d" % i, [128, 256]) for i in range(2)]
        hb = [fw.sb("hhb%d" % i, [128, 256]) for i in range(2)]
        ab = [fw.sb("hab%d" % i, [128, 256]) for i in range(2)]
        a2 = [fw.sb("hab2%d" % i, [128, 256]) for i in range(2)]
        asum = self.PS[7]
        for i in range(nT):
            ps3 = self.PS[2 + i % 2]
            fw.mm(ps3[:, 0:512], h2[:, i * 128:(i + 1) * 128], w3[:, :])
            w_ = win[i % 2]
            fw.act(w_[:], adec[:], AF.Exp, scale=negt[:, i:i + 1])
            f_, b_, a_ = hf[i % 2], hb[i % 2], ab[i % 2]
            fw.tt(f_[:], ps3[:, 0:256], w_[:], ALU.mult)
            fw.tt(b_[:], ps3[:, 256:512], w_[:], ALU.mult)
            if i == 0:
                fw.memset(b_[0:1, :], 0.0, eng="dve")
            fw.tt(AU[:, i, 0:256].k(("f", i)), f_[:], b_[:], ALU.add, eng="pool")
            fw.tt(AU[:, i, 512:768].k(("f", i)), b_[:], f_[:], ALU.subtract, eng="pool")
            fw.act(a_[:], f_[:], AF.Abs)
            fw.act(a2[i % 2][:], b_[:], AF.Abs)
            fw.tt(a_[:], a_[:], a2[i % 2][:], ALU.add)
            fw.mm(asum[:, 0:256], ones, a_[:], start=(i == 0), stop=(i == nT - 1))
        fw.op("dve", lambda e: e.reciprocal(out=rn[:].ap, in_=asum[:, 0:256].ap), [asum[:, 0:256]], [rn[:]])
        fw.release(m2)
        m2 = fw.mark()
        tin = [fw.sb("hyt%d" % i, [128, 6, 514]) for i in range(2)]
        cv = [fw.sb("hyc%d" % i, [128, 6, 512]) for i in range(2)]
        wv = [fw.sb("hyw%d" % i, [128, 2, 512]) for i in range(2)]
        for g in range(nG):
            tok0 = base + g * TB
            t = tin[g % 2]
            self.load_halo(t, 0, 6, tok0, TB)
            c_ = cv[g % 2]
            for j in range(6):
                self.conv3(c_[:, j, 0:TB], t[:, j, :], PP["hycw"] + 3 * j, TB,
                           bias=self.pp[:, PP["hycb"] + j:PP["hycb"] + j + 1], eng=("dve" if j % 2 == 0 else "pool"))
            fw.cp(X0[:, :, g * TB:(g + 1) * TB].k(g), c_[:, 0:2, 0:TB], eng="act")
            w_ = wv[g % 2]
            fw.tt(w_[:, :, 0:TB], c_[:, 2:4, 0:TB], c_[:, 4:6, 0:TB], ALU.mult)
            for cc in range(2):
                fw.act(WB[:, cc, g * TB:(g + 1) * TB].k((g, cc)), w_[:, cc, 0:TB], AF.Copy,
                       scale=self.pp[:, PP["hybias"] + cc:PP["hybias"] + cc + 1])
            for jp in range(tpb // 2):
                ps = self.PS[(g * 2 + jp) % 4]
                for jj in range(2):
                    jt = jp * 2 + jj
                    for cc in range(2):
                        fw.tr(ps[:, (jj * 2 + cc) * 128:(jj * 2 + cc + 1) * 128], w_[:, cc, jt * 128:(jt + 1) * 128], ident)
                i0 = g * tpb + jp * 2
                fw.cp(AU[:, i0:i0 + 2, 256:512].k(("u", i0)), ps[:].r("p (j c) -> p j c", j=2),
                      eng=("dve" if jp % 2 == 0 else "act"))
        fw.release(m2)
        m2 = fw.mark()
        tfr = [fw.sb("tfr%d" % i, [128, nT * 128], BF16) for i in range(4)]
        gcn = [fw.sb("gcn%d" % i, [128, 256]) for i in range(2)]
        gsn = [fw.sb("gsn%d" % i, [128, 256]) for i in range(2)]
        tq = [fw.sb("tq%d" % i, [128, 256]) for i in range(4)]
        for kc in range(nT):
            tabs = (tfr[(kc % 2) * 2], tfr[(kc % 2) * 2 + 1])
            fw.dma("sp", tabs[0][:], tf_d[0, kc])
            fw.dma("pool", tabs[1][:], tf_d[1, kc])
            pc = self.PS[(kc % 2) * 2]
            pS = self.PS[(kc % 2) * 2 + 1]
            for i in range(nT):
                fw.mm(pc[:, 0:512], tabs[0][:, i * 128:(i + 1) * 128], AU[:, i, 0:512], start=(i == 0), stop=(i == nT - 1))
            for i in range(nT):
                fw.mm(pS[:, 0:512], tabs[1][:, i * 128:(i + 1) * 128], AU[:, i, 256:768], start=(i == 0), stop=(i == nT - 1))
            gc_, gs_ = gcn[kc % 2], gsn[kc % 2]
            fw.tt(gc_[:], pc[:, 0:256], rn[:], ALU.mult)
            fw.tt(gs_[:], pS[:, 256:512], rn[:], ALU.mult)
            t1, t2, t3, t4 = tq
            fw.tt(t1[:], pc[:, 256:512], gc_[:], ALU.mult)
            fw.tt(t2[:], pS[:, 0:256], gs_[:], ALU.mult)
            fw.tt(ZB[:, kc, 0:256].k(kc), t1[:], t2[:], ALU.add, eng="pool")
            fw.tt(t3[:], pS[:, 0:256], gc_[:], ALU.mult)
            fw.tt(t4[:], pc[:, 256:512], gs_[:], ALU.mult)
            fw.tt(ZB[:, kc, 256:512].k(kc), t3[:], t4[:], ALU.subtract, eng="pool")
        fw.release(m2)
        m2 = fw.mark()
        KP = min(8, nT)
        tir = [fw.sb("tir%d" % i, [128, KP * TB], BF16) for i in range(4)]
        yt = [fw.sb("hyy%d" % i, [128, 512]) for i in range(2)]
        ob = [fw.sb("hyo%d" % i, [128, 2, 512], BF16) for i in range(2)]
        npiece = nT // KP
        ri = 0
        for tg in range(nG):
            tok0 = base + tg * TB
            pa = [self.PS[4 + (tg % 2) * 2], self.PS[5 + (tg % 2) * 2]]
            for piece in range(npiece):
                for j in range(2):
                    tab = tir[ri % 4]
                    fw.dma("sp" if ri % 2 == 0 else "pool", tab[:], ti_d[j, tg][:, piece * KP * TB:(piece + 1) * KP * TB])
                    ri += 1
                    for kk in range(KP):
                        kc = piece * KP + kk
                        first = (piece == 0 and j == 0 and kk == 0)
                        last = (piece == npiece - 1 and j == 1 and kk == KP - 1)
                        for cc in range(2):
                            fw.mm(pa[cc][:, 0:TB], ZB[:, kc, j * 256 + cc * 128:j * 256 + (cc + 1) * 128],
                                  tab[:, kk * TB:(kk + 1) * TB], start=first, stop=last)
            o = ob[tg % 2]
            for cc in range(2):
                y = yt[cc]
                fw.stt(y[:, 0:TB], pa[cc][:, 0:TB], 1.0 / Lx, WB[:, cc, tg * TB:(tg + 1) * TB], ALU.mult, ALU.add)
                fw.tt(o[:, cc, 0:TB], y[:, 0:TB], X0[:, cc, tg * TB:(tg + 1) * TB], ALU.mult, eng="pool")
            fw.dma("pool", self.brv()[:, 0:2, tok0:tok0 + TB].k(("hy", tok0)), o[:, :, 0:TB])
        fw.release(m2)
        fw.release(m)

    def phase_hgrn_v1(self, l):
        fw = self.fw
        m = fw.mark()
        ident = self.c(C_IDENT)
        ones64 = self.cst[0:64, C_ONES:C_ONES + 64]
        scanm = self.c(C_SCAN)
        cmask = self.c(C_CMASK, 8)
        uv = self.uv()
        hgv = self.hgo[:].r("v (h t) -> v h t", h=4)
        gv = self.uT[2560:2816, :].r("(h v) t -> v h t", v=64)
        bro = self.brT[512:768, :].r("(h v) t -> v h t", v=64)
        epsb = fw.sb("epsb", [128, 1])
        fw.memset(epsb[:], RMS_EPS)
        lbv = fw.sb("lbv", [128, 4])
        oml = fw.sb("oml", [128, 4])
        if l == 0:
            fw.memset(lbv[:], 0.0)
            fw.memset(oml[:], 1.0)
        else:
            for d in range(2):
                for cc in range(2):
                    c1 = PP["lbl"] + (d * 2 + 1) * 2 + cc
                    c0 = PP["lbl"] + (d * 2 + 0) * 2 + cc
                    j = d * 2 + cc
                    fw.tt(lbv[:, j:j + 1], self.pp[:, c1:c1 + 1], self.pp[:, c0:c0 + 1], ALU.subtract)
            fw.act(lbv[:], lbv[:], AF.Exp, scale=-1.0)
            fw.ts(lbv[:], lbv[:], 1.0, ALU.add)
            fw.op("dve", lambda e: e.reciprocal(out=lbv[:].ap, in_=lbv[:].ap), [lbv[:]], [lbv[:]])
            fw.ts(oml[:], lbv[:], -1.0, ALU.mult, 1.0, ALU.add)
        S = [[fw.sb("hgS%d%d" % (cc, p), [128, 8, 64]) for p in range(2)] for cc in range(2)]
        R2 = 2
        qt = [fw.sb("hgq%d" % i, [128, 2, 128]) for i in range(R2)]
        ftl = [fw.sb("hgf%d" % i, [128, 2, 128]) for i in range(R2)]
        vt = [fw.sb("hgv%d" % i, [128, 2, 128]) for i in range(R2)]
        sg = fw.sb("hgsg", [128, 2, 128])
        gg = [fw.sb("hggg%d" % i, [128, 128]) for i in range(2)]
        lf = [fw.sb("hglf%d" % i, [128, 128]) for i in range(2)]
        kk = [fw.sb("hgkk%d" % i, [128, 128]) for i in range(2)]
        bp = [fw.sb("hgbp%d" % i, [128, 128]) for i in range(2)]
        bb = [fw.sb("hgbb%d" % i, [128, 128]) for i in range(2)]
        tm = [fw.sb("hgtm%d" % i, [128, 128]) for i in range(2)]
        qd = [fw.sb("hgqd%d" % i, [128, 128]) for i in range(2)]
        ki = [fw.sb("hgki%d" % i, [128, 128]) for i in range(2)]
        ke = [fw.sb("hgke%d" % i, [128, 128]) for i in range(2)]
        dec = [fw.sb("hgdec%d" % i, [128, 8]) for i in range(2)]
        ketok = [fw.sb("hgket%d" % i, [128, 128]) for i in range(2)]
        vtok = [fw.sb("hgvt%d" % i, [128, 128]) for i in range(2)]
        vexp = [fw.sb("hgvx%d" % i, [128, 8, 128]) for i in range(2)]
        atm = [fw.sb("hgatm%d" % i, [128, 128]) for i in range(4)]
        osb = [fw.sb("hgo%d" % i, [64, 512]) for i in range(2)]
        of = [fw.sb("hgof%d" % i, [64, 512]) for i in range(2)]
        gt = [fw.sb("hggt%d" % i, [64, 512]) for i in range(2)]
        sq = fw.sb("hgsq", [64, 512])
        rs = fw.sb("hgrs", [64, 512])
        sgt = fw.sb("hgsgt", [64, 512])
        obf = [fw.sb("hgob%d" % i, [64, 512], BF16) for i in range(2)]
        ng = self.pp[0:64, PP["hgng"]:PP["hgng"] + 1]
        for d in range(getattr(self, "hg_passes", 2)):
            order = list(range(34)) if d == 0 else [1, 0] + list(range(33, 1, -1))
            MI = self.c(C_MIF) if d == 0 else self.c(C_MIB)
            for cc in range(2):
                fw.memset(S[cc][0][:, 0, :], 0.0)
            for it, tile in enumerate(order):
                if it >= getattr(self, "hg_limit", 99):
                    break
                tok0 = tile * 128
                need_o = not (l == DEPTH - 1 and tile < 2)
                par = it % 2
                q_, f_, v_ = qt[it % R2], ftl[it % R2], vt[it % R2]
                fw.dma("sp", q_[:], uv[:, 12:14, tok0:tok0 + 128])
                fc = 14 if d == 0 else 16
                fw.dma("sp", f_[:], uv[:, fc:fc + 2, tok0:tok0 + 128])
                fw.dma("sp", v_[:], uv[:, 18:20, tok0:tok0 + 128])
                fw.act(sg[:], f_[:], AF.Exp, scale=-1.0)
                fw.ts(sg[:], sg[:], 1.0, ALU.add, eng="pool")
                fw.op("dve", lambda e: e.reciprocal(out=sg[:].ap, in_=sg[:].ap), [sg[:]], [sg[:]])
                pso = self.PS[6]
                pso2 = self.PS[7]
                for cc in range(2):
                    j = d * 2 + cc
                    g_, l_, k_, b_, bb_, t_ = gg[cc], lf[cc], kk[cc], bp[cc], bb[cc], tm[cc]
                    fw.ts(g_[:], sg[:, cc, :], oml[:, j:j + 1], ALU.mult, lbv[:, j:j + 1], ALU.add)
                    fw.act(l_[:], g_[:], AF.Ln)
                    fw.ts(k_[:], g_[:], -1.0, ALU.mult, 1.0, ALU.add, eng="pool")
                    fw.scan(b_[:], scanm, l_[:], 0.0, ALU.mult, ALU.add)
                    b3 = b_[:].r("p (n s) -> p n s", s=16)
                    tot = b3[:, :, 15:16]
                    if d == 0:
                        bcur = b_
                    else:
                        fw.tt(t_[:], l_[:], b_[:], ALU.subtract)
                        fw.tt(bb_[:].r("p (n s) -> p n s", s=16), t_[:].r("p (n s) -> p n s", s=16),
                              tot.bc([128, 8, 16]), ALU.add)
                        bcur = bb_
                    fw.act(dec[cc][:], b3[:, :, 15], AF.Exp)
                    fw.act(t_[:], bcur[:], AF.Exp)
                    fw.stt(qd[cc][:], q_[:, cc, :], 0.125, t_[:], ALU.mult, ALU.mult)
                    fw.act(t_[:], bcur[:], AF.Exp, scale=-1.0)
                    fw.tt(ki[cc][:], k_[:], t_[:], ALU.mult)
                    fw.tt(t_[:].r("p (n s) -> p n s", s=16), tot.bc([128, 8, 16]),
                          bcur[:].r("p (n s) -> p n s", s=16), ALU.subtract)
                    fw.act(t_[:], t_[:], AF.Exp)
                    fw.tt(ke[cc][:], k_[:], t_[:], ALU.mult, eng="pool")
                    if getattr(self, "hg_stage", 9) < 1:
                        continue
                    pst = self.PS[cc]
                    fw.tr(pst[:, 0:128], ke[cc][:], ident)
                    fw.tr(pst[:, 128:256], v_[:, cc, :], ident)
                    fw.cp(ketok[cc][:], pst[:, 0:128], eng="act")
                    fw.cp(vtok[cc][:], pst[:, 128:256], eng="act")
                    fw.tt(vexp[cc][:], vtok[cc][:].un(1).bc([128, 8, 128]), cmask.un(2).bc([128, 8, 128]), ALU.mult)
                    for h in range(2):
                        if getattr(self, "hg_stage", 9) < 2:
                            continue
                        hh = cc * 2 + h
                        rows = slice(h * 64, (h + 1) * 64)
                        if need_o:
                            pa = self.PS[2 + h]
                            fw.mm(pa[:, 0:128], ki[cc][rows, :], qd[cc][rows, :])
                            am = atm[hh]
                            fw.tt(am[:], pa[:, 0:128], MI, ALU.mult)
                            fw.mm(pso[0:64, hh * 128:(hh + 1) * 128].k(hh), vtok[cc][:, h * 64:(h + 1) * 64], am[:])
                        pkv = self.PS[4 + h]
                        fw.mm(pkv[:, 0:512], ketok[cc][:], vexp[cc][:, :, h * 64:(h + 1) * 64])
                        Scur = S[cc][par]
                        Snxt = S[cc][1 - par]
                        for jj in range(8 if getattr(self, "hg_stage", 9) >= 3 else 0):
                            n = jj if d == 0 else 7 - jj
                            if need_o:
                                fw.mm(pso2[0:64, hh * 128 + n * 16:hh * 128 + (n + 1) * 16].k(hh),
                                      Scur[rows, jj, :].k((h, jj)), qd[cc][rows, n * 16:(n + 1) * 16])
                            dst = Scur[rows, jj + 1, :].k((h, jj + 1)) if jj < 7 else Snxt[rows, 0, :].k((h, 0))
                            fw.stt(dst, Scur[rows, jj, :].k((h, jj)), dec[cc][rows, n:n + 1],
                                   pkv[rows, n * 64:(n + 1) * 64], ALU.mult, ALU.add)
                if not need_o or getattr(self, "hg_stage", 9) < 4:
                    continue
                o_ = osb[it % 2]
                fw.cp(o_[:], pso[0:64, :], eng="act")
                fw.tt(o_[:], o_[:], pso2[0:64, :], ALU.add)
                if d == 0:
                    fw.dma("pool", hgv[:, :, tok0:tok0 + 128].k(("t", tile)), o_[:].r("v (h t) -> v h t", h=4))
                    continue
                of_ = of[it % 2]
                g2 = gt[it % 2]
                fw.dma("pool", of_[:].r("v (h t) -> v h t", h=4), hgv[:, :, tok0:tok0 + 128].k(("t", tile)))
                fw.dma("pool", g2[:].r("v (h t) -> v h t", h=4), gv[:, :, tok0:tok0 + 128])
                fw.tt(o_[:], o_[:], of_[:], ALU.add)
                fw.tt(sq[:], o_[:], o_[:], ALU.mult, eng="pool")
                pss = self.PS[0]
                fw.mm(pss[0:64, 0:512], ones64, sq[:])
                fw.act(rs[:], pss[0:64, 0:512], AF.Ln, bias=epsb[0:64, :], scale=1.0 / 64.0)
                fw.act(rs[:], rs[:], AF.Exp, scale=-0.5)
                fw.tt(o_[:], o_[:], rs[:], ALU.mult)
                fw.act(sgt[:], g2[:], AF.Exp, scale=-1.0)
                fw.ts(sgt[:], sgt[:], 1.0, ALU.add, eng="pool")
                fw.op("dve", lambda e: e.reciprocal(out=sgt[:].ap, in_=sgt[:].ap), [sgt[:]], [sgt[:]])
                fw.tt(sgt[:], sgt[:], g2[:], ALU.mult, eng="pool")
                ob_ = obf[it % 2]
                fw.stt(ob_[:], o_[:], ng, sgt[:], ALU.mult, ALU.mult)
                fw.dma("pool", bro[:, :, tok0:tok0 + 128].k(("hg", tile)), ob_[:].r("v (h t) -> v h t", h=4))
            fw.barrier()
        fw.release(m)

    def phase_hgrn(self, l):
        fw = self.fw
        m = fw.mark()
        id64 = self.cst[0:64, C_IDENT:C_IDENT + 64]
        ones64 = self.cst[0:64, C_ONES:C_ONES + 64]
        MI = [self.c(C_MIF), self.c(C_MIB)]
        CM = [self.c(C_CMASK, 8), self.c(C_CMASKR, 8)]
        hgv = [self.hgo[:].r("v (h t) -> v h t", h=4), self.hgo2[:].r("v (h t) -> v h t", h=4)]
        uq = self.uT[1536:1792, :].r("(h d) t -> d h t", d=64)
        uf = [self.uT[1792:2048, :].r("(h d) t -> d h t", d=64), self.uT[2048:2304, :].r("(h d) t -> d h t", d=64)]
        ui = self.uT[2304:2560, :].r("(h d) t -> d h t", d=64)
        gv = self.uT[2560:2816, :].r("(h v) t -> v h t", v=64)
        bro = self.brT[512:768, :].r("(h v) t -> v h t", v=64)
        epsb = fw.sb("epsb", [64, 1])
        fw.memset(epsb[:], RMS_EPS)
        lbv = fw.sb("lbv", [64, 8])
        oml = fw.sb("oml", [64, 8])
        if l > 0:
            for d in range(2):
                c1 = PP["lbl64"] + (d * 2 + 1) * 4
                c0 = PP["lbl64"] + (d * 2 + 0) * 4
                fw.tt(lbv[:, d * 4:(d + 1) * 4], self.pp[0:64, c1:c1 + 4], self.pp[0:64, c0:c0 + 4], ALU.subtract)
            fw.act(lbv[:], lbv[:], AF.Exp, scale=-1.0)
            fw.ts(lbv[:], lbv[:], 1.0, ALU.add)
            fw.op("dve", lambda e: e.reciprocal(out=lbv[:].ap, in_=lbv[:].ap), [lbv[:]], [lbv[:]])
            fw.ts(oml[:], lbv[:], -1.0, ALU.mult, 1.0, ALU.add)
        SM = fw.sb("hg_sm", [64, 2048])
        fw.cp(SM[:].r("p (a b) -> p a b", b=16), self.cst[0:64, C_SCAN16:C_SCAN16 + 16].un(1).bc([64, 128, 16]))
        S = fw.sb("hg_S", [64, 9, 8, 64])
        fw.memset(S[:, 0, :, :], 0.0)
        F = 2048
        qin = fw.sb("hg_qin", [64, 4, 512])
        fin = fw.sb("hg_fin", [64, 4, 512])
        T = [fw.sb("hg_T%d" % i, [64, F]) for i in range(5)]
        qd32 = [fw.sb("hg_qd32%d" % d, [64, 4, 512]) for d in range(2)]
        qd16 = [fw.sb("hg_qd16%d" % d, [64, 4, 512], BF16) for d in range(2)]
        ki16 = [fw.sb("hg_ki16%d" % d, [64, 4, 512], BF16) for d in range(2)]
        ke32 = [fw.sb("hg_ke32%d" % d, [64, 4, 512]) for d in range(2)]
        vin = [fw.sb("hg_vin%d" % d, [64, 4, 512]) for d in range(2)]
        dec = [fw.sb("hg_dec%d" % d, [64, 4, 32]) for d in range(2)]
        ketok = [fw.sb("hg_ket%d" % d, [128, 256], BF16) for d in range(2)]
        vtok = [fw.sb("hg_vt%d" % d, [128, 256], BF16) for d in range(2)]
        vexp = [fw.sb("hg_vx%d" % d, [128, 8, 256], BF16) for d in range(2)]
        atm = [fw.sb("hg_atm%d" % d, [128, 4, 128], BF16) for d in range(2)]
        kvs = [fw.sb("hg_kvs%d" % d, [64, 8, 4, 64]) for d in range(2)]
        osb = [fw.sb("hg_o%d" % d, [64, 512]) for d in range(2)]
        ot = [fw.sb("hg_ot%d" % d, [64, 512]) for d in range(2)]
        CH = ["dve", "pool"]

        def prep(d, tok0, TBk):
            n = 4 * TBk
            nch = TBk // 16
            v3 = lambda b: b[:, 0:n].r("p (h t) -> p h t", h=4)
            c3 = lambda b: b[:, 0:n].r("p (c s) -> p c s", s=16)
            fw.dma("sp", qin[:, :, 0:TBk], uq[:, :, tok0:tok0 + TBk])
            fw.dma("sp", fin[:, :, 0:TBk], uf[d][:, :, tok0:tok0 + TBk])
            fw.dma("sp", vin[d][:, :, 0:TBk], ui[:, :, tok0:tok0 + TBk])
            sg, lf, kk, b_, t_ = T
            fw.act(v3(sg), fin[:, :, 0:TBk], AF.Exp, scale=-1.0)
            fw.ts(sg[:, 0:n], sg[:, 0:n], 1.0, ALU.add, eng="pool")
            fw.op("dve", lambda e: e.reciprocal(out=sg[:, 0:n].ap, in_=sg[:, 0:n].ap), [sg[:, 0:n]], [sg[:, 0:n]])
            if l > 0:
                for h in range(4):
                    j = d * 4 + h
                    fw.ts(sg[:, h * TBk:(h + 1) * TBk], sg[:, h * TBk:(h + 1) * TBk], oml[:, j:j + 1], ALU.mult,
                          lbv[:, j:j + 1], ALU.add)
            fw.act(lf[:, 0:n], sg[:, 0:n], AF.Ln)
            fw.ts(kk[:, 0:n], sg[:, 0:n], -1.0, ALU.mult, 1.0, ALU.add, eng="pool")
            fw.scan(b_[:, 0:n], SM[:, 0:n], lf[:, 0:n], 0.0, ALU.mult, ALU.add)
            tot = c3(b_)[:, :, 15:16]
            fw.act(dec[d][:, :, 0:nch], b_[:, 0:n].r("p (h c s) -> p h c s", h=4, s=16)[:, :, :, 15], AF.Exp)
            if d == 1:
                fw.tt(lf[:, 0:n], lf[:, 0:n], b_[:, 0:n], ALU.subtract, eng="pool")
                fw.tt(c3(lf), c3(lf), tot.bc([64, n // 16, 16]), ALU.add)
                bcur = lf
            else:
                bcur = b_
            fw.act(t_[:, 0:n], bcur[:, 0:n], AF.Exp)
            fw.stt(qd32[d][:, :, 0:TBk], qin[:, :, 0:TBk], 0.125, v3(t_), ALU.mult, ALU.mult)
            fw.cp(qd16[d][:, :, 0:TBk], qd32[d][:, :, 0:TBk], eng="pool")
            fw.act(t_[:, 0:n], bcur[:, 0:n], AF.Exp, scale=-1.0)
            fw.tt(ki16[d][:, :, 0:TBk], v3(kk), v3(t_), ALU.mult)
            fw.tt(c3(t_), tot.bc([64, n // 16, 16]), c3(bcur), ALU.subtract, eng="pool")
            fw.act(t_[:, 0:n], t_[:, 0:n], AF.Exp)
            fw.tt(ke32[d][:, :, 0:TBk], v3(kk), v3(t_), ALU.mult)

        def tile_pre(d, off, tile, need_o):
            pst = self.PS[d]
            for h in range(4):
                fw.tr(pst[:, h * 64:(h + 1) * 64], ke32[d][:, h, off:off + 128], id64)
                fw.tr(pst[:, 256 + h * 64:256 + (h + 1) * 64], vin[d][:, h, off:off + 128], id64)
            fw.cp(ketok[d][:], pst[:, 0:256], eng="act")
            fw.cp(vtok[d][:], pst[:, 256:512], eng="act")
            fw.tt(vexp[d][:], vtok[d][:].un(1).bc([128, 8, 256]), CM[d].un(2).bc([128, 8, 256]), ALU.mult,
                  eng=("dve" if d == 1 else "pool"))
            poi = self.PS[3 + d * 2]
            if need_o:
                pat = self.PS[2]
                for h in range(4):
                    fw.mm(pat[:, h * 128:(h + 1) * 128].k(h), ki16[d][:, h, off:off + 128], qd16[d][:, h, off:off + 128])
                fw.tt(atm[d][:], pat[:].r("p (h t) -> p h t", h=4), MI[d].un(1).bc([128, 4, 128]), ALU.mult)
                for h in range(4):
                    fw.mm(poi[0:64, h * 128:(h + 1) * 128].k(h), vtok[d][:, h * 64:(h + 1) * 64], atm[d][:, h, :])
            pkv = self.PS[7]
            for h in range(4):
                fw.mm(pkv[0:64, 0:512], ketok[d][:, h * 64:(h + 1) * 64], vexp[d][:, :, h * 64:(h + 1) * 64])
                fw.cp(kvs[d][:, :, h, :].k(h), pkv[0:64, 0:512].r("p (j v) -> p j v", j=8), eng="act")

        def chain_step(d, j, off, need_o):
            pox = self.PS[4 + d * 2]
            ce = CH[d]
            dsl = slice(d * 4, (d + 1) * 4)
            c0 = off // 16
            n = j if d == 0 else 7 - j
            if need_o:
                for h in range(4):
                    fw.mm(pox[0:64, h * 128 + n * 16:h * 128 + (n + 1) * 16].k(h),
                          S[:, j, d * 4 + h, :].k((d, j)), qd32[d][:, h, off + n * 16:off + (n + 1) * 16])
            dcb = dec[d][:, :, c0 + n:c0 + n + 1].bc([64, 4, 64])
            dst = S[:, j + 1, dsl, :].k((d, j + 1)) if j < 7 else S[:, 0, dsl, :].k((d, 0))
            fw.tt(S[:, 8, dsl, :].k((d, 8)) if j == 7 else dst, S[:, j, dsl, :].k((d, j)), dcb, ALU.mult, eng=ce)
            src = S[:, 8, dsl, :].k((d, 8)) if j == 7 else dst
            fw.tt(dst, src, kvs[d][:, j, :, :], ALU.add, eng=ce)

        def tile_post(d, tile, need_o):
            if not need_o:
                return
            tok0 = tile * 128
            poi = self.PS[3 + d * 2]
            pox = self.PS[4 + d * 2]
            o_ = osb[d]
            fw.cp(o_[:], poi[0:64, :], eng="act")
            fw.tt(o_[:], o_[:], pox[0:64, :], ALU.add, eng="dve")
            fw.dma("pool", hgv[d][:, :, tok0:tok0 + 128].k(("t", tile)), o_[:].r("v (h t) -> v h t", h=4))

        steps = [(0, 0, 256)] + [(LC + 512 * i, LC + 512 * (7 - i), 512) for i in range(8)]
        for si, (tf0, tb0, TBk) in enumerate(steps):
            prep(0, tf0, TBk)
            prep(1, tb0, TBk)
            nt = TBk // 128
            for i in range(nt):
                info = []
                for d in range(2):
                    off = i * 128 if d == 0 else (nt - 1 - i) * 128
                    t0 = (tf0 if d == 0 else tb0) + off
                    tile = t0 // 128
                    need_o = not (l == DEPTH - 1 and tile < 2)
                    info.append((off, tile, need_o))
                    tile_pre(d, off, tile, need_o)
                for j in range(8):
                    for d in range(2):
                        chain_step(d, j, info[d][0], info[d][2])
                for d in range(2):
                    tile_post(d, info[d][1], info[d][2])
        fw.barrier()
        A = T[0:3]
        obf3 = ki16[0]
        ng = self.pp[0:64, PP["hgng"]:PP["hgng"] + 1]
        for gi, (tok0, TB) in enumerate(GROUPS):
            if gi == 0 and l == DEPTH - 1:
                continue
            n = 4 * TB
            v3 = lambda b: b[:, 0:n].r("p (h t) -> p h t", h=4)
            o1, o2, g2 = qd32[0], qd32[1], ke32[0]
            fw.dma("sp", o1[:, :, 0:TB], hgv[0][:, :, tok0:tok0 + TB])
            fw.dma("sp", o2[:, :, 0:TB], hgv[1][:, :, tok0:tok0 + TB])
            fw.dma("pool", g2[:, :, 0:TB], gv[:, :, tok0:tok0 + TB])
            o_, sq_, sg_ = A
            fw.tt(v3(o_), o1[:, :, 0:TB], o2[:, :, 0:TB], ALU.add)
            fw.tt(sq_[:, 0:n], o_[:, 0:n], o_[:, 0:n], ALU.mult, eng="pool")
            for h in range(4):
                pss = self.PS[h % 2]
                fw.mm(pss[0:64, 0:TB], ones64, sq_[:, h * TB:(h + 1) * TB])
                fw.act(sq_[:, h * TB:(h + 1) * TB].k(h), pss[0:64, 0:TB], AF.Ln, bias=epsb[:], scale=1.0 / 64.0)
            fw.act(sq_[:, 0:n], sq_[:, 0:n], AF.Exp, scale=-0.5)
            fw.tt(o_[:, 0:n], o_[:, 0:n], sq_[:, 0:n], ALU.mult)
            fw.act(v3(sg_), g2[:, :, 0:TB], AF.Exp, scale=-1.0)
            fw.ts(sg_[:, 0:n], sg_[:, 0:n], 1.0, ALU.add, eng="pool")
            fw.op("dve", lambda e: e.reciprocal(out=sg_[:, 0:n].ap, in_=sg_[:, 0:n].ap), [sg_[:, 0:n]], [sg_[:, 0:n]])
            fw.tt(v3(sg_), v3(sg_), g2[:, :, 0:TB], ALU.mult, eng="pool")
            fw.stt(obf3[:, :, 0:TB], v3(o_), ng, v3(sg_), ALU.mult, ALU.mult)
            fw.dma("pool", bro[:, :, tok0:tok0 + TB].k(("hg", gi)), obf3[:, :, 0:TB])
        fw.release(m)

    def phase_ssd(self, l):
        fw = self.fw
        m = fw.mark()
        ident = self.c(C_IDENT)
        ones = self.c(C_ONES)
        uv = self.uv()
        brv = self.brv()
        dtb = self.rb[:, RB["dtbias"]:RB["dtbias"] + 8]
        dsk = self.rb[:, RB["ssdd"]:RB["ssdd"] + 4]
        ngb = self.rb[:, RB["ssdng"]:RB["ssdng"] + 256]
        onec = fw.sb("sd_one", [128, 1])
        fw.memset(onec[:], 1.0)
        epsb = fw.sb("sd_eps", [128, 1])
        fw.memset(epsb[:], RMS_EPS)
        nea = fw.sb("sd_nea", [128, 8])
        fw.act(nea[:], self.rb[:, RB["alog"]:RB["alog"] + 8], AF.Exp)
        fw.ts(nea[:], nea[:], -1.0, ALU.mult)
        ST = fw.sb("sd_ST", [128, 4, 64])
        xin = [fw.sb("sd_xin%d" % i, [128, 4, 130]) for i in range(2)]
        dtr = [fw.sb("sd_dtr%d" % i, [128, 8]) for i in range(2)]
        zin = [fw.sb("sd_zin%d" % i, [128, 2, 128]) for i in range(2)]
        xc = fw.sb("sd_xc", [128, 4, 128])
        sg = fw.sb("sd_sg", [128, 4, 128])
        xbc = [fw.sb("sd_xbc%d" % i, [128, 4, 128]) for i in range(2)]
        xstok = [fw.sb("sd_xst%d" % i, [128, 256]) for i in range(2)]
        btok = [fw.sb("sd_bt%d" % i, [128, 128]) for i in range(2)]
        dtt = fw.sb("sd_dtt", [128, 8])
        atok = fw.sb("sd_atok", [128, 8])
        negcum = fw.sb("sd_negcum", [128, 4])
        expcum = fw.sb("sd_expcum", [128, 4])
        totE = fw.sb("sd_totE", [128, 4])
        edec = fw.sb("sd_edec", [128, 4])
        cbs = [fw.sb("sd_cbs%d" % i, [128, 256]) for i in range(2)]
        xdt = [fw.sb("sd_xdt%d" % i, [128, 4, 64]) for i in range(2)]
        arow = [fw.sb("sd_arow%d" % i, [128, 128]) for i in range(2)]
        E = [fw.sb("sd_E%d" % i, [128, 128]) for i in range(2)]
        W = [fw.sb("sd_W%d" % i, [128, 128]) for i in range(2)]
        bdec = [fw.sb("sd_bdec%d" % i, [128, 128]) for i in range(2)]
        ydg = fw.sb("sd_ydg", [128, 256])
        y = [fw.sb("sd_y%d" % i, [128, 256]) for i in range(2)]
        yfl = [fw.sb("sd_yf%d" % i, [128, 256]) for i in range(2)]
        xsd = fw.sb("sd_xsd", [128, 256])
        zs = fw.sb("sd_zs", [128, 256])
        zt = fw.sb("sd_zt", [128, 256])
        yg = fw.sb("sd_yg", [128, 256])
        ysq = fw.sb("sd_ysq", [128, 256])
        ss = fw.sb("sd_ss", [128, 1])
        ob = [fw.sb("sd_ob%d" % i, [128, 2, 128], BF16) for i in range(2)]
        for d in range(2):
            order = list(range(34)) if d == 0 else [1, 0] + list(range(33, 1, -1))
            TRI = self.c(C_TRIF) if d == 0 else self.c(C_TRIB)
            NEGM = self.c(C_NEGF) if d == 0 else self.c(C_NEGB)
            j0 = d * 4
            fw.memset(ST[:], 0.0)
            for it, tile in enumerate(order):
                if it >= getattr(self, "sd_limit", 99):
                    break
                tok0 = tile * 128
                need_o = not (l == DEPTH - 1 and tile < 2)
                xi = xin[it % 2]
                self.load_halo(xi, 24, 4, tok0, 128)
                dr = dtr[it % 2]
                fw.dma("sp", dr[:], self.dtT[tok0:tok0 + 128, :])
                if d == 1 and need_o:
                    zi = zin[it % 2]
                    fw.dma("sp", zi[:], uv[:, 22:24, tok0:tok0 + 128])
                for j in range(4):
                    self.conv3(xc[:, j, :], xi[:, j, :], PP["sdcw"] + 3 * j, 128,
                               bias=self.pp[:, PP["sdcb"] + j:PP["sdcb"] + j + 1])
                fw.act(sg[:], xc[:], AF.Exp, scale=-1.0)
                fw.ts(sg[:], sg[:], 1.0, ALU.add, eng="pool")
                fw.op("dve", lambda e: e.reciprocal(out=sg[:].ap, in_=sg[:].ap), [sg[:]], [sg[:]])
                xb = xbc[it % 2]
                fw.tt(xb[:], xc[:], sg[:], ALU.mult, eng="pool")
                pt0 = self.PS[0]
                pt1 = self.PS[1]
                for j in range(3):
                    fw.tr(pt0[:, j * 128:(j + 1) * 128], xb[:, j, :], ident)
                xs_ = xstok[it % 2]
                bt_ = btok[it % 2]
                fw.cp(xs_[:], pt0[:, 0:256], eng="act")
                fw.cp(bt_[:], pt0[:, 256:384], eng="act")
                fw.tt(dtt[:], dr[:], dtb, ALU.add)
                fw.act(dtt[:], dtt[:], AF.Exp)
                fw.act(dtt[:], dtt[:], AF.Ln, bias=onec[:], scale=1.0)
                fw.tt(atok[:], dtt[:], nea[:], ALU.mult)
                fw.mm(pt1[:, 16:20], TRI, atok[:, j0:j0 + 4])
                fw.mm(pt1[:, 24:28], ones, atok[:, j0:j0 + 4])
                fw.ts(negcum[:], pt1[:, 16:20], -1.0, ALU.mult)
                fw.act(expcum[:], pt1[:, 16:20], AF.Exp)
                fw.act(totE[:], pt1[:, 24:28], AF.Exp)
                fw.tt(edec[:], pt1[:, 24:28], negcum[:], ALU.add)
                fw.act(edec[:], edec[:], AF.Exp)
                cb_ = cbs[it % 2]
                for g in range(2):
                    gr = slice(g * 64, (g + 1) * 64)
                    pcb = self.PS[2 + g]
                    fw.mm(pcb[:, 0:128], xb[gr, 2, :], xb[gr, 3, :])
                    fw.cp(cb_[:, g * 128:(g + 1) * 128].k(g), pcb[:, 0:128], eng="act")
                xd = xdt[it % 2]
                fw.tt(xd[:], xs_[:].r("p (h q) -> p h q", h=4), dtt[:, j0:j0 + 4].un(2).bc([128, 4, 64]), ALU.mult)
                py = self.PS[5]
                pyos = [self.PS[6], self.PS[2]]
                pss = self.PS[7]
                for h in range(4):
                    g = h // 2
                    gr = slice(g * 64, (g + 1) * 64)
                    ar = arow[h % 2]
                    fw.act(ar[:], TRI, AF.Copy, scale=atok[:, j0 + h:j0 + h + 1])
                    pd = self.PS[3 + h % 2]
                    fw.mm(pd[:, 0:128], ones, ar[:], start=True, stop=False)
                    fw.mm(pd[:, 0:128], ident, NEGM, start=False, stop=True)
                    e_ = E[h % 2]
                    fw.act(e_[:], pd[:, 0:128], AF.Exp, bias=negcum[:, h:h + 1])
                    w_ = W[h % 2]
                    fw.tt(w_[:], e_[:], cb_[:, g * 128:(g + 1) * 128].k(g), ALU.mult, eng="pool")
                    if need_o:
                        fw.mm(py[:, h * 64:(h + 1) * 64].k(h), w_[:], xd[:, h, :])
                        fw.mm(pyos[g][:, (h % 2) * 64:(h % 2 + 1) * 64].k(h), xb[gr, 3, :], ST[gr, h, :].k(h))
                    bd = bdec[h % 2]
                    fw.ts(bd[:], bt_[:], edec[:, h:h + 1], ALU.mult)
                    fw.mm(pss[:, h * 64:(h + 1) * 64].k(h), bd[:], xd[:, h, :])
                    fw.stt(ST[gr, h, :].k(h), ST[gr, h, :].k(h), totE[gr, h:h + 1], pss[gr, h * 64:(h + 1) * 64].k(h),
                           ALU.mult, ALU.add)
                if not need_o:
                    continue
                fw.cp(ydg[:], py[:, 0:256], eng="act")
                y_ = y[it % 2]
                for h in range(4):
                    cs = slice(h * 64, (h + 1) * 64)
                    fw.stt(y_[:, cs], pyos[h // 2][:, (h % 2) * 64:(h % 2 + 1) * 64].k(h), expcum[:, h:h + 1], ydg[:, cs],
                           ALU.mult, ALU.add)
                if d == 0:
                    fw.dma("pool", self.yf[tok0:tok0 + 128, :].k(("t", tile)), y_[:])
                    continue
                yf_ = yfl[it % 2]
                fw.dma("pool", yf_[:], self.yf[tok0:tok0 + 128, :].k(("t", tile)))
                fw.tt(xsd[:].r("p (h q) -> p h q", h=4), xs_[:].r("p (h q) -> p h q", h=4),
                      dsk.un(2).bc([128, 4, 64]), ALU.mult, eng="pool")
                fw.tt(y_[:], y_[:], yf_[:], ALU.add)
                fw.tt(y_[:], y_[:], xsd[:], ALU.add)
                pz = self.PS[4]
                for cc in range(2):
                    fw.tr(pz[:, 256 + cc * 128:256 + (cc + 1) * 128], zi[:, cc, :], ident)
                fw.cp(zt[:], pz[:, 256:512], eng="act")
                fw.act(zs[:], zt[:], AF.Exp, scale=-1.0)
                fw.ts(zs[:], zs[:], 1.0, ALU.add, eng="pool")
                fw.op("dve", lambda e: e.reciprocal(out=zs[:].ap, in_=zs[:].ap), [zs[:]], [zs[:]])
                fw.tt(zs[:], zs[:], zt[:], ALU.mult, eng="pool")
                fw.tt(yg[:], y_[:], zs[:], ALU.mult)
                fw.tt(ysq[:], yg[:], yg[:], ALU.mult, eng="pool")
                fw.red(ss[:], ysq[:], ALU.add)
                fw.act(ss[:], ss[:], AF.Ln, bias=epsb[:], scale=1.0 / 256.0)
                fw.act(ss[:], ss[:], AF.Exp, scale=-0.5)
                fw.stt(yg[:], yg[:], ss[:], ngb, ALU.mult, ALU.mult)
                po = self.PS[1]
                for cc in range(2):
                    fw.tr(po[:, 256 + cc * 128:256 + (cc + 1) * 128], yg[:, cc * 128:(cc + 1) * 128], ident)
                o_ = ob[it % 2]
                fw.cp(o_[:], po[:, 256:512].r("p (c t) -> p c t", c=2), eng="act")
                fw.dma("pool", brv[:, 6:8, tok0:tok0 + 128].k(("sd", tile)), o_[:])
            fw.barrier()
        fw.release(m)

    def layernorm_fm(self, x, TB, gname, out, W):
        fw = self.fw
        ones = self.c(C_ONES)
        ps_s, ps_q = self.PS[6], self.PS[7]
        for Dc in range(KC):
            fw.mm(ps_s[:, 0:TB], ones, x[:, Dc, 0:TB], start=(Dc == 0), stop=(Dc == KC - 1))
        for Dc in range(KC):
            sq = W["sq"][Dc % 2]
            fw.act(sq[:, 0:TB], x[:, Dc, 0:TB], AF.Square)
            fw.mm(ps_q[:, 0:TB], ones, sq[:, 0:TB], start=(Dc == 0), stop=(Dc == KC - 1))
        mean, msq, rstd, mr = W["mean"], W["msq"], W["rstd"], W["mr"]
        fw.ts(mean[:, 0:TB], ps_s[:, 0:TB], 1.0 / D, ALU.mult)
        fw.tt(msq[:, 0:TB], mean[:, 0:TB], mean[:, 0:TB], ALU.mult, eng="pool")
        fw.stt(rstd[:, 0:TB], ps_q[:, 0:TB], 1.0 / D, msq[:, 0:TB], ALU.mult, ALU.subtract)
        fw.act(rstd[:, 0:TB], rstd[:, 0:TB], AF.Ln, bias=W["lneps"][:], scale=1.0)
        fw.act(rstd[:, 0:TB], rstd[:, 0:TB], AF.Exp, scale=-0.5)
        fw.tt(mr[:, 0:TB], mean[:, 0:TB], rstd[:, 0:TB], ALU.mult, eng="pool")
        g0 = PP[gname + "g"]
        b0 = PP[gname + "b"]
        for Dc in range(KC):
            t = W["t"][Dc % 2]
            fw.tt(t[:, 0:TB], x[:, Dc, 0:TB], rstd[:, 0:TB], ALU.mult)
            fw.tt(t[:, 0:TB], t[:, 0:TB], mr[:, 0:TB], ALU.subtract, eng="pool")
            fw.act(out[:, Dc, 0:TB].k(Dc), t[:, 0:TB], AF.Identity, bias=self.pp[:, b0 + Dc:b0 + Dc + 1],
                   scale=self.pp[:, g0 + Dc:g0 + Dc + 1])

    def ln_work(self):
        fw = self.fw
        W = {"sq": [fw.sb("ln_sq%d" % i, [128, 512]) for i in range(2)],
             "t": [fw.sb("ln_t%d" % i, [128, 512]) for i in range(2)],
             "mean": fw.sb("ln_mean", [128, 512]), "msq": fw.sb("ln_msq", [128, 512]),
             "rstd": fw.sb("ln_rstd", [128, 512]), "mr": fw.sb("ln_mr", [128, 512]),
             "lneps": fw.sb("ln_eps", [128, 1])}
        fw.memset(W["lneps"][:], LN_EPS)
        return W

    def phase_merge(self, l):
        fw = self.fw
        m = fw.mark()
        ident = self.c(C_IDENT)
        wbr = fw.sb("wbr", [128, 8, D], BF16)
        wo = fw.sb("wo", [128, KC, D], BF16)
        wr = fw.sb("wr", [128, KC, NE])
        m1 = fw.mark()
        stage = [fw.sb("mst%d" % i, [128, 4 * D]) for i in range(2)]
        stb = [fw.sb("mstb%d" % i, [128, 4 * D], BF16) for i in range(2)]
        wgv = self.w_gate_d[l].r("(kc p) n -> p kc n", p=128)
        wov = self.w_o_d[l].r("(kc p) n -> p kc n", p=128)
        wgc5 = self.wgc[:].r("dc p (kc k c) -> p dc kc k c", kc=KC, k=4)
        i = 0
        for kc in range(KC):
            self.load_cast(stb[kc % 2][:], wgv[:, kc, :], stage, 4 * D, i)
            for k in range(4):
                fw.dma("pool", wgc5[:, :, kc, k, :].k(("wgc", kc, k)),
                       stb[kc % 2][:, k * D:(k + 1) * D].r("p (dc c) -> p dc c", dc=8))
            i += 1
        for kc in range(KC):
            self.load_cast(wo[:, kc, :].k(kc), wov[:, kc, :], stage, D, i)
            i += 1
        for k in range(4):
            for cc in range(2):
                self.load_cast(wbr[:, k * 2 + cc, :].k(k * 2 + cc), self.w_br_d[l, k, cc * 128:(cc + 1) * 128, :], stage, D, i)
                i += 1
        fw.dma("sp", wr[:], self.w_router_d[:].r("(kc p) e -> p kc e", p=128))
        fw.release(m1)
        W = self.ln_work()
        rt = [fw.sb("mrt%d" % i, [128, KC, 512]) for i in range(1)]
        ht = [fw.sb("mht%d" % i, [128, KC, 512], BF16) for i in range(1)]
        bt = [fw.sb("mbt%d" % i, [128, 8, 512], BF16) for i in range(1)]
        wgr = [fw.sb("mwg%d" % i, [128, KC, 4, 128], BF16) for i in range(3)]
        yb = fw.sb("myb", [128, KC, 512], BF16)
        gs = [fw.sb("mgs%d" % i, [128, 512]) for i in range(2)]
        yacc = fw.sb("myacc", [128, 512])
        ytmp = [fw.sb("mytmp%d" % i, [128, 512]) for i in range(2)]
        h2 = fw.sb("mh2", [128, KC, 512])
        gT = fw.sb("mgT", [16, 512])
        R = {n: fw.sb("r_" + n, [128, w]) for n, w in
             [("lg", 16), ("mx", 1), ("e", 16), ("ss", 1), ("sc", 16), ("sel", 16), ("m1", 4), ("eq", 16), ("x2", 16),
              ("m2", 4), ("gsc", 4), ("gm", 1), ("ing", 4), ("t2m", 16), ("selm", 16), ("w", 16), ("ws", 1), ("gate", 16)]}
        resv = self.resT[:].r("(kc p) t -> p kc t", p=128)
        l1v = self.lat1T[:].r("(kc p) t -> p kc t", p=128)
        h2v = self.h2T[:].r("(kc p) t -> p kc t", p=128)
        brv = self.brv()
        brb = self.rb[:, RB["brouter"]:RB["brouter"] + 16]
        wi = 0
        for gi, (tok0, TB) in enumerate(GROUPS):
            if gi == 0 and l == DEPTH - 1:
                continue
            md = self.mod_for(gi)
            r, h, br = rt[0], ht[0], bt[0]
            res1 = r
            lat1 = r
            h2b = h
            fw.dma("sp", r[:, :, 0:TB], resv[:, :, tok0:tok0 + TB])
            fw.dma("pool", br[:, :, 0:TB], brv[:, :, tok0:tok0 + TB])
            for kc in range(KC):
                fw.act(h[:, kc, 0:TB].k(kc), r[:, kc, 0:TB].k(kc), AF.Identity, bias=md[:, kc:kc + 1], scale=md[:, 8 + kc:9 + kc])
            for kc in range(KC):
                fw.ts(r[:, kc, 0:TB].k(kc), r[:, kc, 0:TB].k(kc), ALPHA, ALU.mult, eng="pool")
            for Dc in range(KC):
                wg = wgr[wi % 3]
                wi += 1
                fw.dma("sp", wg[:], self.wgc[Dc].r("p (kc k c) -> p kc k c", kc=KC, k=4))
                for k in range(4):
                    pg = self.PS[k % 2]
                    for kc in range(KC):
                        fw.mm(pg[:, 0:TB], wg[:, kc, k, :], h[:, kc, 0:TB],
                              start=(kc == 0), stop=(kc == KC - 1))
                    pp_ = self.PS[2 + k % 2]
                    for cc in range(2):
                        fw.mm(pp_[:, 0:TB], wbr[:, k * 2 + cc, Dc * 128:(Dc + 1) * 128], br[:, k * 2 + cc, 0:TB],
                              start=(cc == 0), stop=(cc == 1))
                    g_ = gs[k % 2]
                    bcol = PP["bgate"] + k * 8 + Dc
                    fw.act(g_[:, 0:TB], pg[:, 0:TB], AF.Sigmoid, bias=self.pp[:, bcol:bcol + 1])
                    if k == 0:
                        fw.tt(yacc[:, 0:TB], g_[:, 0:TB], pp_[:, 0:TB], ALU.mult)
                    else:
                        t_ = ytmp[k % 2]
                        fw.tt(t_[:, 0:TB], g_[:, 0:TB], pp_[:, 0:TB], ALU.mult)
                        dst = yacc[:, 0:TB] if k < 3 else yb[:, Dc, 0:TB].k(Dc)
                        fw.tt(dst, yacc[:, 0:TB], t_[:, 0:TB], ALU.add, eng="pool")
            g1c = 16
            for Dc in range(KC):
                po = self.PS[4 + Dc % 2]
                for kc in range(KC):
                    fw.mm(po[:, 0:TB], wo[:, kc, Dc * 128:(Dc + 1) * 128], yb[:, kc, 0:TB], start=(kc == 0), stop=(kc == KC - 1))
                fw.stt(res1[:, Dc, 0:TB].k(Dc), po[:, 0:TB], md[:, g1c + Dc:g1c + Dc + 1], r[:, Dc, 0:TB].k(Dc), ALU.mult, ALU.add)
            self.layernorm_fm(res1, TB, "ln1", lat1, W)
            fw.dma("pool", l1v[:, :, tok0:tok0 + TB].k(("g", gi)), lat1[:, :, 0:TB])
            for kc in range(KC):
                fw.act(h2[:, kc, 0:TB].k(kc), lat1[:, kc, 0:TB].k(kc), AF.Identity, bias=md[:, 24 + kc:25 + kc], scale=md[:, 32 + kc:33 + kc])
                fw.cp(h2b[:, kc, 0:TB].k(kc), h2[:, kc, 0:TB].k(kc), eng="pool")
            fw.dma("pool", h2v[:, :, tok0:tok0 + TB].k(("g", gi)), h2b[:, :, 0:TB])
            for jt in range(TB // 128):
                pl = self.PS[0]
                for kc in range(KC):
                    fw.mm(pl[:, 0:16], h2[:, kc, jt * 128:(jt + 1) * 128], wr[:, kc, :], start=(kc == 0), stop=(kc == KC - 1))
                fw.cp(R["lg"][:], pl[:, 0:16], eng="act")
                fw.red(R["mx"][:], R["lg"][:], ALU.max)
                fw.ts(R["mx"][:], R["mx"][:], -1.0, ALU.mult)
                fw.act(R["e"][:], R["lg"][:], AF.Exp, bias=R["mx"][:])
                fw.red(R["ss"][:], R["e"][:], ALU.add)
                fw.op("dve", lambda e: e.reciprocal(out=R["ss"][:].ap, in_=R["ss"][:].ap), [R["ss"][:]], [R["ss"][:]])
                fw.ts(R["sc"][:], R["e"][:], R["ss"][:], ALU.mult)
                fw.tt(R["sel"][:], R["sc"][:], brb, ALU.add)
                sel3 = R["sel"][:].r("p (g e) -> p g e", g=4)
                fw.red(R["m1"][:], sel3, ALU.max)
                fw.tt(R["eq"][:].r("p (g e) -> p g e", g=4), sel3, R["m1"][:].un(2).bc([128, 4, 4]), ALU.is_equal)
                fw.stt(R["x2"][:], R["eq"][:], -1e30, R["sel"][:], ALU.mult, ALU.add)
                fw.red(R["m2"][:], R["x2"][:].r("p (g e) -> p g e", g=4), ALU.max)
                fw.tt(R["gsc"][:], R["m1"][:], R["m2"][:], ALU.add)
                fw.red(R["gm"][:], R["gsc"][:], ALU.max)
                fw.ts(R["ing"][:], R["gsc"][:], R["gm"][:], ALU.is_ge)
                fw.tt(R["t2m"][:].r("p (g e) -> p g e", g=4), sel3, R["m2"][:].un(2).bc([128, 4, 4]), ALU.is_ge)
                fw.tt(R["selm"][:].r("p (g e) -> p g e", g=4), R["t2m"][:].r("p (g e) -> p g e", g=4),
                      R["ing"][:].un(2).bc([128, 4, 4]), ALU.mult)
                fw.tt(R["w"][:], R["sc"][:], R["selm"][:], ALU.mult)
                fw.red(R["ws"][:], R["w"][:], ALU.add)
                fw.op("dve", lambda e: e.reciprocal(out=R["ws"][:].ap, in_=R["ws"][:].ap), [R["ws"][:]], [R["ws"][:]])
                fw.ts(R["gate"][:], R["w"][:], R["ws"][:], ALU.mult)
                pt = self.PS[1]
                fw.tr(pt[0:16, 0:128], R["gate"][:], ident)
                fw.cp(gT[:, jt * 128:(jt + 1) * 128].k(jt), pt[0:16, 0:128], eng="act")
            fw.dma("pool", self.gateT[:, tok0:tok0 + TB].k(("g", gi)), gT[:, 0:TB])
        fw.release(m)

    def phase_wcast(self, l):
        fw = self.fw
        m = fw.mark()
        st = [fw.sb("wc_s%d" % i, [128, 4096]) for i in range(3)]
        sb_ = [fw.sb("wc_b%d" % i, [128, 4096], BF16) for i in range(3)]
        i = 0
        for e in range(NE):
            for (src, dst, pat) in ((self.w_e1_d, self.we1b, "(kc p) f -> p kc f"),
                                    (self.w_e3_d, self.we3b, "(kc p) f -> p kc f"),
                                    (self.w_e2_d, self.we2b, "(fc p) d -> p fc d")):
                a, b = st[i % 3], sb_[i % 3]
                n0 = 8 if src is not self.w_e2_d else 4
                fw.dma("sp", a[:].r("p (a b) -> p a b", a=n0), src[l, e].r(pat, p=128))
                fw.cp(b[:], a[:], eng=("dve", "act", "pool")[i % 3])
                fw.dma("pool", dst[e].r(pat, p=128).k(("e", e)), b[:].r("p (a b) -> p a b", a=n0))
                i += 1
        fw.release(m)

    def phase_moe(self, l):
        fw = self.fw
        m = fw.mark()
        ident = self.c(C_IDENT)
        W = self.ln_work()
        last = (l == DEPTH - 1)
        h2t = [fw.sb("e_h2%d" % i, [128, KC, 512], BF16) for i in range(2)]
        l1t = [fw.sb("e_l1%d" % i, [128, KC, 512]) for i in range(2)]
        gTt = [fw.sb("e_gT%d" % i, [16, 512]) for i in range(2)]
        w1r = [fw.sb("e_w1%d" % i, [128, KC, DFF], BF16) for i in range(2)]
        w3r = [fw.sb("e_w3%d" % i, [128, KC, DFF], BF16) for i in range(2)]
        w2r = [fw.sb("e_w2%d" % i, [128, 4, D], BF16) for i in range(2)]
        gb = [fw.sb("e_gb%d" % i, [128, 512]) for i in range(2)]
        s1 = [fw.sb("e_s1%d" % i, [128, 512]) for i in range(2)]
        tq = [fw.sb("e_t%d" % i, [128, 512]) for i in range(2)]
        ab = [fw.sb("e_a%d" % i, [128, 4, 512], BF16) for i in range(2)]
        accs = [fw.sb("e_acc%d" % i, [128, KC, 512]) for i in range(2)]
        otok = [fw.sb("e_ot%d" % i, [128, D]) for i in range(2)]
        resv = self.resT[:].r("(kc p) t -> p kc t", p=128)
        l1v = self.lat1T[:].r("(kc p) t -> p kc t", p=128)
        h2v = self.h2T[:].r("(kc p) t -> p kc t", p=128)
        ei = 0
        pending = None
        loaded = False
        for gi, (tok0, TB) in enumerate(GROUPS):
            if gi == 0 and last:
                continue
            md = self.mod_for(gi)
            h2, l1, gT = h2t[gi % 2], l1t[gi % 2], gTt[gi % 2]
            acc = accs[gi % 2]

            def group_loads(gj):
                t0_, TB_ = GROUPS[gj]
                fw.dma("sp", h2t[gj % 2][:, :, 0:TB_], h2v[:, :, t0_:t0_ + TB_])
                fw.dma("sp", l1t[gj % 2][:, :, 0:TB_], l1v[:, :, t0_:t0_ + TB_])
                fw.dma("sp", gTt[gj % 2][:, 0:TB_], self.gateT[:, t0_:t0_ + TB_])
            if not loaded:
                group_loads(gi)
                loaded = True
            for e in range(NE):
                w1, w3, w2 = w1r[ei % 2], w3r[ei % 2], w2r[ei % 2]
                fw.dma("sp", w1[:], self.we1b[e].r("(kc p) f -> p kc f", p=128))
                fw.dma("pool", w3[:], self.we3b[e].r("(kc p) f -> p kc f", p=128))
                fw.dma("sp", w2[:], self.we2b[e].r("(fc p) d -> p fc d", p=128))
                pgb = self.PS[6]
                fw.mm(pgb[:, 0:TB], self.cst[0:16, C_SEL + e * 128:C_SEL + (e + 1) * 128], gT[:, 0:TB])
                g_ = gb[ei % 2]
                fw.cp(g_[:, 0:TB], pgb[:, 0:TB], eng="act")
                a_ = ab[ei % 2]
                for f in range(4):
                    p1 = self.PS[f % 2]
                    p3 = self.PS[2 + f % 2]
                    for kc in range(KC):
                        fw.mm(p1[:, 0:TB], w1[:, kc, f * 128:(f + 1) * 128], h2[:, kc, 0:TB], start=(kc == 0), stop=(kc == KC - 1))
                    for kc in range(KC):
                        fw.mm(p3[:, 0:TB], w3[:, kc, f * 128:(f + 1) * 128], h2[:, kc, 0:TB], start=(kc == 0), stop=(kc == KC - 1))
                    s_ = s1[f % 2]
                    fw.act(s_[:, 0:TB], p1[:, 0:TB], AF.Silu)
                    t_ = tq[f % 2]
                    fw.tt(t_[:, 0:TB], s_[:, 0:TB], p3[:, 0:TB], ALU.mult)
                    fw.tt(a_[:, f, 0:TB].k(f), t_[:, 0:TB], g_[:, 0:TB], ALU.mult, eng="pool")
                for Dc in range(KC):
                    po = self.PS[4 + Dc % 2]
                    for f in range(4):
                        fw.mm(po[:, 0:TB], w2[:, f, Dc * 128:(Dc + 1) * 128], a_[:, f, 0:TB], start=(f == 0), stop=(f == 3))
                    if e == 0:
                        fw.cp(acc[:, Dc, 0:TB].k(Dc), po[:, 0:TB], eng="act")
                    else:
                        fw.tt(acc[:, Dc, 0:TB].k(Dc), acc[:, Dc, 0:TB].k(Dc), po[:, 0:TB], ALU.add)
                ei += 1
                if e == 1:
                    if pending is not None:
                        pending()
                        pending = None
                    if gi + 1 < len(GROUPS):
                        group_loads(gi + 1)

            def epilogue(gi=gi, tok0=tok0, TB=TB, md=md, acc=acc, l1=l1):
                res2 = acc
                lat2 = acc
                g2c = 40
                for Dc in range(KC):
                    fw.ts(l1[:, Dc, 0:TB].k(Dc), l1[:, Dc, 0:TB].k(Dc), ALPHA, ALU.mult, eng="pool")
                    fw.stt(res2[:, Dc, 0:TB].k(Dc), acc[:, Dc, 0:TB].k(Dc), md[:, g2c + Dc:g2c + Dc + 1], l1[:, Dc, 0:TB].k(Dc),
                           ALU.mult, ALU.add)
                self.layernorm_fm(res2, TB, "ln2", lat2, W)
                if not last:
                    fw.dma("pool", resv[:, :, tok0:tok0 + TB].k(("g", gi)), lat2[:, :, 0:TB])
                if last and gi > 0:
                    for jt in range(TB // 128):
                        o_ = otok[jt % 2]
                        for half in range(2):
                            ps = self.PS[6 + half]
                            for q in range(4):
                                kc = half * 4 + q
                                fw.tr(ps[:, q * 128:(q + 1) * 128], lat2[:, kc, jt * 128:(jt + 1) * 128], ident)
                            fw.cp(o_[:, half * 512:(half + 1) * 512].k(half), ps[:, 0:512], eng=("dve" if half == 0 else "act"))
                        r0 = tok0 - LC + jt * 128
                        fw.dma("sp", self.out_d[r0:r0 + 128, :].k(("o", r0)), o_[:])
            pending = epilogue
        if pending is not None:
            pending()
        fw.release(m)


def _core_inputs(inp, b, shared, consts):
    cc = np.empty((128, KC, 2), np.float32)
    cc[:, :, 0] = np.asarray(inp["c"][b], np.float32).reshape(KC, 128).T
    cc[:, :, 1] = np.asarray(inp["c_ctx"], np.float32).reshape(KC, 128).T
    m = {"x": np.ascontiguousarray(inp["x"][b], np.float32),
         "ctx": np.ascontiguousarray(inp["ctx"][b], np.float32),
         "cc": cc.reshape(128, KC * 2)}
    m.update(consts)
    m.update(shared)
    return m


_PROG = {}


def kernel(**inputs):
    if "p" not in _PROG:
        _PROG["p"] = Prog()
    prog = _PROG["p"]
    consts = _constants()
    shared = _shared_inputs(inputs)
    n = 8
    in_maps = [_core_inputs(inputs, b, shared, consts) for b in range(n)]
    res = run_bass_kernel_spmd(prog.nc, in_maps, core_ids=list(range(n)))
    return np.stack([np.asarray(r["out"], np.float32) for r in res.results], 0)
```

```python
import math
import numpy as np
import ml_dtypes
import concourse.bass as bass
import concourse.mybir as mybir
from concourse.bass_utils import run_bass_kernel_spmd

F32 = mybir.dt.float32
BF16 = mybir.dt.bfloat16
AF = mybir.ActivationFunctionType
ALU = mybir.AluOpType
AX = mybir.AxisListType

D = 1024
L = 4096
LC = 256
NT = L + LC
KC = 8
DEPTH = 2
IN_COLS = 3592
UROWS = 29 * 128
NEG = -30000.0
ALPHA = (2 * DEPTH) ** 0.25
LN_EPS = 1e-5
RMS_EPS = 1e-6
NE = 16
DFF = 512
SEM_LIMIT = 30000


class Buf:
    def __init__(self, name, t):
        self.name = name
        self.t = t
        self.st = {}

    def __getitem__(self, idx):
        return View(self, self.t[idx], None)

    def k(self, key):
        return View(self, self.t[:], key)


class View:
    def __init__(self, buf, ap, key):
        self.buf = buf
        self.ap = ap
        self.key = key

    def __getitem__(self, idx):
        return View(self.buf, self.ap[idx], self.key)

    def k(self, key):
        return View(self.buf, self.ap, key)

    def r(self, pat, **kw):
        return View(self.buf, self.ap.rearrange(pat, **kw), self.key)

    def bc(self, shape):
        return View(self.buf, self.ap.to_broadcast(list(shape)), self.key)

    def un(self, axis):
        return View(self.buf, self.ap.unsqueeze(axis), self.key)


def _ap(x):
    return x.ap if isinstance(x, View) else x


class FW:
    NDMA = 8
    LIMIT = 10 ** 9
    POOL_TO = "dve"

    def __init__(self, nc):
        self.nc = nc
        self.eng = {"pe": nc.tensor, "dve": nc.vector, "act": nc.scalar,
                    "pool": nc.gpsimd, "sp": nc.sync}
        self.sem = {}
        self.cnt = {}
        self.seen = {}
        self.last = {}
        self._guards = []
        self._semguards = []
        self.bufs = []
        self._nsem = 0
        for e in self.eng:
            self.sem[e] = self._newsem("s_" + e)
            self.cnt[e] = 0
            self.seen[e] = {}
            self.last[e] = []
        self.dsem = {}
        self.dcnt = {}
        self.dlast = {}
        self.drr = {}
        for q in ("sp", "act", "pool"):
            self.dsem[q] = [self._newsem("d_%s%d" % (q, i)) for i in range(self.NDMA)]
            self.dcnt[q] = [0] * self.NDMA
            self.dlast[q] = [None] * self.NDMA
            self.drr[q] = 0
        self.n_inst = 0
        self.n_wait = 0

    def _newsem(self, name):
        self._nsem += 1
        g = self.nc.semaphore("%s_%d" % (name, self._nsem))
        s = g.__enter__()
        self._semguards.append(g)
        return s

    def sb(self, name, shape, dt=F32):
        self._nalloc = getattr(self, "_nalloc", 0) + 1
        g = self.nc.sbuf_tensor("sb%d_%s" % (self._nalloc, name), list(shape), dt)
        t = g.__enter__()
        self._guards.append(g)
        b = Buf(name, t)
        self.bufs.append(b)
        return b

    def ps(self, name, shape, dt=F32):
        g = self.nc.psum_tensor("pp_" + name, list(shape), dt)
        t = g.__enter__()
        self._guards.append(g)
        b = Buf(name, t)
        self.bufs.append(b)
        return b

    def dram(self, name, shape, dt=F32, kind="Internal"):
        t = self.nc.dram_tensor(name, list(shape), dt, kind=kind)
        b = Buf(name, t.ap())
        self.bufs.append(b)
        return b

    def mark(self):
        return len(self._guards)

    def release(self, mark):
        self.barrier()
        while len(self._guards) > mark:
            g = self._guards.pop()
            g.__exit__(None, None, None)

    def close(self):
        while self._guards:
            g = self._guards.pop()
            g.__exit__(None, None, None)
        while self._semguards:
            g = self._semguards.pop()
            g.__exit__(None, None, None)

    def _deps(self, reads, writes):
        deps = []
        for a in reads:
            b, k = a.buf, a.key
            for kk, s in b.st.items():
                if k is None or kk is None or kk == k:
                    if s[0] is not None:
                        deps.append(s[0])
        for a in writes:
            b, k = a.buf, a.key
            for kk, s in b.st.items():
                if k is None or kk is None or kk == k:
                    if s[0] is not None:
                        deps.append(s[0])
                    deps.extend(s[1])
        return deps

    def _record(self, reads, writes, tok):
        for a in reads:
            s = a.buf.st.setdefault(a.key, [None, []])
            s[1].append(tok)
            if len(s[1]) > 48:
                m = {}
                for (sm, v) in s[1]:
                    if id(sm) not in m or m[id(sm)][1] < v:
                        m[id(sm)] = (sm, v)
                s[1] = list(m.values())
        for a in writes:
            if a.key is None:
                a.buf.st = {None: [tok, []]}
            else:
                a.buf.st[a.key] = [tok, []]

    def _emit_waits(self, e, deps, same_engine=True):
        engine = self.eng[e]
        seen = self.seen[e]
        need = {}
        for (sm, v) in deps:
            if (not same_engine) and sm is self.sem[e]:
                continue
            if seen.get(id(sm), 0) >= v:
                continue
            if id(sm) not in need or need[id(sm)][1] < v:
                need[id(sm)] = (sm, v)
        for (sm, v) in need.values():
            engine.wait_ge(sm, v)
            seen[id(sm)] = v
            self.n_wait += 1

    def op(self, e, fn, reads=(), writes=(), same_engine=None):
        if e == "pool":
            e = FW.POOL_TO
        if same_engine is None:
            same_engine = (e != "pe")
        if self.n_inst >= FW.LIMIT:
            return None
        reads = [r for r in reads if isinstance(r, View)]
        writes = [w for w in writes if isinstance(w, View)]
        deps = self._deps(reads, writes)
        self._emit_waits(e, deps, same_engine)
        if self.cnt[e] >= SEM_LIMIT:
            self.sem[e] = self._newsem("s_" + e)
            self.cnt[e] = 0
        ins = fn(self.eng[e])
        self.cnt[e] += 1
        ins.then_inc(self.sem[e], 1)
        tok = (self.sem[e], self.cnt[e])
        self.last[e] = [tok] + [t for t in self.last[e] if t[0] is not self.sem[e]][:1]
        self._record(reads, writes, tok)
        self.n_inst += 1
        return tok

    def dma(self, q, out, in_, **kw):
        if self.n_inst >= FW.LIMIT:
            return None
        reads, writes = [in_], [out]
        deps = self._deps(reads, writes)
        slot = self.drr[q]
        self.drr[q] = (slot + 1) % self.NDMA
        if self.dlast[q][slot] is not None:
            deps.append(self.dlast[q][slot])
        self._emit_waits(q, deps, True)
        if self.dcnt[q][slot] >= SEM_LIMIT:
            self.dsem[q][slot] = self._newsem("d_%s%d" % (q, slot))
            self.dcnt[q][slot] = 0
        sm = self.dsem[q][slot]
        ins = self.eng[q].dma_start(out=out.ap, in_=in_.ap, **kw)
        self.dcnt[q][slot] += 16
        ins.then_inc(sm, 16)
        tok = (sm, self.dcnt[q][slot])
        self.dlast[q][slot] = tok
        self._record(reads, writes, tok)
        self.n_inst += 1
        return tok

    def all_tokens(self):
        toks = []
        for e in self.eng:
            toks.extend(self.last[e])
        for q in self.dlast:
            toks.extend(t for t in self.dlast[q] if t is not None)
        return toks

    def barrier(self):
        toks = self.all_tokens()
        for e in self.eng:
            self._emit_waits(e, toks, True)
        for b in self.bufs:
            b.st = {}

    def mm(self, out, lhsT, rhs, start=True, stop=True):
        return self.op("pe", lambda e: e.matmul(out.ap, lhsT=lhsT.ap, rhs=rhs.ap, start=start, stop=stop),
                       [lhsT, rhs], [out])

    def tr(self, out, in_, ident):
        return self.op("pe", lambda e: e.transpose(out.ap, in_.ap, ident.ap), [in_, ident], [out])

    def act(self, out, in_, func, bias=0.0, scale=1.0, accum=None, eng="act"):
        kw = {}
        if accum is not None:
            kw["accum_out"] = accum.ap
        return self.op("act", lambda e: e.activation(out=out.ap, in_=in_.ap, func=func, bias=_ap(bias),
                                                     scale=_ap(scale), **kw),
                       [in_, bias, scale], [out, accum])

    def tt(self, out, a, b, op, eng="dve"):
        return self.op(eng, lambda e: e.tensor_tensor(out=out.ap, in0=a.ap, in1=b.ap, op=op), [a, b], [out])

    def ts(self, out, a, s1, op0, s2=None, op1=None, eng="dve", accum=None):
        kw = {}
        if op1 is not None:
            kw["op1"] = op1
        if accum is not None:
            kw["accum_out"] = accum.ap
        return self.op(eng, lambda e: e.tensor_scalar(out=out.ap, in0=a.ap, scalar1=_ap(s1), scalar2=_ap(s2),
                                                      op0=op0, **kw), [a, s1, s2], [out, accum])

    def stt(self, out, a, s, b, op0, op1, eng="dve"):
        return self.op("dve", lambda e: e.scalar_tensor_tensor(out=out.ap, in0=a.ap, scalar=_ap(s), in1=b.ap,
                                                             op0=op0, op1=op1), [a, s, b], [out])

    def cp(self, out, in_, eng="dve"):
        if eng == "act":
            return self.op("act", lambda e: e.copy(out=out.ap, in_=in_.ap), [in_], [out])
        return self.op(eng, lambda e: e.tensor_copy(out=out.ap, in_=in_.ap), [in_], [out])

    def memset(self, out, val, eng="pool"):
        return self.op("dve" if eng == "pool" else eng, lambda e: e.memset(out.ap, val), [], [out])

    def red(self, out, in_, op, axis=AX.X, eng="dve"):
        return self.op(eng, lambda e: e.tensor_reduce(out=out.ap, in_=in_.ap, axis=axis, op=op), [in_], [out])

    def scan(self, out, d0, d1, init, op0, op1):
        return self.op("dve", lambda e: e.tensor_tensor_scan(out=out.ap, data0=d0.ap, data1=d1.ap,
                                                             initial=_ap(init), op0=op0, op1=op1),
                       [d0, d1, init], [out])


C_IDENT, C_ONES, C_TRIF, C_TRIB, C_NEGF, C_NEGB, C_MIF, C_MIB, C_SCAN = [i * 128 for i in range(9)]
C_CMASK = 1152
C_SEL = 1160
C_NEGTL = C_SEL + 16 * 128
C_NEGTC = C_NEGTL + 32
C_CMASKR = C_NEGTC + 8
C_SCAN16 = C_CMASKR + 8
NCST = C_SCAN16 + 16

PP = {}
_off = 0
for _n, _w in [("hycw", 18), ("hycb", 6), ("sccw", 6), ("sdcw", 12), ("sdcb", 4), ("hybias", 2), ("lbl", 8),
               ("hgng", 1), ("bgate", 32), ("ln1g", 8), ("ln1b", 8), ("ln2g", 8), ("ln2b", 8), ("bada", 48),
               ("hyb1", 1), ("hyf1", 1), ("hyb2", 1), ("hyf2", 1), ("lbl64", 16)]:
    PP[_n] = _off
    _off += _w
NPP = _off
RB = {}
_off = 0
for _n, _w in [("hydecay", 256), ("ssdng", 256), ("dtbias", 8), ("alog", 8), ("ssdd", 4), ("brouter", 16)]:
    RB[_n] = _off
    _off += _w
NRB = _off

_CONST_CACHE = {}


def _sincos_1d(pos, dim):
    omega = 1.0 / (10000.0 ** (np.arange(dim // 2, dtype=np.float32) / np.float32(dim // 2)))
    ang = pos.astype(np.float32)[:, None] * omega[None].astype(np.float32)
    return np.concatenate([np.sin(ang), np.cos(ang)], -1).astype(np.float32)


def _hy_feat(Lx):
    t = np.linspace(0.0, 1.0, Lx, dtype=np.float32)[:, None]
    bands = np.linspace(1e-4, 15, 16, dtype=np.float32)
    ang = (np.float32(2.0 * math.pi / Lx)) * np.arange(Lx, dtype=np.float32)[:, None] * bands[None]
    feat = np.concatenate([t, np.cos(ang), -np.sin(ang)], -1).astype(np.float32)
    return feat, t[:, 0]


def _dft_tables(Lx, TB):
    N = 2 * Lx
    nT = Lx // 128
    t = np.arange(Lx, dtype=np.int64)
    k = np.arange(Lx, dtype=np.int64)
    m = ((2 * k[None, :] + 1) * t[:, None]) % (2 * N)
    ang = m.astype(np.float64) * (math.pi / N)
    tabs = [np.cos(ang), np.sin(ang)]
    tf = np.empty((2, nT, 128, nT, 128), dtype=ml_dtypes.bfloat16)
    ti = np.empty((2, Lx // TB, 128, nT, TB), dtype=ml_dtypes.bfloat16)
    for j, T in enumerate(tabs):
        T4 = T.reshape(nT, 128, nT, 128)
        tf[j] = T4.transpose(2, 1, 0, 3).astype(ml_dtypes.bfloat16)
        Tt = T.T.reshape(nT, 128, Lx // TB, TB)
        ti[j] = Tt.transpose(2, 1, 0, 3).astype(ml_dtypes.bfloat16)
    return tf.reshape(2, nT, 128, nT * 128), ti.reshape(2, Lx // TB, 128, nT * TB)


def _constants():
    if _CONST_CACHE:
        return _CONST_CACHE
    cst = np.zeros((128, NCST), np.float32)
    r = np.arange(128)
    cst[:, C_IDENT:C_IDENT + 128] = np.eye(128)
    cst[:, C_ONES:C_ONES + 128] = 1.0
    cst[:, C_TRIF:C_TRIF + 128] = (r[:, None] <= r[None, :])
    cst[:, C_TRIB:C_TRIB + 128] = (r[:, None] >= r[None, :])
    cst[:, C_NEGF:C_NEGF + 128] = np.where(r[:, None] > r[None, :], NEG, 0.0)
    cst[:, C_NEGB:C_NEGB + 128] = np.where(r[:, None] < r[None, :], NEG, 0.0)
    same = (r[:, None] // 16) == (r[None, :] // 16)
    cst[:, C_MIF:C_MIF + 128] = same & (r[:, None] <= r[None, :])
    cst[:, C_MIB:C_MIB + 128] = same & (r[:, None] >= r[None, :])
    cst[:, C_SCAN:C_SCAN + 128] = ((r % 16) != 0)[None, :]
    cst[:, C_CMASK:C_CMASK + 8] = (r[:, None] // 16) == np.arange(8)[None, :]
    cst[:, C_CMASKR:C_CMASKR + 8] = (r[:, None] // 16) == (7 - np.arange(8))[None, :]
    cst[:, C_SCAN16:C_SCAN16 + 16] = (np.arange(16) != 0)[None, :]
    sel = np.zeros((128, 16, 128), np.float32)
    for e in range(16):
        sel[e, e, :] = 1.0
    cst[:, C_SEL:C_SEL + 2048] = sel.reshape(128, 2048)
    featL, tL = _hy_feat(L)
    featC, tC = _hy_feat(LC)
    cst[:, C_NEGTL:C_NEGTL + 32] = -tL.reshape(32, 128).T
    cst[:, C_NEGTC:C_NEGTC + 2] = -tC.reshape(2, 128).T
    feat = np.concatenate([featC.T, featL.T], 1).astype(np.float32)
    rows = L // 64
    row = np.repeat(np.arange(rows), 64)
    col = np.tile(np.arange(64), rows)
    pos = np.concatenate([_sincos_1d(row, D // 2), _sincos_1d(col, D // 2)], -1).astype(np.float32)
    tfl, til = _dft_tables(L, 512)
    tfc, tic = _dft_tables(LC, 256)
    _CONST_CACHE.update(cst=cst, feat=np.ascontiguousarray(feat), pos=pos, tfl=tfl, til=til, tfc=tfc, tic=tic)
    return _CONST_CACHE


def _chunks(v, n):
    return np.ascontiguousarray(np.asarray(v, np.float32).reshape(n, 128).T)


def _shared_inputs(inp):
    pp = np.zeros((DEPTH, 128, NPP), np.float32)
    rb = np.zeros((DEPTH, 128, NRB), np.float32)
    for l in range(DEPTH):
        def put(name, arr):
            arr = np.asarray(arr, np.float32)
            pp[l, :arr.shape[0], PP[name]:PP[name] + arr.shape[1]] = arr
        cw = inp["hy_conv_w"][l]
        put("hycw", cw.reshape(3, 6, 128).transpose(2, 1, 0).reshape(128, 18))
        put("hycb", _chunks(inp["hy_conv_b"][l], 6))
        put("sccw", inp["sc_conv_w"][l].reshape(3, 2, 128).transpose(2, 1, 0).reshape(128, 6))
        put("sdcw", inp["ssd_conv_w"][l].reshape(3, 4, 128).transpose(2, 1, 0).reshape(128, 12))
        put("sdcb", _chunks(inp["ssd_conv_b"][l], 4))
        put("hybias", _chunks(inp["hy_bias"][l], 2))
        lbl = inp["hg_lb_logits"]
        put("lbl", lbl.reshape(2, DEPTH, 2, 128).transpose(3, 0, 1, 2).reshape(128, 8))
        put("lbl64", lbl.reshape(2, DEPTH, 4, 64).transpose(3, 0, 1, 2).reshape(64, 16))
        put("hgng", np.asarray(inp["hg_norm_g"][l]).reshape(64, 1))
        put("bgate", _chunks(inp["b_gate"][l], 32))
        put("ln1g", _chunks(inp["ln1_g"][l], 8))
        put("ln1b", _chunks(inp["ln1_b"][l], 8))
        put("ln2g", _chunks(inp["ln2_g"][l], 8))
        put("ln2b", _chunks(inp["ln2_b"][l], 8))
        put("bada", _chunks(inp["b_ada"][l], 48))
        put("hyb1", np.asarray(inp["hy_b1"][l]).reshape(64, 1))
        put("hyf1", np.asarray(inp["hy_freq1"][l]).reshape(64, 1))
        put("hyb2", np.asarray(inp["hy_b2"][l]).reshape(64, 1))
        put("hyf2", np.asarray(inp["hy_freq2"][l]).reshape(64, 1))

        def rput(name, row):
            row = np.asarray(row, np.float32).reshape(1, -1)
            rb[l, :, RB[name]:RB[name] + row.shape[1]] = np.broadcast_to(row, (128, row.shape[1]))
        rput("hydecay", inp["hy_decay"][l])
        rput("ssdng", inp["ssd_norm_g"][l])
        rput("dtbias", inp["ssd_dt_bias"][l])
        rput("alog", inp["ssd_a_log"][l])
        rput("ssdd", inp["ssd_d"][l])
        rput("brouter", inp["b_router"])
    sh = {"pp": pp, "rb": rb}
    for n in ("w_ada", "w_in", "hy_w1", "hy_w2", "hy_w3", "w_gate", "w_br", "w_o", "w_router",
              "w_e1", "w_e3", "w_e2"):
        sh[n] = np.ascontiguousarray(np.asarray(inp[n], np.float32))
    return sh


GROUPS = [(0, LC)] + [(LC + 512 * i, 512) for i in range(L // 512)]


class Prog:
    def __init__(self, dbg=(), stop_after=None, layers=DEPTH):
        self.dbg = set(dbg)
        self.stop_after = stop_after
        self.layers = layers
        nc = bass.Bass("TRN2", target_bir_lowering=False)
        self.nc = nc
        fw = FW(nc)
        self.fw = fw
        din = lambda name, shape, dt=F32: fw.dram(name, shape, dt, kind="ExternalInput")
        self.x_d = din("x", [L, D])
        self.ctx_d = din("ctx", [LC, D])
        self.cc_d = din("cc", [128, KC * 2])
        self.pos_d = din("pos", [L, D])
        self.cst_d = din("cst", [128, NCST])
        self.feat_d = din("feat", [33, NT])
        self.tfl_d = din("tfl", [2, 32, 128, 32 * 128], BF16)
        self.til_d = din("til", [2, 8, 128, 32 * 512], BF16)
        self.tfc_d = din("tfc", [2, 2, 128, 2 * 128], BF16)
        self.tic_d = din("tic", [2, 1, 128, 2 * 256], BF16)
        self.pp_d = din("pp", [DEPTH, 128, NPP])
        self.rb_d = din("rb", [DEPTH, 128, NRB])
        self.w_ada_d = din("w_ada", [DEPTH, D, 6 * D])
        self.w_in_d = din("w_in", [DEPTH, D, IN_COLS])
        self.hy_w1_d = din("hy_w1", [DEPTH, 33, 64])
        self.hy_w2_d = din("hy_w2", [DEPTH, 64, 64])
        self.hy_w3_d = din("hy_w3", [DEPTH, 64, 512])
        self.w_gate_d = din("w_gate", [DEPTH, D, 4 * D])
        self.w_br_d = din("w_br", [DEPTH, 4, 256, D])
        self.w_o_d = din("w_o", [DEPTH, D, D])
        self.w_router_d = din("w_router", [D, NE])
        self.w_e1_d = din("w_e1", [DEPTH, NE, D, DFF])
        self.w_e3_d = din("w_e3", [DEPTH, NE, D, DFF])
        self.w_e2_d = din("w_e2", [DEPTH, NE, DFF, D])
        self.out_d = fw.dram("out", [L, D], F32, kind="ExternalOutput")

        def scratch(name, shape, dt=F32):
            kind = "ExternalOutput" if name in self.dbg else "Internal"
            return fw.dram(name, shape, dt, kind=kind)
        self.resT = scratch("resT", [D, NT])
        self.uT = scratch("uT", [UROWS, NT])
        self.brT = scratch("brT", [D, NT], BF16)
        self.lat1T = scratch("lat1T", [D, NT])
        self.h2T = scratch("h2T", [D, NT], BF16)
        self.gateT = scratch("gateT", [NE, NT])
        self.hgo = scratch("hgo", [64, 4 * NT])
        self.hgo2 = scratch("hgo2", [64, 4 * NT])
        self.yf = scratch("yf", [NT, 256])
        self.yb = scratch("yb", [NT, 256])
        self.dtT = scratch("dtT", [NT, 8])
        self.wgc = scratch("wgc", [8, 128, KC * 4 * 128], BF16)
        self.modd = scratch("modd", [128, 96])
        self.we1b = scratch("we1b", [NE, D, DFF], BF16)
        self.we3b = scratch("we3b", [NE, D, DFF], BF16)
        self.we2b = scratch("we2b", [NE, DFF, D], BF16)

        self.cst = fw.sb("cst", [128, NCST])
        fw.dma("sp", self.cst[:], self.cst_d[:])
        self.PS = [fw.ps("ps%d" % i, [128, 512]) for i in range(8)]
        self.pp = fw.sb("pp", [128, NPP])
        self.rb = fw.sb("rb", [128, NRB])
        self.modL = fw.sb("modL", [128, 48])
        self.modC = fw.sb("modC", [128, 48])
        self.build()
        fw.barrier()
        fw.close()

    def c(self, off, w=128, rows=128):
        return self.cst[0:rows, off:off + w]

    def build(self):
        fw = self.fw
        self.phase_init()
        if self.stop_after == "init":
            return
        for l in range(self.layers):
            self.l = l
            fw.dma("sp", self.pp[:], self.pp_d[l])
            fw.dma("sp", self.rb[:], self.rb_d[l])
            self.phase_mod(l)
            if self.stop_after in ("mod", "mod%d" % l):
                return
            self.phase_p1(l)
            if self.stop_after in ("p1", "p1%d" % l):
                return
            self.phase_wcast(l)
            self.phase_sc(l)
            if self.stop_after in ("sc", "sc%d" % l):
                return
            self.phase_hyena(l, ctx=False)
            if l < DEPTH - 1:
                self.phase_hyena(l, ctx=True)
            if self.stop_after in ("hy", "hy%d" % l):
                return
            self.phase_hgrn(l)
            if self.stop_after in ("hg", "hg%d" % l):
                return
            self.phase_ssd(l)
            if self.stop_after in ("ssd", "ssd%d" % l):
                return
            self.phase_merge(l)
            if self.stop_after in ("merge", "merge%d" % l):
                return
            self.phase_moe(l)
            if self.stop_after == "moe%d" % l:
                return

    def phase_init(self):
        fw = self.fw
        m = fw.mark()
        xt = [fw.sb("xt%d" % i, [128, D]) for i in range(2)]
        pt = [fw.sb("pt%d" % i, [128, D]) for i in range(2)]
        st = [fw.sb("st%d" % i, [128, KC, 512]) for i in range(2)]
        ident = self.c(C_IDENT)
        resv = self.resT[:].r("(kc p) t -> p kc t", p=128)
        ti = 0
        for gi, (tok0, TB) in enumerate(GROUPS):
            sg = st[gi % 2]
            for j in range(TB // 128):
                a = xt[ti % 2]
                if gi == 0:
                    fw.dma("sp", a[:], self.ctx_d[j * 128:(j + 1) * 128, :])
                else:
                    r0 = tok0 - LC + j * 128
                    fw.dma("sp", a[:], self.x_d[r0:r0 + 128, :])
                    p = pt[ti % 2]
                    fw.dma("pool", p[:], self.pos_d[r0:r0 + 128, :])
                    fw.tt(a[:], a[:], p[:], ALU.add)
                for half in range(2):
                    ps = self.PS[(ti * 2 + half) % 4]
                    for q in range(4):
                        kc = half * 4 + q
                        fw.tr(ps[:, q * 128:(q + 1) * 128], a[:, kc * 128:(kc + 1) * 128], ident)
                    dst = sg[:, half * 4:(half + 1) * 4, j * 128:(j + 1) * 128]
                    src = ps[:].r("p (q t) -> p q t", q=4)
                    if half == 0:
                        fw.cp(dst, src, eng="dve")
                    else:
                        fw.cp(dst, src, eng="act")
                ti += 1
            fw.dma("sp", resv[:, :, tok0:tok0 + TB].k(("g", gi)), sg[:, :, 0:TB])
        fw.release(m)

    def phase_mod(self, l):
        fw = self.fw
        m = fw.mark()
        cs = fw.sb("cs", [128, KC * 2])
        fw.dma("sp", cs[:], self.cc_d[:])
        csil = fw.sb("csil", [128, KC * 2])
        fw.act(csil[:], cs[:], AF.Silu)
        wa = [fw.sb("wa%d" % i, [128, KC, 1024]) for i in range(2)]
        ps = self.PS[0]
        wv = self.w_ada_d[l].r("(kc p) n -> p kc n", p=128)
        for blk in range(6):
            w = wa[blk % 2]
            fw.dma("sp" if blk % 2 == 0 else "pool", w[:], wv[:, :, blk * 1024:(blk + 1) * 1024])
            for jj in range(8):
                j = blk * 8 + jj
                for kc in range(KC):
                    fw.mm(ps[:, j * 2:j * 2 + 2], w[:, kc, jj * 128:(jj + 1) * 128], csil[:, kc * 2:kc * 2 + 2],
                          start=(kc == 0), stop=(kc == KC - 1))
        psv = ps[:, 0:96].r("p (j t) -> p j t", t=2)
        bada = self.pp[:, PP["bada"]:PP["bada"] + 48]
        fw.tt(self.modL[:], psv[:, :, 0], bada, ALU.add)
        fw.tt(self.modC[:], psv[:, :, 1], bada, ALU.add)
        for mm_ in (self.modL, self.modC):
            for o in (8, 32):
                fw.ts(mm_[:, o:o + 8], mm_[:, o:o + 8], 1.0, ALU.add)
        if "modd" in self.dbg:
            fw.dma("sp", self.modd[:, 0:48], self.modL[:])
            fw.dma("sp", self.modd[:, 48:96], self.modC[:])
        fw.release(m)

    def mod_for(self, gi):
        return self.modC if gi == 0 else self.modL

    def load_cast(self, dst, src_view, stage, n, i):
        fw = self.fw
        s = stage[i % len(stage)]
        fw.dma("sp" if i % 2 == 0 else "pool", s[:, 0:n], src_view)
        eng = ("dve", "act", "pool")[i % 3]
        fw.cp(dst, s[:, 0:n], eng=eng)

    def phase_p1(self, l):
        fw = self.fw
        m = fw.mark()
        wb = fw.sb("winb", [128, KC, UROWS], BF16)
        stage = [fw.sb("wst%d" % i, [128, IN_COLS]) for i in range(2)]
        fw.memset(wb[:, :, IN_COLS:UROWS], 0.0)
        wv = self.w_in_d[l].r("(kc p) n -> p kc n", p=128)
        for kc in range(KC):
            self.load_cast(wb[:, kc, 0:IN_COLS].k(kc), wv[:, kc, :], stage, IN_COLS, kc)
        rt = [fw.sb("rt%d" % i, [128, KC, 512]) for i in range(2)]
        ht = [fw.sb("ht%d" % i, [128, KC, 512], BF16) for i in range(2)]
        og = [fw.sb("og%d" % i, [128, 4, 512]) for i in range(3)]
        dts = [fw.sb("dts%d" % i, [128, 8]) for i in range(2)]
        resv = self.resT[:].r("(kc p) t -> p kc t", p=128)
        uv = self.uT[:].r("(c p) t -> p c t", p=128)
        oi = 0
        for gi, (tok0, TB) in enumerate(GROUPS):
            r = rt[gi % 2]
            h = ht[gi % 2]
            md = self.mod_for(gi)
            fw.dma("sp", r[:, :, 0:TB], resv[:, :, tok0:tok0 + TB])
            for kc in range(KC):
                fw.act(h[:, kc, 0:TB].k(kc), r[:, kc, 0:TB], AF.Identity, bias=md[:, kc:kc + 1], scale=md[:, 8 + kc:9 + kc])
            for c0 in range(0, 29, 4):
                nchunk = min(4, 29 - c0)
                o = og[oi % 3]
                for cj in range(nchunk):
                    cidx = c0 + cj
                    ps = self.PS[cidx % 4]
                    for kc in range(KC):
                        fw.mm(ps[:, 0:TB], wb[:, kc, cidx * 128:(cidx + 1) * 128], h[:, kc, 0:TB],
                              start=(kc == 0), stop=(kc == KC - 1))
                    fw.cp(o[:, cj, 0:TB].k(cj), ps[:, 0:TB], eng=("dve" if cidx % 2 == 0 else "act"))
                fw.dma("sp" if oi % 2 == 0 else "pool", uv[:, c0:c0 + nchunk, tok0:tok0 + TB].k(("g", gi, c0)),
                       o[:, 0:nchunk, 0:TB])
                oi += 1
            for jt in range(TB // 128):
                ps = self.PS[4 + jt % 2]
                for kc in range(KC):
                    fw.mm(ps[:, 0:8], h[:, kc, jt * 128:(jt + 1) * 128], wb[:, kc, 3584:3592],
                          start=(kc == 0), stop=(kc == KC - 1))
                dq = dts[jt % 2]
                fw.cp(dq[:], ps[:, 0:8], eng="act")
                fw.dma("pool", self.dtT[tok0 + jt * 128:tok0 + (jt + 1) * 128, :].k(("dt", tok0, jt)), dq[:])
        fw.release(m)

    def uv(self):
        return self.uT[:].r("(c p) t -> p c t", p=128)

    def brv(self):
        return self.brT[:].r("(c p) t -> p c t", p=128)

    def load_halo(self, dst, c0, nch, tok0, TB, q="sp"):
        fw = self.fw
        s0, s1 = (0, LC) if tok0 < LC else (LC, NT)
        lo = tok0 - 1
        hi = tok0 + TB + 1
        d0 = 0
        if lo < s0:
            fw.memset(dst[:, 0:nch, 0:1], 0.0)
            lo += 1
            d0 = 1
        if hi > s1:
            fw.memset(dst[:, 0:nch, TB + 1:TB + 2], 0.0)
            hi -= 1
        fw.dma(q, dst[:, 0:nch, d0:d0 + (hi - lo)], self.uv()[:, c0:c0 + nch, lo:hi])

    def conv3(self, out, src, wcol, TB, bias=None, eng="dve"):
        fw = self.fw
        w = lambda k: self.pp[:, wcol + k:wcol + k + 1]
        if bias is not None:
            fw.ts(out, src[:, 0:TB], w(0), ALU.mult, bias, ALU.add, eng=eng)
        else:
            fw.ts(out, src[:, 0:TB], w(0), ALU.mult, eng=eng)
        fw.stt(out, src[:, 1:TB + 1], w(1), out, ALU.mult, ALU.add)
        fw.stt(out, src[:, 2:TB + 2], w(2), out, ALU.mult, ALU.add)

    def phase_sc(self, l):
        fw = self.fw
        m = fw.mark()
        tin = [fw.sb("sct%d" % i, [128, 6, 514]) for i in range(2)]
        mm_ = [fw.sb("scm%d" % i, [128, 2, 514]) for i in range(2)]
        acc = [fw.sb("sca%d" % i, [128, 2, 512]) for i in range(2)]
        ob = [fw.sb("sco%d" % i, [128, 2, 512], BF16) for i in range(2)]
        for gi, (tok0, TB) in enumerate(GROUPS):
            if gi == 0 and l == DEPTH - 1:
                continue
            t = tin[gi % 2]
            self.load_halo(t, 6, 6, tok0, TB)
            mv = mm_[gi % 2]
            fw.tt(mv[:, :, 0:TB + 2], t[:, 2:4, 0:TB + 2], t[:, 4:6, 0:TB + 2], ALU.mult)
            a = acc[gi % 2]
            o = ob[gi % 2]
            for cc in range(2):
                self.conv3(a[:, cc, 0:TB], mv[:, cc, :], PP["sccw"] + cc * 3, TB, eng=("dve" if cc == 0 else "pool"))
                fw.tt(o[:, cc, 0:TB], a[:, cc, 0:TB], t[:, cc, 1:TB + 1], ALU.mult, eng=("dve" if cc == 0 else "pool"))
            fw.dma("pool", self.brv()[:, 2:4, tok0:tok0 + TB].k(("sc", gi)), o[:, :, 0:TB])
        fw.release(m)

    def phase_hyena(self, l, ctx):
        fw = self.fw
        m = fw.mark()
        Lx = LC if ctx else L
        base = 0 if ctx else LC
        TB = 256 if ctx else 512
        nT = Lx // 128
        nG = Lx // TB
        tpb = TB // 128
        tf_d = self.tfc_d if ctx else self.tfl_d
        ti_d = self.tic_d if ctx else self.til_d
        negt = self.c(C_NEGTC, 2) if ctx else self.c(C_NEGTL, 32)
        ident = self.c(C_IDENT)
        ones = self.c(C_ONES)
        AU = fw.sb("AU", [128, nT, 768], BF16)
        ZB = fw.sb("ZB", [128, nT, 512], BF16)
        X0 = fw.sb("X0", [128, 2, Lx], BF16)
        WB = fw.sb("WB", [128, 2, Lx], BF16)
        rn = fw.sb("rn", [128, 256])
        m2 = fw.mark()
        w1 = fw.sb("hw1", [33, 64])
        w2 = fw.sb("hw2", [64, 64])
        w3 = fw.sb("hw3", [64, 512])
        fw.dma("sp", w1[:], self.hy_w1_d[l])
        fw.dma("sp", w2[:], self.hy_w2_d[l])
        fw.dma("sp", w3[:], self.hy_w3_d[l])
        bf = fw.sb("hbf", [64, 2])
        ppc = lambda n: self.pp[0:64, PP[n]:PP[n] + 1]
        fw.tt(bf[:, 0:1], ppc("hyb1"), ppc("hyf1"), ALU.mult)
        fw.tt(bf[:, 1:2], ppc("hyb2"), ppc("hyf2"), ALU.mult)
        h1 = fw.sb("hh1", [64, Lx])
        h2 = fw.sb("hh2", [64, Lx])
        ft = [fw.sb("hft%d" % i, [33, 512]) for i in range(2)]
        tmp = [fw.sb("htmp%d" % i, [64, 512]) for i in range(2)]
        TF = min(512, Lx)
        tmpk = [fw.sb("htmpk%d" % i, [64, 512]) for i in range(2)]
        MAGIC = 12582912.0
        for layer in range(2):
            for g in range(Lx // TF):
                ps = self.PS[g % 2]
                if layer == 0:
                    f = ft[g % 2]
                    fw.dma("sp", f[:, 0:TF], self.feat_d[:, base + g * TF:base + (g + 1) * TF])
                    fw.mm(ps[0:64, 0:TF], w1[:, :], f[:, 0:TF])
                    fq, bq, dst = ppc("hyf1"), bf[:, 0:1], h1
                else:
                    fw.mm(ps[0:64, 0:TF], w2[:, :], h1[:, g * TF:(g + 1) * TF])
                    fq, bq, dst = ppc("hyf2"), bf[:, 1:2], h2
                t = tmp[g % 2]
                fw.ts(t[:, 0:TF], ps[0:64, 0:TF], fq, ALU.mult, bq, ALU.add)
                kq = tmpk[g % 2]
                fw.ts(kq[:, 0:TF], t[:, 0:TF], 1.0 / (2.0 * math.pi), ALU.mult, MAGIC, ALU.add)
                fw.ts(kq[:, 0:TF], kq[:, 0:TF], -MAGIC, ALU.add)
                fw.stt(t[:, 0:TF], kq[:, 0:TF], -2.0 * math.pi, t[:, 0:TF], ALU.mult, ALU.add)
                fw.ts(t[:, 0:TF], t[:, 0:TF], -3.141592, ALU.max, 3.141592, ALU.min)
                fw.act(dst[:, g * TF:(g + 1) * TF].k(g), t[:, 0:TF], AF.Sin)
        adec = fw.sb("adec", [128, 256])
        dv = self.rb[:, RB["hydecay"]:RB["hydecay"] + 256]
        fw.stt(adec[:], dv, -1.0, dv, ALU.mult, ALU.max)
        win = [fw.sb("hwin%d" % i, [128, 256]) for i in range(2)]
        hf = [fw.sb("hhf%d" % i, [128, 256]) for i in range(2)]
        hb = [fw.sb("hhb%d" % i, [128, 256]) for i in range(2)]
        ab = [fw.sb("hab%d" % i, [128, 256]) for i in range(2)]
        a2 = [fw.sb("hab2%d" % i, [128, 256]) for i in range(2)]
        asum = self.PS[7]
        for i in range(nT):
            ps3 = self.PS[2 + i % 2]
            fw.mm(ps3[:, 0:512], h2[:, i * 128:(i + 1) * 128], w3[:, :])
            w_ = win[i % 2]
            fw.act(w_[:], adec[:], AF.Exp, scale=negt[:, i:i + 1])
            f_, b_, a_ = hf[i % 2], hb[i % 2], ab[i % 2]
            fw.tt(f_[:], ps3[:, 0:256], w_[:], ALU.mult)
            fw.tt(b_[:], ps3[:, 256:512], w_[:], ALU.mult)
            if i == 0:
                fw.memset(b_[0:1, :], 0.0, eng="dve")
            fw.tt(AU[:, i, 0:256].k(("f", i)), f_[:], b_[:], ALU.add, eng="pool")
            fw.tt(AU[:, i, 512:768].k(("f", i)), b_[:], f_[:], ALU.subtract, eng="pool")
            fw.act(a_[:], f_[:], AF.Abs)
            fw.act(a2[i % 2][:], b_[:], AF.Abs)
            fw.tt(a_[:], a_[:], a2[i % 2][:], ALU.add)
            fw.mm(asum[:, 0:256], ones, a_[:], start=(i == 0), stop=(i == nT - 1))
        fw.op("dve", lambda e: e.reciprocal(out=rn[:].ap, in_=asum[:, 0:256].ap), [asum[:, 0:256]], [rn[:]])
        fw.release(m2)
        m2 = fw.mark()
        tin = [fw.sb("hyt%d" % i, [128, 6, 514]) for i in range(2)]
        cv = [fw.sb("hyc%d" % i, [128, 6, 512]) for i in range(2)]
        wv = [fw.sb("hyw%d" % i, [128, 2, 512]) for i in range(2)]
        for g in range(nG):
            tok0 = base + g * TB
            t = tin[g % 2]
            self.load_halo(t, 0, 6, tok0, TB)
            c_ = cv[g % 2]
            for j in range(6):
                self.conv3(c_[:, j, 0:TB], t[:, j, :], PP["hycw"] + 3 * j, TB,
                           bias=self.pp[:, PP["hycb"] + j:PP["hycb"] + j + 1], eng=("dve" if j % 2 == 0 else "pool"))
            fw.cp(X0[:, :, g * TB:(g + 1) * TB].k(g), c_[:, 0:2, 0:TB], eng="act")
            w_ = wv[g % 2]
            fw.tt(w_[:, :, 0:TB], c_[:, 2:4, 0:TB], c_[:, 4:6, 0:TB], ALU.mult)
            for cc in range(2):
                fw.act(WB[:, cc, g * TB:(g + 1) * TB].k((g, cc)), w_[:, cc, 0:TB], AF.Copy,
                       scale=self.pp[:, PP["hybias"] + cc:PP["hybias"] + cc + 1])
            for jp in range(tpb // 2):
                ps = self.PS[(g * 2 + jp) % 4]
                for jj in range(2):
                    jt = jp * 2 + jj
                    for cc in range(2):
                        fw.tr(ps[:, (jj * 2 + cc) * 128:(jj * 2 + cc + 1) * 128], w_[:, cc, jt * 128:(jt + 1) * 128], ident)
                i0 = g * tpb + jp * 2
                fw.cp(AU[:, i0:i0 + 2, 256:512].k(("u", i0)), ps[:].r("p (j c) -> p j c", j=2),
                      eng=("dve" if jp % 2 == 0 else "act"))
        fw.release(m2)
        m2 = fw.mark()
        tfr = [fw.sb("tfr%d" % i, [128, nT * 128], BF16) for i in range(4)]
        gcn = [fw.sb("gcn%d" % i, [128, 256]) for i in range(2)]
        gsn = [fw.sb("gsn%d" % i, [128, 256]) for i in range(2)]
        tq = [fw.sb("tq%d" % i, [128, 256]) for i in range(4)]
        for kc in range(nT):
            tabs = (tfr[(kc % 2) * 2], tfr[(kc % 2) * 2 + 1])
            fw.dma("sp", tabs[0][:], tf_d[0, kc])
            fw.dma("pool", tabs[1][:], tf_d[1, kc])
            pc = self.PS[(kc % 2) * 2]
            pS = self.PS[(kc % 2) * 2 + 1]
            for i in range(nT):
                fw.mm(pc[:, 0:512], tabs[0][:, i * 128:(i + 1) * 128], AU[:, i, 0:512], start=(i == 0), stop=(i == nT - 1))
            for i in range(nT):
                fw.mm(pS[:, 0:512], tabs[1][:, i * 128:(i + 1) * 128], AU[:, i, 256:768], start=(i == 0), stop=(i == nT - 1))
            gc_, gs_ = gcn[kc % 2], gsn[kc % 2]
            fw.tt(gc_[:], pc[:, 0:256], rn[:], ALU.mult)
            fw.tt(gs_[:], pS[:, 256:512], rn[:], ALU.mult)
            t1, t2, t3, t4 = tq
            fw.tt(t1[:], pc[:, 256:512], gc_[:], ALU.mult)
            fw.tt(t2[:], pS[:, 0:256], gs_[:], ALU.mult)
            fw.tt(ZB[:, kc, 0:256].k(kc), t1[:], t2[:], ALU.add, eng="pool")
            fw.tt(t3[:], pS[:, 0:256], gc_[:], ALU.mult)
            fw.tt(t4[:], pc[:, 256:512], gs_[:], ALU.mult)
            fw.tt(ZB[:, kc, 256:512].k(kc), t3[:], t4[:], ALU.subtract, eng="pool")
        fw.release(m2)
        m2 = fw.mark()
        KP = min(8, nT)
        tir = [fw.sb("tir%d" % i, [128, KP * TB], BF16) for i in range(4)]
        yt = [fw.sb("hyy%d" % i, [128, 512]) for i in range(2)]
        ob = [fw.sb("hyo%d" % i, [128, 2, 512], BF16) for i in range(2)]
        npiece = nT // KP
        ri = 0
        for tg in range(nG):
            tok0 = base + tg * TB
            pa = [self.PS[4 + (tg % 2) * 2], self.PS[5 + (tg % 2) * 2]]
            for piece in range(npiece):
                for j in range(2):
                    tab = tir[ri % 4]
                    fw.dma("sp" if ri % 2 == 0 else "pool", tab[:], ti_d[j, tg][:, piece * KP * TB:(piece + 1) * KP * TB])
                    ri += 1
                    for kk in range(KP):
                        kc = piece * KP + kk
                        first = (piece == 0 and j == 0 and kk == 0)
                        last = (piece == npiece - 1 and j == 1 and kk == KP - 1)
                        for cc in range(2):
                            fw.mm(pa[cc][:, 0:TB], ZB[:, kc, j * 256 + cc * 128:j * 256 + (cc + 1) * 128],
                                  tab[:, kk * TB:(kk + 1) * TB], start=first, stop=last)
            o = ob[tg % 2]
            for cc in range(2):
                y = yt[cc]
                fw.stt(y[:, 0:TB], pa[cc][:, 0:TB], 1.0 / Lx, WB[:, cc, tg * TB:(tg + 1) * TB], ALU.mult, ALU.add)
                fw.tt(o[:, cc, 0:TB], y[:, 0:TB], X0[:, cc, tg * TB:(tg + 1) * TB], ALU.mult, eng="pool")
            fw.dma("pool", self.brv()[:, 0:2, tok0:tok0 + TB].k(("hy", tok0)), o[:, :, 0:TB])
        fw.release(m2)
        fw.release(m)

    def phase_hgrn_v1(self, l):
        fw = self.fw
        m = fw.mark()
        ident = self.c(C_IDENT)
        ones64 = self.cst[0:64, C_ONES:C_ONES + 64]
        scanm = self.c(C_SCAN)
        cmask = self.c(C_CMASK, 8)
        uv = self.uv()
        hgv = self.hgo[:].r("v (h t) -> v h t", h=4)
        gv = self.uT[2560:2816, :].r("(h v) t -> v h t", v=64)
        bro = self.brT[512:768, :].r("(h v) t -> v h t", v=64)
        epsb = fw.sb("epsb", [128, 1])
        fw.memset(epsb[:], RMS_EPS)
        lbv = fw.sb("lbv", [128, 4])
        oml = fw.sb("oml", [128, 4])
        if l == 0:
            fw.memset(lbv[:], 0.0)
            fw.memset(oml[:], 1.0)
        else:
            for d in range(2):
                for cc in range(2):
                    c1 = PP["lbl"] + (d * 2 + 1) * 2 + cc
                    c0 = PP["lbl"] + (d * 2 + 0) * 2 + cc
                    j = d * 2 + cc
                    fw.tt(lbv[:, j:j + 1], self.pp[:, c1:c1 + 1], self.pp[:, c0:c0 + 1], ALU.subtract)
            fw.act(lbv[:], lbv[:], AF.Exp, scale=-1.0)
            fw.ts(lbv[:], lbv[:], 1.0, ALU.add)
            fw.op("dve", lambda e: e.reciprocal(out=lbv[:].ap, in_=lbv[:].ap), [lbv[:]], [lbv[:]])
            fw.ts(oml[:], lbv[:], -1.0, ALU.mult, 1.0, ALU.add)
        S = [[fw.sb("hgS%d%d" % (cc, p), [128, 8, 64]) for p in range(2)] for cc in range(2)]
        R2 = 2
        qt = [fw.sb("hgq%d" % i, [128, 2, 128]) for i in range(R2)]
        ftl = [fw.sb("hgf%d" % i, [128, 2, 128]) for i in range(R2)]
        vt = [fw.sb("hgv%d" % i, [128, 2, 128]) for i in range(R2)]
        sg = fw.sb("hgsg", [128, 2, 128])
        gg = [fw.sb("hggg%d" % i, [128, 128]) for i in range(2)]
        lf = [fw.sb("hglf%d" % i, [128, 128]) for i in range(2)]
        kk = [fw.sb("hgkk%d" % i, [128, 128]) for i in range(2)]
        bp = [fw.sb("hgbp%d" % i, [128, 128]) for i in range(2)]
        bb = [fw.sb("hgbb%d" % i, [128, 128]) for i in range(2)]
        tm = [fw.sb("hgtm%d" % i, [128, 128]) for i in range(2)]
        qd = [fw.sb("hgqd%d" % i, [128, 128]) for i in range(2)]
        ki = [fw.sb("hgki%d" % i, [128, 128]) for i in range(2)]
        ke = [fw.sb("hgke%d" % i, [128, 128]) for i in range(2)]
        dec = [fw.sb("hgdec%d" % i, [128, 8]) for i in range(2)]
        ketok = [fw.sb("hgket%d" % i, [128, 128]) for i in range(2)]
        vtok = [fw.sb("hgvt%d" % i, [128, 128]) for i in range(2)]
        vexp = [fw.sb("hgvx%d" % i, [128, 8, 128]) for i in range(2)]
        atm = [fw.sb("hgatm%d" % i, [128, 128]) for i in range(4)]
        osb = [fw.sb("hgo%d" % i, [64, 512]) for i in range(2)]
        of = [fw.sb("hgof%d" % i, [64, 512]) for i in range(2)]
        gt = [fw.sb("hggt%d" % i, [64, 512]) for i in range(2)]
        sq = fw.sb("hgsq", [64, 512])
        rs = fw.sb("hgrs", [64, 512])
        sgt = fw.sb("hgsgt", [64, 512])
        obf = [fw.sb("hgob%d" % i, [64, 512], BF16) for i in range(2)]
        ng = self.pp[0:64, PP["hgng"]:PP["hgng"] + 1]
        for d in range(getattr(self, "hg_passes", 2)):
            order = list(range(34)) if d == 0 else [1, 0] + list(range(33, 1, -1))
            MI = self.c(C_MIF) if d == 0 else self.c(C_MIB)
            for cc in range(2):
                fw.memset(S[cc][0][:, 0, :], 0.0)
            for it, tile in enumerate(order):
                if it >= getattr(self, "hg_limit", 99):
                    break
                tok0 = tile * 128
                need_o = not (l == DEPTH - 1 and tile < 2)
                par = it % 2
                q_, f_, v_ = qt[it % R2], ftl[it % R2], vt[it % R2]
                fw.dma("sp", q_[:], uv[:, 12:14, tok0:tok0 + 128])
                fc = 14 if d == 0 else 16
                fw.dma("sp", f_[:], uv[:, fc:fc + 2, tok0:tok0 + 128])
                fw.dma("sp", v_[:], uv[:, 18:20, tok0:tok0 + 128])
                fw.act(sg[:], f_[:], AF.Exp, scale=-1.0)
                fw.ts(sg[:], sg[:], 1.0, ALU.add, eng="pool")
                fw.op("dve", lambda e: e.reciprocal(out=sg[:].ap, in_=sg[:].ap), [sg[:]], [sg[:]])
                pso = self.PS[6]
                pso2 = self.PS[7]
                for cc in range(2):
                    j = d * 2 + cc
                    g_, l_, k_, b_, bb_, t_ = gg[cc], lf[cc], kk[cc], bp[cc], bb[cc], tm[cc]
                    fw.ts(g_[:], sg[:, cc, :], oml[:, j:j + 1], ALU.mult, lbv[:, j:j + 1], ALU.add)
                    fw.act(l_[:], g_[:], AF.Ln)
                    fw.ts(k_[:], g_[:], -1.0, ALU.mult, 1.0, ALU.add, eng="pool")
                    fw.scan(b_[:], scanm, l_[:], 0.0, ALU.mult, ALU.add)
                    b3 = b_[:].r("p (n s) -> p n s", s=16)
                    tot = b3[:, :, 15:16]
                    if d == 0:
                        bcur = b_
                    else:
                        fw.tt(t_[:], l_[:], b_[:], ALU.subtract)
                        fw.tt(bb_[:].r("p (n s) -> p n s", s=16), t_[:].r("p (n s) -> p n s", s=16),
                              tot.bc([128, 8, 16]), ALU.add)
                        bcur = bb_
                    fw.act(dec[cc][:], b3[:, :, 15], AF.Exp)
                    fw.act(t_[:], bcur[:], AF.Exp)
                    fw.stt(qd[cc][:], q_[:, cc, :], 0.125, t_[:], ALU.mult, ALU.mult)
                    fw.act(t_[:], bcur[:], AF.Exp, scale=-1.0)
                    fw.tt(ki[cc][:], k_[:], t_[:], ALU.mult)
                    fw.tt(t_[:].r("p (n s) -> p n s", s=16), tot.bc([128, 8, 16]),
                          bcur[:].r("p (n s) -> p n s", s=16), ALU.subtract)
                    fw.act(t_[:], t_[:], AF.Exp)
                    fw.tt(ke[cc][:], k_[:], t_[:], ALU.mult, eng="pool")
                    if getattr(self, "hg_stage", 9) < 1:
                        continue
                    pst = self.PS[cc]
                    fw.tr(pst[:, 0:128], ke[cc][:], ident)
                    fw.tr(pst[:, 128:256], v_[:, cc, :], ident)
                    fw.cp(ketok[cc][:], pst[:, 0:128], eng="act")
                    fw.cp(vtok[cc][:], pst[:, 128:256], eng="act")
                    fw.tt(vexp[cc][:], vtok[cc][:].un(1).bc([128, 8, 128]), cmask.un(2).bc([128, 8, 128]), ALU.mult)
                    for h in range(2):
                        if getattr(self, "hg_stage", 9) < 2:
                            continue
                        hh = cc * 2 + h
                        rows = slice(h * 64, (h + 1) * 64)
                        if need_o:
                            pa = self.PS[2 + h]
                            fw.mm(pa[:, 0:128], ki[cc][rows, :], qd[cc][rows, :])
                            am = atm[hh]
                            fw.tt(am[:], pa[:, 0:128], MI, ALU.mult)
                            fw.mm(pso[0:64, hh * 128:(hh + 1) * 128].k(hh), vtok[cc][:, h * 64:(h + 1) * 64], am[:])
                        pkv = self.PS[4 + h]
                        fw.mm(pkv[:, 0:512], ketok[cc][:], vexp[cc][:, :, h * 64:(h + 1) * 64])
                        Scur = S[cc][par]
                        Snxt = S[cc][1 - par]
                        for jj in range(8 if getattr(self, "hg_stage", 9) >= 3 else 0):
                            n = jj if d == 0 else 7 - jj
                            if need_o:
                                fw.mm(pso2[0:64, hh * 128 + n * 16:hh * 128 + (n + 1) * 16].k(hh),
                                      Scur[rows, jj, :].k((h, jj)), qd[cc][rows, n * 16:(n + 1) * 16])
                            dst = Scur[rows, jj + 1, :].k((h, jj + 1)) if jj < 7 else Snxt[rows, 0, :].k((h, 0))
                            fw.stt(dst, Scur[rows, jj, :].k((h, jj)), dec[cc][rows, n:n + 1],
                                   pkv[rows, n * 64:(n + 1) * 64], ALU.mult, ALU.add)
                if not need_o or getattr(self, "hg_stage", 9) < 4:
                    continue
                o_ = osb[it % 2]
                fw.cp(o_[:], pso[0:64, :], eng="act")
                fw.tt(o_[:], o_[:], pso2[0:64, :], ALU.add)
                if d == 0:
                    fw.dma("pool", hgv[:, :, tok0:tok0 + 128].k(("t", tile)), o_[:].r("v (h t) -> v h t", h=4))
                    continue
                of_ = of[it % 2]
                g2 = gt[it % 2]
                fw.dma("pool", of_[:].r("v (h t) -> v h t", h=4), hgv[:, :, tok0:tok0 + 128].k(("t", tile)))
                fw.dma("pool", g2[:].r("v (h t) -> v h t", h=4), gv[:, :, tok0:tok0 + 128])
                fw.tt(o_[:], o_[:], of_[:], ALU.add)
                fw.tt(sq[:], o_[:], o_[:], ALU.mult, eng="pool")
                pss = self.PS[0]
                fw.mm(pss[0:64, 0:512], ones64, sq[:])
                fw.act(rs[:], pss[0:64, 0:512], AF.Ln, bias=epsb[0:64, :], scale=1.0 / 64.0)
                fw.act(rs[:], rs[:], AF.Exp, scale=-0.5)
                fw.tt(o_[:], o_[:], rs[:], ALU.mult)
                fw.act(sgt[:], g2[:], AF.Exp, scale=-1.0)
                fw.ts(sgt[:], sgt[:], 1.0, ALU.add, eng="pool")
                fw.op("dve", lambda e: e.reciprocal(out=sgt[:].ap, in_=sgt[:].ap), [sgt[:]], [sgt[:]])
                fw.tt(sgt[:], sgt[:], g2[:], ALU.mult, eng="pool")
                ob_ = obf[it % 2]
                fw.stt(ob_[:], o_[:], ng, sgt[:], ALU.mult, ALU.mult)
                fw.dma("pool", bro[:, :, tok0:tok0 + 128].k(("hg", tile)), ob_[:].r("v (h t) -> v h t", h=4))
            fw.barrier()
        fw.release(m)

    def phase_hgrn(self, l):
        fw = self.fw
        m = fw.mark()
        id64 = self.cst[0:64, C_IDENT:C_IDENT + 64]
        ones64 = self.cst[0:64, C_ONES:C_ONES + 64]
        MI = [self.c(C_MIF), self.c(C_MIB)]
        CM = [self.c(C_CMASK, 8), self.c(C_CMASKR, 8)]
        hgv = [self.hgo[:].r("v (h t) -> v h t", h=4), self.hgo2[:].r("v (h t) -> v h t", h=4)]
        uq = self.uT[1536:1792, :].r("(h d) t -> d h t", d=64)
        uf = [self.uT[1792:2048, :].r("(h d) t -> d h t", d=64), self.uT[2048:2304, :].r("(h d) t -> d h t", d=64)]
        ui = self.uT[2304:2560, :].r("(h d) t -> d h t", d=64)
        gv = self.uT[2560:2816, :].r("(h v) t -> v h t", v=64)
        bro = self.brT[512:768, :].r("(h v) t -> v h t", v=64)
        epsb = fw.sb("epsb", [64, 1])
        fw.memset(epsb[:], RMS_EPS)
        lbv = fw.sb("lbv", [64, 8])
        oml = fw.sb("oml", [64, 8])
        if l > 0:
            for d in range(2):
                c1 = PP["lbl64"] + (d * 2 + 1) * 4
                c0 = PP["lbl64"] + (d * 2 + 0) * 4
                fw.tt(lbv[:, d * 4:(d + 1) * 4], self.pp[0:64, c1:c1 + 4], self.pp[0:64, c0:c0 + 4], ALU.subtract)
            fw.act(lbv[:], lbv[:], AF.Exp, scale=-1.0)
            fw.ts(lbv[:], lbv[:], 1.0, ALU.add)
            fw.op("dve", lambda e: e.reciprocal(out=lbv[:].ap, in_=lbv[:].ap), [lbv[:]], [lbv[:]])
            fw.ts(oml[:], lbv[:], -1.0, ALU.mult, 1.0, ALU.add)
        SM = fw.sb("hg_sm", [64, 2048])
        fw.cp(SM[:].r("p (a b) -> p a b", b=16), self.cst[0:64, C_SCAN16:C_SCAN16 + 16].un(1).bc([64, 128, 16]))
        S = fw.sb("hg_S", [64, 9, 8, 64])
        fw.memset(S[:, 0, :, :], 0.0)
        F = 2048
        qin = fw.sb("hg_qin", [64, 4, 512])
        fin = fw.sb("hg_fin", [64, 4, 512])
        T = [fw.sb("hg_T%d" % i, [64, F]) for i in range(5)]
        qd32 = [fw.sb("hg_qd32%d" % d, [64, 4, 512]) for d in range(2)]
        qd16 = [fw.sb("hg_qd16%d" % d, [64, 4, 512], BF16) for d in range(2)]
        ki16 = [fw.sb("hg_ki16%d" % d, [64, 4, 512], BF16) for d in range(2)]
        ke32 = [fw.sb("hg_ke32%d" % d, [64, 4, 512]) for d in range(2)]
        vin = [fw.sb("hg_vin%d" % d, [64, 4, 512]) for d in range(2)]
        dec = [fw.sb("hg_dec%d" % d, [64, 4, 32]) for d in range(2)]
        ketok = [fw.sb("hg_ket%d" % d, [128, 256], BF16) for d in range(2)]
        vtok = [fw.sb("hg_vt%d" % d, [128, 256], BF16) for d in range(2)]
        vexp = [fw.sb("hg_vx%d" % d, [128, 8, 256], BF16) for d in range(2)]
        atm = [fw.sb("hg_atm%d" % d, [128, 4, 128], BF16) for d in range(2)]
        kvs = [fw.sb("hg_kvs%d" % d, [64, 8, 4, 64]) for d in range(2)]
        osb = [fw.sb("hg_o%d" % d, [64, 512]) for d in range(2)]
        ot = [fw.sb("hg_ot%d" % d, [64, 512]) for d in range(2)]
        CH = ["dve", "pool"]

        def prep(d, tok0, TBk):
            n = 4 * TBk
            nch = TBk // 16
            v3 = lambda b: b[:, 0:n].r("p (h t) -> p h t", h=4)
            c3 = lambda b: b[:, 0:n].r("p (c s) -> p c s", s=16)
            fw.dma("sp", qin[:, :, 0:TBk], uq[:, :, tok0:tok0 + TBk])
            fw.dma("sp", fin[:, :, 0:TBk], uf[d][:, :, tok0:tok0 + TBk])
            fw.dma("sp", vin[d][:, :, 0:TBk], ui[:, :, tok0:tok0 + TBk])
            sg, lf, kk, b_, t_ = T
            fw.act(v3(sg), fin[:, :, 0:TBk], AF.Exp, scale=-1.0)
            fw.ts(sg[:, 0:n], sg[:, 0:n], 1.0, ALU.add, eng="pool")
            fw.op("dve", lambda e: e.reciprocal(out=sg[:, 0:n].ap, in_=sg[:, 0:n].ap), [sg[:, 0:n]], [sg[:, 0:n]])
            if l > 0:
                for h in range(4):
                    j = d * 4 + h
                    fw.ts(sg[:, h * TBk:(h + 1) * TBk], sg[:, h * TBk:(h + 1) * TBk], oml[:, j:j + 1], ALU.mult,
                          lbv[:, j:j + 1], ALU.add)
            fw.act(lf[:, 0:n], sg[:, 0:n], AF.Ln)
            fw.ts(kk[:, 0:n], sg[:, 0:n], -1.0, ALU.mult, 1.0, ALU.add, eng="pool")
            fw.scan(b_[:, 0:n], SM[:, 0:n], lf[:, 0:n], 0.0, ALU.mult, ALU.add)
            tot = c3(b_)[:, :, 15:16]
            fw.act(dec[d][:, :, 0:nch], b_[:, 0:n].r("p (h c s) -> p h c s", h=4, s=16)[:, :, :, 15], AF.Exp)
            if d == 1:
                fw.tt(lf[:, 0:n], lf[:, 0:n], b_[:, 0:n], ALU.subtract, eng="pool")
                fw.tt(c3(lf), c3(lf), tot.bc([64, n // 16, 16]), ALU.add)
                bcur = lf
            else:
                bcur = b_
            fw.act(t_[:, 0:n], bcur[:, 0:n], AF.Exp)
            fw.stt(qd32[d][:, :, 0:TBk], qin[:, :, 0:TBk], 0.125, v3(t_), ALU.mult, ALU.mult)
            fw.cp(qd16[d][:, :, 0:TBk], qd32[d][:, :, 0:TBk], eng="pool")
            fw.act(t_[:, 0:n], bcur[:, 0:n], AF.Exp, scale=-1.0)
            fw.tt(ki16[d][:, :, 0:TBk], v3(kk), v3(t_), ALU.mult)
            fw.tt(c3(t_), tot.bc([64, n // 16, 16]), c3(bcur), ALU.subtract, eng="pool")
            fw.act(t_[:, 0:n], t_[:, 0:n], AF.Exp)
            fw.tt(ke32[d][:, :, 0:TBk], v3(kk), v3(t_), ALU.mult)

        def tile_pre(d, off, tile, need_o):
            pst = self.PS[d]
            for h in range(4):
                fw.tr(pst[:, h * 64:(h + 1) * 64], ke32[d][:, h, off:off + 128], id64)
                fw.tr(pst[:, 256 + h * 64:256 + (h + 1) * 64], vin[d][:, h, off:off + 128], id64)
            fw.cp(ketok[d][:], pst[:, 0:256], eng="act")
            fw.cp(vtok[d][:], pst[:, 256:512], eng="act")
            fw.tt(vexp[d][:], vtok[d][:].un(1).bc([128, 8, 256]), CM[d].un(2).bc([128, 8, 256]), ALU.mult,
                  eng=("dve" if d == 1 else "pool"))
            poi = self.PS[3 + d * 2]
            if need_o:
                pat = self.PS[2]
                for h in range(4):
                    fw.mm(pat[:, h * 128:(h + 1) * 128].k(h), ki16[d][:, h, off:off + 128], qd16[d][:, h, off:off + 128])
                fw.tt(atm[d][:], pat[:].r("p (h t) -> p h t", h=4), MI[d].un(1).bc([128, 4, 128]), ALU.mult)
                for h in range(4):
                    fw.mm(poi[0:64, h * 128:(h + 1) * 128].k(h), vtok[d][:, h * 64:(h + 1) * 64], atm[d][:, h, :])
            pkv = self.PS[7]
            for h in range(4):
                fw.mm(pkv[0:64, 0:512], ketok[d][:, h * 64:(h + 1) * 64], vexp[d][:, :, h * 64:(h + 1) * 64])
                fw.cp(kvs[d][:, :, h, :].k(h), pkv[0:64, 0:512].r("p (j v) -> p j v", j=8), eng="act")

        def chain_step(d, j, off, need_o):
            pox = self.PS[4 + d * 2]
            ce = CH[d]
            dsl = slice(d * 4, (d + 1) * 4)
            c0 = off // 16
            n = j if d == 0 else 7 - j
            if need_o:
                for h in range(4):
                    fw.mm(pox[0:64, h * 128 + n * 16:h * 128 + (n + 1) * 16].k(h),
                          S[:, j, d * 4 + h, :].k((d, j)), qd32[d][:, h, off + n * 16:off + (n + 1) * 16])
            dcb = dec[d][:, :, c0 + n:c0 + n + 1].bc([64, 4, 64])
            dst = S[:, j + 1, dsl, :].k((d, j + 1)) if j < 7 else S[:, 0, dsl, :].k((d, 0))
            fw.tt(S[:, 8, dsl, :].k((d, 8)) if j == 7 else dst, S[:, j, dsl, :].k((d, j)), dcb, ALU.mult, eng=ce)
            src = S[:, 8, dsl, :].k((d, 8)) if j == 7 else dst
            fw.tt(dst, src, kvs[d][:, j, :, :], ALU.add, eng=ce)

        def tile_post(d, tile, need_o):
            if not need_o:
                return
            tok0 = tile * 128
            poi = self.PS[3 + d * 2]
            pox = self.PS[4 + d * 2]
            o_ = osb[d]
            fw.cp(o_[:], poi[0:64, :], eng="act")
            fw.tt(o_[:], o_[:], pox[0:64, :], ALU.add, eng="dve")
            fw.dma("pool", hgv[d][:, :, tok0:tok0 + 128].k(("t", tile)), o_[:].r("v (h t) -> v h t", h=4))

        steps = [(0, 0, 256)] + [(LC + 512 * i, LC + 512 * (7 - i), 512) for i in range(8)]
        for si, (tf0, tb0, TBk) in enumerate(steps):
            prep(0, tf0, TBk)
            prep(1, tb0, TBk)
            nt = TBk // 128
            for i in range(nt):
                info = []
                for d in range(2):
                    off = i * 128 if d == 0 else (nt - 1 - i) * 128
                    t0 = (tf0 if d == 0 else tb0) + off
                    tile = t0 // 128
                    need_o = not (l == DEPTH - 1 and tile < 2)
                    info.append((off, tile, need_o))
                    tile_pre(d, off, tile, need_o)
                for j in range(8):
                    for d in range(2):
                        chain_step(d, j, info[d][0], info[d][2])
                for d in range(2):
                    tile_post(d, info[d][1], info[d][2])
        fw.barrier()
        A = T[0:3]
        obf3 = ki16[0]
        ng = self.pp[0:64, PP["hgng"]:PP["hgng"] + 1]
        for gi, (tok0, TB) in enumerate(GROUPS):
            if gi == 0 and l == DEPTH - 1:
                continue
            n = 4 * TB
            v3 = lambda b: b[:, 0:n].r("p (h t) -> p h t", h=4)
            o1, o2, g2 = qd32[0], qd32[1], ke32[0]
            fw.dma("sp", o1[:, :, 0:TB], hgv[0][:, :, tok0:tok0 + TB])
            fw.dma("sp", o2[:, :, 0:TB], hgv[1][:, :, tok0:tok0 + TB])
            fw.dma("pool", g2[:, :, 0:TB], gv[:, :, tok0:tok0 + TB])
            o_, sq_, sg_ = A
            fw.tt(v3(o_), o1[:, :, 0:TB], o2[:, :, 0:TB], ALU.add)
            fw.tt(sq_[:, 0:n], o_[:, 0:n], o_[:, 0:n], ALU.mult, eng="pool")
            for h in range(4):
                pss = self.PS[h % 2]
                fw.mm(pss[0:64, 0:TB], ones64, sq_[:, h * TB:(h + 1) * TB])
                fw.act(sq_[:, h * TB:(h + 1) * TB].k(h), pss[0:64, 0:TB], AF.Ln, bias=epsb[:], scale=1.0 / 64.0)
            fw.act(sq_[:, 0:n], sq_[:, 0:n], AF.Exp, scale=-0.5)
            fw.tt(o_[:, 0:n], o_[:, 0:n], sq_[:, 0:n], ALU.mult)
            fw.act(v3(sg_), g2[:, :, 0:TB], AF.Exp, scale=-1.0)
            fw.ts(sg_[:, 0:n], sg_[:, 0:n], 1.0, ALU.add, eng="pool")
            fw.op("dve", lambda e: e.reciprocal(out=sg_[:, 0:n].ap, in_=sg_[:, 0:n].ap), [sg_[:, 0:n]], [sg_[:, 0:n]])
            fw.tt(v3(sg_), v3(sg_), g2[:, :, 0:TB], ALU.mult, eng="pool")
            fw.stt(obf3[:, :, 0:TB], v3(o_), ng, v3(sg_), ALU.mult, ALU.mult)
            fw.dma("pool", bro[:, :, tok0:tok0 + TB].k(("hg", gi)), obf3[:, :, 0:TB])
        fw.release(m)

    def phase_ssd(self, l):
        fw = self.fw
        m = fw.mark()
        ident = self.c(C_IDENT)
        ones = self.c(C_ONES)
        uv = self.uv()
        brv = self.brv()
        dtb = self.rb[:, RB["dtbias"]:RB["dtbias"] + 8]
        dsk = self.rb[:, RB["ssdd"]:RB["ssdd"] + 4]
        ngb = self.rb[:, RB["ssdng"]:RB["ssdng"] + 256]
        onec = fw.sb("sd_one", [128, 1])
        fw.memset(onec[:], 1.0)
        epsb = fw.sb("sd_eps", [128, 1])
        fw.memset(epsb[:], RMS_EPS)
        nea = fw.sb("sd_nea", [128, 8])
        fw.act(nea[:], self.rb[:, RB["alog"]:RB["alog"] + 8], AF.Exp)
        fw.ts(nea[:], nea[:], -1.0, ALU.mult)
        yD = [self.yf, self.yb]
        B = []
        for d in range(2):
            b = {}
            b["ST"] = fw.sb("sd_ST%d" % d, [128, 4, 64])
            fw.memset(b["ST"][:], 0.0)
            b["xin"] = [fw.sb("sd_xin%d%d" % (d, i), [128, 4, 130]) for i in range(2)]
            b["dtr"] = [fw.sb("sd_dtr%d%d" % (d, i), [128, 8]) for i in range(2)]
            for nm, shp in (("xc", [128, 4, 128]), ("sg", [128, 4, 128]), ("xb", [128, 4, 128]), ("xs", [128, 256]),
                            ("bt", [128, 128]), ("dtt", [128, 8]), ("atok", [128, 8]), ("negcum", [128, 4]),
                            ("expcum", [128, 4]), ("totE", [128, 4]), ("edec", [128, 4]), ("cb", [128, 256]),
                            ("xd", [128, 4, 64]), ("ydg", [128, 256]), ("y", [128, 256]), ("xsd", [128, 256])):
                b[nm] = fw.sb("sd_%s%d" % (nm, d), shp)
            for nm in ("arow", "E", "W", "bdec"):
                b[nm] = [fw.sb("sd_%s%d%d" % (nm, d, i), [128, 128]) for i in range(2)]
            B.append(b)
        PX, PY = self.PS[0], self.PS[1]
        PT = [self.PS[2], self.PS[3]]
        PD = [self.PS[4], self.PS[5]]
        PYD = self.PS[6]
        PSS = self.PS[7]

        def tile_gen(d, it, tile):
            b = B[d]
            TRI = self.c(C_TRIF) if d == 0 else self.c(C_TRIB)
            NEGM = self.c(C_NEGF) if d == 0 else self.c(C_NEGB)
            j0 = d * 4
            tok0 = tile * 128
            need_o = not (l == DEPTH - 1 and tile < 2)
            xi = b["xin"][it % 2]
            self.load_halo(xi, 24, 4, tok0, 128)
            dr = b["dtr"][it % 2]
            fw.dma("sp", dr[:], self.dtT[tok0:tok0 + 128, :])
            xc, sg, xb = b["xc"], b["sg"], b["xb"]
            for j in range(4):
                self.conv3(xc[:, j, :], xi[:, j, :], PP["sdcw"] + 3 * j, 128,
                           bias=self.pp[:, PP["sdcb"] + j:PP["sdcb"] + j + 1])
                yield
            fw.act(sg[:], xc[:], AF.Exp, scale=-1.0)
            yield
            fw.ts(sg[:], sg[:], 1.0, ALU.add)
            fw.op("dve", lambda e: e.reciprocal(out=sg[:].ap, in_=sg[:].ap), [sg[:]], [sg[:]])
            fw.tt(xb[:], xc[:], sg[:], ALU.mult)
            yield
            pt = PT[d]
            for j in range(3):
                fw.tr(pt[:, j * 128:(j + 1) * 128], xb[:, j, :], ident)
            xs_, bt_, dtt, atok = b["xs"], b["bt"], b["dtt"], b["atok"]
            fw.cp(xs_[:], pt[:, 0:256], eng="act")
            fw.cp(bt_[:], pt[:, 256:384], eng="act")
            yield
            fw.tt(dtt[:], dr[:], dtb, ALU.add)
            fw.act(dtt[:], dtt[:], AF.Exp)
            fw.act(dtt[:], dtt[:], AF.Ln, bias=onec[:], scale=1.0)
            fw.tt(atok[:], dtt[:], nea[:], ALU.mult)
            yield
            fw.mm(pt[:, 384:388], TRI, atok[:, j0:j0 + 4])
            fw.mm(pt[:, 392:396], ones, atok[:, j0:j0 + 4])
            negcum, expcum, totE, edec = b["negcum"], b["expcum"], b["totE"], b["edec"]
            fw.ts(negcum[:], pt[:, 384:388], -1.0, ALU.mult)
            fw.act(expcum[:], pt[:, 384:388], AF.Exp)
            fw.act(totE[:], pt[:, 392:396], AF.Exp)
            fw.tt(edec[:], pt[:, 392:396], negcum[:], ALU.add)
            fw.act(edec[:], edec[:], AF.Exp)
            yield
            cb_ = b["cb"]
            for g in range(2):
                gr = slice(g * 64, (g + 1) * 64)
                pcb = (PX, PY)[g]
                fw.mm(pcb[:, d * 128:(d + 1) * 128], xb[gr, 2, :], xb[gr, 3, :])
                fw.cp(cb_[:, g * 128:(g + 1) * 128].k(g), pcb[:, d * 128:(d + 1) * 128], eng="act")
            xd = b["xd"]
            fw.tt(xd[:], xs_[:].r("p (h q) -> p h q", h=4), dtt[:, j0:j0 + 4].un(2).bc([128, 4, 64]), ALU.mult)
            yield
            ST = b["ST"]
            for h in range(4):
                g = h // 2
                gr = slice(g * 64, (g + 1) * 64)
                ar = b["arow"][h % 2]
                fw.act(ar[:], TRI, AF.Copy, scale=atok[:, j0 + h:j0 + h + 1])
                pd = PD[d]
                hc = slice((h % 2) * 128, (h % 2 + 1) * 128)
                fw.mm(pd[:, hc], ones, ar[:], start=True, stop=False)
                fw.mm(pd[:, hc], ident, NEGM, start=False, stop=True)
                e_ = b["E"][h % 2]
                fw.act(e_[:], pd[:, hc], AF.Exp, bias=negcum[:, h:h + 1])
                yield
                w_ = b["W"][h % 2]
                fw.tt(w_[:], e_[:], cb_[:, g * 128:(g + 1) * 128].k(g), ALU.mult)
                yc = slice(d * 256 + h * 64, d * 256 + (h + 1) * 64)
                pyo = (PX, PY)[g]
                yoc = slice(256 + d * 128 + (h % 2) * 64, 256 + d * 128 + (h % 2 + 1) * 64)
                if need_o:
                    fw.mm(PYD[:, yc], w_[:], xd[:, h, :])
                    fw.mm(pyo[:, yoc], xb[gr, 3, :], ST[gr, h, :].k(h))
                bd = b["bdec"][h % 2]
                fw.ts(bd[:], bt_[:], edec[:, h:h + 1], ALU.mult)
                fw.mm(PSS[:, yc].k((d, h)), bd[:], xd[:, h, :])
                fw.stt(ST[gr, h, :].k(h), ST[gr, h, :].k(h), totE[gr, h:h + 1], PSS[gr, yc].k((d, h)), ALU.mult, ALU.add)
                yield
            if not need_o:
                return
            ydg, y_ = b["ydg"], b["y"]
            fw.cp(ydg[:], PYD[:, d * 256:(d + 1) * 256], eng="act")
            for h in range(4):
                g = h // 2
                pyo = (PX, PY)[g]
                yoc = slice(256 + d * 128 + (h % 2) * 64, 256 + d * 128 + (h % 2 + 1) * 64)
                cs = slice(h * 64, (h + 1) * 64)
                fw.stt(y_[:, cs], pyo[:, yoc], expcum[:, h:h + 1], ydg[:, cs], ALU.mult, ALU.add)
            yield
            if d == 0:
                xsd = b["xsd"]
                fw.tt(xsd[:].r("p (h q) -> p h q", h=4), xs_[:].r("p (h q) -> p h q", h=4),
                      dsk.un(2).bc([128, 4, 64]), ALU.mult)
                fw.tt(y_[:], y_[:], xsd[:], ALU.add)
            fw.dma("pool", yD[d][tok0:tok0 + 128, :].k(("t", tile)), y_[:])
            yield

        of = list(range(34))
        ob = [1, 0] + list(range(33, 1, -1))
        for it in range(34):
            if it >= getattr(self, "sd_limit", 99):
                break
            gens = [tile_gen(0, it, of[it]), tile_gen(1, it, ob[it])]
            alive = [True, True]
            while any(alive):
                for d in range(2):
                    if alive[d]:
                        try:
                            next(gens[d])
                        except StopIteration:
                            alive[d] = False
        fw.barrier()
        yfl = [fw.sb("sd_ryf%d" % i, [128, 256]) for i in range(2)]
        ybl = [fw.sb("sd_ryb%d" % i, [128, 256]) for i in range(2)]
        zin = [fw.sb("sd_rz%d" % i, [128, 2, 128]) for i in range(2)]
        zt = fw.sb("sd_rzt", [128, 256])
        zs = fw.sb("sd_rzs", [128, 256])
        yg = fw.sb("sd_ryg", [128, 256])
        ysq = fw.sb("sd_rysq", [128, 256])
        ss = fw.sb("sd_rss", [128, 1])
        ob_ = [fw.sb("sd_rob%d" % i, [128, 2, 128], BF16) for i in range(2)]
        for tile in range(34):
            if l == DEPTH - 1 and tile < 2:
                continue
            tok0 = tile * 128
            a, b2, zi = yfl[tile % 2], ybl[tile % 2], zin[tile % 2]
            fw.dma("sp", a[:], self.yf[tok0:tok0 + 128, :])
            fw.dma("sp", b2[:], self.yb[tok0:tok0 + 128, :])
            fw.dma("sp", zi[:], uv[:, 22:24, tok0:tok0 + 128])
            pz = self.PS[tile % 2]
            for cc in range(2):
                fw.tr(pz[:, cc * 128:(cc + 1) * 128], zi[:, cc, :], ident)
            fw.cp(zt[:], pz[:, 0:256], eng="act")
            fw.act(zs[:], zt[:], AF.Exp, scale=-1.0)
            fw.ts(zs[:], zs[:], 1.0, ALU.add)
            fw.op("dve", lambda e: e.reciprocal(out=zs[:].ap, in_=zs[:].ap), [zs[:]], [zs[:]])
            fw.tt(zs[:], zs[:], zt[:], ALU.mult)
            fw.tt(yg[:], a[:], b2[:], ALU.add)
            fw.tt(yg[:], yg[:], zs[:], ALU.mult)
            fw.act(ysq[:], yg[:], AF.Square)
            fw.red(ss[:], ysq[:], ALU.add)
            fw.act(ss[:], ss[:], AF.Ln, bias=epsb[:], scale=1.0 / 256.0)
            fw.act(ss[:], ss[:], AF.Exp, scale=-0.5)
            fw.stt(yg[:], yg[:], ss[:], ngb, ALU.mult, ALU.mult)
            po = self.PS[2 + tile % 2]
            for cc in range(2):
                fw.tr(po[:, cc * 128:(cc + 1) * 128], yg[:, cc * 128:(cc + 1) * 128], ident)
            o_ = ob_[tile % 2]
            fw.cp(o_[:], po[:, 0:256].r("p (c t) -> p c t", c=2), eng="act")
            fw.dma("pool", brv[:, 6:8, tok0:tok0 + 128].k(("sd", tile)), o_[:])
        fw.release(m)

    def layernorm_fm(self, x, TB, gname, out, W):
        fw = self.fw
        ones = self.c(C_ONES)
        ps_s, ps_q = self.PS[6], self.PS[7]
        for Dc in range(KC):
            fw.mm(ps_s[:, 0:TB], ones, x[:, Dc, 0:TB], start=(Dc == 0), stop=(Dc == KC - 1))
        for Dc in range(KC):
            sq = W["sq"][Dc % 2]
            fw.act(sq[:, 0:TB], x[:, Dc, 0:TB], AF.Square)
            fw.mm(ps_q[:, 0:TB], ones, sq[:, 0:TB], start=(Dc == 0), stop=(Dc == KC - 1))
        mean, msq, rstd, mr = W["mean"], W["msq"], W["rstd"], W["mr"]
        fw.ts(mean[:, 0:TB], ps_s[:, 0:TB], 1.0 / D, ALU.mult)
        fw.tt(msq[:, 0:TB], mean[:, 0:TB], mean[:, 0:TB], ALU.mult, eng="pool")
        fw.stt(rstd[:, 0:TB], ps_q[:, 0:TB], 1.0 / D, msq[:, 0:TB], ALU.mult, ALU.subtract)
        fw.act(rstd[:, 0:TB], rstd[:, 0:TB], AF.Ln, bias=W["lneps"][:], scale=1.0)
        fw.act(rstd[:, 0:TB], rstd[:, 0:TB], AF.Exp, scale=-0.5)
        fw.tt(mr[:, 0:TB], mean[:, 0:TB], rstd[:, 0:TB], ALU.mult, eng="pool")
        g0 = PP[gname + "g"]
        b0 = PP[gname + "b"]
        for Dc in range(KC):
            t = W["t"][Dc % 2]
            fw.tt(t[:, 0:TB], x[:, Dc, 0:TB], rstd[:, 0:TB], ALU.mult)
            fw.tt(t[:, 0:TB], t[:, 0:TB], mr[:, 0:TB], ALU.subtract, eng="pool")
            fw.act(out[:, Dc, 0:TB].k(Dc), t[:, 0:TB], AF.Identity, bias=self.pp[:, b0 + Dc:b0 + Dc + 1],
                   scale=self.pp[:, g0 + Dc:g0 + Dc + 1])

    def ln_work(self):
        fw = self.fw
        W = {"sq": [fw.sb("ln_sq%d" % i, [128, 512]) for i in range(2)],
             "t": [fw.sb("ln_t%d" % i, [128, 512]) for i in range(2)],
             "mean": fw.sb("ln_mean", [128, 512]), "msq": fw.sb("ln_msq", [128, 512]),
             "rstd": fw.sb("ln_rstd", [128, 512]), "mr": fw.sb("ln_mr", [128, 512]),
             "lneps": fw.sb("ln_eps", [128, 1])}
        fw.memset(W["lneps"][:], LN_EPS)
        return W

    def phase_merge(self, l):
        fw = self.fw
        m = fw.mark()
        ident = self.c(C_IDENT)
        wbr = fw.sb("wbr", [128, 8, D], BF16)
        wo = fw.sb("wo", [128, KC, D], BF16)
        wr = fw.sb("wr", [128, KC, NE])
        m1 = fw.mark()
        stage = [fw.sb("mst%d" % i, [128, 4 * D]) for i in range(2)]
        stb = [fw.sb("mstb%d" % i, [128, 4 * D], BF16) for i in range(2)]
        wgv = self.w_gate_d[l].r("(kc p) n -> p kc n", p=128)
        wov = self.w_o_d[l].r("(kc p) n -> p kc n", p=128)
        wgc5 = self.wgc[:].r("dc p (kc k c) -> p dc kc k c", kc=KC, k=4)
        i = 0
        for kc in range(KC):
            self.load_cast(stb[kc % 2][:], wgv[:, kc, :], stage, 4 * D, i)
            for k in range(4):
                fw.dma("pool", wgc5[:, :, kc, k, :].k(("wgc", kc, k)),
                       stb[kc % 2][:, k * D:(k + 1) * D].r("p (dc c) -> p dc c", dc=8))
            i += 1
        for kc in range(KC):
            self.load_cast(wo[:, kc, :].k(kc), wov[:, kc, :], stage, D, i)
            i += 1
        for k in range(4):
            for cc in range(2):
                self.load_cast(wbr[:, k * 2 + cc, :].k(k * 2 + cc), self.w_br_d[l, k, cc * 128:(cc + 1) * 128, :], stage, D, i)
                i += 1
        fw.dma("sp", wr[:], self.w_router_d[:].r("(kc p) e -> p kc e", p=128))
        fw.release(m1)
        W = self.ln_work()
        rt = [fw.sb("mrt%d" % i, [128, KC, 512]) for i in range(1)]
        ht = [fw.sb("mht%d" % i, [128, KC, 512], BF16) for i in range(1)]
        bt = [fw.sb("mbt%d" % i, [128, 8, 512], BF16) for i in range(1)]
        wgr = [fw.sb("mwg%d" % i, [128, KC, 4, 128], BF16) for i in range(3)]
        yb = fw.sb("myb", [128, KC, 512], BF16)
        gs = [fw.sb("mgs%d" % i, [128, 512]) for i in range(2)]
        yacc = fw.sb("myacc", [128, 512])
        ytmp = [fw.sb("mytmp%d" % i, [128, 512]) for i in range(2)]
        h2 = fw.sb("mh2", [128, KC, 512])
        gT = fw.sb("mgT", [16, 512])
        R = {n: fw.sb("r_" + n, [128, w]) for n, w in
             [("lg", 16), ("mx", 1), ("e", 16), ("ss", 1), ("sc", 16), ("sel", 16), ("m1", 4), ("eq", 16), ("x2", 16),
              ("m2", 4), ("gsc", 4), ("gm", 1), ("ing", 4), ("t2m", 16), ("selm", 16), ("w", 16), ("ws", 1), ("gate", 16)]}
        resv = self.resT[:].r("(kc p) t -> p kc t", p=128)
        l1v = self.lat1T[:].r("(kc p) t -> p kc t", p=128)
        h2v = self.h2T[:].r("(kc p) t -> p kc t", p=128)
        brv = self.brv()
        brb = self.rb[:, RB["brouter"]:RB["brouter"] + 16]
        wi = 0
        for gi, (tok0, TB) in enumerate(GROUPS):
            if gi == 0 and l == DEPTH - 1:
                continue
            md = self.mod_for(gi)
            r, h, br = rt[0], ht[0], bt[0]
            res1 = r
            lat1 = r
            h2b = h
            fw.dma("sp", r[:, :, 0:TB], resv[:, :, tok0:tok0 + TB])
            fw.dma("pool", br[:, :, 0:TB], brv[:, :, tok0:tok0 + TB])
            for kc in range(KC):
                fw.act(h[:, kc, 0:TB].k(kc), r[:, kc, 0:TB].k(kc), AF.Identity, bias=md[:, kc:kc + 1], scale=md[:, 8 + kc:9 + kc])
            for kc in range(KC):
                fw.ts(r[:, kc, 0:TB].k(kc), r[:, kc, 0:TB].k(kc), ALPHA, ALU.mult, eng="pool")
            for Dc in range(KC):
                wg = wgr[wi % 3]
                wi += 1
                fw.dma("sp", wg[:], self.wgc[Dc].r("p (kc k c) -> p kc k c", kc=KC, k=4))
                for k in range(4):
                    pg = self.PS[k % 2]
                    for kc in range(KC):
                        fw.mm(pg[:, 0:TB], wg[:, kc, k, :], h[:, kc, 0:TB],
                              start=(kc == 0), stop=(kc == KC - 1))
                    pp_ = self.PS[2 + k % 2]
                    for cc in range(2):
                        fw.mm(pp_[:, 0:TB], wbr[:, k * 2 + cc, Dc * 128:(Dc + 1) * 128], br[:, k * 2 + cc, 0:TB],
                              start=(cc == 0), stop=(cc == 1))
                    g_ = gs[k % 2]
                    bcol = PP["bgate"] + k * 8 + Dc
                    fw.act(g_[:, 0:TB], pg[:, 0:TB], AF.Sigmoid, bias=self.pp[:, bcol:bcol + 1])
                    if k == 0:
                        fw.tt(yacc[:, 0:TB], g_[:, 0:TB], pp_[:, 0:TB], ALU.mult)
                    else:
                        t_ = ytmp[k % 2]
                        fw.tt(t_[:, 0:TB], g_[:, 0:TB], pp_[:, 0:TB], ALU.mult)
                        dst = yacc[:, 0:TB] if k < 3 else yb[:, Dc, 0:TB].k(Dc)
                        fw.tt(dst, yacc[:, 0:TB], t_[:, 0:TB], ALU.add, eng="pool")
            g1c = 16
            for Dc in range(KC):
                po = self.PS[4 + Dc % 2]
                for kc in range(KC):
                    fw.mm(po[:, 0:TB], wo[:, kc, Dc * 128:(Dc + 1) * 128], yb[:, kc, 0:TB], start=(kc == 0), stop=(kc == KC - 1))
                fw.stt(res1[:, Dc, 0:TB].k(Dc), po[:, 0:TB], md[:, g1c + Dc:g1c + Dc + 1], r[:, Dc, 0:TB].k(Dc), ALU.mult, ALU.add)
            self.layernorm_fm(res1, TB, "ln1", lat1, W)
            fw.dma("pool", l1v[:, :, tok0:tok0 + TB].k(("g", gi)), lat1[:, :, 0:TB])
            for kc in range(KC):
                fw.act(h2[:, kc, 0:TB].k(kc), lat1[:, kc, 0:TB].k(kc), AF.Identity, bias=md[:, 24 + kc:25 + kc], scale=md[:, 32 + kc:33 + kc])
                fw.cp(h2b[:, kc, 0:TB].k(kc), h2[:, kc, 0:TB].k(kc), eng="pool")
            fw.dma("pool", h2v[:, :, tok0:tok0 + TB].k(("g", gi)), h2b[:, :, 0:TB])
            for jt in range(TB // 128):
                pl = self.PS[0]
                for kc in range(KC):
                    fw.mm(pl[:, 0:16], h2[:, kc, jt * 128:(jt + 1) * 128], wr[:, kc, :], start=(kc == 0), stop=(kc == KC - 1))
                fw.cp(R["lg"][:], pl[:, 0:16], eng="act")
                fw.red(R["mx"][:], R["lg"][:], ALU.max)
                fw.ts(R["mx"][:], R["mx"][:], -1.0, ALU.mult)
                fw.act(R["e"][:], R["lg"][:], AF.Exp, bias=R["mx"][:])
                fw.red(R["ss"][:], R["e"][:], ALU.add)
                fw.op("dve", lambda e: e.reciprocal(out=R["ss"][:].ap, in_=R["ss"][:].ap), [R["ss"][:]], [R["ss"][:]])
                fw.ts(R["sc"][:], R["e"][:], R["ss"][:], ALU.mult)
                fw.tt(R["sel"][:], R["sc"][:], brb, ALU.add)
                sel3 = R["sel"][:].r("p (g e) -> p g e", g=4)
                fw.red(R["m1"][:], sel3, ALU.max)
                fw.tt(R["eq"][:].r("p (g e) -> p g e", g=4), sel3, R["m1"][:].un(2).bc([128, 4, 4]), ALU.is_equal)
                fw.stt(R["x2"][:], R["eq"][:], -1e30, R["sel"][:], ALU.mult, ALU.add)
                fw.red(R["m2"][:], R["x2"][:].r("p (g e) -> p g e", g=4), ALU.max)
                fw.tt(R["gsc"][:], R["m1"][:], R["m2"][:], ALU.add)
                fw.red(R["gm"][:], R["gsc"][:], ALU.max)
                fw.ts(R["ing"][:], R["gsc"][:], R["gm"][:], ALU.is_ge)
                fw.tt(R["t2m"][:].r("p (g e) -> p g e", g=4), sel3, R["m2"][:].un(2).bc([128, 4, 4]), ALU.is_ge)
                fw.tt(R["selm"][:].r("p (g e) -> p g e", g=4), R["t2m"][:].r("p (g e) -> p g e", g=4),
                      R["ing"][:].un(2).bc([128, 4, 4]), ALU.mult)
                fw.tt(R["w"][:], R["sc"][:], R["selm"][:], ALU.mult)
                fw.red(R["ws"][:], R["w"][:], ALU.add)
                fw.op("dve", lambda e: e.reciprocal(out=R["ws"][:].ap, in_=R["ws"][:].ap), [R["ws"][:]], [R["ws"][:]])
                fw.ts(R["gate"][:], R["w"][:], R["ws"][:], ALU.mult)
                pt = self.PS[1]
                fw.tr(pt[0:16, 0:128], R["gate"][:], ident)
                fw.cp(gT[:, jt * 128:(jt + 1) * 128].k(jt), pt[0:16, 0:128], eng="act")
            fw.dma("pool", self.gateT[:, tok0:tok0 + TB].k(("g", gi)), gT[:, 0:TB])
        fw.release(m)

    def phase_wcast(self, l):
        fw = self.fw
        m = fw.mark()
        st = [fw.sb("wc_s%d" % i, [128, 4096]) for i in range(3)]
        sb_ = [fw.sb("wc_b%d" % i, [128, 4096], BF16) for i in range(3)]
        i = 0
        for e in range(NE):
            for (src, dst, pat) in ((self.w_e1_d, self.we1b, "(kc p) f -> p kc f"),
                                    (self.w_e3_d, self.we3b, "(kc p) f -> p kc f"),
                                    (self.w_e2_d, self.we2b, "(fc p) d -> p fc d")):
                a, b = st[i % 3], sb_[i % 3]
                n0 = 8 if src is not self.w_e2_d else 4
                fw.dma("sp", a[:].r("p (a b) -> p a b", a=n0), src[l, e].r(pat, p=128))
                fw.cp(b[:], a[:], eng=("dve", "act", "pool")[i % 3])
                fw.dma("pool", dst[e].r(pat, p=128).k(("e", e)), b[:].r("p (a b) -> p a b", a=n0))
                i += 1
        fw.release(m)

    def phase_moe(self, l):
        fw = self.fw
        m = fw.mark()
        ident = self.c(C_IDENT)
        W = self.ln_work()
        last = (l == DEPTH - 1)
        h2t = [fw.sb("e_h2%d" % i, [128, KC, 512], BF16) for i in range(2)]
        l1t = [fw.sb("e_l1%d" % i, [128, KC, 512]) for i in range(2)]
        gTt = [fw.sb("e_gT%d" % i, [16, 512]) for i in range(2)]
        w1r = [fw.sb("e_w1%d" % i, [128, KC, DFF], BF16) for i in range(2)]
        w3r = [fw.sb("e_w3%d" % i, [128, KC, DFF], BF16) for i in range(2)]
        w2r = [fw.sb("e_w2%d" % i, [128, 4, D], BF16) for i in range(2)]
        gb = [fw.sb("e_gb%d" % i, [128, 512]) for i in range(2)]
        s1 = [fw.sb("e_s1%d" % i, [128, 512]) for i in range(2)]
        tq = [fw.sb("e_t%d" % i, [128, 512]) for i in range(2)]
        ab = [fw.sb("e_a%d" % i, [128, 4, 512], BF16) for i in range(2)]
        accs = [fw.sb("e_acc%d" % i, [128, KC, 512]) for i in range(2)]
        otok = [fw.sb("e_ot%d" % i, [128, D]) for i in range(2)]
        resv = self.resT[:].r("(kc p) t -> p kc t", p=128)
        l1v = self.lat1T[:].r("(kc p) t -> p kc t", p=128)
        h2v = self.h2T[:].r("(kc p) t -> p kc t", p=128)
        ei = 0
        pending = None
        loaded = False
        for gi, (tok0, TB) in enumerate(GROUPS):
            if gi == 0 and last:
                continue
            md = self.mod_for(gi)
            h2, l1, gT = h2t[gi % 2], l1t[gi % 2], gTt[gi % 2]
            acc = accs[gi % 2]

            def group_loads(gj):
                t0_, TB_ = GROUPS[gj]
                fw.dma("sp", h2t[gj % 2][:, :, 0:TB_], h2v[:, :, t0_:t0_ + TB_])
                fw.dma("sp", l1t[gj % 2][:, :, 0:TB_], l1v[:, :, t0_:t0_ + TB_])
                fw.dma("sp", gTt[gj % 2][:, 0:TB_], self.gateT[:, t0_:t0_ + TB_])
            if not loaded:
                group_loads(gi)
                loaded = True
            for e in range(NE):
                w1, w3, w2 = w1r[ei % 2], w3r[ei % 2], w2r[ei % 2]
                fw.dma("sp", w1[:], self.we1b[e].r("(kc p) f -> p kc f", p=128))
                fw.dma("pool", w3[:], self.we3b[e].r("(kc p) f -> p kc f", p=128))
                fw.dma("sp", w2[:], self.we2b[e].r("(fc p) d -> p fc d", p=128))
                pgb = self.PS[6]
                fw.mm(pgb[:, 0:TB], self.cst[0:16, C_SEL + e * 128:C_SEL + (e + 1) * 128], gT[:, 0:TB])
                g_ = gb[ei % 2]
                fw.cp(g_[:, 0:TB], pgb[:, 0:TB], eng="act")
                a_ = ab[ei % 2]
                for f in range(4):
                    p1 = self.PS[f % 2]
                    p3 = self.PS[2 + f % 2]
                    for kc in range(KC):
                        fw.mm(p1[:, 0:TB], w1[:, kc, f * 128:(f + 1) * 128], h2[:, kc, 0:TB], start=(kc == 0), stop=(kc == KC - 1))
                    for kc in range(KC):
                        fw.mm(p3[:, 0:TB], w3[:, kc, f * 128:(f + 1) * 128], h2[:, kc, 0:TB], start=(kc == 0), stop=(kc == KC - 1))
                    s_ = s1[f % 2]
                    fw.act(s_[:, 0:TB], p1[:, 0:TB], AF.Silu)
                    t_ = tq[f % 2]
                    fw.tt(t_[:, 0:TB], s_[:, 0:TB], p3[:, 0:TB], ALU.mult)
                    fw.tt(a_[:, f, 0:TB].k(f), t_[:, 0:TB], g_[:, 0:TB], ALU.mult, eng="pool")
                for Dc in range(KC):
                    po = self.PS[4 + Dc % 2]
                    for f in range(4):
                        fw.mm(po[:, 0:TB], w2[:, f, Dc * 128:(Dc + 1) * 128], a_[:, f, 0:TB], start=(f == 0), stop=(f == 3))
                    if e == 0:
                        fw.cp(acc[:, Dc, 0:TB].k(Dc), po[:, 0:TB], eng="act")
                    else:
                        fw.tt(acc[:, Dc, 0:TB].k(Dc), acc[:, Dc, 0:TB].k(Dc), po[:, 0:TB], ALU.add)
                ei += 1
                if e == 1:
                    if pending is not None:
                        pending()
                        pending = None
                    if gi + 1 < len(GROUPS):
                        group_loads(gi + 1)

            def epilogue(gi=gi, tok0=tok0, TB=TB, md=md, acc=acc, l1=l1):
                res2 = acc
                lat2 = acc
                g2c = 40
                for Dc in range(KC):
                    fw.ts(l1[:, Dc, 0:TB].k(Dc), l1[:, Dc, 0:TB].k(Dc), ALPHA, ALU.mult, eng="pool")
                    fw.stt(res2[:, Dc, 0:TB].k(Dc), acc[:, Dc, 0:TB].k(Dc), md[:, g2c + Dc:g2c + Dc + 1], l1[:, Dc, 0:TB].k(Dc),
                           ALU.mult, ALU.add)
                self.layernorm_fm(res2, TB, "ln2", lat2, W)
                if not last:
                    fw.dma("pool", resv[:, :, tok0:tok0 + TB].k(("g", gi)), lat2[:, :, 0:TB])
                if last and gi > 0:
                    for jt in range(TB // 128):
                        o_ = otok[jt % 2]
                        for half in range(2):
                            ps = self.PS[6 + half]
                            for q in range(4):
                                kc = half * 4 + q
                                fw.tr(ps[:, q * 128:(q + 1) * 128], lat2[:, kc, jt * 128:(jt + 1) * 128], ident)
                            fw.cp(o_[:, half * 512:(half + 1) * 512].k(half), ps[:, 0:512], eng=("dve" if half == 0 else "act"))
                        r0 = tok0 - LC + jt * 128
                        fw.dma("sp", self.out_d[r0:r0 + 128, :].k(("o", r0)), o_[:])
            pending = epilogue
        if pending is not None:
            pending()
        fw.release(m)


def _core_inputs(inp, b, shared, consts):
    cc = np.empty((128, KC, 2), np.float32)
    cc[:, :, 0] = np.asarray(inp["c"][b], np.float32).reshape(KC, 128).T
    cc[:, :, 1] = np.asarray(inp["c_ctx"], np.float32).reshape(KC, 128).T
    m = {"x": np.ascontiguousarray(inp["x"][b], np.float32),
         "ctx": np.ascontiguousarray(inp["ctx"][b], np.float32),
         "cc": cc.reshape(128, KC * 2)}
    m.update(consts)
    m.update(shared)
    return m


_PROG = {}


def kernel(**inputs):
    if "p" not in _PROG:
        _PROG["p"] = Prog()
    prog = _PROG["p"]
    consts = _constants()
    shared = _shared_inputs(inputs)
    n = 8
    in_maps = [_core_inputs(inputs, b, shared, consts) for b in range(n)]
    res = run_bass_kernel_spmd(prog.nc, in_maps, core_ids=list(range(n)))
    return np.stack([np.asarray(r["out"], np.float32) for r in res.results], 0)
```

```python
import math
import numpy as np
import ml_dtypes
import concourse.bass as bass
import concourse.mybir as mybir
from concourse.bass_utils import run_bass_kernel_spmd

F32 = mybir.dt.float32
BF16 = mybir.dt.bfloat16
AF = mybir.ActivationFunctionType
ALU = mybir.AluOpType
AX = mybir.AxisListType

D = 1024
L = 4096
LC = 256
NT = L + LC
KC = 8
DEPTH = 2
IN_COLS = 3592
UROWS = 29 * 128
NEG = -30000.0
ALPHA = (2 * DEPTH) ** 0.25
LN_EPS = 1e-5
RMS_EPS = 1e-6
NE = 16
DFF = 512
SEM_LIMIT = 30000


class Buf:
    def __init__(self, name, t):
        self.name = name
        self.t = t
        self.st = {}

    def __getitem__(self, idx):
        return View(self, self.t[idx], None)

    def k(self, key):
        return View(self, self.t[:], key)


class View:
    def __init__(self, buf, ap, key):
        self.buf = buf
        self.ap = ap
        self.key = key

    def __getitem__(self, idx):
        return View(self.buf, self.ap[idx], self.key)

    def k(self, key):
        return View(self.buf, self.ap, key)

    def r(self, pat, **kw):
        return View(self.buf, self.ap.rearrange(pat, **kw), self.key)

    def bc(self, shape):
        return View(self.buf, self.ap.to_broadcast(list(shape)), self.key)

    def un(self, axis):
        return View(self.buf, self.ap.unsqueeze(axis), self.key)


def _ap(x):
    return x.ap if isinstance(x, View) else x


class FW:
    NDMA = 8
    LIMIT = 10 ** 9
    POOL_TO = "dve"

    def __init__(self, nc):
        self.nc = nc
        self.eng = {"pe": nc.tensor, "dve": nc.vector, "act": nc.scalar,
                    "pool": nc.gpsimd, "sp": nc.sync}
        self.sem = {}
        self.cnt = {}
        self.seen = {}
        self.last = {}
        self._guards = []
        self._semguards = []
        self.bufs = []
        self._nsem = 0
        for e in self.eng:
            self.sem[e] = self._newsem("s_" + e)
            self.cnt[e] = 0
            self.seen[e] = {}
            self.last[e] = []
        self.dsem = {}
        self.dcnt = {}
        self.dlast = {}
        self.drr = {}
        for q in ("sp", "act", "pool"):
            self.dsem[q] = [self._newsem("d_%s%d" % (q, i)) for i in range(self.NDMA)]
            self.dcnt[q] = [0] * self.NDMA
            self.dlast[q] = [None] * self.NDMA
            self.drr[q] = 0
        self.n_inst = 0
        self.n_wait = 0

    def _newsem(self, name):
        self._nsem += 1
        g = self.nc.semaphore("%s_%d" % (name, self._nsem))
        s = g.__enter__()
        self._semguards.append(g)
        return s

    def sb(self, name, shape, dt=F32):
        self._nalloc = getattr(self, "_nalloc", 0) + 1
        g = self.nc.sbuf_tensor("sb%d_%s" % (self._nalloc, name), list(shape), dt)
        t = g.__enter__()
        self._guards.append(g)
        b = Buf(name, t)
        self.bufs.append(b)
        return b

    def ps(self, name, shape, dt=F32):
        g = self.nc.psum_tensor("pp_" + name, list(shape), dt)
        t = g.__enter__()
        self._guards.append(g)
        b = Buf(name, t)
        self.bufs.append(b)
        return b

    def dram(self, name, shape, dt=F32, kind="Internal"):
        t = self.nc.dram_tensor(name, list(shape), dt, kind=kind)
        b = Buf(name, t.ap())
        self.bufs.append(b)
        return b

    def mark(self):
        return len(self._guards)

    def release(self, mark):
        self.barrier()
        while len(self._guards) > mark:
            g = self._guards.pop()
            g.__exit__(None, None, None)

    def close(self):
        while self._guards:
            g = self._guards.pop()
            g.__exit__(None, None, None)
        while self._semguards:
            g = self._semguards.pop()
            g.__exit__(None, None, None)

    def _deps(self, reads, writes):
        deps = []
        for a in reads:
            b, k = a.buf, a.key
            for kk, s in b.st.items():
                if k is None or kk is None or kk == k:
                    if s[0] is not None:
                        deps.append(s[0])
        for a in writes:
            b, k = a.buf, a.key
            for kk, s in b.st.items():
                if k is None or kk is None or kk == k:
                    if s[0] is not None:
                        deps.append(s[0])
                    deps.extend(s[1])
        return deps

    def _record(self, reads, writes, tok):
        for a in reads:
            s = a.buf.st.setdefault(a.key, [None, []])
            s[1].append(tok)
            if len(s[1]) > 48:
                m = {}
                for (sm, v) in s[1]:
                    if id(sm) not in m or m[id(sm)][1] < v:
                        m[id(sm)] = (sm, v)
                s[1] = list(m.values())
        for a in writes:
            if a.key is None:
                a.buf.st = {None: [tok, []]}
            else:
                a.buf.st[a.key] = [tok, []]

    def _emit_waits(self, e, deps, same_engine=True):
        engine = self.eng[e]
        seen = self.seen[e]
        need = {}
        for (sm, v) in deps:
            if (not same_engine) and sm is self.sem[e]:
                continue
            if seen.get(id(sm), 0) >= v:
                continue
            if id(sm) not in need or need[id(sm)][1] < v:
                need[id(sm)] = (sm, v)
        for (sm, v) in need.values():
            engine.wait_ge(sm, v)
            seen[id(sm)] = v
            self.n_wait += 1

    def op(self, e, fn, reads=(), writes=(), same_engine=None):
        if e == "pool":
            e = FW.POOL_TO
        if same_engine is None:
            same_engine = (e != "pe")
        if self.n_inst >= FW.LIMIT:
            return None
        reads = [r for r in reads if isinstance(r, View)]
        writes = [w for w in writes if isinstance(w, View)]
        deps = self._deps(reads, writes)
        self._emit_waits(e, deps, same_engine)
        if self.cnt[e] >= SEM_LIMIT:
            self.sem[e] = self._newsem("s_" + e)
            self.cnt[e] = 0
        ins = fn(self.eng[e])
        self.cnt[e] += 1
        ins.then_inc(self.sem[e], 1)
        tok = (self.sem[e], self.cnt[e])
        self.last[e] = [tok] + [t for t in self.last[e] if t[0] is not self.sem[e]][:1]
        self._record(reads, writes, tok)
        self.n_inst += 1
        return tok

    def dma(self, q, out, in_, **kw):
        if self.n_inst >= FW.LIMIT:
            return None
        reads, writes = [in_], [out]
        deps = self._deps(reads, writes)
        slot = self.drr[q]
        self.drr[q] = (slot + 1) % self.NDMA
        if self.dlast[q][slot] is not None:
            deps.append(self.dlast[q][slot])
        self._emit_waits(q, deps, True)
        if self.dcnt[q][slot] >= SEM_LIMIT:
            self.dsem[q][slot] = self._newsem("d_%s%d" % (q, slot))
            self.dcnt[q][slot] = 0
        sm = self.dsem[q][slot]
        ins = self.eng[q].dma_start(out=out.ap, in_=in_.ap, **kw)
        self.dcnt[q][slot] += 16
        ins.then_inc(sm, 16)
        tok = (sm, self.dcnt[q][slot])
        self.dlast[q][slot] = tok
        self._record(reads, writes, tok)
        self.n_inst += 1
        return tok

    def all_tokens(self):
        toks = []
        for e in self.eng:
            toks.extend(self.last[e])
        for q in self.dlast:
            toks.extend(t for t in self.dlast[q] if t is not None)
        return toks

    def barrier(self):
        toks = self.all_tokens()
        for e in self.eng:
            self._emit_waits(e, toks, True)
        for b in self.bufs:
            b.st = {}

    def mm(self, out, lhsT, rhs, start=True, stop=True):
        return self.op("pe", lambda e: e.matmul(out.ap, lhsT=lhsT.ap, rhs=rhs.ap, start=start, stop=stop),
                       [lhsT, rhs], [out])

    def tr(self, out, in_, ident):
        return self.op("pe", lambda e: e.transpose(out.ap, in_.ap, ident.ap), [in_, ident], [out])

    def act(self, out, in_, func, bias=0.0, scale=1.0, accum=None, eng="act"):
        kw = {}
        if accum is not None:
            kw["accum_out"] = accum.ap
        return self.op("act", lambda e: e.activation(out=out.ap, in_=in_.ap, func=func, bias=_ap(bias),
                                                     scale=_ap(scale), **kw),
                       [in_, bias, scale], [out, accum])

    def tt(self, out, a, b, op, eng="dve"):
        return self.op(eng, lambda e: e.tensor_tensor(out=out.ap, in0=a.ap, in1=b.ap, op=op), [a, b], [out])

    def ts(self, out, a, s1, op0, s2=None, op1=None, eng="dve", accum=None):
        kw = {}
        if op1 is not None:
            kw["op1"] = op1
        if accum is not None:
            kw["accum_out"] = accum.ap
        return self.op(eng, lambda e: e.tensor_scalar(out=out.ap, in0=a.ap, scalar1=_ap(s1), scalar2=_ap(s2),
                                                      op0=op0, **kw), [a, s1, s2], [out, accum])

    def stt(self, out, a, s, b, op0, op1, eng="dve"):
        return self.op("dve", lambda e: e.scalar_tensor_tensor(out=out.ap, in0=a.ap, scalar=_ap(s), in1=b.ap,
                                                             op0=op0, op1=op1), [a, s, b], [out])

    def cp(self, out, in_, eng="dve"):
        if eng == "act":
            return self.op("act", lambda e: e.copy(out=out.ap, in_=in_.ap), [in_], [out])
        return self.op(eng, lambda e: e.tensor_copy(out=out.ap, in_=in_.ap), [in_], [out])

    def memset(self, out, val, eng="pool"):
        return self.op("dve" if eng == "pool" else eng, lambda e: e.memset(out.ap, val), [], [out])

    def red(self, out, in_, op, axis=AX.X, eng="dve"):
        return self.op(eng, lambda e: e.tensor_reduce(out=out.ap, in_=in_.ap, axis=axis, op=op), [in_], [out])

    def scan(self, out, d0, d1, init, op0, op1):
        return self.op("dve", lambda e: e.tensor_tensor_scan(out=out.ap, data0=d0.ap, data1=d1.ap,
                                                             initial=_ap(init), op0=op0, op1=op1),
                       [d0, d1, init], [out])


C_IDENT, C_ONES, C_TRIF, C_TRIB, C_NEGF, C_NEGB, C_MIF, C_MIB, C_SCAN = [i * 128 for i in range(9)]
C_CMASK = 1152
C_SEL = 1160
C_NEGTL = C_SEL + 16 * 128
C_NEGTC = C_NEGTL + 32
C_CMASKR = C_NEGTC + 8
C_SCAN16 = C_CMASKR + 8
NCST = C_SCAN16 + 16

PP = {}
_off = 0
for _n, _w in [("hycw", 18), ("hycb", 6), ("sccw", 6), ("sdcw", 12), ("sdcb", 4), ("hybias", 2), ("lbl", 8),
               ("hgng", 1), ("bgate", 32), ("ln1g", 8), ("ln1b", 8), ("ln2g", 8), ("ln2b", 8), ("bada", 48),
               ("hyb1", 1), ("hyf1", 1), ("hyb2", 1), ("hyf2", 1), ("lbl64", 16)]:
    PP[_n] = _off
    _off += _w
NPP = _off
RB = {}
_off = 0
for _n, _w in [("hydecay", 256), ("ssdng", 256), ("dtbias", 8), ("alog", 8), ("ssdd", 4), ("brouter", 16)]:
    RB[_n] = _off
    _off += _w
NRB = _off

_CONST_CACHE = {}


def _sincos_1d(pos, dim):
    omega = 1.0 / (10000.0 ** (np.arange(dim // 2, dtype=np.float32) / np.float32(dim // 2)))
    ang = pos.astype(np.float32)[:, None] * omega[None].astype(np.float32)
    return np.concatenate([np.sin(ang), np.cos(ang)], -1).astype(np.float32)


def _hy_feat(Lx):
    t = np.linspace(0.0, 1.0, Lx, dtype=np.float32)[:, None]
    bands = np.linspace(1e-4, 15, 16, dtype=np.float32)
    ang = (np.float32(2.0 * math.pi / Lx)) * np.arange(Lx, dtype=np.float32)[:, None] * bands[None]
    feat = np.concatenate([t, np.cos(ang), -np.sin(ang)], -1).astype(np.float32)
    return feat, t[:, 0]


def _dft_tables(Lx, TB):
    N = 2 * Lx
    nT = Lx // 128
    t = np.arange(Lx, dtype=np.int64)
    k = np.arange(Lx, dtype=np.int64)
    m = ((2 * k[None, :] + 1) * t[:, None]) % (2 * N)
    ang = m.astype(np.float64) * (math.pi / N)
    tabs = [np.cos(ang), np.sin(ang)]
    tf = np.empty((2, nT, 128, nT, 128), dtype=ml_dtypes.bfloat16)
    ti = np.empty((2, Lx // TB, 128, nT, TB), dtype=ml_dtypes.bfloat16)
    for j, T in enumerate(tabs):
        T4 = T.reshape(nT, 128, nT, 128)
        tf[j] = T4.transpose(2, 1, 0, 3).astype(ml_dtypes.bfloat16)
        Tt = T.T.reshape(nT, 128, Lx // TB, TB)
        ti[j] = Tt.transpose(2, 1, 0, 3).astype(ml_dtypes.bfloat16)
    return tf.reshape(2, nT, 128, nT * 128), ti.reshape(2, Lx // TB, 128, nT * TB)


def _constants():
    if _CONST_CACHE:
        return _CONST_CACHE
    cst = np.zeros((128, NCST), np.float32)
    r = np.arange(128)
    cst[:, C_IDENT:C_IDENT + 128] = np.eye(128)
    cst[:, C_ONES:C_ONES + 128] = 1.0
    cst[:, C_TRIF:C_TRIF + 128] = (r[:, None] <= r[None, :])
    cst[:, C_TRIB:C_TRIB + 128] = (r[:, None] >= r[None, :])
    cst[:, C_NEGF:C_NEGF + 128] = np.where(r[:, None] > r[None, :], NEG, 0.0)
    cst[:, C_NEGB:C_NEGB + 128] = np.where(r[:, None] < r[None, :], NEG, 0.0)
    same = (r[:, None] // 16) == (r[None, :] // 16)
    cst[:, C_MIF:C_MIF + 128] = same & (r[:, None] <= r[None, :])
    cst[:, C_MIB:C_MIB + 128] = same & (r[:, None] >= r[None, :])
    cst[:, C_SCAN:C_SCAN + 128] = ((r % 16) != 0)[None, :]
    cst[:, C_CMASK:C_CMASK + 8] = (r[:, None] // 16) == np.arange(8)[None, :]
    cst[:, C_CMASKR:C_CMASKR + 8] = (r[:, None] // 16) == (7 - np.arange(8))[None, :]
    cst[:, C_SCAN16:C_SCAN16 + 16] = (np.arange(16) != 0)[None, :]
    sel = np.zeros((128, 16, 128), np.float32)
    for e in range(16):
        sel[e, e, :] = 1.0
    cst[:, C_SEL:C_SEL + 2048] = sel.reshape(128, 2048)
    featL, tL = _hy_feat(L)
    featC, tC = _hy_feat(LC)
    cst[:, C_NEGTL:C_NEGTL + 32] = -tL.reshape(32, 128).T
    cst[:, C_NEGTC:C_NEGTC + 2] = -tC.reshape(2, 128).T
    feat = np.concatenate([featC.T, featL.T], 1).astype(np.float32)
    rows = L // 64
    row = np.repeat(np.arange(rows), 64)
    col = np.tile(np.arange(64), rows)
    pos = np.concatenate([_sincos_1d(row, D // 2), _sincos_1d(col, D // 2)], -1).astype(np.float32)
    tfl, til = _dft_tables(L, 512)
    tfc, tic = _dft_tables(LC, 256)
    _CONST_CACHE.update(cst=cst, feat=np.ascontiguousarray(feat), pos=pos, tfl=tfl, til=til, tfc=tfc, tic=tic)
    return _CONST_CACHE


def _chunks(v, n):
    return np.ascontiguousarray(np.asarray(v, np.float32).reshape(n, 128).T)


def _shared_inputs(inp):
    pp = np.zeros((DEPTH, 128, NPP), np.float32)
    rb = np.zeros((DEPTH, 128, NRB), np.float32)
    for l in range(DEPTH):
        def put(name, arr):
            arr = np.asarray(arr, np.float32)
            pp[l, :arr.shape[0], PP[name]:PP[name] + arr.shape[1]] = arr
        cw = inp["hy_conv_w"][l]
        put("hycw", cw.reshape(3, 6, 128).transpose(2, 1, 0).reshape(128, 18))
        put("hycb", _chunks(inp["hy_conv_b"][l], 6))
        put("sccw", inp["sc_conv_w"][l].reshape(3, 2, 128).transpose(2, 1, 0).reshape(128, 6))
        put("sdcw", inp["ssd_conv_w"][l].reshape(3, 4, 128).transpose(2, 1, 0).reshape(128, 12))
        put("sdcb", _chunks(inp["ssd_conv_b"][l], 4))
        put("hybias", _chunks(inp["hy_bias"][l], 2))
        lbl = inp["hg_lb_logits"]
        put("lbl", lbl.reshape(2, DEPTH, 2, 128).transpose(3, 0, 1, 2).reshape(128, 8))
        put("lbl64", lbl.reshape(2, DEPTH, 4, 64).transpose(3, 0, 1, 2).reshape(64, 16))
        put("hgng", np.asarray(inp["hg_norm_g"][l]).reshape(64, 1))
        put("bgate", _chunks(inp["b_gate"][l], 32))
        put("ln1g", _chunks(inp["ln1_g"][l], 8))
        put("ln1b", _chunks(inp["ln1_b"][l], 8))
        put("ln2g", _chunks(inp["ln2_g"][l], 8))
        put("ln2b", _chunks(inp["ln2_b"][l], 8))
        put("bada", _chunks(inp["b_ada"][l], 48))
        put("hyb1", np.asarray(inp["hy_b1"][l]).reshape(64, 1))
        put("hyf1", np.asarray(inp["hy_freq1"][l]).reshape(64, 1))
        put("hyb2", np.asarray(inp["hy_b2"][l]).reshape(64, 1))
        put("hyf2", np.asarray(inp["hy_freq2"][l]).reshape(64, 1))

        def rput(name, row):
            row = np.asarray(row, np.float32).reshape(1, -1)
            rb[l, :, RB[name]:RB[name] + row.shape[1]] = np.broadcast_to(row, (128, row.shape[1]))
        rput("hydecay", inp["hy_decay"][l])
        rput("ssdng", inp["ssd_norm_g"][l])
        rput("dtbias", inp["ssd_dt_bias"][l])
        rput("alog", inp["ssd_a_log"][l])
        rput("ssdd", inp["ssd_d"][l])
        rput("brouter", inp["b_router"])
    sh = {"pp": pp, "rb": rb}
    for n in ("w_ada", "w_in", "hy_w1", "hy_w2", "hy_w3", "w_gate", "w_br", "w_o", "w_router",
              "w_e1", "w_e3", "w_e2"):
        sh[n] = np.ascontiguousarray(np.asarray(inp[n], np.float32))
    return sh


GROUPS = [(0, LC)] + [(LC + 512 * i, 512) for i in range(L // 512)]


class Prog:
    def __init__(self, dbg=(), stop_after=None, layers=DEPTH):
        self.dbg = set(dbg)
        self.stop_after = stop_after
        self.layers = layers
        nc = bass.Bass("TRN2", target_bir_lowering=False)
        self.nc = nc
        fw = FW(nc)
        self.fw = fw
        din = lambda name, shape, dt=F32: fw.dram(name, shape, dt, kind="ExternalInput")
        self.x_d = din("x", [L, D])
        self.ctx_d = din("ctx", [LC, D])
        self.cc_d = din("cc", [128, KC * 2])
        self.pos_d = din("pos", [L, D])
        self.cst_d = din("cst", [128, NCST])
        self.feat_d = din("feat", [33, NT])
        self.tfl_d = din("tfl", [2, 32, 128, 32 * 128], BF16)
        self.til_d = din("til", [2, 8, 128, 32 * 512], BF16)
        self.tfc_d = din("tfc", [2, 2, 128, 2 * 128], BF16)
        self.tic_d = din("tic", [2, 1, 128, 2 * 256], BF16)
        self.pp_d = din("pp", [DEPTH, 128, NPP])
        self.rb_d = din("rb", [DEPTH, 128, NRB])
        self.w_ada_d = din("w_ada", [DEPTH, D, 6 * D])
        self.w_in_d = din("w_in", [DEPTH, D, IN_COLS])
        self.hy_w1_d = din("hy_w1", [DEPTH, 33, 64])
        self.hy_w2_d = din("hy_w2", [DEPTH, 64, 64])
        self.hy_w3_d = din("hy_w3", [DEPTH, 64, 512])
        self.w_gate_d = din("w_gate", [DEPTH, D, 4 * D])
        self.w_br_d = din("w_br", [DEPTH, 4, 256, D])
        self.w_o_d = din("w_o", [DEPTH, D, D])
        self.w_router_d = din("w_router", [D, NE])
        self.w_e1_d = din("w_e1", [DEPTH, NE, D, DFF])
        self.w_e3_d = din("w_e3", [DEPTH, NE, D, DFF])
        self.w_e2_d = din("w_e2", [DEPTH, NE, DFF, D])
        self.out_d = fw.dram("out", [L, D], F32, kind="ExternalOutput")

        def scratch(name, shape, dt=F32):
            kind = "ExternalOutput" if name in self.dbg else "Internal"
            return fw.dram(name, shape, dt, kind=kind)
        self.resT = scratch("resT", [D, NT])
        self.uT = scratch("uT", [UROWS, NT])
        self.brT = scratch("brT", [D, NT], BF16)
        self.lat1T = scratch("lat1T", [D, NT])
        self.h2T = scratch("h2T", [D, NT], BF16)
        self.gateT = scratch("gateT", [NE, NT])
        self.hgo = scratch("hgo", [64, 4 * NT])
        self.hgo2 = scratch("hgo2", [64, 4 * NT])
        self.yf = scratch("yf", [NT, 256])
        self.yb = scratch("yb", [NT, 256])
        self.dtT = scratch("dtT", [NT, 8])
        self.wgc = scratch("wgc", [8, 128, KC * 4 * 128], BF16)
        self.modd = scratch("modd", [128, 96])
        self.we1b = scratch("we1b", [NE, D, DFF], BF16)
        self.we3b = scratch("we3b", [NE, D, DFF], BF16)
        self.we2b = scratch("we2b", [NE, DFF, D], BF16)

        self.cst = fw.sb("cst", [128, NCST])
        fw.dma("sp", self.cst[:], self.cst_d[:])
        self.PS = [fw.ps("ps%d" % i, [128, 512]) for i in range(8)]
        self.pp = fw.sb("pp", [128, NPP])
        self.rb = fw.sb("rb", [128, NRB])
        self.modL = fw.sb("modL", [128, 48])
        self.modC = fw.sb("modC", [128, 48])
        self.build()
        fw.barrier()
        fw.close()

    def c(self, off, w=128, rows=128):
        return self.cst[0:rows, off:off + w]

    def build(self):
        fw = self.fw
        self.phase_init()
        if self.stop_after == "init":
            return
        for l in range(self.layers):
            self.l = l
            fw.dma("sp", self.pp[:], self.pp_d[l])
            fw.dma("sp", self.rb[:], self.rb_d[l])
            self.phase_mod(l)
            if self.stop_after in ("mod", "mod%d" % l):
                return
            self.phase_p1(l)
            if self.stop_after in ("p1", "p1%d" % l):
                return
            self.phase_wcast(l)
            self.phase_sc(l)
            if self.stop_after in ("sc", "sc%d" % l):
                return
            self.phase_hyena(l, ctx=False)
            if l < DEPTH - 1:
                self.phase_hyena(l, ctx=True)
            if self.stop_after in ("hy", "hy%d" % l):
                return
            self.phase_hgrn(l)
            if self.stop_after in ("hg", "hg%d" % l):
                return
            self.phase_ssd(l)
            if self.stop_after in ("ssd", "ssd%d" % l):
                return
            self.phase_merge(l)
            if self.stop_after in ("merge", "merge%d" % l):
                return
            self.phase_moe(l)
            if self.stop_after == "moe%d" % l:
                return

    def phase_init(self):
        fw = self.fw
        m = fw.mark()
        xt = [fw.sb("xt%d" % i, [128, D]) for i in range(2)]
        pt = [fw.sb("pt%d" % i, [128, D]) for i in range(2)]
        st = [fw.sb("st%d" % i, [128, KC, 512]) for i in range(2)]
        ident = self.c(C_IDENT)
        resv = self.resT[:].r("(kc p) t -> p kc t", p=128)
        ti = 0
        for gi, (tok0, TB) in enumerate(GROUPS):
            sg = st[gi % 2]
            for j in range(TB // 128):
                a = xt[ti % 2]
                if gi == 0:
                    fw.dma("sp", a[:], self.ctx_d[j * 128:(j + 1) * 128, :])
                else:
                    r0 = tok0 - LC + j * 128
                    fw.dma("sp", a[:], self.x_d[r0:r0 + 128, :])
                    p = pt[ti % 2]
                    fw.dma("pool", p[:], self.pos_d[r0:r0 + 128, :])
                    fw.tt(a[:], a[:], p[:], ALU.add)
                for half in range(2):
                    ps = self.PS[(ti * 2 + half) % 4]
                    for q in range(4):
                        kc = half * 4 + q
                        fw.tr(ps[:, q * 128:(q + 1) * 128], a[:, kc * 128:(kc + 1) * 128], ident)
                    dst = sg[:, half * 4:(half + 1) * 4, j * 128:(j + 1) * 128]
                    src = ps[:].r("p (q t) -> p q t", q=4)
                    if half == 0:
                        fw.cp(dst, src, eng="dve")
                    else:
                        fw.cp(dst, src, eng="act")
                ti += 1
            fw.dma("sp", resv[:, :, tok0:tok0 + TB].k(("g", gi)), sg[:, :, 0:TB])
        fw.release(m)

    def phase_mod(self, l):
        fw = self.fw
        m = fw.mark()
        cs = fw.sb("cs", [128, KC * 2])
        fw.dma("sp", cs[:], self.cc_d[:])
        csil = fw.sb("csil", [128, KC * 2])
        fw.act(csil[:], cs[:], AF.Silu)
        wa = [fw.sb("wa%d" % i, [128, KC, 1024]) for i in range(2)]
        ps = self.PS[0]
        wv = self.w_ada_d[l].r("(kc p) n -> p kc n", p=128)
        for blk in range(6):
            w = wa[blk % 2]
            fw.dma("sp" if blk % 2 == 0 else "pool", w[:], wv[:, :, blk * 1024:(blk + 1) * 1024])
            for jj in range(8):
                j = blk * 8 + jj
                for kc in range(KC):
                    fw.mm(ps[:, j * 2:j * 2 + 2], w[:, kc, jj * 128:(jj + 1) * 128], csil[:, kc * 2:kc * 2 + 2],
                          start=(kc == 0), stop=(kc == KC - 1))
        psv = ps[:, 0:96].r("p (j t) -> p j t", t=2)
        bada = self.pp[:, PP["bada"]:PP["bada"] + 48]
        fw.tt(self.modL[:], psv[:, :, 0], bada, ALU.add)
        fw.tt(self.modC[:], psv[:, :, 1], bada, ALU.add)
        for mm_ in (self.modL, self.modC):
            for o in (8, 32):
                fw.ts(mm_[:, o:o + 8], mm_[:, o:o + 8], 1.0, ALU.add)
        if "modd" in self.dbg:
            fw.dma("sp", self.modd[:, 0:48], self.modL[:])
            fw.dma("sp", self.modd[:, 48:96], self.modC[:])
        fw.release(m)

    def mod_for(self, gi):
        return self.modC if gi == 0 else self.modL

    def load_cast(self, dst, src_view, stage, n, i):
        fw = self.fw
        s = stage[i % len(stage)]
        fw.dma("sp" if i % 2 == 0 else "pool", s[:, 0:n], src_view)
        eng = ("dve", "act", "pool")[i % 3]
        fw.cp(dst, s[:, 0:n], eng=eng)

    def phase_p1(self, l):
        fw = self.fw
        m = fw.mark()
        wb = fw.sb("winb", [128, KC, UROWS], BF16)
        stage = [fw.sb("wst%d" % i, [128, IN_COLS]) for i in range(2)]
        fw.memset(wb[:, :, IN_COLS:UROWS], 0.0)
        wv = self.w_in_d[l].r("(kc p) n -> p kc n", p=128)
        for kc in range(KC):
            self.load_cast(wb[:, kc, 0:IN_COLS].k(kc), wv[:, kc, :], stage, IN_COLS, kc)
        rt = [fw.sb("rt%d" % i, [128, KC, 512]) for i in range(2)]
        ht = [fw.sb("ht%d" % i, [128, KC, 512], BF16) for i in range(2)]
        og = [fw.sb("og%d" % i, [128, 4, 512]) for i in range(3)]
        dts = [fw.sb("dts%d" % i, [128, 8]) for i in range(2)]
        resv = self.resT[:].r("(kc p) t -> p kc t", p=128)
        uv = self.uT[:].r("(c p) t -> p c t", p=128)
        oi = 0
        for gi, (tok0, TB) in enumerate(GROUPS):
            r = rt[gi % 2]
            h = ht[gi % 2]
            md = self.mod_for(gi)
            fw.dma("sp", r[:, :, 0:TB], resv[:, :, tok0:tok0 + TB])
            for kc in range(KC):
                fw.act(h[:, kc, 0:TB].k(kc), r[:, kc, 0:TB], AF.Identity, bias=md[:, kc:kc + 1], scale=md[:, 8 + kc:9 + kc])
            for c0 in range(0, 29, 4):
                nchunk = min(4, 29 - c0)
                o = og[oi % 3]
                for cj in range(nchunk):
                    cidx = c0 + cj
                    ps = self.PS[cidx % 4]
                    for kc in range(KC):
                        fw.mm(ps[:, 0:TB], wb[:, kc, cidx * 128:(cidx + 1) * 128], h[:, kc, 0:TB],
                              start=(kc == 0), stop=(kc == KC - 1))
                    fw.cp(o[:, cj, 0:TB].k(cj), ps[:, 0:TB], eng=("dve" if cidx % 2 == 0 else "act"))
                fw.dma("sp" if oi % 2 == 0 else "pool", uv[:, c0:c0 + nchunk, tok0:tok0 + TB].k(("g", gi, c0)),
                       o[:, 0:nchunk, 0:TB])
                oi += 1
            for jt in range(TB // 128):
                ps = self.PS[4 + jt % 2]
                for kc in range(KC):
                    fw.mm(ps[:, 0:8], h[:, kc, jt * 128:(jt + 1) * 128], wb[:, kc, 3584:3592],
                          start=(kc == 0), stop=(kc == KC - 1))
                dq = dts[jt % 2]
                fw.cp(dq[:], ps[:, 0:8], eng="act")
                fw.dma("pool", self.dtT[tok0 + jt * 128:tok0 + (jt + 1) * 128, :].k(("dt", tok0, jt)), dq[:])
        fw.release(m)

    def uv(self):
        return self.uT[:].r("(c p) t -> p c t", p=128)

    def brv(self):
        return self.brT[:].r("(c p) t -> p c t", p=128)

    def load_halo(self, dst, c0, nch, tok0, TB, q="sp"):
        fw = self.fw
        s0, s1 = (0, LC) if tok0 < LC else (LC, NT)
        lo = tok0 - 1
        hi = tok0 + TB + 1
        d0 = 0
        if lo < s0:
            fw.memset(dst[:, 0:nch, 0:1], 0.0)
            lo += 1
            d0 = 1
        if hi > s1:
            fw.memset(dst[:, 0:nch, TB + 1:TB + 2], 0.0)
            hi -= 1
        fw.dma(q, dst[:, 0:nch, d0:d0 + (hi - lo)], self.uv()[:, c0:c0 + nch, lo:hi])

    def conv3(self, out, src, wcol, TB, bias=None, eng="dve"):
        fw = self.fw
        w = lambda k: self.pp[:, wcol + k:wcol + k + 1]
        if bias is not None:
            fw.ts(out, src[:, 0:TB], w(0), ALU.mult, bias, ALU.add, eng=eng)
        else:
            fw.ts(out, src[:, 0:TB], w(0), ALU.mult, eng=eng)
        fw.stt(out, src[:, 1:TB + 1], w(1), out, ALU.mult, ALU.add)
        fw.stt(out, src[:, 2:TB + 2], w(2), out, ALU.mult, ALU.add)

    def phase_sc(self, l):
        fw = self.fw
        m = fw.mark()
        tin = [fw.sb("sct%d" % i, [128, 6, 514]) for i in range(2)]
        mm_ = [fw.sb("scm%d" % i, [128, 2, 514]) for i in range(2)]
        acc = [fw.sb("sca%d" % i, [128, 2, 512]) for i in range(2)]
        ob = [fw.sb("sco%d" % i, [128, 2, 512], BF16) for i in range(2)]
        for gi, (tok0, TB) in enumerate(GROUPS):
            if gi == 0 and l == DEPTH - 1:
                continue
            t = tin[gi % 2]
            self.load_halo(t, 6, 6, tok0, TB)
            mv = mm_[gi % 2]
            fw.tt(mv[:, :, 0:TB + 2], t[:, 2:4, 0:TB + 2], t[:, 4:6, 0:TB + 2], ALU.mult)
            a = acc[gi % 2]
            o = ob[gi % 2]
            for cc in range(2):
                self.conv3(a[:, cc, 0:TB], mv[:, cc, :], PP["sccw"] + cc * 3, TB, eng=("dve" if cc == 0 else "pool"))
                fw.tt(o[:, cc, 0:TB], a[:, cc, 0:TB], t[:, cc, 1:TB + 1], ALU.mult, eng=("dve" if cc == 0 else "pool"))
            fw.dma("pool", self.brv()[:, 2:4, tok0:tok0 + TB].k(("sc", gi)), o[:, :, 0:TB])
        fw.release(m)

    def phase_hyena(self, l, ctx):
        fw = self.fw
        m = fw.mark()
        Lx = LC if ctx else L
        base = 0 if ctx else LC
        TB = 256 if ctx else 512
        nT = Lx // 128
        nG = Lx // TB
        tpb = TB // 128
        tf_d = self.tfc_d if ctx else self.tfl_d
        ti_d = self.tic_d if ctx else self.til_d
        negt = self.c(C_NEGTC, 2) if ctx else self.c(C_NEGTL, 32)
        ident = self.c(C_IDENT)
        ones = self.c(C_ONES)
        AU = fw.sb("AU", [128, nT, 768], BF16)
        ZB = fw.sb("ZB", [128, nT, 512], BF16)
        X0 = fw.sb("X0", [128, 2, Lx], BF16)
        WB = fw.sb("WB", [128, 2, Lx], BF16)
        rn = fw.sb("rn", [128, 256])
        m2 = fw.mark()
        w1 = fw.sb("hw1", [33, 64])
        w2 = fw.sb("hw2", [64, 64])
        w3 = fw.sb("hw3", [64, 512])
        fw.dma("sp", w1[:], self.hy_w1_d[l])
        fw.dma("sp", w2[:], self.hy_w2_d[l])
        fw.dma("sp", w3[:], self.hy_w3_d[l])
        bf = fw.sb("hbf", [64, 2])
        ppc = lambda n: self.pp[0:64, PP[n]:PP[n] + 1]
        fw.tt(bf[:, 0:1], ppc("hyb1"), ppc("hyf1"), ALU.mult)
        fw.tt(bf[:, 1:2], ppc("hyb2"), ppc("hyf2"), ALU.mult)
        h1 = fw.sb("hh1", [64, Lx])
        h2 = fw.sb("hh2", [64, Lx])
        ft = [fw.sb("hft%d" % i, [33, 512]) for i in range(2)]
        tmp = [fw.sb("htmp%d" % i, [64, 512]) for i in range(2)]
        TF = min(512, Lx)
        tmpk = [fw.sb("htmpk%d" % i, [64, 512]) for i in range(2)]
        MAGIC = 12582912.0
        for layer in range(2):
            for g in range(Lx // TF):
                ps = self.PS[g % 2]
                if layer == 0:
                    f = ft[g % 2]
                    fw.dma("sp", f[:, 0:TF], self.feat_d[:, base + g * TF:base + (g + 1) * TF])
                    fw.mm(ps[0:64, 0:TF], w1[:, :], f[:, 0:TF])
                    fq, bq, dst = ppc("hyf1"), bf[:, 0:1], h1
                else:
                    fw.mm(ps[0:64, 0:TF], w2[:, :], h1[:, g * TF:(g + 1) * TF])
                    fq, bq, dst = ppc("hyf2"), bf[:, 1:2], h2
                t = tmp[g % 2]
                fw.ts(t[:, 0:TF], ps[0:64, 0:TF], fq, ALU.mult, bq, ALU.add)
                kq = tmpk[g % 2]
                fw.ts(kq[:, 0:TF], t[:, 0:TF], 1.0 / (2.0 * math.pi), ALU.mult, MAGIC, ALU.add)
                fw.ts(kq[:, 0:TF], kq[:, 0:TF], -MAGIC, ALU.add)
                fw.stt(t[:, 0:TF], kq[:, 0:TF], -2.0 * math.pi, t[:, 0:TF], ALU.mult, ALU.add)
                fw.ts(t[:, 0:TF], t[:, 0:TF], -3.141592, ALU.max, 3.141592, ALU.min)
                fw.act(dst[:, g * TF:(g + 1) * TF].k(g), t[:, 0:TF], AF.Sin)
        adec = fw.sb("adec", [128, 256])
        dv = self.rb[:, RB["hydecay"]:RB["hydecay"] + 256]
        fw.stt(adec[:], dv, -1.0, dv, ALU.mult, ALU.max)
        win = [fw.sb("hwin%d" % i, [128, 256]) for i in range(2)]
        hf = [fw.sb("hhf%d" % i, [128, 256]) for i in range(2)]
        hb = [fw.sb("hhb%d" % i, [128, 256]) for i in range(2)]
        ab = [fw.sb("hab%d" % i, [128, 256]) for i in range(2)]
        a2 = [fw.sb("hab2%d" % i, [128, 256]) for i in range(2)]
        asum = self.PS[7]
        for i in range(nT):
            ps3 = self.PS[2 + i % 2]
            fw.mm(ps3[:, 0:512], h2[:, i * 128:(i + 1) * 128], w3[:, :])
            w_ = win[i % 2]
            fw.act(w_[:], adec[:], AF.Exp, scale=negt[:, i:i + 1])
            f_, b_, a_ = hf[i % 2], hb[i % 2], ab[i % 2]
            fw.tt(f_[:], ps3[:, 0:256], w_[:], ALU.mult)
            fw.tt(b_[:], ps3[:, 256:512], w_[:], ALU.mult)
            if i == 0:
                fw.memset(b_[0:1, :], 0.0, eng="dve")
            fw.tt(AU[:, i, 0:256].k(("f", i)), f_[:], b_[:], ALU.add, eng="pool")
            fw.tt(AU[:, i, 512:768].k(("f", i)), b_[:], f_[:], ALU.subtract, eng="pool")
            fw.act(a_[:], f_[:], AF.Abs)
            fw.act(a2[i % 2][:], b_[:], AF.Abs)
            fw.tt(a_[:], a_[:], a2[i % 2][:], ALU.add)
            fw.mm(asum[:, 0:256], ones, a_[:], start=(i == 0), stop=(i == nT - 1))
        fw.op("dve", lambda e: e.reciprocal(out=rn[:].ap, in_=asum[:, 0:256].ap), [asum[:, 0:256]], [rn[:]])
        fw.release(m2)
        m2 = fw.mark()
        tin = [fw.sb("hyt%d" % i, [128, 6, 514]) for i in range(2)]
        cv = [fw.sb("hyc%d" % i, [128, 6, 512]) for i in range(2)]
        wv = [fw.sb("hyw%d" % i, [128, 2, 512]) for i in range(2)]
        for g in range(nG):
            tok0 = base + g * TB
            t = tin[g % 2]
            self.load_halo(t, 0, 6, tok0, TB)
            c_ = cv[g % 2]
            for j in range(6):
                self.conv3(c_[:, j, 0:TB], t[:, j, :], PP["hycw"] + 3 * j, TB,
                           bias=self.pp[:, PP["hycb"] + j:PP["hycb"] + j + 1], eng=("dve" if j % 2 == 0 else "pool"))
            fw.cp(X0[:, :, g * TB:(g + 1) * TB].k(g), c_[:, 0:2, 0:TB], eng="act")
            w_ = wv[g % 2]
            fw.tt(w_[:, :, 0:TB], c_[:, 2:4, 0:TB], c_[:, 4:6, 0:TB], ALU.mult)
            for cc in range(2):
                fw.act(WB[:, cc, g * TB:(g + 1) * TB].k((g, cc)), w_[:, cc, 0:TB], AF.Copy,
                       scale=self.pp[:, PP["hybias"] + cc:PP["hybias"] + cc + 1])
            for jp in range(tpb // 2):
                ps = self.PS[(g * 2 + jp) % 4]
                for jj in range(2):
                    jt = jp * 2 + jj
                    for cc in range(2):
                        fw.tr(ps[:, (jj * 2 + cc) * 128:(jj * 2 + cc + 1) * 128], w_[:, cc, jt * 128:(jt + 1) * 128], ident)
                i0 = g * tpb + jp * 2
                fw.cp(AU[:, i0:i0 + 2, 256:512].k(("u", i0)), ps[:].r("p (j c) -> p j c", j=2),
                      eng=("dve" if jp % 2 == 0 else "act"))
        fw.release(m2)
        m2 = fw.mark()
        tfr = [fw.sb("tfr%d" % i, [128, nT * 128], BF16) for i in range(4)]
        gcn = [fw.sb("gcn%d" % i, [128, 256]) for i in range(2)]
        gsn = [fw.sb("gsn%d" % i, [128, 256]) for i in range(2)]
        tq = [fw.sb("tq%d" % i, [128, 256]) for i in range(4)]
        for kc in range(nT):
            tabs = (tfr[(kc % 2) * 2], tfr[(kc % 2) * 2 + 1])
            fw.dma("sp", tabs[0][:], tf_d[0, kc])
            fw.dma("pool", tabs[1][:], tf_d[1, kc])
            pc = self.PS[(kc % 2) * 2]
            pS = self.PS[(kc % 2) * 2 + 1]
            for i in range(nT):
                fw.mm(pc[:, 0:512], tabs[0][:, i * 128:(i + 1) * 128], AU[:, i, 0:512], start=(i == 0), stop=(i == nT - 1))
            for i in range(nT):
                fw.mm(pS[:, 0:512], tabs[1][:, i * 128:(i + 1) * 128], AU[:, i, 256:768], start=(i == 0), stop=(i == nT - 1))
            gc_, gs_ = gcn[kc % 2], gsn[kc % 2]
            fw.tt(gc_[:], pc[:, 0:256], rn[:], ALU.mult)
            fw.tt(gs_[:], pS[:, 256:512], rn[:], ALU.mult)
            t1, t2, t3, t4 = tq
            fw.tt(t1[:], pc[:, 256:512], gc_[:], ALU.mult)
            fw.tt(t2[:], pS[:, 0:256], gs_[:], ALU.mult)
            fw.tt(ZB[:, kc, 0:256].k(kc), t1[:], t2[:], ALU.add, eng="pool")
            fw.tt(t3[:], pS[:, 0:256], gc_[:], ALU.mult)
            fw.tt(t4[:], pc[:, 256:512], gs_[:], ALU.mult)
            fw.tt(ZB[:, kc, 256:512].k(kc), t3[:], t4[:], ALU.subtract, eng="pool")
        fw.release(m2)
        m2 = fw.mark()
        KP = min(8, nT)
        tir = [fw.sb("tir%d" % i, [128, KP * TB], BF16) for i in range(4)]
        yt = [fw.sb("hyy%d" % i, [128, 512]) for i in range(2)]
        ob = [fw.sb("hyo%d" % i, [128, 2, 512], BF16) for i in range(2)]
        npiece = nT // KP
        ri = 0
        for tg in range(nG):
            tok0 = base + tg * TB
            pa = [self.PS[4 + (tg % 2) * 2], self.PS[5 + (tg % 2) * 2]]
            for piece in range(npiece):
                for j in range(2):
                    tab = tir[ri % 4]
                    fw.dma("sp" if ri % 2 == 0 else "pool", tab[:], ti_d[j, tg][:, piece * KP * TB:(piece + 1) * KP * TB])
                    ri += 1
                    for kk in range(KP):
                        kc = piece * KP + kk
                        first = (piece == 0 and j == 0 and kk == 0)
                        last = (piece == npiece - 1 and j == 1 and kk == KP - 1)
                        for cc in range(2):
                            fw.mm(pa[cc][:, 0:TB], ZB[:, kc, j * 256 + cc * 128:j * 256 + (cc + 1) * 128],
                                  tab[:, kk * TB:(kk + 1) * TB], start=first, stop=last)
            o = ob[tg % 2]
            for cc in range(2):
                y = yt[cc]
                fw.stt(y[:, 0:TB], pa[cc][:, 0:TB], 1.0 / Lx, WB[:, cc, tg * TB:(tg + 1) * TB], ALU.mult, ALU.add)
                fw.tt(o[:, cc, 0:TB], y[:, 0:TB], X0[:, cc, tg * TB:(tg + 1) * TB], ALU.mult, eng="pool")
            fw.dma("pool", self.brv()[:, 0:2, tok0:tok0 + TB].k(("hy", tok0)), o[:, :, 0:TB])
        fw.release(m2)
        fw.release(m)

    def phase_hgrn_v1(self, l):
        fw = self.fw
        m = fw.mark()
        ident = self.c(C_IDENT)
        ones64 = self.cst[0:64, C_ONES:C_ONES + 64]
        scanm = self.c(C_SCAN)
        cmask = self.c(C_CMASK, 8)
        uv = self.uv()
        hgv = self.hgo[:].r("v (h t) -> v h t", h=4)
        gv = self.uT[2560:2816, :].r("(h v) t -> v h t", v=64)
        bro = self.brT[512:768, :].r("(h v) t -> v h t", v=64)
        epsb = fw.sb("epsb", [128, 1])
        fw.memset(epsb[:], RMS_EPS)
        lbv = fw.sb("lbv", [128, 4])
        oml = fw.sb("oml", [128, 4])
        if l == 0:
            fw.memset(lbv[:], 0.0)
            fw.memset(oml[:], 1.0)
        else:
            for d in range(2):
                for cc in range(2):
                    c1 = PP["lbl"] + (d * 2 + 1) * 2 + cc
                    c0 = PP["lbl"] + (d * 2 + 0) * 2 + cc
                    j = d * 2 + cc
                    fw.tt(lbv[:, j:j + 1], self.pp[:, c1:c1 + 1], self.pp[:, c0:c0 + 1], ALU.subtract)
            fw.act(lbv[:], lbv[:], AF.Exp, scale=-1.0)
            fw.ts(lbv[:], lbv[:], 1.0, ALU.add)
            fw.op("dve", lambda e: e.reciprocal(out=lbv[:].ap, in_=lbv[:].ap), [lbv[:]], [lbv[:]])
            fw.ts(oml[:], lbv[:], -1.0, ALU.mult, 1.0, ALU.add)
        S = [[fw.sb("hgS%d%d" % (cc, p), [128, 8, 64]) for p in range(2)] for cc in range(2)]
        R2 = 2
        qt = [fw.sb("hgq%d" % i, [128, 2, 128]) for i in range(R2)]
        ftl = [fw.sb("hgf%d" % i, [128, 2, 128]) for i in range(R2)]
        vt = [fw.sb("hgv%d" % i, [128, 2, 128]) for i in range(R2)]
        sg = fw.sb("hgsg", [128, 2, 128])
        gg = [fw.sb("hggg%d" % i, [128, 128]) for i in range(2)]
        lf = [fw.sb("hglf%d" % i, [128, 128]) for i in range(2)]
        kk = [fw.sb("hgkk%d" % i, [128, 128]) for i in range(2)]
        bp = [fw.sb("hgbp%d" % i, [128, 128]) for i in range(2)]
        bb = [fw.sb("hgbb%d" % i, [128, 128]) for i in range(2)]
        tm = [fw.sb("hgtm%d" % i, [128, 128]) for i in range(2)]
        qd = [fw.sb("hgqd%d" % i, [128, 128]) for i in range(2)]
        ki = [fw.sb("hgki%d" % i, [128, 128]) for i in range(2)]
        ke = [fw.sb("hgke%d" % i, [128, 128]) for i in range(2)]
        dec = [fw.sb("hgdec%d" % i, [128, 8]) for i in range(2)]
        ketok = [fw.sb("hgket%d" % i, [128, 128]) for i in range(2)]
        vtok = [fw.sb("hgvt%d" % i, [128, 128]) for i in range(2)]
        vexp = [fw.sb("hgvx%d" % i, [128, 8, 128]) for i in range(2)]
        atm = [fw.sb("hgatm%d" % i, [128, 128]) for i in range(4)]
        osb = [fw.sb("hgo%d" % i, [64, 512]) for i in range(2)]
        of = [fw.sb("hgof%d" % i, [64, 512]) for i in range(2)]
        gt = [fw.sb("hggt%d" % i, [64, 512]) for i in range(2)]
        sq = fw.sb("hgsq", [64, 512])
        rs = fw.sb("hgrs", [64, 512])
        sgt = fw.sb("hgsgt", [64, 512])
        obf = [fw.sb("hgob%d" % i, [64, 512], BF16) for i in range(2)]
        ng = self.pp[0:64, PP["hgng"]:PP["hgng"] + 1]
        for d in range(getattr(self, "hg_passes", 2)):
            order = list(range(34)) if d == 0 else [1, 0] + list(range(33, 1, -1))
            MI = self.c(C_MIF) if d == 0 else self.c(C_MIB)
            for cc in range(2):
                fw.memset(S[cc][0][:, 0, :], 0.0)
            for it, tile in enumerate(order):
                if it >= getattr(self, "hg_limit", 99):
                    break
                tok0 = tile * 128
                need_o = not (l == DEPTH - 1 and tile < 2)
                par = it % 2
                q_, f_, v_ = qt[it % R2], ftl[it % R2], vt[it % R2]
                fw.dma("sp", q_[:], uv[:, 12:14, tok0:tok0 + 128])
                fc = 14 if d == 0 else 16
                fw.dma("sp", f_[:], uv[:, fc:fc + 2, tok0:tok0 + 128])
                fw.dma("sp", v_[:], uv[:, 18:20, tok0:tok0 + 128])
                fw.act(sg[:], f_[:], AF.Exp, scale=-1.0)
                fw.ts(sg[:], sg[:], 1.0, ALU.add, eng="pool")
                fw.op("dve", lambda e: e.reciprocal(out=sg[:].ap, in_=sg[:].ap), [sg[:]], [sg[:]])
                pso = self.PS[6]
                pso2 = self.PS[7]
                for cc in range(2):
                    j = d * 2 + cc
                    g_, l_, k_, b_, bb_, t_ = gg[cc], lf[cc], kk[cc], bp[cc], bb[cc], tm[cc]
                    fw.ts(g_[:], sg[:, cc, :], oml[:, j:j + 1], ALU.mult, lbv[:, j:j + 1], ALU.add)
                    fw.act(l_[:], g_[:], AF.Ln)
                    fw.ts(k_[:], g_[:], -1.0, ALU.mult, 1.0, ALU.add, eng="pool")
                    fw.scan(b_[:], scanm, l_[:], 0.0, ALU.mult, ALU.add)
                    b3 = b_[:].r("p (n s) -> p n s", s=16)
                    tot = b3[:, :, 15:16]
                    if d == 0:
                        bcur = b_
                    else:
                        fw.tt(t_[:], l_[:], b_[:], ALU.subtract)
                        fw.tt(bb_[:].r("p (n s) -> p n s", s=16), t_[:].r("p (n s) -> p n s", s=16),
                              tot.bc([128, 8, 16]), ALU.add)
                        bcur = bb_
                    fw.act(dec[cc][:], b3[:, :, 15], AF.Exp)
                    fw.act(t_[:], bcur[:], AF.Exp)
                    fw.stt(qd[cc][:], q_[:, cc, :], 0.125, t_[:], ALU.mult, ALU.mult)
                    fw.act(t_[:], bcur[:], AF.Exp, scale=-1.0)
                    fw.tt(ki[cc][:], k_[:], t_[:], ALU.mult)
                    fw.tt(t_[:].r("p (n s) -> p n s", s=16), tot.bc([128, 8, 16]),
                          bcur[:].r("p (n s) -> p n s", s=16), ALU.subtract)
                    fw.act(t_[:], t_[:], AF.Exp)
                    fw.tt(ke[cc][:], k_[:], t_[:], ALU.mult, eng="pool")
                    if getattr(self, "hg_stage", 9) < 1:
                        continue
                    pst = self.PS[cc]
                    fw.tr(pst[:, 0:128], ke[cc][:], ident)
                    fw.tr(pst[:, 128:256], v_[:, cc, :], ident)
                    fw.cp(ketok[cc][:], pst[:, 0:128], eng="act")
                    fw.cp(vtok[cc][:], pst[:, 128:256], eng="act")
                    fw.tt(vexp[cc][:], vtok[cc][:].un(1).bc([128, 8, 128]), cmask.un(2).bc([128, 8, 128]), ALU.mult)
                    for h in range(2):
                        if getattr(self, "hg_stage", 9) < 2:
                            continue
                        hh = cc * 2 + h
                        rows = slice(h * 64, (h + 1) * 64)
                        if need_o:
                            pa = self.PS[2 + h]
                            fw.mm(pa[:, 0:128], ki[cc][rows, :], qd[cc][rows, :])
                            am = atm[hh]
                            fw.tt(am[:], pa[:, 0:128], MI, ALU.mult)
                            fw.mm(pso[0:64, hh * 128:(hh + 1) * 128].k(hh), vtok[cc][:, h * 64:(h + 1) * 64], am[:])
                        pkv = self.PS[4 + h]
                        fw.mm(pkv[:, 0:512], ketok[cc][:], vexp[cc][:, :, h * 64:(h + 1) * 64])
                        Scur = S[cc][par]
                        Snxt = S[cc][1 - par]
                        for jj in range(8 if getattr(self, "hg_stage", 9) >= 3 else 0):
                            n = jj if d == 0 else 7 - jj
                            if need_o:
                                fw.mm(pso2[0:64, hh * 128 + n * 16:hh * 128 + (n + 1) * 16].k(hh),
                                      Scur[rows, jj, :].k((h, jj)), qd[cc][rows, n * 16:(n + 1) * 16])
                            dst = Scur[rows, jj + 1, :].k((h, jj + 1)) if jj < 7 else Snxt[rows, 0, :].k((h, 0))
                            fw.stt(dst, Scur[rows, jj, :].k((h, jj)), dec[cc][rows, n:n + 1],
                                   pkv[rows, n * 64:(n + 1) * 64], ALU.mult, ALU.add)
                if not need_o or getattr(self, "hg_stage", 9) < 4:
                    continue
                o_ = osb[it % 2]
                fw.cp(o_[:], pso[0:64, :], eng="act")
                fw.tt(o_[:], o_[:], pso2[0:64, :], ALU.add)
                if d == 0:
                    fw.dma("pool", hgv[:, :, tok0:tok0 + 128].k(("t", tile)), o_[:].r("v (h t) -> v h t", h=4))
                    continue
                of_ = of[it % 2]
                g2 = gt[it % 2]
                fw.dma("pool", of_[:].r("v (h t) -> v h t", h=4), hgv[:, :, tok0:tok0 + 128].k(("t", tile)))
                fw.dma("pool", g2[:].r("v (h t) -> v h t", h=4), gv[:, :, tok0:tok0 + 128])
                fw.tt(o_[:], o_[:], of_[:], ALU.add)
                fw.tt(sq[:], o_[:], o_[:], ALU.mult, eng="pool")
                pss = self.PS[0]
                fw.mm(pss[0:64, 0:512], ones64, sq[:])
                fw.act(rs[:], pss[0:64, 0:512], AF.Ln, bias=epsb[0:64, :], scale=1.0 / 64.0)
                fw.act(rs[:], rs[:], AF.Exp, scale=-0.5)
                fw.tt(o_[:], o_[:], rs[:], ALU.mult)
                fw.act(sgt[:], g2[:], AF.Exp, scale=-1.0)
                fw.ts(sgt[:], sgt[:], 1.0, ALU.add, eng="pool")
                fw.op("dve", lambda e: e.reciprocal(out=sgt[:].ap, in_=sgt[:].ap), [sgt[:]], [sgt[:]])
                fw.tt(sgt[:], sgt[:], g2[:], ALU.mult, eng="pool")
                ob_ = obf[it % 2]
                fw.stt(ob_[:], o_[:], ng, sgt[:], ALU.mult, ALU.mult)
                fw.dma("pool", bro[:, :, tok0:tok0 + 128].k(("hg", tile)), ob_[:].r("v (h t) -> v h t", h=4))
            fw.barrier()
        fw.release(m)

    def phase_hgrn(self, l):
        fw = self.fw
        m = fw.mark()
        id64 = self.cst[0:64, C_IDENT:C_IDENT + 64]
        ones64 = self.cst[0:64, C_ONES:C_ONES + 64]
        MI = [self.c(C_MIF), self.c(C_MIB)]
        CM = [self.c(C_CMASK, 8), self.c(C_CMASKR, 8)]
        hgv = [self.hgo[:].r("v (h t) -> v h t", h=4), self.hgo2[:].r("v (h t) -> v h t", h=4)]
        uq = self.uT[1536:1792, :].r("(h d) t -> d h t", d=64)
        uf = [self.uT[1792:2048, :].r("(h d) t -> d h t", d=64), self.uT[2048:2304, :].r("(h d) t -> d h t", d=64)]
        ui = self.uT[2304:2560, :].r("(h d) t -> d h t", d=64)
        gv = self.uT[2560:2816, :].r("(h v) t -> v h t", v=64)
        bro = self.brT[512:768, :].r("(h v) t -> v h t", v=64)
        epsb = fw.sb("epsb", [64, 1])
        fw.memset(epsb[:], RMS_EPS)
        lbv = fw.sb("lbv", [64, 8])
        oml = fw.sb("oml", [64, 8])
        if l > 0:
            for d in range(2):
                c1 = PP["lbl64"] + (d * 2 + 1) * 4
                c0 = PP["lbl64"] + (d * 2 + 0) * 4
                fw.tt(lbv[:, d * 4:(d + 1) * 4], self.pp[0:64, c1:c1 + 4], self.pp[0:64, c0:c0 + 4], ALU.subtract)
            fw.act(lbv[:], lbv[:], AF.Exp, scale=-1.0)
            fw.ts(lbv[:], lbv[:], 1.0, ALU.add)
            fw.op("dve", lambda e: e.reciprocal(out=lbv[:].ap, in_=lbv[:].ap), [lbv[:]], [lbv[:]])
            fw.ts(oml[:], lbv[:], -1.0, ALU.mult, 1.0, ALU.add)
        SM = fw.sb("hg_sm", [64, 2048])
        fw.cp(SM[:].r("p (a b) -> p a b", b=16), self.cst[0:64, C_SCAN16:C_SCAN16 + 16].un(1).bc([64, 128, 16]))
        S = fw.sb("hg_S", [64, 9, 8, 64])
        fw.memset(S[:, 0, :, :], 0.0)
        F = 2048
        qin = fw.sb("hg_qin", [64, 4, 512])
        fin = fw.sb("hg_fin", [64, 4, 512])
        T = [fw.sb("hg_T%d" % i, [64, F]) for i in range(5)]
        qd32 = [fw.sb("hg_qd32%d" % d, [64, 4, 512]) for d in range(2)]
        qd16 = [fw.sb("hg_qd16%d" % d, [64, 4, 512], BF16) for d in range(2)]
        ki16 = [fw.sb("hg_ki16%d" % d, [64, 4, 512], BF16) for d in range(2)]
        ke32 = [fw.sb("hg_ke32%d" % d, [64, 4, 512]) for d in range(2)]
        vin = [fw.sb("hg_vin%d" % d, [64, 4, 512]) for d in range(2)]
        dec = [fw.sb("hg_dec%d" % d, [64, 4, 32]) for d in range(2)]
        ketok = [fw.sb("hg_ket%d" % d, [128, 256], BF16) for d in range(2)]
        vtok = [fw.sb("hg_vt%d" % d, [128, 256], BF16) for d in range(2)]
        vexp = [fw.sb("hg_vx%d" % d, [128, 8, 256], BF16) for d in range(2)]
        atm = [fw.sb("hg_atm%d" % d, [128, 4, 128], BF16) for d in range(2)]
        kvs = [fw.sb("hg_kvs%d" % d, [64, 8, 4, 64]) for d in range(2)]
        osb = [fw.sb("hg_o%d" % d, [64, 512]) for d in range(2)]
        ot = [fw.sb("hg_ot%d" % d, [64, 512]) for d in range(2)]
        CH = ["dve", "pool"]

        def prep(d, tok0, TBk):
            n = 4 * TBk
            nch = TBk // 16
            v3 = lambda b: b[:, 0:n].r("p (h t) -> p h t", h=4)
            c3 = lambda b: b[:, 0:n].r("p (c s) -> p c s", s=16)
            fw.dma("sp", qin[:, :, 0:TBk], uq[:, :, tok0:tok0 + TBk])
            fw.dma("sp", fin[:, :, 0:TBk], uf[d][:, :, tok0:tok0 + TBk])
            fw.dma("sp", vin[d][:, :, 0:TBk], ui[:, :, tok0:tok0 + TBk])
            sg, lf, kk, b_, t_ = T
            fw.act(v3(sg), fin[:, :, 0:TBk], AF.Exp, scale=-1.0)
            fw.ts(sg[:, 0:n], sg[:, 0:n], 1.0, ALU.add, eng="pool")
            fw.op("dve", lambda e: e.reciprocal(out=sg[:, 0:n].ap, in_=sg[:, 0:n].ap), [sg[:, 0:n]], [sg[:, 0:n]])
            if l > 0:
                for h in range(4):
                    j = d * 4 + h
                    fw.ts(sg[:, h * TBk:(h + 1) * TBk], sg[:, h * TBk:(h + 1) * TBk], oml[:, j:j + 1], ALU.mult,
                          lbv[:, j:j + 1], ALU.add)
            fw.act(lf[:, 0:n], sg[:, 0:n], AF.Ln)
            fw.ts(kk[:, 0:n], sg[:, 0:n], -1.0, ALU.mult, 1.0, ALU.add, eng="pool")
            fw.scan(b_[:, 0:n], SM[:, 0:n], lf[:, 0:n], 0.0, ALU.mult, ALU.add)
            tot = c3(b_)[:, :, 15:16]
            fw.act(dec[d][:, :, 0:nch], b_[:, 0:n].r("p (h c s) -> p h c s", h=4, s=16)[:, :, :, 15], AF.Exp)
            if d == 1:
                fw.tt(lf[:, 0:n], lf[:, 0:n], b_[:, 0:n], ALU.subtract, eng="pool")
                fw.tt(c3(lf), c3(lf), tot.bc([64, n // 16, 16]), ALU.add)
                bcur = lf
            else:
                bcur = b_
            fw.act(t_[:, 0:n], bcur[:, 0:n], AF.Exp)
            fw.stt(qd32[d][:, :, 0:TBk], qin[:, :, 0:TBk], 0.125, v3(t_), ALU.mult, ALU.mult)
            fw.cp(qd16[d][:, :, 0:TBk], qd32[d][:, :, 0:TBk], eng="pool")
            fw.act(t_[:, 0:n], bcur[:, 0:n], AF.Exp, scale=-1.0)
            fw.tt(ki16[d][:, :, 0:TBk], v3(kk), v3(t_), ALU.mult)
            fw.tt(c3(t_), tot.bc([64, n // 16, 16]), c3(bcur), ALU.subtract, eng="pool")
            fw.act(t_[:, 0:n], t_[:, 0:n], AF.Exp)
            fw.tt(ke32[d][:, :, 0:TBk], v3(kk), v3(t_), ALU.mult)

        def tile_pre(d, off, tile, need_o):
            pst = self.PS[d]
            for h in range(4):
                fw.tr(pst[:, h * 64:(h + 1) * 64], ke32[d][:, h, off:off + 128], id64)
                fw.tr(pst[:, 256 + h * 64:256 + (h + 1) * 64], vin[d][:, h, off:off + 128], id64)
            fw.cp(ketok[d][:], pst[:, 0:256], eng="act")
            fw.cp(vtok[d][:], pst[:, 256:512], eng="act")
            fw.tt(vexp[d][:], vtok[d][:].un(1).bc([128, 8, 256]), CM[d].un(2).bc([128, 8, 256]), ALU.mult,
                  eng=("dve" if d == 1 else "pool"))
            poi = self.PS[3 + d * 2]
            if need_o:
                pat = self.PS[2]
                for h in range(4):
                    fw.mm(pat[:, h * 128:(h + 1) * 128].k(h), ki16[d][:, h, off:off + 128], qd16[d][:, h, off:off + 128])
                fw.tt(atm[d][:], pat[:].r("p (h t) -> p h t", h=4), MI[d].un(1).bc([128, 4, 128]), ALU.mult)
                for h in range(4):
                    fw.mm(poi[0:64, h * 128:(h + 1) * 128].k(h), vtok[d][:, h * 64:(h + 1) * 64], atm[d][:, h, :])
            pkv = self.PS[7]
            for h in range(4):
                fw.mm(pkv[0:64, 0:512], ketok[d][:, h * 64:(h + 1) * 64], vexp[d][:, :, h * 64:(h + 1) * 64])
                fw.cp(kvs[d][:, :, h, :].k(h), pkv[0:64, 0:512].r("p (j v) -> p j v", j=8), eng="act")

        def chain_step(d, j, off, need_o):
            pox = self.PS[4 + d * 2]
            ce = CH[d]
            dsl = slice(d * 4, (d + 1) * 4)
            c0 = off // 16
            n = j if d == 0 else 7 - j
            if need_o:
                for h in range(4):
                    fw.mm(pox[0:64, h * 128 + n * 16:h * 128 + (n + 1) * 16].k(h),
                          S[:, j, d * 4 + h, :].k((d, j)), qd32[d][:, h, off + n * 16:off + (n + 1) * 16])
            dcb = dec[d][:, :, c0 + n:c0 + n + 1].bc([64, 4, 64])
            dst = S[:, j + 1, dsl, :].k((d, j + 1)) if j < 7 else S[:, 0, dsl, :].k((d, 0))
            fw.tt(S[:, 8, dsl, :].k((d, 8)) if j == 7 else dst, S[:, j, dsl, :].k((d, j)), dcb, ALU.mult, eng=ce)
            src = S[:, 8, dsl, :].k((d, 8)) if j == 7 else dst
            fw.tt(dst, src, kvs[d][:, j, :, :], ALU.add, eng=ce)

        def tile_post(d, tile, need_o):
            if not need_o:
                return
            tok0 = tile * 128
            poi = self.PS[3 + d * 2]
            pox = self.PS[4 + d * 2]
            o_ = osb[d]
            fw.cp(o_[:], poi[0:64, :], eng="act")
            fw.tt(o_[:], o_[:], pox[0:64, :], ALU.add, eng="dve")
            fw.dma("pool", hgv[d][:, :, tok0:tok0 + 128].k(("t", tile)), o_[:].r("v (h t) -> v h t", h=4))

        steps = [(0, 0, 256)] + [(LC + 512 * i, LC + 512 * (7 - i), 512) for i in range(8)]
        for si, (tf0, tb0, TBk) in enumerate(steps):
            prep(0, tf0, TBk)
            prep(1, tb0, TBk)
            nt = TBk // 128
            for i in range(nt):
                info = []
                for d in range(2):
                    off = i * 128 if d == 0 else (nt - 1 - i) * 128
                    t0 = (tf0 if d == 0 else tb0) + off
                    tile = t0 // 128
                    need_o = not (l == DEPTH - 1 and tile < 2)
                    info.append((off, tile, need_o))
                    tile_pre(d, off, tile, need_o)
                for j in range(8):
                    for d in range(2):
                        chain_step(d, j, info[d][0], info[d][2])
                for d in range(2):
                    tile_post(d, info[d][1], info[d][2])
        fw.barrier()
        A = T[0:3]
        obf3 = ki16[0]
        ng = self.pp[0:64, PP["hgng"]:PP["hgng"] + 1]
        for gi, (tok0, TB) in enumerate(GROUPS):
            if gi == 0 and l == DEPTH - 1:
                continue
            n = 4 * TB
            v3 = lambda b: b[:, 0:n].r("p (h t) -> p h t", h=4)
            o1, o2, g2 = qd32[0], qd32[1], ke32[0]
            fw.dma("sp", o1[:, :, 0:TB], hgv[0][:, :, tok0:tok0 + TB])
            fw.dma("sp", o2[:, :, 0:TB], hgv[1][:, :, tok0:tok0 + TB])
            fw.dma("pool", g2[:, :, 0:TB], gv[:, :, tok0:tok0 + TB])
            o_, sq_, sg_ = A
            fw.tt(v3(o_), o1[:, :, 0:TB], o2[:, :, 0:TB], ALU.add)
            fw.tt(sq_[:, 0:n], o_[:, 0:n], o_[:, 0:n], ALU.mult, eng="pool")
            for h in range(4):
                pss = self.PS[h % 2]
                fw.mm(pss[0:64, 0:TB], ones64, sq_[:, h * TB:(h + 1) * TB])
                fw.act(sq_[:, h * TB:(h + 1) * TB].k(h), pss[0:64, 0:TB], AF.Ln, bias=epsb[:], scale=1.0 / 64.0)
            fw.act(sq_[:, 0:n], sq_[:, 0:n], AF.Exp, scale=-0.5)
            fw.tt(o_[:, 0:n], o_[:, 0:n], sq_[:, 0:n], ALU.mult)
            fw.act(v3(sg_), g2[:, :, 0:TB], AF.Exp, scale=-1.0)
            fw.ts(sg_[:, 0:n], sg_[:, 0:n], 1.0, ALU.add, eng="pool")
            fw.op("dve", lambda e: e.reciprocal(out=sg_[:, 0:n].ap, in_=sg_[:, 0:n].ap), [sg_[:, 0:n]], [sg_[:, 0:n]])
            fw.tt(v3(sg_), v3(sg_), g2[:, :, 0:TB], ALU.mult, eng="pool")
            fw.stt(obf3[:, :, 0:TB], v3(o_), ng, v3(sg_), ALU.mult, ALU.mult)
            fw.dma("pool", bro[:, :, tok0:tok0 + TB].k(("hg", gi)), obf3[:, :, 0:TB])
        fw.release(m)

    def phase_ssd(self, l):
        fw = self.fw
        m = fw.mark()
        ident = self.c(C_IDENT)
        ones = self.c(C_ONES)
        uv = self.uv()
        brv = self.brv()
        dtb = self.rb[:, RB["dtbias"]:RB["dtbias"] + 8]
        dsk = self.rb[:, RB["ssdd"]:RB["ssdd"] + 4]
        ngb = self.rb[:, RB["ssdng"]:RB["ssdng"] + 256]
        onec = fw.sb("sd_one", [128, 1])
        fw.memset(onec[:], 1.0)
        epsb = fw.sb("sd_eps", [128, 1])
        fw.memset(epsb[:], RMS_EPS)
        nea = fw.sb("sd_nea", [128, 8])
        fw.act(nea[:], self.rb[:, RB["alog"]:RB["alog"] + 8], AF.Exp)
        fw.ts(nea[:], nea[:], -1.0, ALU.mult)
        yD = [self.yf, self.yb]
        B = []
        for d in range(2):
            b = {}
            b["ST"] = fw.sb("sd_ST%d" % d, [128, 4, 64])
            fw.memset(b["ST"][:], 0.0)
            b["xin"] = [fw.sb("sd_xin%d%d" % (d, i), [128, 4, 130]) for i in range(2)]
            b["dtr"] = [fw.sb("sd_dtr%d%d" % (d, i), [128, 8]) for i in range(2)]
            for nm, shp in (("xc", [128, 4, 128]), ("sg", [128, 4, 128]), ("xb", [128, 4, 128]), ("xs", [128, 256]),
                            ("bt", [128, 128]), ("dtt", [128, 8]), ("atok", [128, 8]), ("negcum", [128, 4]),
                            ("expcum", [128, 4]), ("totE", [128, 4]), ("edec", [128, 4]), ("cb", [128, 256]),
                            ("ydg", [128, 256]), ("y", [128, 256]), ("xsd", [128, 256])):
                b[nm] = fw.sb("sd_%s%d" % (nm, d), shp)
            b["xd"] = fw.sb("sd_xd%d" % d, [128, 4, 64], BF16)
            for nm in ("arow", "E"):
                b[nm] = [fw.sb("sd_%s%d%d" % (nm, d, i), [128, 128]) for i in range(2)]
            for nm in ("W", "bdec"):
                b[nm] = [fw.sb("sd_%s%d%d" % (nm, d, i), [128, 128], BF16) for i in range(2)]
            B.append(b)
        PX, PY = self.PS[0], self.PS[1]
        PT = [self.PS[2], self.PS[3]]
        PD = [self.PS[4], self.PS[5]]
        PYD = self.PS[6]
        PSS = self.PS[7]

        def tile_gen(d, it, tile):
            b = B[d]
            TRI = self.c(C_TRIF) if d == 0 else self.c(C_TRIB)
            NEGM = self.c(C_NEGF) if d == 0 else self.c(C_NEGB)
            j0 = d * 4
            tok0 = tile * 128
            need_o = not (l == DEPTH - 1 and tile < 2)
            xi = b["xin"][it % 2]
            self.load_halo(xi, 24, 4, tok0, 128)
            dr = b["dtr"][it % 2]
            fw.dma("sp", dr[:], self.dtT[tok0:tok0 + 128, :])
            xc, sg, xb = b["xc"], b["sg"], b["xb"]
            for j in range(4):
                self.conv3(xc[:, j, :], xi[:, j, :], PP["sdcw"] + 3 * j, 128,
                           bias=self.pp[:, PP["sdcb"] + j:PP["sdcb"] + j + 1])
                yield
            fw.act(sg[:], xc[:], AF.Exp, scale=-1.0)
            yield
            fw.ts(sg[:], sg[:], 1.0, ALU.add)
            fw.op("dve", lambda e: e.reciprocal(out=sg[:].ap, in_=sg[:].ap), [sg[:]], [sg[:]])
            fw.tt(xb[:], xc[:], sg[:], ALU.mult)
            yield
            pt = PT[d]
            for j in range(3):
                fw.tr(pt[:, j * 128:(j + 1) * 128], xb[:, j, :], ident)
            xs_, bt_, dtt, atok = b["xs"], b["bt"], b["dtt"], b["atok"]
            fw.cp(xs_[:], pt[:, 0:256], eng="act")
            fw.cp(bt_[:], pt[:, 256:384], eng="act")
            yield
            fw.tt(dtt[:], dr[:], dtb, ALU.add)
            fw.act(dtt[:], dtt[:], AF.Exp)
            fw.act(dtt[:], dtt[:], AF.Ln, bias=onec[:], scale=1.0)
            fw.tt(atok[:], dtt[:], nea[:], ALU.mult)
            yield
            fw.mm(pt[:, 384:388], TRI, atok[:, j0:j0 + 4])
            fw.mm(pt[:, 392:396], ones, atok[:, j0:j0 + 4])
            negcum, expcum, totE, edec = b["negcum"], b["expcum"], b["totE"], b["edec"]
            fw.ts(negcum[:], pt[:, 384:388], -1.0, ALU.mult)
            fw.act(expcum[:], pt[:, 384:388], AF.Exp)
            fw.act(totE[:], pt[:, 392:396], AF.Exp)
            fw.tt(edec[:], pt[:, 392:396], negcum[:], ALU.add)
            fw.act(edec[:], edec[:], AF.Exp)
            yield
            cb_ = b["cb"]
            for g in range(2):
                gr = slice(g * 64, (g + 1) * 64)
                pcb = (PX, PY)[g]
                fw.mm(pcb[:, d * 128:(d + 1) * 128], xb[gr, 2, :], xb[gr, 3, :])
                fw.cp(cb_[:, g * 128:(g + 1) * 128].k(g), pcb[:, d * 128:(d + 1) * 128], eng="act")
            xd = b["xd"]
            fw.tt(xd[:], xs_[:].r("p (h q) -> p h q", h=4), dtt[:, j0:j0 + 4].un(2).bc([128, 4, 64]), ALU.mult)
            yield
            ST = b["ST"]
            for h in range(4):
                g = h // 2
                gr = slice(g * 64, (g + 1) * 64)
                ar = b["arow"][h % 2]
                fw.act(ar[:], TRI, AF.Copy, scale=atok[:, j0 + h:j0 + h + 1])
                pd = PD[d]
                hc = slice((h % 2) * 128, (h % 2 + 1) * 128)
                fw.mm(pd[:, hc], ones, ar[:], start=True, stop=False)
                fw.mm(pd[:, hc], ident, NEGM, start=False, stop=True)
                e_ = b["E"][h % 2]
                fw.act(e_[:], pd[:, hc], AF.Exp, bias=negcum[:, h:h + 1])
                yield
                w_ = b["W"][h % 2]
                fw.tt(w_[:], e_[:], cb_[:, g * 128:(g + 1) * 128].k(g), ALU.mult)
                yc = slice(d * 256 + h * 64, d * 256 + (h + 1) * 64)
                pyo = (PX, PY)[g]
                yoc = slice(256 + d * 128 + (h % 2) * 64, 256 + d * 128 + (h % 2 + 1) * 64)
                if need_o:
                    fw.mm(PYD[:, yc], w_[:], xd[:, h, :])
                    fw.mm(pyo[:, yoc], xb[gr, 3, :], ST[gr, h, :].k(h))
                bd = b["bdec"][h % 2]
                fw.ts(bd[:], bt_[:], edec[:, h:h + 1], ALU.mult)
                fw.mm(PSS[:, yc].k((d, h)), bd[:], xd[:, h, :])
                fw.stt(ST[gr, h, :].k(h), ST[gr, h, :].k(h), totE[gr, h:h + 1], PSS[gr, yc].k((d, h)), ALU.mult, ALU.add)
                yield
            if not need_o:
                return
            ydg, y_ = b["ydg"], b["y"]
            fw.cp(ydg[:], PYD[:, d * 256:(d + 1) * 256], eng="act")
            for h in range(4):
                g = h // 2
                pyo = (PX, PY)[g]
                yoc = slice(256 + d * 128 + (h % 2) * 64, 256 + d * 128 + (h % 2 + 1) * 64)
                cs = slice(h * 64, (h + 1) * 64)
                fw.stt(y_[:, cs], pyo[:, yoc], expcum[:, h:h + 1], ydg[:, cs], ALU.mult, ALU.add)
            yield
            if d == 0:
                xsd = b["xsd"]
                fw.tt(xsd[:].r("p (h q) -> p h q", h=4), xs_[:].r("p (h q) -> p h q", h=4),
                      dsk.un(2).bc([128, 4, 64]), ALU.mult)
                fw.tt(y_[:], y_[:], xsd[:], ALU.add)
            fw.dma("pool", yD[d][tok0:tok0 + 128, :].k(("t", tile)), y_[:])
            yield

        of = list(range(34))
        ob = [1, 0] + list(range(33, 1, -1))
        for it in range(34):
            if it >= getattr(self, "sd_limit", 99):
                break
            gens = [tile_gen(0, it, of[it]), tile_gen(1, it, ob[it])]
            alive = [True, True]
            while any(alive):
                for d in range(2):
                    if alive[d]:
                        try:
                            next(gens[d])
                        except StopIteration:
                            alive[d] = False
        fw.barrier()
        yfl = [fw.sb("sd_ryf%d" % i, [128, 256]) for i in range(2)]
        ybl = [fw.sb("sd_ryb%d" % i, [128, 256]) for i in range(2)]
        zin = [fw.sb("sd_rz%d" % i, [128, 2, 128]) for i in range(2)]
        zt = fw.sb("sd_rzt", [128, 256])
        zs = fw.sb("sd_rzs", [128, 256])
        yg = fw.sb("sd_ryg", [128, 256])
        ysq = fw.sb("sd_rysq", [128, 256])
        ss = fw.sb("sd_rss", [128, 1])
        ob_ = [fw.sb("sd_rob%d" % i, [128, 2, 128], BF16) for i in range(2)]
        for tile in range(34):
            if l == DEPTH - 1 and tile < 2:
                continue
            tok0 = tile * 128
            a, b2, zi = yfl[tile % 2], ybl[tile % 2], zin[tile % 2]
            fw.dma("sp", a[:], self.yf[tok0:tok0 + 128, :])
            fw.dma("sp", b2[:], self.yb[tok0:tok0 + 128, :])
            fw.dma("sp", zi[:], uv[:, 22:24, tok0:tok0 + 128])
            pz = self.PS[tile % 2]
            for cc in range(2):
                fw.tr(pz[:, cc * 128:(cc + 1) * 128], zi[:, cc, :], ident)
            fw.cp(zt[:], pz[:, 0:256], eng="act")
            fw.act(zs[:], zt[:], AF.Exp, scale=-1.0)
            fw.ts(zs[:], zs[:], 1.0, ALU.add)
            fw.op("dve", lambda e: e.reciprocal(out=zs[:].ap, in_=zs[:].ap), [zs[:]], [zs[:]])
            fw.tt(zs[:], zs[:], zt[:], ALU.mult)
            fw.tt(yg[:], a[:], b2[:], ALU.add)
            fw.tt(yg[:], yg[:], zs[:], ALU.mult)
            fw.act(ysq[:], yg[:], AF.Square)
            fw.red(ss[:], ysq[:], ALU.add)
            fw.act(ss[:], ss[:], AF.Ln, bias=epsb[:], scale=1.0 / 256.0)
            fw.act(ss[:], ss[:], AF.Exp, scale=-0.5)
            fw.stt(yg[:], yg[:], ss[:], ngb, ALU.mult, ALU.mult)
            po = self.PS[2 + tile % 2]
            for cc in range(2):
                fw.tr(po[:, cc * 128:(cc + 1) * 128], yg[:, cc * 128:(cc + 1) * 128], ident)
            o_ = ob_[tile % 2]
            fw.cp(o_[:], po[:, 0:256].r("p (c t) -> p c t", c=2), eng="act")
            fw.dma("pool", brv[:, 6:8, tok0:tok0 + 128].k(("sd", tile)), o_[:])
        fw.release(m)

    def layernorm_fm(self, x, TB, gname, out, W):
        fw = self.fw
        ones = self.c(C_ONES)
        ps_s, ps_q = self.PS[6], self.PS[7]
        for Dc in range(KC):
            fw.mm(ps_s[:, 0:TB], ones, x[:, Dc, 0:TB], start=(Dc == 0), stop=(Dc == KC - 1))
        for Dc in range(KC):
            sq = W["sq"][Dc % 2]
            fw.act(sq[:, 0:TB], x[:, Dc, 0:TB], AF.Square)
            fw.mm(ps_q[:, 0:TB], ones, sq[:, 0:TB], start=(Dc == 0), stop=(Dc == KC - 1))
        mean, msq, rstd, mr = W["mean"], W["msq"], W["rstd"], W["mr"]
        fw.ts(mean[:, 0:TB], ps_s[:, 0:TB], 1.0 / D, ALU.mult)
        fw.tt(msq[:, 0:TB], mean[:, 0:TB], mean[:, 0:TB], ALU.mult, eng="pool")
        fw.stt(rstd[:, 0:TB], ps_q[:, 0:TB], 1.0 / D, msq[:, 0:TB], ALU.mult, ALU.subtract)
        fw.act(rstd[:, 0:TB], rstd[:, 0:TB], AF.Ln, bias=W["lneps"][:], scale=1.0)
        fw.act(rstd[:, 0:TB], rstd[:, 0:TB], AF.Exp, scale=-0.5)
        fw.tt(mr[:, 0:TB], mean[:, 0:TB], rstd[:, 0:TB], ALU.mult, eng="pool")
        g0 = PP[gname + "g"]
        b0 = PP[gname + "b"]
        for Dc in range(KC):
            t = W["t"][Dc % 2]
            fw.tt(t[:, 0:TB], x[:, Dc, 0:TB], rstd[:, 0:TB], ALU.mult)
            fw.tt(t[:, 0:TB], t[:, 0:TB], mr[:, 0:TB], ALU.subtract, eng="pool")
            fw.act(out[:, Dc, 0:TB].k(Dc), t[:, 0:TB], AF.Identity, bias=self.pp[:, b0 + Dc:b0 + Dc + 1],
                   scale=self.pp[:, g0 + Dc:g0 + Dc + 1])

    def ln_work(self):
        fw = self.fw
        W = {"sq": [fw.sb("ln_sq%d" % i, [128, 512]) for i in range(2)],
             "t": [fw.sb("ln_t%d" % i, [128, 512]) for i in range(2)],
             "mean": fw.sb("ln_mean", [128, 512]), "msq": fw.sb("ln_msq", [128, 512]),
             "rstd": fw.sb("ln_rstd", [128, 512]), "mr": fw.sb("ln_mr", [128, 512]),
             "lneps": fw.sb("ln_eps", [128, 1])}
        fw.memset(W["lneps"][:], LN_EPS)
        return W

    def phase_merge(self, l):
        fw = self.fw
        m = fw.mark()
        ident = self.c(C_IDENT)
        wbr = fw.sb("wbr", [128, 8, D], BF16)
        wo = fw.sb("wo", [128, KC, D], BF16)
        wr = fw.sb("wr", [128, KC, NE])
        m1 = fw.mark()
        stage = [fw.sb("mst%d" % i, [128, 4 * D]) for i in range(2)]
        stb = [fw.sb("mstb%d" % i, [128, 4 * D], BF16) for i in range(2)]
        wgv = self.w_gate_d[l].r("(kc p) n -> p kc n", p=128)
        wov = self.w_o_d[l].r("(kc p) n -> p kc n", p=128)
        wgc5 = self.wgc[:].r("dc p (kc k c) -> p dc kc k c", kc=KC, k=4)
        i = 0
        for kc in range(KC):
            self.load_cast(stb[kc % 2][:], wgv[:, kc, :], stage, 4 * D, i)
            for k in range(4):
                fw.dma("pool", wgc5[:, :, kc, k, :].k(("wgc", kc, k)),
                       stb[kc % 2][:, k * D:(k + 1) * D].r("p (dc c) -> p dc c", dc=8))
            i += 1
        for kc in range(KC):
            self.load_cast(wo[:, kc, :].k(kc), wov[:, kc, :], stage, D, i)
            i += 1
        for k in range(4):
            for cc in range(2):
                self.load_cast(wbr[:, k * 2 + cc, :].k(k * 2 + cc), self.w_br_d[l, k, cc * 128:(cc + 1) * 128, :], stage, D, i)
                i += 1
        fw.dma("sp", wr[:], self.w_router_d[:].r("(kc p) e -> p kc e", p=128))
        fw.release(m1)
        W = self.ln_work()
        rt = [fw.sb("mrt%d" % i, [128, KC, 512]) for i in range(1)]
        ht = [fw.sb("mht%d" % i, [128, KC, 512], BF16) for i in range(1)]
        bt = [fw.sb("mbt%d" % i, [128, 8, 512], BF16) for i in range(1)]
        wgr = [fw.sb("mwg%d" % i, [128, KC, 4, 128], BF16) for i in range(3)]
        yb = fw.sb("myb", [128, KC, 512], BF16)
        gs = [fw.sb("mgs%d" % i, [128, 512]) for i in range(2)]
        yacc = fw.sb("myacc", [128, 512])
        ytmp = [fw.sb("mytmp%d" % i, [128, 512]) for i in range(2)]
        h2 = fw.sb("mh2", [128, KC, 512])
        gT = fw.sb("mgT", [16, 512])
        R = {n: fw.sb("r_" + n, [128, w]) for n, w in
             [("lg", 16), ("mx", 1), ("e", 16), ("ss", 1), ("sc", 16), ("sel", 16), ("m1", 4), ("eq", 16), ("x2", 16),
              ("m2", 4), ("gsc", 4), ("gm", 1), ("ing", 4), ("t2m", 16), ("selm", 16), ("w", 16), ("ws", 1), ("gate", 16)]}
        resv = self.resT[:].r("(kc p) t -> p kc t", p=128)
        l1v = self.lat1T[:].r("(kc p) t -> p kc t", p=128)
        h2v = self.h2T[:].r("(kc p) t -> p kc t", p=128)
        brv = self.brv()
        brb = self.rb[:, RB["brouter"]:RB["brouter"] + 16]
        wi = 0
        for gi, (tok0, TB) in enumerate(GROUPS):
            if gi == 0 and l == DEPTH - 1:
                continue
            md = self.mod_for(gi)
            r, h, br = rt[0], ht[0], bt[0]
            res1 = r
            lat1 = r
            h2b = h
            fw.dma("sp", r[:, :, 0:TB], resv[:, :, tok0:tok0 + TB])
            fw.dma("pool", br[:, :, 0:TB], brv[:, :, tok0:tok0 + TB])
            for kc in range(KC):
                fw.act(h[:, kc, 0:TB].k(kc), r[:, kc, 0:TB].k(kc), AF.Identity, bias=md[:, kc:kc + 1], scale=md[:, 8 + kc:9 + kc])
            for kc in range(KC):
                fw.ts(r[:, kc, 0:TB].k(kc), r[:, kc, 0:TB].k(kc), ALPHA, ALU.mult, eng="pool")
            for Dc in range(KC):
                wg = wgr[wi % 3]
                wi += 1
                fw.dma("sp", wg[:], self.wgc[Dc].r("p (kc k c) -> p kc k c", kc=KC, k=4))
                for k in range(4):
                    pg = self.PS[k % 2]
                    for kc in range(KC):
                        fw.mm(pg[:, 0:TB], wg[:, kc, k, :], h[:, kc, 0:TB],
                              start=(kc == 0), stop=(kc == KC - 1))
                    pp_ = self.PS[2 + k % 2]
                    for cc in range(2):
                        fw.mm(pp_[:, 0:TB], wbr[:, k * 2 + cc, Dc * 128:(Dc + 1) * 128], br[:, k * 2 + cc, 0:TB],
                              start=(cc == 0), stop=(cc == 1))
                    g_ = gs[k % 2]
                    bcol = PP["bgate"] + k * 8 + Dc
                    fw.act(g_[:, 0:TB], pg[:, 0:TB], AF.Sigmoid, bias=self.pp[:, bcol:bcol + 1])
                    if k == 0:
                        fw.tt(yacc[:, 0:TB], g_[:, 0:TB], pp_[:, 0:TB], ALU.mult)
                    else:
                        t_ = ytmp[k % 2]
                        fw.tt(t_[:, 0:TB], g_[:, 0:TB], pp_[:, 0:TB], ALU.mult)
                        dst = yacc[:, 0:TB] if k < 3 else yb[:, Dc, 0:TB].k(Dc)
                        fw.tt(dst, yacc[:, 0:TB], t_[:, 0:TB], ALU.add, eng="pool")
            g1c = 16
            for Dc in range(KC):
                po = self.PS[4 + Dc % 2]
                for kc in range(KC):
                    fw.mm(po[:, 0:TB], wo[:, kc, Dc * 128:(Dc + 1) * 128], yb[:, kc, 0:TB], start=(kc == 0), stop=(kc == KC - 1))
                fw.stt(res1[:, Dc, 0:TB].k(Dc), po[:, 0:TB], md[:, g1c + Dc:g1c + Dc + 1], r[:, Dc, 0:TB].k(Dc), ALU.mult, ALU.add)
            self.layernorm_fm(res1, TB, "ln1", lat1, W)
            fw.dma("pool", l1v[:, :, tok0:tok0 + TB].k(("g", gi)), lat1[:, :, 0:TB])
            for kc in range(KC):
                fw.act(h2[:, kc, 0:TB].k(kc), lat1[:, kc, 0:TB].k(kc), AF.Identity, bias=md[:, 24 + kc:25 + kc], scale=md[:, 32 + kc:33 + kc])
                fw.cp(h2b[:, kc, 0:TB].k(kc), h2[:, kc, 0:TB].k(kc), eng="pool")
            fw.dma("pool", h2v[:, :, tok0:tok0 + TB].k(("g", gi)), h2b[:, :, 0:TB])
            for jt in range(TB // 128):
                pl = self.PS[0]
                for kc in range(KC):
                    fw.mm(pl[:, 0:16], h2[:, kc, jt * 128:(jt + 1) * 128], wr[:, kc, :], start=(kc == 0), stop=(kc == KC - 1))
                fw.cp(R["lg"][:], pl[:, 0:16], eng="act")
                fw.red(R["mx"][:], R["lg"][:], ALU.max)
                fw.ts(R["mx"][:], R["mx"][:], -1.0, ALU.mult)
                fw.act(R["e"][:], R["lg"][:], AF.Exp, bias=R["mx"][:])
                fw.red(R["ss"][:], R["e"][:], ALU.add)
                fw.op("dve", lambda e: e.reciprocal(out=R["ss"][:].ap, in_=R["ss"][:].ap), [R["ss"][:]], [R["ss"][:]])
                fw.ts(R["sc"][:], R["e"][:], R["ss"][:], ALU.mult)
                fw.tt(R["sel"][:], R["sc"][:], brb, ALU.add)
                sel3 = R["sel"][:].r("p (g e) -> p g e", g=4)
                fw.red(R["m1"][:], sel3, ALU.max)
                fw.tt(R["eq"][:].r("p (g e) -> p g e", g=4), sel3, R["m1"][:].un(2).bc([128, 4, 4]), ALU.is_equal)
                fw.stt(R["x2"][:], R["eq"][:], -1e30, R["sel"][:], ALU.mult, ALU.add)
                fw.red(R["m2"][:], R["x2"][:].r("p (g e) -> p g e", g=4), ALU.max)
                fw.tt(R["gsc"][:], R["m1"][:], R["m2"][:], ALU.add)
                fw.red(R["gm"][:], R["gsc"][:], ALU.max)
                fw.ts(R["ing"][:], R["gsc"][:], R["gm"][:], ALU.is_ge)
                fw.tt(R["t2m"][:].r("p (g e) -> p g e", g=4), sel3, R["m2"][:].un(2).bc([128, 4, 4]), ALU.is_ge)
                fw.tt(R["selm"][:].r("p (g e) -> p g e", g=4), R["t2m"][:].r("p (g e) -> p g e", g=4),
                      R["ing"][:].un(2).bc([128, 4, 4]), ALU.mult)
                fw.tt(R["w"][:], R["sc"][:], R["selm"][:], ALU.mult)
                fw.red(R["ws"][:], R["w"][:], ALU.add)
                fw.op("dve", lambda e: e.reciprocal(out=R["ws"][:].ap, in_=R["ws"][:].ap), [R["ws"][:]], [R["ws"][:]])
                fw.ts(R["gate"][:], R["w"][:], R["ws"][:], ALU.mult)
                pt = self.PS[1]
                fw.tr(pt[0:16, 0:128], R["gate"][:], ident)
                fw.cp(gT[:, jt * 128:(jt + 1) * 128].k(jt), pt[0:16, 0:128], eng="act")
            fw.dma("pool", self.gateT[:, tok0:tok0 + TB].k(("g", gi)), gT[:, 0:TB])
        fw.release(m)

    def phase_wcast(self, l):
        fw = self.fw
        m = fw.mark()
        st = [fw.sb("wc_s%d" % i, [128, 4096]) for i in range(3)]
        sb_ = [fw.sb("wc_b%d" % i, [128, 4096], BF16) for i in range(3)]
        i = 0
        for e in range(NE):
            for (src, dst, pat) in ((self.w_e1_d, self.we1b, "(kc p) f -> p kc f"),
                                    (self.w_e3_d, self.we3b, "(kc p) f -> p kc f"),
                                    (self.w_e2_d, self.we2b, "(fc p) d -> p fc d")):
                a, b = st[i % 3], sb_[i % 3]
                n0 = 8 if src is not self.w_e2_d else 4
                fw.dma("sp", a[:].r("p (a b) -> p a b", a=n0), src[l, e].r(pat, p=128))
                fw.cp(b[:], a[:], eng=("dve", "act", "pool")[i % 3])
                fw.dma("pool", dst[e].r(pat, p=128).k(("e", e)), b[:].r("p (a b) -> p a b", a=n0))
                i += 1
        fw.release(m)

    def phase_moe(self, l):
        fw = self.fw
        m = fw.mark()
        ident = self.c(C_IDENT)
        W = self.ln_work()
        last = (l == DEPTH - 1)
        h2t = [fw.sb("e_h2%d" % i, [128, KC, 512], BF16) for i in range(2)]
        l1t = [fw.sb("e_l1%d" % i, [128, KC, 512]) for i in range(2)]
        gTt = [fw.sb("e_gT%d" % i, [16, 512]) for i in range(2)]
        w1r = [fw.sb("e_w1%d" % i, [128, KC, DFF], BF16) for i in range(2)]
        w3r = [fw.sb("e_w3%d" % i, [128, KC, DFF], BF16) for i in range(2)]
        w2r = [fw.sb("e_w2%d" % i, [128, 4, D], BF16) for i in range(2)]
        gb = [fw.sb("e_gb%d" % i, [128, 512]) for i in range(2)]
        s1 = [fw.sb("e_s1%d" % i, [128, 512]) for i in range(2)]
        tq = [fw.sb("e_t%d" % i, [128, 512]) for i in range(2)]
        ab = [fw.sb("e_a%d" % i, [128, 4, 512], BF16) for i in range(2)]
        accs = [fw.sb("e_acc%d" % i, [128, KC, 512]) for i in range(2)]
        otok = [fw.sb("e_ot%d" % i, [128, D]) for i in range(2)]
        resv = self.resT[:].r("(kc p) t -> p kc t", p=128)
        l1v = self.lat1T[:].r("(kc p) t -> p kc t", p=128)
        h2v = self.h2T[:].r("(kc p) t -> p kc t", p=128)
        ei = 0
        pending = None
        loaded = False
        for gi, (tok0, TB) in enumerate(GROUPS):
            if gi == 0 and last:
                continue
            md = self.mod_for(gi)
            h2, l1, gT = h2t[gi % 2], l1t[gi % 2], gTt[gi % 2]
            acc = accs[gi % 2]

            def group_loads(gj):
                t0_, TB_ = GROUPS[gj]
                fw.dma("sp", h2t[gj % 2][:, :, 0:TB_], h2v[:, :, t0_:t0_ + TB_])
                fw.dma("sp", l1t[gj % 2][:, :, 0:TB_], l1v[:, :, t0_:t0_ + TB_])
                fw.dma("sp", gTt[gj % 2][:, 0:TB_], self.gateT[:, t0_:t0_ + TB_])
            if not loaded:
                group_loads(gi)
                loaded = True
            for e in range(NE):
                w1, w3, w2 = w1r[ei % 2], w3r[ei % 2], w2r[ei % 2]
                fw.dma("sp", w1[:], self.we1b[e].r("(kc p) f -> p kc f", p=128))
                fw.dma("pool", w3[:], self.we3b[e].r("(kc p) f -> p kc f", p=128))
                fw.dma("sp", w2[:], self.we2b[e].r("(fc p) d -> p fc d", p=128))
                pgb = self.PS[6]
                fw.mm(pgb[:, 0:TB], self.cst[0:16, C_SEL + e * 128:C_SEL + (e + 1) * 128], gT[:, 0:TB])
                g_ = gb[ei % 2]
                fw.cp(g_[:, 0:TB], pgb[:, 0:TB], eng="act")
                a_ = ab[ei % 2]
                for f in range(4):
                    p1 = self.PS[f % 2]
                    p3 = self.PS[2 + f % 2]
                    for kc in range(KC):
                        fw.mm(p1[:, 0:TB], w1[:, kc, f * 128:(f + 1) * 128], h2[:, kc, 0:TB], start=(kc == 0), stop=(kc == KC - 1))
                    for kc in range(KC):
                        fw.mm(p3[:, 0:TB], w3[:, kc, f * 128:(f + 1) * 128], h2[:, kc, 0:TB], start=(kc == 0), stop=(kc == KC - 1))
                    s_ = s1[f % 2]
                    fw.act(s_[:, 0:TB], p1[:, 0:TB], AF.Silu)
                    t_ = tq[f % 2]
                    fw.tt(t_[:, 0:TB], s_[:, 0:TB], p3[:, 0:TB], ALU.mult)
                    fw.tt(a_[:, f, 0:TB].k(f), t_[:, 0:TB], g_[:, 0:TB], ALU.mult, eng="pool")
                for Dc in range(KC):
                    po = self.PS[4 + Dc % 2]
                    for f in range(4):
                        fw.mm(po[:, 0:TB], w2[:, f, Dc * 128:(Dc + 1) * 128], a_[:, f, 0:TB], start=(f == 0), stop=(f == 3))
                    if e == 0:
                        fw.cp(acc[:, Dc, 0:TB].k(Dc), po[:, 0:TB], eng="act")
                    else:
                        fw.tt(acc[:, Dc, 0:TB].k(Dc), acc[:, Dc, 0:TB].k(Dc), po[:, 0:TB], ALU.add)
                ei += 1
                if e == 1:
                    if pending is not None:
                        pending()
                        pending = None
                    if gi + 1 < len(GROUPS):
                        group_loads(gi + 1)

            def epilogue(gi=gi, tok0=tok0, TB=TB, md=md, acc=acc, l1=l1):
                res2 = acc
                lat2 = acc
                g2c = 40
                for Dc in range(KC):
                    fw.ts(l1[:, Dc, 0:TB].k(Dc), l1[:, Dc, 0:TB].k(Dc), ALPHA, ALU.mult, eng="pool")
                    fw.stt(res2[:, Dc, 0:TB].k(Dc), acc[:, Dc, 0:TB].k(Dc), md[:, g2c + Dc:g2c + Dc + 1], l1[:, Dc, 0:TB].k(Dc),
                           ALU.mult, ALU.add)
                self.layernorm_fm(res2, TB, "ln2", lat2, W)
                if not last:
                    fw.dma("pool", resv[:, :, tok0:tok0 + TB].k(("g", gi)), lat2[:, :, 0:TB])
                if last and gi > 0:
                    for jt in range(TB // 128):
                        o_ = otok[jt % 2]
                        for half in range(2):
                            ps = self.PS[6 + half]
                            for q in range(4):
                                kc = half * 4 + q
                                fw.tr(ps[:, q * 128:(q + 1) * 128], lat2[:, kc, jt * 128:(jt + 1) * 128], ident)
                            fw.cp(o_[:, half * 512:(half + 1) * 512].k(half), ps[:, 0:512], eng=("dve" if half == 0 else "act"))
                        r0 = tok0 - LC + jt * 128
                        fw.dma("sp", self.out_d[r0:r0 + 128, :].k(("o", r0)), o_[:])
            pending = epilogue
        if pending is not None:
            pending()
        fw.release(m)


def _core_inputs(inp, b, shared, consts):
    cc = np.empty((128, KC, 2), np.float32)
    cc[:, :, 0] = np.asarray(inp["c"][b], np.float32).reshape(KC, 128).T
    cc[:, :, 1] = np.asarray(inp["c_ctx"], np.float32).reshape(KC, 128).T
    m = {"x": np.ascontiguousarray(inp["x"][b], np.float32),
         "ctx": np.ascontiguousarray(inp["ctx"][b], np.float32),
         "cc": cc.reshape(128, KC * 2)}
    m.update(consts)
    m.update(shared)
    return m


_PROG = {}


def kernel(**inputs):
    if "p" not in _PROG:
        _PROG["p"] = Prog()
    prog = _PROG["p"]
    consts = _constants()
    shared = _shared_inputs(inputs)
    n = 8
    in_maps = [_core_inputs(inputs, b, shared, consts) for b in range(n)]
    res = run_bass_kernel_spmd(prog.nc, in_maps, core_ids=list(range(n)))
    return np.stack([np.asarray(r["out"], np.float32) for r in res.results], 0)
```

```python
import math
import numpy as np
import ml_dtypes
import concourse.bass as bass
import concourse.mybir as mybir
from concourse.bass_utils import run_bass_kernel_spmd

F32 = mybir.dt.float32
BF16 = mybir.dt.bfloat16
AF = mybir.ActivationFunctionType
ALU = mybir.AluOpType
AX = mybir.AxisListType

D = 1024
L = 4096
LC = 256
NT = L + LC
KC = 8
DEPTH = 2
IN_COLS = 3592
UROWS = 29 * 128
NEG = -30000.0
ALPHA = (2 * DEPTH) ** 0.25
LN_EPS = 1e-5
RMS_EPS = 1e-6
NE = 16
DFF = 512
SEM_LIMIT = 30000


class Buf:
    def __init__(self, name, t):
        self.name = name
        self.t = t
        self.st = {}

    def __getitem__(self, idx):
        return View(self, self.t[idx], None)

    def k(self, key):
        return View(self, self.t[:], key)


class View:
    def __init__(self, buf, ap, key):
        self.buf = buf
        self.ap = ap
        self.key = key

    def __getitem__(self, idx):
        return View(self.buf, self.ap[idx], self.key)

    def k(self, key):
        return View(self.buf, self.ap, key)

    def r(self, pat, **kw):
        return View(self.buf, self.ap.rearrange(pat, **kw), self.key)

    def bc(self, shape):
        return View(self.buf, self.ap.to_broadcast(list(shape)), self.key)

    def un(self, axis):
        return View(self.buf, self.ap.unsqueeze(axis), self.key)


def _ap(x):
    return x.ap if isinstance(x, View) else x


class FW:
    NDMA = 8
    LIMIT = 10 ** 9
    POOL_TO = "dve"

    def __init__(self, nc):
        self.nc = nc
        self.eng = {"pe": nc.tensor, "dve": nc.vector, "act": nc.scalar,
                    "pool": nc.gpsimd, "sp": nc.sync}
        self.sem = {}
        self.cnt = {}
        self.seen = {}
        self.last = {}
        self._guards = []
        self._semguards = []
        self.bufs = []
        self._nsem = 0
        for e in self.eng:
            self.sem[e] = self._newsem("s_" + e)
            self.cnt[e] = 0
            self.seen[e] = {}
            self.last[e] = []
        self.dsem = {}
        self.dcnt = {}
        self.dlast = {}
        self.drr = {}
        for q in ("sp", "act", "pool"):
            self.dsem[q] = [self._newsem("d_%s%d" % (q, i)) for i in range(self.NDMA)]
            self.dcnt[q] = [0] * self.NDMA
            self.dlast[q] = [None] * self.NDMA
            self.drr[q] = 0
        self.n_inst = 0
        self.n_wait = 0

    def _newsem(self, name):
        self._nsem += 1
        g = self.nc.semaphore("%s_%d" % (name, self._nsem))
        s = g.__enter__()
        self._semguards.append(g)
        return s

    def sb(self, name, shape, dt=F32):
        self._nalloc = getattr(self, "_nalloc", 0) + 1
        g = self.nc.sbuf_tensor("sb%d_%s" % (self._nalloc, name), list(shape), dt)
        t = g.__enter__()
        self._guards.append(g)
        b = Buf(name, t)
        self.bufs.append(b)
        return b

    def ps(self, name, shape, dt=F32):
        g = self.nc.psum_tensor("pp_" + name, list(shape), dt)
        t = g.__enter__()
        self._guards.append(g)
        b = Buf(name, t)
        self.bufs.append(b)
        return b

    def dram(self, name, shape, dt=F32, kind="Internal"):
        t = self.nc.dram_tensor(name, list(shape), dt, kind=kind)
        b = Buf(name, t.ap())
        self.bufs.append(b)
        return b

    def mark(self):
        return len(self._guards)

    def release(self, mark):
        self.barrier()
        while len(self._guards) > mark:
            g = self._guards.pop()
            g.__exit__(None, None, None)

    def close(self):
        while self._guards:
            g = self._guards.pop()
            g.__exit__(None, None, None)
        while self._semguards:
            g = self._semguards.pop()
            g.__exit__(None, None, None)

    def _deps(self, reads, writes):
        deps = []
        for a in reads:
            b, k = a.buf, a.key
            for kk, s in b.st.items():
                if k is None or kk is None or kk == k:
                    if s[0] is not None:
                        deps.append(s[0])
        for a in writes:
            b, k = a.buf, a.key
            for kk, s in b.st.items():
                if k is None or kk is None or kk == k:
                    if s[0] is not None:
                        deps.append(s[0])
                    deps.extend(s[1])
        return deps

    def _record(self, reads, writes, tok):
        for a in reads:
            s = a.buf.st.setdefault(a.key, [None, []])
            s[1].append(tok)
            if len(s[1]) > 48:
                m = {}
                for (sm, v) in s[1]:
                    if id(sm) not in m or m[id(sm)][1] < v:
                        m[id(sm)] = (sm, v)
                s[1] = list(m.values())
        for a in writes:
            if a.key is None:
                a.buf.st = {None: [tok, []]}
            else:
                a.buf.st[a.key] = [tok, []]

    def _emit_waits(self, e, deps, same_engine=True):
        engine = self.eng[e]
        seen = self.seen[e]
        need = {}
        for (sm, v) in deps:
            if (not same_engine) and sm is self.sem[e]:
                continue
            if seen.get(id(sm), 0) >= v:
                continue
            if id(sm) not in need or need[id(sm)][1] < v:
                need[id(sm)] = (sm, v)
        for (sm, v) in need.values():
            engine.wait_ge(sm, v)
            seen[id(sm)] = v
            self.n_wait += 1

    def op(self, e, fn, reads=(), writes=(), same_engine=None):
        if e == "pool":
            e = FW.POOL_TO
        if same_engine is None:
            same_engine = (e != "pe")
        if self.n_inst >= FW.LIMIT:
            return None
        reads = [r for r in reads if isinstance(r, View)]
        writes = [w for w in writes if isinstance(w, View)]
        deps = self._deps(reads, writes)
        self._emit_waits(e, deps, same_engine)
        if self.cnt[e] >= SEM_LIMIT:
            self.sem[e] = self._newsem("s_" + e)
            self.cnt[e] = 0
        ins = fn(self.eng[e])
        self.cnt[e] += 1
        ins.then_inc(self.sem[e], 1)
        tok = (self.sem[e], self.cnt[e])
        self.last[e] = [tok] + [t for t in self.last[e] if t[0] is not self.sem[e]][:1]
        self._record(reads, writes, tok)
        self.n_inst += 1
        return tok

    def dma(self, q, out, in_, **kw):
        if self.n_inst >= FW.LIMIT:
            return None
        reads, writes = [in_], [out]
        deps = self._deps(reads, writes)
        slot = self.drr[q]
        self.drr[q] = (slot + 1) % self.NDMA
        if self.dlast[q][slot] is not None:
            deps.append(self.dlast[q][slot])
        self._emit_waits(q, deps, True)
        if self.dcnt[q][slot] >= SEM_LIMIT:
            self.dsem[q][slot] = self._newsem("d_%s%d" % (q, slot))
            self.dcnt[q][slot] = 0
        sm = self.dsem[q][slot]
        ins = self.eng[q].dma_start(out=out.ap, in_=in_.ap, **kw)
        self.dcnt[q][slot] += 16
        ins.then_inc(sm, 16)
        tok = (sm, self.dcnt[q][slot])
        self.dlast[q][slot] = tok
        self._record(reads, writes, tok)
        self.n_inst += 1
        return tok

    def all_tokens(self):
        toks = []
        for e in self.eng:
            toks.extend(self.last[e])
        for q in self.dlast:
            toks.extend(t for t in self.dlast[q] if t is not None)
        return toks

    def barrier(self):
        toks = self.all_tokens()
        for e in self.eng:
            self._emit_waits(e, toks, True)
        for b in self.bufs:
            b.st = {}

    def mm(self, out, lhsT, rhs, start=True, stop=True):
        return self.op("pe", lambda e: e.matmul(out.ap, lhsT=lhsT.ap, rhs=rhs.ap, start=start, stop=stop),
                       [lhsT, rhs], [out])

    def tr(self, out, in_, ident):
        return self.op("pe", lambda e: e.transpose(out.ap, in_.ap, ident.ap), [in_, ident], [out])

    def act(self, out, in_, func, bias=0.0, scale=1.0, accum=None, eng="act"):
        kw = {}
        if accum is not None:
            kw["accum_out"] = accum.ap
        return self.op("act", lambda e: e.activation(out=out.ap, in_=in_.ap, func=func, bias=_ap(bias),
                                                     scale=_ap(scale), **kw),
                       [in_, bias, scale], [out, accum])

    def tt(self, out, a, b, op, eng="dve"):
        return self.op(eng, lambda e: e.tensor_tensor(out=out.ap, in0=a.ap, in1=b.ap, op=op), [a, b], [out])

    def ts(self, out, a, s1, op0, s2=None, op1=None, eng="dve", accum=None):
        kw = {}
        if op1 is not None:
            kw["op1"] = op1
        if accum is not None:
            kw["accum_out"] = accum.ap
        return self.op(eng, lambda e: e.tensor_scalar(out=out.ap, in0=a.ap, scalar1=_ap(s1), scalar2=_ap(s2),
                                                      op0=op0, **kw), [a, s1, s2], [out, accum])

    def stt(self, out, a, s, b, op0, op1, eng="dve"):
        return self.op("dve", lambda e: e.scalar_tensor_tensor(out=out.ap, in0=a.ap, scalar=_ap(s), in1=b.ap,
                                                             op0=op0, op1=op1), [a, s, b], [out])

    def cp(self, out, in_, eng="dve"):
        if eng == "act":
            return self.op("act", lambda e: e.copy(out=out.ap, in_=in_.ap), [in_], [out])
        return self.op(eng, lambda e: e.tensor_copy(out=out.ap, in_=in_.ap), [in_], [out])

    def memset(self, out, val, eng="pool"):
        return self.op("dve" if eng == "pool" else eng, lambda e: e.memset(out.ap, val), [], [out])

    def red(self, out, in_, op, axis=AX.X, eng="dve"):
        return self.op(eng, lambda e: e.tensor_reduce(out=out.ap, in_=in_.ap, axis=axis, op=op), [in_], [out])

    def scan(self, out, d0, d1, init, op0, op1):
        return self.op("dve", lambda e: e.tensor_tensor_scan(out=out.ap, data0=d0.ap, data1=d1.ap,
                                                             initial=_ap(init), op0=op0, op1=op1),
                       [d0, d1, init], [out])


C_IDENT, C_ONES, C_TRIF, C_TRIB, C_NEGF, C_NEGB, C_MIF, C_MIB, C_SCAN = [i * 128 for i in range(9)]
C_CMASK = 1152
C_SEL = 1160
C_NEGTL = C_SEL + 16 * 128
C_NEGTC = C_NEGTL + 32
C_CMASKR = C_NEGTC + 8
C_SCAN16 = C_CMASKR + 8
NCST = C_SCAN16 + 16

PP = {}
_off = 0
for _n, _w in [("hycw", 18), ("hycb", 6), ("sccw", 6), ("sdcw", 12), ("sdcb", 4), ("hybias", 2), ("lbl", 8),
               ("hgng", 1), ("bgate", 32), ("ln1g", 8), ("ln1b", 8), ("ln2g", 8), ("ln2b", 8), ("bada", 48),
               ("hyb1", 1), ("hyf1", 1), ("hyb2", 1), ("hyf2", 1), ("lbl64", 16)]:
    PP[_n] = _off
    _off += _w
NPP = _off
RB = {}
_off = 0
for _n, _w in [("hydecay", 256), ("ssdng", 256), ("dtbias", 8), ("alog", 8), ("ssdd", 4), ("brouter", 16)]:
    RB[_n] = _off
    _off += _w
NRB = _off

_CONST_CACHE = {}


def _sincos_1d(pos, dim):
    omega = 1.0 / (10000.0 ** (np.arange(dim // 2, dtype=np.float32) / np.float32(dim // 2)))
    ang = pos.astype(np.float32)[:, None] * omega[None].astype(np.float32)
    return np.concatenate([np.sin(ang), np.cos(ang)], -1).astype(np.float32)


def _hy_feat(Lx):
    t = np.linspace(0.0, 1.0, Lx, dtype=np.float32)[:, None]
    bands = np.linspace(1e-4, 15, 16, dtype=np.float32)
    ang = (np.float32(2.0 * math.pi / Lx)) * np.arange(Lx, dtype=np.float32)[:, None] * bands[None]
    feat = np.concatenate([t, np.cos(ang), -np.sin(ang)], -1).astype(np.float32)
    return feat, t[:, 0]


def _dft_tables(Lx, TB):
    N = 2 * Lx
    nT = Lx // 128
    t = np.arange(Lx, dtype=np.int64)
    k = np.arange(Lx, dtype=np.int64)
    m = ((2 * k[None, :] + 1) * t[:, None]) % (2 * N)
    ang = m.astype(np.float64) * (math.pi / N)
    tabs = [np.cos(ang), np.sin(ang)]
    tf = np.empty((2, nT, 128, nT, 128), dtype=ml_dtypes.bfloat16)
    ti = np.empty((2, Lx // TB, 128, nT, TB), dtype=ml_dtypes.bfloat16)
    for j, T in enumerate(tabs):
        T4 = T.reshape(nT, 128, nT, 128)
        tf[j] = T4.transpose(2, 1, 0, 3).astype(ml_dtypes.bfloat16)
        Tt = T.T.reshape(nT, 128, Lx // TB, TB)
        ti[j] = Tt.transpose(2, 1, 0, 3).astype(ml_dtypes.bfloat16)
    return tf.reshape(2, nT, 128, nT * 128), ti.reshape(2, Lx // TB, 128, nT * TB)


def _constants():
    if _CONST_CACHE:
        return _CONST_CACHE
    cst = np.zeros((128, NCST), np.float32)
    r = np.arange(128)
    cst[:, C_IDENT:C_IDENT + 128] = np.eye(128)
    cst[:, C_ONES:C_ONES + 128] = 1.0
    cst[:, C_TRIF:C_TRIF + 128] = (r[:, None] <= r[None, :])
    cst[:, C_TRIB:C_TRIB + 128] = (r[:, None] >= r[None, :])
    cst[:, C_NEGF:C_NEGF + 128] = np.where(r[:, None] > r[None, :], NEG, 0.0)
    cst[:, C_NEGB:C_NEGB + 128] = np.where(r[:, None] < r[None, :], NEG, 0.0)
    same = (r[:, None] // 16) == (r[None, :] // 16)
    cst[:, C_MIF:C_MIF + 128] = same & (r[:, None] <= r[None, :])
    cst[:, C_MIB:C_MIB + 128] = same & (r[:, None] >= r[None, :])
    cst[:, C_SCAN:C_SCAN + 128] = ((r % 16) != 0)[None, :]
    cst[:, C_CMASK:C_CMASK + 8] = (r[:, None] // 16) == np.arange(8)[None, :]
    cst[:, C_CMASKR:C_CMASKR + 8] = (r[:, None] // 16) == (7 - np.arange(8))[None, :]
    cst[:, C_SCAN16:C_SCAN16 + 16] = (np.arange(16) != 0)[None, :]
    sel = np.zeros((128, 16, 128), np.float32)
    for e in range(16):
        sel[e, e, :] = 1.0
    cst[:, C_SEL:C_SEL + 2048] = sel.reshape(128, 2048)
    featL, tL = _hy_feat(L)
    featC, tC = _hy_feat(LC)
    cst[:, C_NEGTL:C_NEGTL + 32] = -tL.reshape(32, 128).T
    cst[:, C_NEGTC:C_NEGTC + 2] = -tC.reshape(2, 128).T
    feat = np.concatenate([featC.T, featL.T], 1).astype(np.float32)
    rows = L // 64
    row = np.repeat(np.arange(rows), 64)
    col = np.tile(np.arange(64), rows)
    pos = np.concatenate([_sincos_1d(row, D // 2), _sincos_1d(col, D // 2)], -1).astype(np.float32)
    tfl, til = _dft_tables(L, 512)
    tfc, tic = _dft_tables(LC, 256)
    _CONST_CACHE.update(cst=cst, feat=np.ascontiguousarray(feat), pos=pos, tfl=tfl, til=til, tfc=tfc, tic=tic)
    return _CONST_CACHE


def _chunks(v, n):
    return np.ascontiguousarray(np.asarray(v, np.float32).reshape(n, 128).T)


def _shared_inputs(inp):
    pp = np.zeros((DEPTH, 128, NPP), np.float32)
    rb = np.zeros((DEPTH, 128, NRB), np.float32)
    for l in range(DEPTH):
        def put(name, arr):
            arr = np.asarray(arr, np.float32)
            pp[l, :arr.shape[0], PP[name]:PP[name] + arr.shape[1]] = arr
        cw = inp["hy_conv_w"][l]
        put("hycw", cw.reshape(3, 6, 128).transpose(2, 1, 0).reshape(128, 18))
        put("hycb", _chunks(inp["hy_conv_b"][l], 6))
        put("sccw", inp["sc_conv_w"][l].reshape(3, 2, 128).transpose(2, 1, 0).reshape(128, 6))
        put("sdcw", inp["ssd_conv_w"][l].reshape(3, 4, 128).transpose(2, 1, 0).reshape(128, 12))
        put("sdcb", _chunks(inp["ssd_conv_b"][l], 4))
        put("hybias", _chunks(inp["hy_bias"][l], 2))
        lbl = inp["hg_lb_logits"]
        put("lbl", lbl.reshape(2, DEPTH, 2, 128).transpose(3, 0, 1, 2).reshape(128, 8))
        put("lbl64", lbl.reshape(2, DEPTH, 4, 64).transpose(3, 0, 1, 2).reshape(64, 16))
        put("hgng", np.asarray(inp["hg_norm_g"][l]).reshape(64, 1))
        put("bgate", _chunks(inp["b_gate"][l], 32))
        put("ln1g", _chunks(inp["ln1_g"][l], 8))
        put("ln1b", _chunks(inp["ln1_b"][l], 8))
        put("ln2g", _chunks(inp["ln2_g"][l], 8))
        put("ln2b", _chunks(inp["ln2_b"][l], 8))
        put("bada", _chunks(inp["b_ada"][l], 48))
        put("hyb1", np.asarray(inp["hy_b1"][l]).reshape(64, 1))
        put("hyf1", np.asarray(inp["hy_freq1"][l]).reshape(64, 1))
        put("hyb2", np.asarray(inp["hy_b2"][l]).reshape(64, 1))
        put("hyf2", np.asarray(inp["hy_freq2"][l]).reshape(64, 1))

        def rput(name, row):
            row = np.asarray(row, np.float32).reshape(1, -1)
            rb[l, :, RB[name]:RB[name] + row.shape[1]] = np.broadcast_to(row, (128, row.shape[1]))
        rput("hydecay", inp["hy_decay"][l])
        rput("ssdng", inp["ssd_norm_g"][l])
        rput("dtbias", inp["ssd_dt_bias"][l])
        rput("alog", inp["ssd_a_log"][l])
        rput("ssdd", inp["ssd_d"][l])
        rput("brouter", inp["b_router"])
    sh = {"pp": pp, "rb": rb}
    for n in ("w_ada", "w_in", "hy_w1", "hy_w2", "hy_w3", "w_gate", "w_br", "w_o", "w_router",
              "w_e1", "w_e3", "w_e2"):
        sh[n] = np.ascontiguousarray(np.asarray(inp[n], np.float32))
    return sh


GROUPS = [(0, LC)] + [(LC + 512 * i, 512) for i in range(L // 512)]


class Prog:
    def __init__(self, dbg=(), stop_after=None, layers=DEPTH):
        self.dbg = set(dbg)
        self.stop_after = stop_after
        self.layers = layers
        nc = bass.Bass("TRN2", target_bir_lowering=False)
        self.nc = nc
        fw = FW(nc)
        self.fw = fw
        din = lambda name, shape, dt=F32: fw.dram(name, shape, dt, kind="ExternalInput")
        self.x_d = din("x", [L, D])
        self.ctx_d = din("ctx", [LC, D])
        self.cc_d = din("cc", [128, KC * 2])
        self.pos_d = din("pos", [L, D])
        self.cst_d = din("cst", [128, NCST])
        self.feat_d = din("feat", [33, NT])
        self.tfl_d = din("tfl", [2, 32, 128, 32 * 128], BF16)
        self.til_d = din("til", [2, 8, 128, 32 * 512], BF16)
        self.tfc_d = din("tfc", [2, 2, 128, 2 * 128], BF16)
        self.tic_d = din("tic", [2, 1, 128, 2 * 256], BF16)
        self.pp_d = din("pp", [DEPTH, 128, NPP])
        self.rb_d = din("rb", [DEPTH, 128, NRB])
        self.w_ada_d = din("w_ada", [DEPTH, D, 6 * D])
        self.w_in_d = din("w_in", [DEPTH, D, IN_COLS])
        self.hy_w1_d = din("hy_w1", [DEPTH, 33, 64])
        self.hy_w2_d = din("hy_w2", [DEPTH, 64, 64])
        self.hy_w3_d = din("hy_w3", [DEPTH, 64, 512])
        self.w_gate_d = din("w_gate", [DEPTH, D, 4 * D])
        self.w_br_d = din("w_br", [DEPTH, 4, 256, D])
        self.w_o_d = din("w_o", [DEPTH, D, D])
        self.w_router_d = din("w_router", [D, NE])
        self.w_e1_d = din("w_e1", [DEPTH, NE, D, DFF])
        self.w_e3_d = din("w_e3", [DEPTH, NE, D, DFF])
        self.w_e2_d = din("w_e2", [DEPTH, NE, DFF, D])
        self.out_d = fw.dram("out", [L, D], F32, kind="ExternalOutput")

        def scratch(name, shape, dt=F32):
            kind = "ExternalOutput" if name in self.dbg else "Internal"
            return fw.dram(name, shape, dt, kind=kind)
        self.resT = scratch("resT", [D, NT])
        self.uT = scratch("uT", [UROWS, NT])
        self.brT = scratch("brT", [D, NT], BF16)
        self.lat1T = scratch("lat1T", [D, NT])
        self.h2T = scratch("h2T", [D, NT], BF16)
        self.gateT = scratch("gateT", [NE, NT])
        self.hgo = scratch("hgo", [64, 4 * NT])
        self.hgo2 = scratch("hgo2", [64, 4 * NT])
        self.yf = scratch("yf", [NT, 256])
        self.yb = scratch("yb", [NT, 256])
        self.dtT = scratch("dtT", [NT, 8])
        self.wgc = scratch("wgc", [8, 128, KC * 4 * 128], BF16)
        self.modd = scratch("modd", [128, 96])
        self.we1b = scratch("we1b", [NE, D, DFF], BF16)
        self.we3b = scratch("we3b", [NE, D, DFF], BF16)
        self.we2b = scratch("we2b", [NE, DFF, D], BF16)

        self.cst = fw.sb("cst", [128, NCST])
        fw.dma("sp", self.cst[:], self.cst_d[:])
        self.PS = [fw.ps("ps%d" % i, [128, 512]) for i in range(8)]
        self.pp = fw.sb("pp", [128, NPP])
        self.rb = fw.sb("rb", [128, NRB])
        self.modL = fw.sb("modL", [128, 48])
        self.modC = fw.sb("modC", [128, 48])
        self.build()
        fw.barrier()
        fw.close()

    def c(self, off, w=128, rows=128):
        return self.cst[0:rows, off:off + w]

    def build(self):
        fw = self.fw
        self.phase_init()
        if self.stop_after == "init":
            return
        for l in range(self.layers):
            self.l = l
            fw.dma("sp", self.pp[:], self.pp_d[l])
            fw.dma("sp", self.rb[:], self.rb_d[l])
            self.phase_mod(l)
            if self.stop_after in ("mod", "mod%d" % l):
                return
            self.phase_p1(l)
            if self.stop_after in ("p1", "p1%d" % l):
                return
            self.phase_wcast(l)
            self.phase_sc(l)
            if self.stop_after in ("sc", "sc%d" % l):
                return
            self.phase_hyena(l, ctx=False)
            if l < DEPTH - 1:
                self.phase_hyena(l, ctx=True)
            if self.stop_after in ("hy", "hy%d" % l):
                return
            self.phase_hgrn(l)
            if self.stop_after in ("hg", "hg%d" % l):
                return
            self.phase_ssd(l)
            if self.stop_after in ("ssd", "ssd%d" % l):
                return
            self.phase_merge(l)
            if self.stop_after in ("merge", "merge%d" % l):
                return
            self.phase_moe(l)
            if self.stop_after == "moe%d" % l:
                return

    def phase_init(self):
        fw = self.fw
        m = fw.mark()
        xt = [fw.sb("xt%d" % i, [128, D]) for i in range(2)]
        pt = [fw.sb("pt%d" % i, [128, D]) for i in range(2)]
        st = [fw.sb("st%d" % i, [128, KC, 512]) for i in range(2)]
        ident = self.c(C_IDENT)
        resv = self.resT[:].r("(kc p) t -> p kc t", p=128)
        ti = 0
        for gi, (tok0, TB) in enumerate(GROUPS):
            sg = st[gi % 2]
            for j in range(TB // 128):
                a = xt[ti % 2]
                if gi == 0:
                    fw.dma("sp", a[:], self.ctx_d[j * 128:(j + 1) * 128, :])
                else:
                    r0 = tok0 - LC + j * 128
                    fw.dma("sp", a[:], self.x_d[r0:r0 + 128, :])
                    p = pt[ti % 2]
                    fw.dma("pool", p[:], self.pos_d[r0:r0 + 128, :])
                    fw.tt(a[:], a[:], p[:], ALU.add)
                for half in range(2):
                    ps = self.PS[(ti * 2 + half) % 4]
                    for q in range(4):
                        kc = half * 4 + q
                        fw.tr(ps[:, q * 128:(q + 1) * 128], a[:, kc * 128:(kc + 1) * 128], ident)
                    dst = sg[:, half * 4:(half + 1) * 4, j * 128:(j + 1) * 128]
                    src = ps[:].r("p (q t) -> p q t", q=4)
                    if half == 0:
                        fw.cp(dst, src, eng="dve")
                    else:
                        fw.cp(dst, src, eng="act")
                ti += 1
            fw.dma("sp", resv[:, :, tok0:tok0 + TB].k(("g", gi)), sg[:, :, 0:TB])
        fw.release(m)

    def phase_mod(self, l):
        fw = self.fw
        m = fw.mark()
        cs = fw.sb("cs", [128, KC * 2])
        fw.dma("sp", cs[:], self.cc_d[:])
        csil = fw.sb("csil", [128, KC * 2])
        fw.act(csil[:], cs[:], AF.Silu)
        wa = [fw.sb("wa%d" % i, [128, KC, 1024]) for i in range(2)]
        ps = self.PS[0]
        wv = self.w_ada_d[l].r("(kc p) n -> p kc n", p=128)
        for blk in range(6):
            w = wa[blk % 2]
            fw.dma("sp" if blk % 2 == 0 else "pool", w[:], wv[:, :, blk * 1024:(blk + 1) * 1024])
            for jj in range(8):
                j = blk * 8 + jj
                for kc in range(KC):
                    fw.mm(ps[:, j * 2:j * 2 + 2], w[:, kc, jj * 128:(jj + 1) * 128], csil[:, kc * 2:kc * 2 + 2],
                          start=(kc == 0), stop=(kc == KC - 1))
        psv = ps[:, 0:96].r("p (j t) -> p j t", t=2)
        bada = self.pp[:, PP["bada"]:PP["bada"] + 48]
        fw.tt(self.modL[:], psv[:, :, 0], bada, ALU.add)
        fw.tt(self.modC[:], psv[:, :, 1], bada, ALU.add)
        for mm_ in (self.modL, self.modC):
            for o in (8, 32):
                fw.ts(mm_[:, o:o + 8], mm_[:, o:o + 8], 1.0, ALU.add)
        if "modd" in self.dbg:
            fw.dma("sp", self.modd[:, 0:48], self.modL[:])
            fw.dma("sp", self.modd[:, 48:96], self.modC[:])
        fw.release(m)

    def mod_for(self, gi):
        return self.modC if gi == 0 else self.modL

    def load_cast(self, dst, src_view, stage, n, i):
        fw = self.fw
        s = stage[i % len(stage)]
        fw.dma("sp" if i % 2 == 0 else "pool", s[:, 0:n], src_view)
        eng = ("dve", "act", "pool")[i % 3]
        fw.cp(dst, s[:, 0:n], eng=eng)

    def phase_p1(self, l):
        fw = self.fw
        m = fw.mark()
        wb = fw.sb("winb", [128, KC, UROWS], BF16)
        stage = [fw.sb("wst%d" % i, [128, IN_COLS]) for i in range(2)]
        fw.memset(wb[:, :, IN_COLS:UROWS], 0.0)
        wv = self.w_in_d[l].r("(kc p) n -> p kc n", p=128)
        for kc in range(KC):
            self.load_cast(wb[:, kc, 0:IN_COLS].k(kc), wv[:, kc, :], stage, IN_COLS, kc)
        rt = [fw.sb("rt%d" % i, [128, KC, 512]) for i in range(2)]
        ht = [fw.sb("ht%d" % i, [128, KC, 512], BF16) for i in range(2)]
        og = [fw.sb("og%d" % i, [128, 4, 512]) for i in range(3)]
        dts = [fw.sb("dts%d" % i, [128, 8]) for i in range(2)]
        resv = self.resT[:].r("(kc p) t -> p kc t", p=128)
        uv = self.uT[:].r("(c p) t -> p c t", p=128)
        oi = 0
        for gi, (tok0, TB) in enumerate(GROUPS):
            r = rt[gi % 2]
            h = ht[gi % 2]
            md = self.mod_for(gi)
            fw.dma("sp", r[:, :, 0:TB], resv[:, :, tok0:tok0 + TB])
            for kc in range(KC):
                fw.act(h[:, kc, 0:TB].k(kc), r[:, kc, 0:TB], AF.Identity, bias=md[:, kc:kc + 1], scale=md[:, 8 + kc:9 + kc])
            for c0 in range(0, 29, 4):
                nchunk = min(4, 29 - c0)
                o = og[oi % 3]
                for cj in range(nchunk):
                    cidx = c0 + cj
                    ps = self.PS[cidx % 4]
                    for kc in range(KC):
                        fw.mm(ps[:, 0:TB], wb[:, kc, cidx * 128:(cidx + 1) * 128], h[:, kc, 0:TB],
                              start=(kc == 0), stop=(kc == KC - 1))
                    fw.cp(o[:, cj, 0:TB].k(cj), ps[:, 0:TB], eng=("dve" if cidx % 2 == 0 else "act"))
                fw.dma("sp" if oi % 2 == 0 else "pool", uv[:, c0:c0 + nchunk, tok0:tok0 + TB].k(("g", gi, c0)),
                       o[:, 0:nchunk, 0:TB])
                oi += 1
            for jt in range(TB // 128):
                ps = self.PS[4 + jt % 2]
                for kc in range(KC):
                    fw.mm(ps[:, 0:8], h[:, kc, jt * 128:(jt + 1) * 128], wb[:, kc, 3584:3592],
                          start=(kc == 0), stop=(kc == KC - 1))
                dq = dts[jt % 2]
                fw.cp(dq[:], ps[:, 0:8], eng="act")
                fw.dma("pool", self.dtT[tok0 + jt * 128:tok0 + (jt + 1) * 128, :].k(("dt", tok0, jt)), dq[:])
        fw.release(m)

    def uv(self):
        return self.uT[:].r("(c p) t -> p c t", p=128)

    def brv(self):
        return self.brT[:].r("(c p) t -> p c t", p=128)

    def load_halo(self, dst, c0, nch, tok0, TB, q="sp"):
        fw = self.fw
        s0, s1 = (0, LC) if tok0 < LC else (LC, NT)
        lo = tok0 - 1
        hi = tok0 + TB + 1
        d0 = 0
        if lo < s0:
            fw.memset(dst[:, 0:nch, 0:1], 0.0)
            lo += 1
            d0 = 1
        if hi > s1:
            fw.memset(dst[:, 0:nch, TB + 1:TB + 2], 0.0)
            hi -= 1
        fw.dma(q, dst[:, 0:nch, d0:d0 + (hi - lo)], self.uv()[:, c0:c0 + nch, lo:hi])

    def conv3(self, out, src, wcol, TB, bias=None, eng="dve"):
        fw = self.fw
        w = lambda k: self.pp[:, wcol + k:wcol + k + 1]
        if bias is not None:
            fw.ts(out, src[:, 0:TB], w(0), ALU.mult, bias, ALU.add, eng=eng)
        else:
            fw.ts(out, src[:, 0:TB], w(0), ALU.mult, eng=eng)
        fw.stt(out, src[:, 1:TB + 1], w(1), out, ALU.mult, ALU.add)
        fw.stt(out, src[:, 2:TB + 2], w(2), out, ALU.mult, ALU.add)

    def phase_sc(self, l):
        fw = self.fw
        m = fw.mark()
        tin = [fw.sb("sct%d" % i, [128, 6, 514]) for i in range(2)]
        mm_ = [fw.sb("scm%d" % i, [128, 2, 514]) for i in range(2)]
        acc = [fw.sb("sca%d" % i, [128, 2, 512]) for i in range(2)]
        ob = [fw.sb("sco%d" % i, [128, 2, 512], BF16) for i in range(2)]
        for gi, (tok0, TB) in enumerate(GROUPS):
            if gi == 0 and l == DEPTH - 1:
                continue
            t = tin[gi % 2]
            self.load_halo(t, 6, 6, tok0, TB)
            mv = mm_[gi % 2]
            fw.tt(mv[:, :, 0:TB + 2], t[:, 2:4, 0:TB + 2], t[:, 4:6, 0:TB + 2], ALU.mult)
            a = acc[gi % 2]
            o = ob[gi % 2]
            for cc in range(2):
                self.conv3(a[:, cc, 0:TB], mv[:, cc, :], PP["sccw"] + cc * 3, TB, eng=("dve" if cc == 0 else "pool"))
                fw.tt(o[:, cc, 0:TB], a[:, cc, 0:TB], t[:, cc, 1:TB + 1], ALU.mult, eng=("dve" if cc == 0 else "pool"))
            fw.dma("pool", self.brv()[:, 2:4, tok0:tok0 + TB].k(("sc", gi)), o[:, :, 0:TB])
        fw.release(m)

    def phase_hyena(self, l, ctx):
        fw = self.fw
        m = fw.mark()
        Lx = LC if ctx else L
        base = 0 if ctx else LC
        TB = 256 if ctx else 512
        nT = Lx // 128
        nG = Lx // TB
        tpb = TB // 128
        tf_d = self.tfc_d if ctx else self.tfl_d
        ti_d = self.tic_d if ctx else self.til_d
        negt = self.c(C_NEGTC, 2) if ctx else self.c(C_NEGTL, 32)
        ident = self.c(C_IDENT)
        ones = self.c(C_ONES)
        AU = fw.sb("AU", [128, nT, 768], BF16)
        ZB = fw.sb("ZB", [128, nT, 512], BF16)
        X0 = fw.sb("X0", [128, 2, Lx], BF16)
        WB = fw.sb("WB", [128, 2, Lx], BF16)
        rn = fw.sb("rn", [128, 256])
        m2 = fw.mark()
        w1 = fw.sb("hw1", [33, 64])
        w2 = fw.sb("hw2", [64, 64])
        w3 = fw.sb("hw3", [64, 512])
        fw.dma("sp", w1[:], self.hy_w1_d[l])
        fw.dma("sp", w2[:], self.hy_w2_d[l])
        fw.dma("sp", w3[:], self.hy_w3_d[l])
        bf = fw.sb("hbf", [64, 2])
        ppc = lambda n: self.pp[0:64, PP[n]:PP[n] + 1]
        fw.tt(bf[:, 0:1], ppc("hyb1"), ppc("hyf1"), ALU.mult)
        fw.tt(bf[:, 1:2], ppc("hyb2"), ppc("hyf2"), ALU.mult)
        h1 = fw.sb("hh1", [64, Lx])
        h2 = fw.sb("hh2", [64, Lx])
        ft = [fw.sb("hft%d" % i, [33, 512]) for i in range(2)]
        tmp = [fw.sb("htmp%d" % i, [64, 512]) for i in range(2)]
        TF = min(512, Lx)
        tmpk = [fw.sb("htmpk%d" % i, [64, 512]) for i in range(2)]
        MAGIC = 12582912.0
        for layer in range(2):
            for g in range(Lx // TF):
                ps = self.PS[g % 2]
                if layer == 0:
                    f = ft[g % 2]
                    fw.dma("sp", f[:, 0:TF], self.feat_d[:, base + g * TF:base + (g + 1) * TF])
                    fw.mm(ps[0:64, 0:TF], w1[:, :], f[:, 0:TF])
                    fq, bq, dst = ppc("hyf1"), bf[:, 0:1], h1
                else:
                    fw.mm(ps[0:64, 0:TF], w2[:, :], h1[:, g * TF:(g + 1) * TF])
                    fq, bq, dst = ppc("hyf2"), bf[:, 1:2], h2
                t = tmp[g % 2]
                fw.ts(t[:, 0:TF], ps[0:64, 0:TF], fq, ALU.mult, bq, ALU.add)
                kq = tmpk[g % 2]
                fw.ts(kq[:, 0:TF], t[:, 0:TF], 1.0 / (2.0 * math.pi), ALU.mult, MAGIC, ALU.add)
                fw.ts(kq[:, 0:TF], kq[:, 0:TF], -MAGIC, ALU.add)
                fw.stt(t[:, 0:TF], kq[:, 0:TF], -2.0 * math.pi, t[:, 0:TF], ALU.mult, ALU.add)
                fw.ts(t[:, 0:TF], t[:, 0:TF], -3.141592, ALU.max, 3.141592, ALU.min)
                fw.act(dst[:, g * TF:(g + 1) * TF].k(g), t[:, 0:TF], AF.Sin)
        adec = fw.sb("adec", [128, 256])
        dv = self.rb[:, RB["hydecay"]:RB["hydecay"] + 256]
        fw.stt(adec[:], dv, -1.0, dv, ALU.mult, ALU.max)
        win = [fw.sb("hwin%d" % i, [128, 256]) for i in range(2)]
        hf = [fw.sb("hhf%d" % i, [128, 256]) for i in range(2)]
        hb = [fw.sb("hhb%d" % i, [128, 256]) for i in range(2)]
        ab = [fw.sb("hab%d" % i, [128, 256]) for i in range(2)]
        a2 = [fw.sb("hab2%d" % i, [128, 256]) for i in range(2)]
        asum = self.PS[7]
        for i in range(nT):
            ps3 = self.PS[2 + i % 2]
            fw.mm(ps3[:, 0:512], h2[:, i * 128:(i + 1) * 128], w3[:, :])
            w_ = win[i % 2]
            fw.act(w_[:], adec[:], AF.Exp, scale=negt[:, i:i + 1])
            f_, b_, a_ = hf[i % 2], hb[i % 2], ab[i % 2]
            fw.tt(f_[:], ps3[:, 0:256], w_[:], ALU.mult)
            fw.tt(b_[:], ps3[:, 256:512], w_[:], ALU.mult)
            if i == 0:
                fw.memset(b_[0:1, :], 0.0, eng="dve")
            fw.tt(AU[:, i, 0:256].k(("f", i)), f_[:], b_[:], ALU.add, eng="pool")
            fw.tt(AU[:, i, 512:768].k(("f", i)), b_[:], f_[:], ALU.subtract, eng="pool")
            fw.act(a_[:], f_[:], AF.Abs)
            fw.act(a2[i % 2][:], b_[:], AF.Abs)
            fw.tt(a_[:], a_[:], a2[i % 2][:], ALU.add)
            fw.mm(asum[:, 0:256], ones, a_[:], start=(i == 0), stop=(i == nT - 1))
        fw.op("dve", lambda e: e.reciprocal(out=rn[:].ap, in_=asum[:, 0:256].ap), [asum[:, 0:256]], [rn[:]])
        fw.release(m2)
        m2 = fw.mark()
        tin = [fw.sb("hyt%d" % i, [128, 6, 514]) for i in range(2)]
        cv = [fw.sb("hyc%d" % i, [128, 6, 512]) for i in range(2)]
        wv = [fw.sb("hyw%d" % i, [128, 2, 512]) for i in range(2)]
        for g in range(nG):
            tok0 = base + g * TB
            t = tin[g % 2]
            self.load_halo(t, 0, 6, tok0, TB)
            c_ = cv[g % 2]
            for j in range(6):
                self.conv3(c_[:, j, 0:TB], t[:, j, :], PP["hycw"] + 3 * j, TB,
                           bias=self.pp[:, PP["hycb"] + j:PP["hycb"] + j + 1], eng=("dve" if j % 2 == 0 else "pool"))
            fw.cp(X0[:, :, g * TB:(g + 1) * TB].k(g), c_[:, 0:2, 0:TB], eng="act")
            w_ = wv[g % 2]
            fw.tt(w_[:, :, 0:TB], c_[:, 2:4, 0:TB], c_[:, 4:6, 0:TB], ALU.mult)
            for cc in range(2):
                fw.act(WB[:, cc, g * TB:(g + 1) * TB].k((g, cc)), w_[:, cc, 0:TB], AF.Copy,
                       scale=self.pp[:, PP["hybias"] + cc:PP["hybias"] + cc + 1])
            for jp in range(tpb // 2):
                ps = self.PS[(g * 2 + jp) % 4]
                for jj in range(2):
                    jt = jp * 2 + jj
                    for cc in range(2):
                        fw.tr(ps[:, (jj * 2 + cc) * 128:(jj * 2 + cc + 1) * 128], w_[:, cc, jt * 128:(jt + 1) * 128], ident)
                i0 = g * tpb + jp * 2
                fw.cp(AU[:, i0:i0 + 2, 256:512].k(("u", i0)), ps[:].r("p (j c) -> p j c", j=2),
                      eng=("dve" if jp % 2 == 0 else "act"))
        fw.release(m2)
        m2 = fw.mark()
        tfr = [fw.sb("tfr%d" % i, [128, nT * 128], BF16) for i in range(4)]
        gcn = [fw.sb("gcn%d" % i, [128, 256]) for i in range(2)]
        gsn = [fw.sb("gsn%d" % i, [128, 256]) for i in range(2)]
        tq = [fw.sb("tq%d" % i, [128, 256]) for i in range(4)]
        for kc in range(nT):
            tabs = (tfr[(kc % 2) * 2], tfr[(kc % 2) * 2 + 1])
            fw.dma("sp", tabs[0][:], tf_d[0, kc])
            fw.dma("pool", tabs[1][:], tf_d[1, kc])
            pc = self.PS[(kc % 2) * 2]
            pS = self.PS[(kc % 2) * 2 + 1]
            for i in range(nT):
                fw.mm(pc[:, 0:512], tabs[0][:, i * 128:(i + 1) * 128], AU[:, i, 0:512], start=(i == 0), stop=(i == nT - 1))
            for i in range(nT):
                fw.mm(pS[:, 0:512], tabs[1][:, i * 128:(i + 1) * 128], AU[:, i, 256:768], start=(i == 0), stop=(i == nT - 1))
            gc_, gs_ = gcn[kc % 2], gsn[kc % 2]
            fw.tt(gc_[:], pc[:, 0:256], rn[:], ALU.mult)
            fw.tt(gs_[:], pS[:, 256:512], rn[:], ALU.mult)
            t1, t2, t3, t4 = tq
            fw.tt(t1[:], pc[:, 256:512], gc_[:], ALU.mult)
            fw.tt(t2[:], pS[:, 0:256], gs_[:], ALU.mult)
            fw.tt(ZB[:, kc, 0:256].k(kc), t1[:], t2[:], ALU.add, eng="pool")
            fw.tt(t3[:], pS[:, 0:256], gc_[:], ALU.mult)
            fw.tt(t4[:], pc[:, 256:512], gs_[:], ALU.mult)
            fw.tt(ZB[:, kc, 256:512].k(kc), t3[:], t4[:], ALU.subtract, eng="pool")
        fw.release(m2)
        m2 = fw.mark()
        KP = min(8, nT)
        tir = [fw.sb("tir%d" % i, [128, KP * TB], BF16) for i in range(4)]
        yt = [fw.sb("hyy%d" % i, [128, 512]) for i in range(2)]
        ob = [fw.sb("hyo%d" % i, [128, 2, 512], BF16) for i in range(2)]
        npiece = nT // KP
        ri = 0
        for tg in range(nG):
            tok0 = base + tg * TB
            pa = [self.PS[4 + (tg % 2) * 2], self.PS[5 + (tg % 2) * 2]]
            for piece in range(npiece):
                for j in range(2):
                    tab = tir[ri % 4]
                    fw.dma("sp" if ri % 2 == 0 else "pool", tab[:], ti_d[j, tg][:, piece * KP * TB:(piece + 1) * KP * TB])
                    ri += 1
                    for kk in range(KP):
                        kc = piece * KP + kk
                        first = (piece == 0 and j == 0 and kk == 0)
                        last = (piece == npiece - 1 and j == 1 and kk == KP - 1)
                        for cc in range(2):
                            fw.mm(pa[cc][:, 0:TB], ZB[:, kc, j * 256 + cc * 128:j * 256 + (cc + 1) * 128],
                                  tab[:, kk * TB:(kk + 1) * TB], start=first, stop=last)
            o = ob[tg % 2]
            for cc in range(2):
                y = yt[cc]
                fw.stt(y[:, 0:TB], pa[cc][:, 0:TB], 1.0 / Lx, WB[:, cc, tg * TB:(tg + 1) * TB], ALU.mult, ALU.add)
                fw.tt(o[:, cc, 0:TB], y[:, 0:TB], X0[:, cc, tg * TB:(tg + 1) * TB], ALU.mult, eng="pool")
            fw.dma("pool", self.brv()[:, 0:2, tok0:tok0 + TB].k(("hy", tok0)), o[:, :, 0:TB])
        fw.release(m2)
        fw.release(m)

    def phase_hgrn_v1(self, l):
        fw = self.fw
        m = fw.mark()
        ident = self.c(C_IDENT)
        ones64 = self.cst[0:64, C_ONES:C_ONES + 64]
        scanm = self.c(C_SCAN)
        cmask = self.c(C_CMASK, 8)
        uv = self.uv()
        hgv = self.hgo[:].r("v (h t) -> v h t", h=4)
        gv = self.uT[2560:2816, :].r("(h v) t -> v h t", v=64)
        bro = self.brT[512:768, :].r("(h v) t -> v h t", v=64)
        epsb = fw.sb("epsb", [128, 1])
        fw.memset(epsb[:], RMS_EPS)
        lbv = fw.sb("lbv", [128, 4])
        oml = fw.sb("oml", [128, 4])
        if l == 0:
            fw.memset(lbv[:], 0.0)
            fw.memset(oml[:], 1.0)
        else:
            for d in range(2):
                for cc in range(2):
                    c1 = PP["lbl"] + (d * 2 + 1) * 2 + cc
                    c0 = PP["lbl"] + (d * 2 + 0) * 2 + cc
                    j = d * 2 + cc
                    fw.tt(lbv[:, j:j + 1], self.pp[:, c1:c1 + 1], self.pp[:, c0:c0 + 1], ALU.subtract)
            fw.act(lbv[:], lbv[:], AF.Exp, scale=-1.0)
            fw.ts(lbv[:], lbv[:], 1.0, ALU.add)
            fw.op("dve", lambda e: e.reciprocal(out=lbv[:].ap, in_=lbv[:].ap), [lbv[:]], [lbv[:]])
            fw.ts(oml[:], lbv[:], -1.0, ALU.mult, 1.0, ALU.add)
        S = [[fw.sb("hgS%d%d" % (cc, p), [128, 8, 64]) for p in range(2)] for cc in range(2)]
        R2 = 2
        qt = [fw.sb("hgq%d" % i, [128, 2, 128]) for i in range(R2)]
        ftl = [fw.sb("hgf%d" % i, [128, 2, 128]) for i in range(R2)]
        vt = [fw.sb("hgv%d" % i, [128, 2, 128]) for i in range(R2)]
        sg = fw.sb("hgsg", [128, 2, 128])
        gg = [fw.sb("hggg%d" % i, [128, 128]) for i in range(2)]
        lf = [fw.sb("hglf%d" % i, [128, 128]) for i in range(2)]
        kk = [fw.sb("hgkk%d" % i, [128, 128]) for i in range(2)]
        bp = [fw.sb("hgbp%d" % i, [128, 128]) for i in range(2)]
        bb = [fw.sb("hgbb%d" % i, [128, 128]) for i in range(2)]
        tm = [fw.sb("hgtm%d" % i, [128, 128]) for i in range(2)]
        qd = [fw.sb("hgqd%d" % i, [128, 128]) for i in range(2)]
        ki = [fw.sb("hgki%d" % i, [128, 128]) for i in range(2)]
        ke = [fw.sb("hgke%d" % i, [128, 128]) for i in range(2)]
        dec = [fw.sb("hgdec%d" % i, [128, 8]) for i in range(2)]
        ketok = [fw.sb("hgket%d" % i, [128, 128]) for i in range(2)]
        vtok = [fw.sb("hgvt%d" % i, [128, 128]) for i in range(2)]
        vexp = [fw.sb("hgvx%d" % i, [128, 8, 128]) for i in range(2)]
        atm = [fw.sb("hgatm%d" % i, [128, 128]) for i in range(4)]
        osb = [fw.sb("hgo%d" % i, [64, 512]) for i in range(2)]
        of = [fw.sb("hgof%d" % i, [64, 512]) for i in range(2)]
        gt = [fw.sb("hggt%d" % i, [64, 512]) for i in range(2)]
        sq = fw.sb("hgsq", [64, 512])
        rs = fw.sb("hgrs", [64, 512])
        sgt = fw.sb("hgsgt", [64, 512])
        obf = [fw.sb("hgob%d" % i, [64, 512], BF16) for i in range(2)]
        ng = self.pp[0:64, PP["hgng"]:PP["hgng"] + 1]
        for d in range(getattr(self, "hg_passes", 2)):
            order = list(range(34)) if d == 0 else [1, 0] + list(range(33, 1, -1))
            MI = self.c(C_MIF) if d == 0 else self.c(C_MIB)
            for cc in range(2):
                fw.memset(S[cc][0][:, 0, :], 0.0)
            for it, tile in enumerate(order):
                if it >= getattr(self, "hg_limit", 99):
                    break
                tok0 = tile * 128
                need_o = not (l == DEPTH - 1 and tile < 2)
                par = it % 2
                q_, f_, v_ = qt[it % R2], ftl[it % R2], vt[it % R2]
                fw.dma("sp", q_[:], uv[:, 12:14, tok0:tok0 + 128])
                fc = 14 if d == 0 else 16
                fw.dma("sp", f_[:], uv[:, fc:fc + 2, tok0:tok0 + 128])
                fw.dma("sp", v_[:], uv[:, 18:20, tok0:tok0 + 128])
                fw.act(sg[:], f_[:], AF.Exp, scale=-1.0)
                fw.ts(sg[:], sg[:], 1.0, ALU.add, eng="pool")
                fw.op("dve", lambda e: e.reciprocal(out=sg[:].ap, in_=sg[:].ap), [sg[:]], [sg[:]])
                pso = self.PS[6]
                pso2 = self.PS[7]
                for cc in range(2):
                    j = d * 2 + cc
                    g_, l_, k_, b_, bb_, t_ = gg[cc], lf[cc], kk[cc], bp[cc], bb[cc], tm[cc]
                    fw.ts(g_[:], sg[:, cc, :], oml[:, j:j + 1], ALU.mult, lbv[:, j:j + 1], ALU.add)
                    fw.act(l_[:], g_[:], AF.Ln)
                    fw.ts(k_[:], g_[:], -1.0, ALU.mult, 1.0, ALU.add, eng="pool")
                    fw.scan(b_[:], scanm, l_[:], 0.0, ALU.mult, ALU.add)
                    b3 = b_[:].r("p (n s) -> p n s", s=16)
                    tot = b3[:, :, 15:16]
                    if d == 0:
                        bcur = b_
                    else:
                        fw.tt(t_[:], l_[:], b_[:], ALU.subtract)
                        fw.tt(bb_[:].r("p (n s) -> p n s", s=16), t_[:].r("p (n s) -> p n s", s=16),
                              tot.bc([128, 8, 16]), ALU.add)
                        bcur = bb_
                    fw.act(dec[cc][:], b3[:, :, 15], AF.Exp)
                    fw.act(t_[:], bcur[:], AF.Exp)
                    fw.stt(qd[cc][:], q_[:, cc, :], 0.125, t_[:], ALU.mult, ALU.mult)
                    fw.act(t_[:], bcur[:], AF.Exp, scale=-1.0)
                    fw.tt(ki[cc][:], k_[:], t_[:], ALU.mult)
                    fw.tt(t_[:].r("p (n s) -> p n s", s=16), tot.bc([128, 8, 16]),
                          bcur[:].r("p (n s) -> p n s", s=16), ALU.subtract)
                    fw.act(t_[:], t_[:], AF.Exp)
                    fw.tt(ke[cc][:], k_[:], t_[:], ALU.mult, eng="pool")
                    if getattr(self, "hg_stage", 9) < 1:
                        continue
                    pst = self.PS[cc]
                    fw.tr(pst[:, 0:128], ke[cc][:], ident)
                    fw.tr(pst[:, 128:256], v_[:, cc, :], ident)
                    fw.cp(ketok[cc][:], pst[:, 0:128], eng="act")
                    fw.cp(vtok[cc][:], pst[:, 128:256], eng="act")
                    fw.tt(vexp[cc][:], vtok[cc][:].un(1).bc([128, 8, 128]), cmask.un(2).bc([128, 8, 128]), ALU.mult)
                    for h in range(2):
                        if getattr(self, "hg_stage", 9) < 2:
                            continue
                        hh = cc * 2 + h
                        rows = slice(h * 64, (h + 1) * 64)
                        if need_o:
                            pa = self.PS[2 + h]
                            fw.mm(pa[:, 0:128], ki[cc][rows, :], qd[cc][rows, :])
                            am = atm[hh]
                            fw.tt(am[:], pa[:, 0:128], MI, ALU.mult)
                            fw.mm(pso[0:64, hh * 128:(hh + 1) * 128].k(hh), vtok[cc][:, h * 64:(h + 1) * 64], am[:])
                        pkv = self.PS[4 + h]
                        fw.mm(pkv[:, 0:512], ketok[cc][:], vexp[cc][:, :, h * 64:(h + 1) * 64])
                        Scur = S[cc][par]
                        Snxt = S[cc][1 - par]
                        for jj in range(8 if getattr(self, "hg_stage", 9) >= 3 else 0):
                            n = jj if d == 0 else 7 - jj
                            if need_o:
                                fw.mm(pso2[0:64, hh * 128 + n * 16:hh * 128 + (n + 1) * 16].k(hh),
                                      Scur[rows, jj, :].k((h, jj)), qd[cc][rows, n * 16:(n + 1) * 16])
                            dst = Scur[rows, jj + 1, :].k((h, jj + 1)) if jj < 7 else Snxt[rows, 0, :].k((h, 0))
                            fw.stt(dst, Scur[rows, jj, :].k((h, jj)), dec[cc][rows, n:n + 1],
                                   pkv[rows, n * 64:(n + 1) * 64], ALU.mult, ALU.add)
                if not need_o or getattr(self, "hg_stage", 9) < 4:
                    continue
                o_ = osb[it % 2]
                fw.cp(o_[:], pso[0:64, :], eng="act")
                fw.tt(o_[:], o_[:], pso2[0:64, :], ALU.add)
                if d == 0:
                    fw.dma("pool", hgv[:, :, tok0:tok0 + 128].k(("t", tile)), o_[:].r("v (h t) -> v h t", h=4))
                    continue
                of_ = of[it % 2]
                g2 = gt[it % 2]
                fw.dma("pool", of_[:].r("v (h t) -> v h t", h=4), hgv[:, :, tok0:tok0 + 128].k(("t", tile)))
                fw.dma("pool", g2[:].r("v (h t) -> v h t", h=4), gv[:, :, tok0:tok0 + 128])
                fw.tt(o_[:], o_[:], of_[:], ALU.add)
                fw.tt(sq[:], o_[:], o_[:], ALU.mult, eng="pool")
                pss = self.PS[0]
                fw.mm(pss[0:64, 0:512], ones64, sq[:])
                fw.act(rs[:], pss[0:64, 0:512], AF.Ln, bias=epsb[0:64, :], scale=1.0 / 64.0)
                fw.act(rs[:], rs[:], AF.Exp, scale=-0.5)
                fw.tt(o_[:], o_[:], rs[:], ALU.mult)
                fw.act(sgt[:], g2[:], AF.Exp, scale=-1.0)
                fw.ts(sgt[:], sgt[:], 1.0, ALU.add, eng="pool")
                fw.op("dve", lambda e: e.reciprocal(out=sgt[:].ap, in_=sgt[:].ap), [sgt[:]], [sgt[:]])
                fw.tt(sgt[:], sgt[:], g2[:], ALU.mult, eng="pool")
                ob_ = obf[it % 2]
                fw.stt(ob_[:], o_[:], ng, sgt[:], ALU.mult, ALU.mult)
                fw.dma("pool", bro[:, :, tok0:tok0 + 128].k(("hg", tile)), ob_[:].r("v (h t) -> v h t", h=4))
            fw.barrier()
        fw.release(m)

    def phase_hgrn(self, l):
        fw = self.fw
        m = fw.mark()
        id64 = self.cst[0:64, C_IDENT:C_IDENT + 64]
        ones64 = self.cst[0:64, C_ONES:C_ONES + 64]
        MI = [self.c(C_MIF), self.c(C_MIB)]
        CM = [self.c(C_CMASK, 8), self.c(C_CMASKR, 8)]
        hgv = [self.hgo[:].r("v (h t) -> v h t", h=4), self.hgo2[:].r("v (h t) -> v h t", h=4)]
        uq = self.uT[1536:1792, :].r("(h d) t -> d h t", d=64)
        uf = [self.uT[1792:2048, :].r("(h d) t -> d h t", d=64), self.uT[2048:2304, :].r("(h d) t -> d h t", d=64)]
        ui = self.uT[2304:2560, :].r("(h d) t -> d h t", d=64)
        gv = self.uT[2560:2816, :].r("(h v) t -> v h t", v=64)
        bro = self.brT[512:768, :].r("(h v) t -> v h t", v=64)
        epsb = fw.sb("epsb", [64, 1])
        fw.memset(epsb[:], RMS_EPS)
        lbv = fw.sb("lbv", [64, 8])
        oml = fw.sb("oml", [64, 8])
        if l > 0:
            for d in range(2):
                c1 = PP["lbl64"] + (d * 2 + 1) * 4
                c0 = PP["lbl64"] + (d * 2 + 0) * 4
                fw.tt(lbv[:, d * 4:(d + 1) * 4], self.pp[0:64, c1:c1 + 4], self.pp[0:64, c0:c0 + 4], ALU.subtract)
            fw.act(lbv[:], lbv[:], AF.Exp, scale=-1.0)
            fw.ts(lbv[:], lbv[:], 1.0, ALU.add)
            fw.op("dve", lambda e: e.reciprocal(out=lbv[:].ap, in_=lbv[:].ap), [lbv[:]], [lbv[:]])
            fw.ts(oml[:], lbv[:], -1.0, ALU.mult, 1.0, ALU.add)
        SM = fw.sb("hg_sm", [64, 2048])
        fw.cp(SM[:].r("p (a b) -> p a b", b=16), self.cst[0:64, C_SCAN16:C_SCAN16 + 16].un(1).bc([64, 128, 16]))
        S = fw.sb("hg_S", [64, 9, 8, 64])
        fw.memset(S[:, 0, :, :], 0.0)
        F = 2048
        qin = fw.sb("hg_qin", [64, 4, 512])
        fin = fw.sb("hg_fin", [64, 4, 512])
        T = [fw.sb("hg_T%d" % i, [64, F]) for i in range(5)]
        qd32 = [fw.sb("hg_qd32%d" % d, [64, 4, 512]) for d in range(2)]
        qd16 = [fw.sb("hg_qd16%d" % d, [64, 4, 512], BF16) for d in range(2)]
        ki16 = [fw.sb("hg_ki16%d" % d, [64, 4, 512], BF16) for d in range(2)]
        ke32 = [fw.sb("hg_ke32%d" % d, [64, 4, 512]) for d in range(2)]
        vin = [fw.sb("hg_vin%d" % d, [64, 4, 512]) for d in range(2)]
        dec = [fw.sb("hg_dec%d" % d, [64, 4, 32]) for d in range(2)]
        ketok = [fw.sb("hg_ket%d" % d, [128, 256], BF16) for d in range(2)]
        vtok = [fw.sb("hg_vt%d" % d, [128, 256], BF16) for d in range(2)]
        vexp = [fw.sb("hg_vx%d" % d, [128, 8, 256], BF16) for d in range(2)]
        atm = [fw.sb("hg_atm%d" % d, [128, 4, 128], BF16) for d in range(2)]
        kvs = [fw.sb("hg_kvs%d" % d, [64, 8, 4, 64]) for d in range(2)]
        osb = [fw.sb("hg_o%d" % d, [64, 512]) for d in range(2)]
        ot = [fw.sb("hg_ot%d" % d, [64, 512]) for d in range(2)]
        CH = ["dve", "pool"]

        def prep(d, tok0, TBk):
            n = 4 * TBk
            nch = TBk // 16
            v3 = lambda b: b[:, 0:n].r("p (h t) -> p h t", h=4)
            c3 = lambda b: b[:, 0:n].r("p (c s) -> p c s", s=16)
            fw.dma("sp", qin[:, :, 0:TBk], uq[:, :, tok0:tok0 + TBk])
            fw.dma("sp", fin[:, :, 0:TBk], uf[d][:, :, tok0:tok0 + TBk])
            fw.dma("sp", vin[d][:, :, 0:TBk], ui[:, :, tok0:tok0 + TBk])
            sg, lf, kk, b_, t_ = T
            fw.act(v3(sg), fin[:, :, 0:TBk], AF.Exp, scale=-1.0)
            fw.ts(sg[:, 0:n], sg[:, 0:n], 1.0, ALU.add, eng="pool")
            fw.op("dve", lambda e: e.reciprocal(out=sg[:, 0:n].ap, in_=sg[:, 0:n].ap), [sg[:, 0:n]], [sg[:, 0:n]])
            if l > 0:
                for h in range(4):
                    j = d * 4 + h
                    fw.ts(sg[:, h * TBk:(h + 1) * TBk], sg[:, h * TBk:(h + 1) * TBk], oml[:, j:j + 1], ALU.mult,
                          lbv[:, j:j + 1], ALU.add)
            fw.act(lf[:, 0:n], sg[:, 0:n], AF.Ln)
            fw.ts(kk[:, 0:n], sg[:, 0:n], -1.0, ALU.mult, 1.0, ALU.add, eng="pool")
            fw.scan(b_[:, 0:n], SM[:, 0:n], lf[:, 0:n], 0.0, ALU.mult, ALU.add)
            tot = c3(b_)[:, :, 15:16]
            fw.act(dec[d][:, :, 0:nch], b_[:, 0:n].r("p (h c s) -> p h c s", h=4, s=16)[:, :, :, 15], AF.Exp)
            if d == 1:
                fw.tt(lf[:, 0:n], lf[:, 0:n], b_[:, 0:n], ALU.subtract, eng="pool")
                fw.tt(c3(lf), c3(lf), tot.bc([64, n // 16, 16]), ALU.add)
                bcur = lf
            else:
                bcur = b_
            fw.act(t_[:, 0:n], bcur[:, 0:n], AF.Exp)
            fw.stt(qd32[d][:, :, 0:TBk], qin[:, :, 0:TBk], 0.125, v3(t_), ALU.mult, ALU.mult)
            fw.cp(qd16[d][:, :, 0:TBk], qd32[d][:, :, 0:TBk], eng="pool")
            fw.act(t_[:, 0:n], bcur[:, 0:n], AF.Exp, scale=-1.0)
            fw.tt(ki16[d][:, :, 0:TBk], v3(kk), v3(t_), ALU.mult)
            fw.tt(c3(t_), tot.bc([64, n // 16, 16]), c3(bcur), ALU.subtract, eng="pool")
            fw.act(t_[:, 0:n], t_[:, 0:n], AF.Exp)
            fw.tt(ke32[d][:, :, 0:TBk], v3(kk), v3(t_), ALU.mult)

        def tile_pre(d, off, tile, need_o):
            pst = self.PS[d]
            for h in range(4):
                fw.tr(pst[:, h * 64:(h + 1) * 64], ke32[d][:, h, off:off + 128], id64)
                fw.tr(pst[:, 256 + h * 64:256 + (h + 1) * 64], vin[d][:, h, off:off + 128], id64)
            fw.cp(ketok[d][:], pst[:, 0:256], eng="act")
            fw.cp(vtok[d][:], pst[:, 256:512], eng="act")
            fw.tt(vexp[d][:], vtok[d][:].un(1).bc([128, 8, 256]), CM[d].un(2).bc([128, 8, 256]), ALU.mult,
                  eng=("dve" if d == 1 else "pool"))
            poi = self.PS[3 + d * 2]
            if need_o:
                pat = self.PS[2]
                for h in range(4):
                    fw.mm(pat[:, h * 128:(h + 1) * 128].k(h), ki16[d][:, h, off:off + 128], qd16[d][:, h, off:off + 128])
                fw.tt(atm[d][:], pat[:].r("p (h t) -> p h t", h=4), MI[d].un(1).bc([128, 4, 128]), ALU.mult)
                for h in range(4):
                    fw.mm(poi[0:64, h * 128:(h + 1) * 128].k(h), vtok[d][:, h * 64:(h + 1) * 64], atm[d][:, h, :])
            pkv = self.PS[7]
            for h in range(4):
                fw.mm(pkv[0:64, 0:512], ketok[d][:, h * 64:(h + 1) * 64], vexp[d][:, :, h * 64:(h + 1) * 64])
                fw.cp(kvs[d][:, :, h, :].k(h), pkv[0:64, 0:512].r("p (j v) -> p j v", j=8), eng="act")

        def chain_step(d, j, off, need_o):
            pox = self.PS[4 + d * 2]
            ce = CH[d]
            dsl = slice(d * 4, (d + 1) * 4)
            c0 = off // 16
            n = j if d == 0 else 7 - j
            if need_o:
                for h in range(4):
                    fw.mm(pox[0:64, h * 128 + n * 16:h * 128 + (n + 1) * 16].k(h),
                          S[:, j, d * 4 + h, :].k((d, j)), qd32[d][:, h, off + n * 16:off + (n + 1) * 16])
            dcb = dec[d][:, :, c0 + n:c0 + n + 1].bc([64, 4, 64])
            dst = S[:, j + 1, dsl, :].k((d, j + 1)) if j < 7 else S[:, 0, dsl, :].k((d, 0))
            fw.tt(S[:, 8, dsl, :].k((d, 8)) if j == 7 else dst, S[:, j, dsl, :].k((d, j)), dcb, ALU.mult, eng=ce)
            src = S[:, 8, dsl, :].k((d, 8)) if j == 7 else dst
            fw.tt(dst, src, kvs[d][:, j, :, :], ALU.add, eng=ce)

        def tile_post(d, tile, need_o):
            if not need_o:
                return
            tok0 = tile * 128
            poi = self.PS[3 + d * 2]
            pox = self.PS[4 + d * 2]
            o_ = osb[d]
            fw.cp(o_[:], poi[0:64, :], eng="act")
            fw.tt(o_[:], o_[:], pox[0:64, :], ALU.add, eng="dve")
            fw.dma("pool", hgv[d][:, :, tok0:tok0 + 128].k(("t", tile)), o_[:].r("v (h t) -> v h t", h=4))

        steps = [(0, 0, 256)] + [(LC + 512 * i, LC + 512 * (7 - i), 512) for i in range(8)]
        for si, (tf0, tb0, TBk) in enumerate(steps):
            prep(0, tf0, TBk)
            prep(1, tb0, TBk)
            nt = TBk // 128
            for i in range(nt):
                info = []
                for d in range(2):
                    off = i * 128 if d == 0 else (nt - 1 - i) * 128
                    t0 = (tf0 if d == 0 else tb0) + off
                    tile = t0 // 128
                    need_o = not (l == DEPTH - 1 and tile < 2)
                    info.append((off, tile, need_o))
                    tile_pre(d, off, tile, need_o)
                for j in range(8):
                    for d in range(2):
                        chain_step(d, j, info[d][0], info[d][2])
                for d in range(2):
                    tile_post(d, info[d][1], info[d][2])
        fw.barrier()
        A = T[0:3]
        obf3 = ki16[0]
        ng = self.pp[0:64, PP["hgng"]:PP["hgng"] + 1]
        for gi, (tok0, TB) in enumerate(GROUPS):
            if gi == 0 and l == DEPTH - 1:
                continue
            n = 4 * TB
            v3 = lambda b: b[:, 0:n].r("p (h t) -> p h t", h=4)
            o1, o2, g2 = qd32[0], qd32[1], ke32[0]
            fw.dma("sp", o1[:, :, 0:TB], hgv[0][:, :, tok0:tok0 + TB])
            fw.dma("sp", o2[:, :, 0:TB], hgv[1][:, :, tok0:tok0 + TB])
            fw.dma("pool", g2[:, :, 0:TB], gv[:, :, tok0:tok0 + TB])
            o_, sq_, sg_ = A
            fw.tt(v3(o_), o1[:, :, 0:TB], o2[:, :, 0:TB], ALU.add)
            fw.tt(sq_[:, 0:n], o_[:, 0:n], o_[:, 0:n], ALU.mult, eng="pool")
            for h in range(4):
                pss = self.PS[h % 2]
                fw.mm(pss[0:64, 0:TB], ones64, sq_[:, h * TB:(h + 1) * TB])
                fw.act(sq_[:, h * TB:(h + 1) * TB].k(h), pss[0:64, 0:TB], AF.Ln, bias=epsb[:], scale=1.0 / 64.0)
            fw.act(sq_[:, 0:n], sq_[:, 0:n], AF.Exp, scale=-0.5)
            fw.tt(o_[:, 0:n], o_[:, 0:n], sq_[:, 0:n], ALU.mult)
            fw.act(v3(sg_), g2[:, :, 0:TB], AF.Exp, scale=-1.0)
            fw.ts(sg_[:, 0:n], sg_[:, 0:n], 1.0, ALU.add, eng="pool")
            fw.op("dve", lambda e: e.reciprocal(out=sg_[:, 0:n].ap, in_=sg_[:, 0:n].ap), [sg_[:, 0:n]], [sg_[:, 0:n]])
            fw.tt(v3(sg_), v3(sg_), g2[:, :, 0:TB], ALU.mult, eng="pool")
            fw.stt(obf3[:, :, 0:TB], v3(o_), ng, v3(sg_), ALU.mult, ALU.mult)
            fw.dma("pool", bro[:, :, tok0:tok0 + TB].k(("hg", gi)), obf3[:, :, 0:TB])
        fw.release(m)

    def phase_ssd(self, l):
        fw = self.fw
        m = fw.mark()
        ident = self.c(C_IDENT)
        ones = self.c(C_ONES)
        uv = self.uv()
        brv = self.brv()
        dtb = self.rb[:, RB["dtbias"]:RB["dtbias"] + 8]
        dsk = self.rb[:, RB["ssdd"]:RB["ssdd"] + 4]
        ngb = self.rb[:, RB["ssdng"]:RB["ssdng"] + 256]
        onec = fw.sb("sd_one", [128, 1])
        fw.memset(onec[:], 1.0)
        epsb = fw.sb("sd_eps", [128, 1])
        fw.memset(epsb[:], RMS_EPS)
        nea = fw.sb("sd_nea", [128, 8])
        fw.act(nea[:], self.rb[:, RB["alog"]:RB["alog"] + 8], AF.Exp)
        fw.ts(nea[:], nea[:], -1.0, ALU.mult)
        yD = [self.yf, self.yb]
        identb = fw.sb("sd_identb", [128, 128], BF16)
        fw.cp(identb[:], ident)
        negb = [fw.sb("sd_negb%d" % d, [128, 128], BF16) for d in range(2)]
        fw.cp(negb[0][:], self.c(C_NEGF))
        fw.cp(negb[1][:], self.c(C_NEGB))
        B = []
        for d in range(2):
            b = {}
            b["ST"] = fw.sb("sd_ST%d" % d, [128, 4, 64])
            fw.memset(b["ST"][:], 0.0)
            b["xin"] = [fw.sb("sd_xin%d%d" % (d, i), [128, 4, 130]) for i in range(2)]
            b["dtr"] = [fw.sb("sd_dtr%d%d" % (d, i), [128, 8]) for i in range(2)]
            for nm, shp in (("xc", [128, 4, 128]), ("sg", [128, 4, 128]), ("xb", [128, 4, 128]), ("xs", [128, 256]),
                            ("bt", [128, 128]), ("dtt", [128, 8]), ("atok", [128, 8]), ("negcum", [128, 4]),
                            ("expcum", [128, 4]), ("totE", [128, 4]), ("edec", [128, 4]), ("cb", [128, 256]),
                            ("ydg", [128, 256]), ("y", [128, 256]), ("xsd", [128, 256])):
                b[nm] = fw.sb("sd_%s%d" % (nm, d), shp)
            b["xd"] = fw.sb("sd_xd%d" % d, [128, 4, 64], BF16)
            for nm in ("arow", "E"):
                b[nm] = [fw.sb("sd_%s%d%d" % (nm, d, i), [128, 128]) for i in range(2)]
            for nm in ("W", "bdec"):
                b[nm] = [fw.sb("sd_%s%d%d" % (nm, d, i), [128, 128], BF16) for i in range(2)]
            B.append(b)
        PX, PY = self.PS[0], self.PS[1]
        PT = [self.PS[2], self.PS[3]]
        PD = [self.PS[4], self.PS[5]]
        PYD = self.PS[6]
        PSS = self.PS[7]

        def tile_gen(d, it, tile):
            b = B[d]
            TRI = self.c(C_TRIF) if d == 0 else self.c(C_TRIB)
            NEGM = self.c(C_NEGF) if d == 0 else self.c(C_NEGB)
            j0 = d * 4
            tok0 = tile * 128
            need_o = not (l == DEPTH - 1 and tile < 2)
            xi = b["xin"][it % 2]
            self.load_halo(xi, 24, 4, tok0, 128)
            dr = b["dtr"][it % 2]
            fw.dma("sp", dr[:], self.dtT[tok0:tok0 + 128, :])
            xc, sg, xb = b["xc"], b["sg"], b["xb"]
            for j in range(4):
                self.conv3(xc[:, j, :], xi[:, j, :], PP["sdcw"] + 3 * j, 128,
                           bias=self.pp[:, PP["sdcb"] + j:PP["sdcb"] + j + 1])
                yield
            fw.act(sg[:], xc[:], AF.Exp, scale=-1.0)
            yield
            fw.ts(sg[:], sg[:], 1.0, ALU.add)
            fw.op("dve", lambda e: e.reciprocal(out=sg[:].ap, in_=sg[:].ap), [sg[:]], [sg[:]])
            fw.tt(xb[:], xc[:], sg[:], ALU.mult)
            yield
            pt = PT[d]
            for j in range(3):
                fw.tr(pt[:, j * 128:(j + 1) * 128], xb[:, j, :], ident)
            xs_, bt_, dtt, atok = b["xs"], b["bt"], b["dtt"], b["atok"]
            fw.cp(xs_[:], pt[:, 0:256], eng="act")
            fw.cp(bt_[:], pt[:, 256:384], eng="act")
            yield
            fw.tt(dtt[:], dr[:], dtb, ALU.add)
            fw.act(dtt[:], dtt[:], AF.Exp)
            fw.act(dtt[:], dtt[:], AF.Ln, bias=onec[:], scale=1.0)
            fw.tt(atok[:], dtt[:], nea[:], ALU.mult)
            yield
            fw.mm(pt[:, 384:388], TRI, atok[:, j0:j0 + 4])
            fw.mm(pt[:, 392:396], ones, atok[:, j0:j0 + 4])
            negcum, expcum, totE, edec = b["negcum"], b["expcum"], b["totE"], b["edec"]
            fw.ts(negcum[:], pt[:, 384:388], -1.0, ALU.mult)
            fw.act(expcum[:], pt[:, 384:388], AF.Exp)
            fw.act(totE[:], pt[:, 392:396], AF.Exp)
            fw.tt(edec[:], pt[:, 392:396], negcum[:], ALU.add)
            fw.act(edec[:], edec[:], AF.Exp)
            yield
            cb_ = b["cb"]
            for g in range(2):
                gr = slice(g * 64, (g + 1) * 64)
                pcb = (PX, PY)[g]
                fw.mm(pcb[:, d * 128:(d + 1) * 128], xb[gr, 2, :], xb[gr, 3, :])
                fw.cp(cb_[:, g * 128:(g + 1) * 128].k(g), pcb[:, d * 128:(d + 1) * 128], eng="act")
            xd = b["xd"]
            fw.tt(xd[:], xs_[:].r("p (h q) -> p h q", h=4), dtt[:, j0:j0 + 4].un(2).bc([128, 4, 64]), ALU.mult)
            yield
            ST = b["ST"]
            for h in range(4):
                g = h // 2
                gr = slice(g * 64, (g + 1) * 64)
                ar = b["arow"][h % 2]
                fw.act(ar[:], TRI, AF.Copy, scale=atok[:, j0 + h:j0 + h + 1])
                pd = PD[d]
                hc = slice((h % 2) * 128, (h % 2 + 1) * 128)
                fw.mm(pd[:, hc], ones, ar[:], start=True, stop=False)
                fw.mm(pd[:, hc], identb[:], negb[d][:], start=False, stop=True)
                e_ = b["E"][h % 2]
                fw.act(e_[:], pd[:, hc], AF.Exp, bias=negcum[:, h:h + 1])
                yield
                w_ = b["W"][h % 2]
                fw.tt(w_[:], e_[:], cb_[:, g * 128:(g + 1) * 128].k(g), ALU.mult)
                yc = slice(d * 256 + h * 64, d * 256 + (h + 1) * 64)
                pyo = (PX, PY)[g]
                yoc = slice(256 + d * 128 + (h % 2) * 64, 256 + d * 128 + (h % 2 + 1) * 64)
                if need_o:
                    fw.mm(PYD[:, yc], w_[:], xd[:, h, :])
                    fw.mm(pyo[:, yoc], xb[gr, 3, :], ST[gr, h, :].k(h))
                bd = b["bdec"][h % 2]
                fw.ts(bd[:], bt_[:], edec[:, h:h + 1], ALU.mult)
                fw.mm(PSS[:, yc].k((d, h)), bd[:], xd[:, h, :])
                fw.stt(ST[gr, h, :].k(h), ST[gr, h, :].k(h), totE[gr, h:h + 1], PSS[gr, yc].k((d, h)), ALU.mult, ALU.add)
                yield
            if not need_o:
                return
            ydg, y_ = b["ydg"], b["y"]
            fw.cp(ydg[:], PYD[:, d * 256:(d + 1) * 256], eng="act")
            for h in range(4):
                g = h // 2
                pyo = (PX, PY)[g]
                yoc = slice(256 + d * 128 + (h % 2) * 64, 256 + d * 128 + (h % 2 + 1) * 64)
                cs = slice(h * 64, (h + 1) * 64)
                fw.stt(y_[:, cs], pyo[:, yoc], expcum[:, h:h + 1], ydg[:, cs], ALU.mult, ALU.add)
            yield
            if d == 0:
                xsd = b["xsd"]
                fw.tt(xsd[:].r("p (h q) -> p h q", h=4), xs_[:].r("p (h q) -> p h q", h=4),
                      dsk.un(2).bc([128, 4, 64]), ALU.mult)
                fw.tt(y_[:], y_[:], xsd[:], ALU.add)
            fw.dma("pool", yD[d][tok0:tok0 + 128, :].k(("t", tile)), y_[:])
            yield

        of = list(range(34))
        ob = [1, 0] + list(range(33, 1, -1))
        for it in range(34):
            if it >= getattr(self, "sd_limit", 99):
                break
            gens = [tile_gen(0, it, of[it]), tile_gen(1, it, ob[it])]
            alive = [True, True]
            while any(alive):
                for d in range(2):
                    if alive[d]:
                        try:
                            next(gens[d])
                        except StopIteration:
                            alive[d] = False
        fw.barrier()
        yfl = [fw.sb("sd_ryf%d" % i, [128, 256]) for i in range(2)]
        ybl = [fw.sb("sd_ryb%d" % i, [128, 256]) for i in range(2)]
        zin = [fw.sb("sd_rz%d" % i, [128, 2, 128]) for i in range(2)]
        zt = fw.sb("sd_rzt", [128, 256])
        zs = fw.sb("sd_rzs", [128, 256])
        yg = fw.sb("sd_ryg", [128, 256])
        ysq = fw.sb("sd_rysq", [128, 256])
        ss = fw.sb("sd_rss", [128, 1])
        ob_ = [fw.sb("sd_rob%d" % i, [128, 2, 128], BF16) for i in range(2)]
        for tile in range(34):
            if l == DEPTH - 1 and tile < 2:
                continue
            tok0 = tile * 128
            a, b2, zi = yfl[tile % 2], ybl[tile % 2], zin[tile % 2]
            fw.dma("sp", a[:], self.yf[tok0:tok0 + 128, :])
            fw.dma("sp", b2[:], self.yb[tok0:tok0 + 128, :])
            fw.dma("sp", zi[:], uv[:, 22:24, tok0:tok0 + 128])
            pz = self.PS[tile % 2]
            for cc in range(2):
                fw.tr(pz[:, cc * 128:(cc + 1) * 128], zi[:, cc, :], ident)
            fw.cp(zt[:], pz[:, 0:256], eng="act")
            fw.act(zs[:], zt[:], AF.Exp, scale=-1.0)
            fw.ts(zs[:], zs[:], 1.0, ALU.add)
            fw.op("dve", lambda e: e.reciprocal(out=zs[:].ap, in_=zs[:].ap), [zs[:]], [zs[:]])
            fw.tt(zs[:], zs[:], zt[:], ALU.mult)
            fw.tt(yg[:], a[:], b2[:], ALU.add)
            fw.tt(yg[:], yg[:], zs[:], ALU.mult)
            fw.act(ysq[:], yg[:], AF.Square)
            fw.red(ss[:], ysq[:], ALU.add)
            fw.act(ss[:], ss[:], AF.Ln, bias=epsb[:], scale=1.0 / 256.0)
            fw.act(ss[:], ss[:], AF.Exp, scale=-0.5)
            fw.stt(yg[:], yg[:], ss[:], ngb, ALU.mult, ALU.mult)
            po = self.PS[2 + tile % 2]
            for cc in range(2):
                fw.tr(po[:, cc * 128:(cc + 1) * 128], yg[:, cc * 128:(cc + 1) * 128], ident)
            o_ = ob_[tile % 2]
            fw.cp(o_[:], po[:, 0:256].r("p (c t) -> p c t", c=2), eng="act")
            fw.dma("pool", brv[:, 6:8, tok0:tok0 + 128].k(("sd", tile)), o_[:])
        fw.release(m)

    def layernorm_fm(self, x, TB, gname, out, W):
        fw = self.fw
        ones = self.c(C_ONES)
        ps_s, ps_q = self.PS[6], self.PS[7]
        for Dc in range(KC):
            fw.mm(ps_s[:, 0:TB], ones, x[:, Dc, 0:TB], start=(Dc == 0), stop=(Dc == KC - 1))
        for Dc in range(KC):
            sq = W["sq"][Dc % 2]
            fw.act(sq[:, 0:TB], x[:, Dc, 0:TB], AF.Square)
            fw.mm(ps_q[:, 0:TB], ones, sq[:, 0:TB], start=(Dc == 0), stop=(Dc == KC - 1))
        mean, msq, rstd, mr = W["mean"], W["msq"], W["rstd"], W["mr"]
        fw.ts(mean[:, 0:TB], ps_s[:, 0:TB], 1.0 / D, ALU.mult)
        fw.tt(msq[:, 0:TB], mean[:, 0:TB], mean[:, 0:TB], ALU.mult, eng="pool")
        fw.stt(rstd[:, 0:TB], ps_q[:, 0:TB], 1.0 / D, msq[:, 0:TB], ALU.mult, ALU.subtract)
        fw.act(rstd[:, 0:TB], rstd[:, 0:TB], AF.Ln, bias=W["lneps"][:], scale=1.0)
        fw.act(rstd[:, 0:TB], rstd[:, 0:TB], AF.Exp, scale=-0.5)
        fw.tt(mr[:, 0:TB], mean[:, 0:TB], rstd[:, 0:TB], ALU.mult, eng="pool")
        g0 = PP[gname + "g"]
        b0 = PP[gname + "b"]
        for Dc in range(KC):
            t = W["t"][Dc % 2]
            fw.tt(t[:, 0:TB], x[:, Dc, 0:TB], rstd[:, 0:TB], ALU.mult)
            fw.tt(t[:, 0:TB], t[:, 0:TB], mr[:, 0:TB], ALU.subtract, eng="pool")
            fw.act(out[:, Dc, 0:TB].k(Dc), t[:, 0:TB], AF.Identity, bias=self.pp[:, b0 + Dc:b0 + Dc + 1],
                   scale=self.pp[:, g0 + Dc:g0 + Dc + 1])

    def ln_work(self):
        fw = self.fw
        W = {"sq": [fw.sb("ln_sq%d" % i, [128, 512]) for i in range(2)],
             "t": [fw.sb("ln_t%d" % i, [128, 512]) for i in range(2)],
             "mean": fw.sb("ln_mean", [128, 512]), "msq": fw.sb("ln_msq", [128, 512]),
             "rstd": fw.sb("ln_rstd", [128, 512]), "mr": fw.sb("ln_mr", [128, 512]),
             "lneps": fw.sb("ln_eps", [128, 1])}
        fw.memset(W["lneps"][:], LN_EPS)
        return W

    def phase_merge(self, l):
        fw = self.fw
        m = fw.mark()
        ident = self.c(C_IDENT)
        wbr = fw.sb("wbr", [128, 8, D], BF16)
        wo = fw.sb("wo", [128, KC, D], BF16)
        wr = fw.sb("wr", [128, KC, NE])
        m1 = fw.mark()
        stage = [fw.sb("mst%d" % i, [128, 4 * D]) for i in range(2)]
        stb = [fw.sb("mstb%d" % i, [128, 4 * D], BF16) for i in range(2)]
        wgv = self.w_gate_d[l].r("(kc p) n -> p kc n", p=128)
        wov = self.w_o_d[l].r("(kc p) n -> p kc n", p=128)
        wgc5 = self.wgc[:].r("dc p (kc k c) -> p dc kc k c", kc=KC, k=4)
        i = 0
        for kc in range(KC):
            self.load_cast(stb[kc % 2][:], wgv[:, kc, :], stage, 4 * D, i)
            for k in range(4):
                fw.dma("pool", wgc5[:, :, kc, k, :].k(("wgc", kc, k)),
                       stb[kc % 2][:, k * D:(k + 1) * D].r("p (dc c) -> p dc c", dc=8))
            i += 1
        for kc in range(KC):
            self.load_cast(wo[:, kc, :].k(kc), wov[:, kc, :], stage, D, i)
            i += 1
        for k in range(4):
            for cc in range(2):
                self.load_cast(wbr[:, k * 2 + cc, :].k(k * 2 + cc), self.w_br_d[l, k, cc * 128:(cc + 1) * 128, :], stage, D, i)
                i += 1
        fw.dma("sp", wr[:], self.w_router_d[:].r("(kc p) e -> p kc e", p=128))
        fw.release(m1)
        W = self.ln_work()
        rt = [fw.sb("mrt%d" % i, [128, KC, 512]) for i in range(1)]
        ht = [fw.sb("mht%d" % i, [128, KC, 512], BF16) for i in range(1)]
        bt = [fw.sb("mbt%d" % i, [128, 8, 512], BF16) for i in range(1)]
        wgr = [fw.sb("mwg%d" % i, [128, KC, 4, 128], BF16) for i in range(3)]
        yb = fw.sb("myb", [128, KC, 512], BF16)
        gs = [fw.sb("mgs%d" % i, [128, 512]) for i in range(2)]
        yacc = fw.sb("myacc", [128, 512])
        ytmp = [fw.sb("mytmp%d" % i, [128, 512]) for i in range(2)]
        h2 = fw.sb("mh2", [128, KC, 512])
        gT = fw.sb("mgT", [16, 512])
        R = {n: fw.sb("r_" + n, [128, w]) for n, w in
             [("lg", 16), ("mx", 1), ("e", 16), ("ss", 1), ("sc", 16), ("sel", 16), ("m1", 4), ("eq", 16), ("x2", 16),
              ("m2", 4), ("gsc", 4), ("gm", 1), ("ing", 4), ("t2m", 16), ("selm", 16), ("w", 16), ("ws", 1), ("gate", 16)]}
        resv = self.resT[:].r("(kc p) t -> p kc t", p=128)
        l1v = self.lat1T[:].r("(kc p) t -> p kc t", p=128)
        h2v = self.h2T[:].r("(kc p) t -> p kc t", p=128)
        brv = self.brv()
        brb = self.rb[:, RB["brouter"]:RB["brouter"] + 16]
        wi = 0
        for gi, (tok0, TB) in enumerate(GROUPS):
            if gi == 0 and l == DEPTH - 1:
                continue
            md = self.mod_for(gi)
            r, h, br = rt[0], ht[0], bt[0]
            res1 = r
            lat1 = r
            h2b = h
            fw.dma("sp", r[:, :, 0:TB], resv[:, :, tok0:tok0 + TB])
            fw.dma("pool", br[:, :, 0:TB], brv[:, :, tok0:tok0 + TB])
            for kc in range(KC):
                fw.act(h[:, kc, 0:TB].k(kc), r[:, kc, 0:TB].k(kc), AF.Identity, bias=md[:, kc:kc + 1], scale=md[:, 8 + kc:9 + kc])
            for kc in range(KC):
                fw.ts(r[:, kc, 0:TB].k(kc), r[:, kc, 0:TB].k(kc), ALPHA, ALU.mult, eng="pool")
            for Dc in range(KC):
                wg = wgr[wi % 3]
                wi += 1
                fw.dma("sp", wg[:], self.wgc[Dc].r("p (kc k c) -> p kc k c", kc=KC, k=4))
                for k in range(4):
                    pg = self.PS[k % 2]
                    for kc in range(KC):
                        fw.mm(pg[:, 0:TB], wg[:, kc, k, :], h[:, kc, 0:TB],
                              start=(kc == 0), stop=(kc == KC - 1))
                    pp_ = self.PS[2 + k % 2]
                    for cc in range(2):
                        fw.mm(pp_[:, 0:TB], wbr[:, k * 2 + cc, Dc * 128:(Dc + 1) * 128], br[:, k * 2 + cc, 0:TB],
                              start=(cc == 0), stop=(cc == 1))
                    g_ = gs[k % 2]
                    bcol = PP["bgate"] + k * 8 + Dc
                    fw.act(g_[:, 0:TB], pg[:, 0:TB], AF.Sigmoid, bias=self.pp[:, bcol:bcol + 1])
                    if k == 0:
                        fw.tt(yacc[:, 0:TB], g_[:, 0:TB], pp_[:, 0:TB], ALU.mult)
                    else:
                        t_ = ytmp[k % 2]
                        fw.tt(t_[:, 0:TB], g_[:, 0:TB], pp_[:, 0:TB], ALU.mult)
                        dst = yacc[:, 0:TB] if k < 3 else yb[:, Dc, 0:TB].k(Dc)
                        fw.tt(dst, yacc[:, 0:TB], t_[:, 0:TB], ALU.add, eng="pool")
            g1c = 16
            for Dc in range(KC):
                po = self.PS[4 + Dc % 2]
                for kc in range(KC):
                    fw.mm(po[:, 0:TB], wo[:, kc, Dc * 128:(Dc + 1) * 128], yb[:, kc, 0:TB], start=(kc == 0), stop=(kc == KC - 1))
                fw.stt(res1[:, Dc, 0:TB].k(Dc), po[:, 0:TB], md[:, g1c + Dc:g1c + Dc + 1], r[:, Dc, 0:TB].k(Dc), ALU.mult, ALU.add)
            self.layernorm_fm(res1, TB, "ln1", lat1, W)
            fw.dma("pool", l1v[:, :, tok0:tok0 + TB].k(("g", gi)), lat1[:, :, 0:TB])
            for kc in range(KC):
                fw.act(h2[:, kc, 0:TB].k(kc), lat1[:, kc, 0:TB].k(kc), AF.Identity, bias=md[:, 24 + kc:25 + kc], scale=md[:, 32 + kc:33 + kc])
                fw.cp(h2b[:, kc, 0:TB].k(kc), h2[:, kc, 0:TB].k(kc), eng="pool")
            fw.dma("pool", h2v[:, :, tok0:tok0 + TB].k(("g", gi)), h2b[:, :, 0:TB])
            for jt in range(TB // 128):
                pl = self.PS[0]
                for kc in range(KC):
                    fw.mm(pl[:, 0:16], h2[:, kc, jt * 128:(jt + 1) * 128], wr[:, kc, :], start=(kc == 0), stop=(kc == KC - 1))
                fw.cp(R["lg"][:], pl[:, 0:16], eng="act")
                fw.red(R["mx"][:], R["lg"][:], ALU.max)
                fw.ts(R["mx"][:], R["mx"][:], -1.0, ALU.mult)
                fw.act(R["e"][:], R["lg"][:], AF.Exp, bias=R["mx"][:])
                fw.red(R["ss"][:], R["e"][:], ALU.add)
                fw.op("dve", lambda e: e.reciprocal(out=R["ss"][:].ap, in_=R["ss"][:].ap), [R["ss"][:]], [R["ss"][:]])
                fw.ts(R["sc"][:], R["e"][:], R["ss"][:], ALU.mult)
                fw.tt(R["sel"][:], R["sc"][:], brb, ALU.add)
                sel3 = R["sel"][:].r("p (g e) -> p g e", g=4)
                fw.red(R["m1"][:], sel3, ALU.max)
                fw.tt(R["eq"][:].r("p (g e) -> p g e", g=4), sel3, R["m1"][:].un(2).bc([128, 4, 4]), ALU.is_equal)
                fw.stt(R["x2"][:], R["eq"][:], -1e30, R["sel"][:], ALU.mult, ALU.add)
                fw.red(R["m2"][:], R["x2"][:].r("p (g e) -> p g e", g=4), ALU.max)
                fw.tt(R["gsc"][:], R["m1"][:], R["m2"][:], ALU.add)
                fw.red(R["gm"][:], R["gsc"][:], ALU.max)
                fw.ts(R["ing"][:], R["gsc"][:], R["gm"][:], ALU.is_ge)
                fw.tt(R["t2m"][:].r("p (g e) -> p g e", g=4), sel3, R["m2"][:].un(2).bc([128, 4, 4]), ALU.is_ge)
                fw.tt(R["selm"][:].r("p (g e) -> p g e", g=4), R["t2m"][:].r("p (g e) -> p g e", g=4),
                      R["ing"][:].un(2).bc([128, 4, 4]), ALU.mult)
                fw.tt(R["w"][:], R["sc"][:], R["selm"][:], ALU.mult)
                fw.red(R["ws"][:], R["w"][:], ALU.add)
                fw.op("dve", lambda e: e.reciprocal(out=R["ws"][:].ap, in_=R["ws"][:].ap), [R["ws"][:]], [R["ws"][:]])
                fw.ts(R["gate"][:], R["w"][:], R["ws"][:], ALU.mult)
                pt = self.PS[1]
                fw.tr(pt[0:16, 0:128], R["gate"][:], ident)
                fw.cp(gT[:, jt * 128:(jt + 1) * 128].k(jt), pt[0:16, 0:128], eng="act")
            fw.dma("pool", self.gateT[:, tok0:tok0 + TB].k(("g", gi)), gT[:, 0:TB])
        fw.release(m)

    def phase_wcast(self, l):
        fw = self.fw
        m = fw.mark()
        st = [fw.sb("wc_s%d" % i, [128, 4096]) for i in range(3)]
        sb_ = [fw.sb("wc_b%d" % i, [128, 4096], BF16) for i in range(3)]
        i = 0
        for e in range(NE):
            for (src, dst, pat) in ((self.w_e1_d, self.we1b, "(kc p) f -> p kc f"),
                                    (self.w_e3_d, self.we3b, "(kc p) f -> p kc f"),
                                    (self.w_e2_d, self.we2b, "(fc p) d -> p fc d")):
                a, b = st[i % 3], sb_[i % 3]
                n0 = 8 if src is not self.w_e2_d else 4
                fw.dma("sp", a[:].r("p (a b) -> p a b", a=n0), src[l, e].r(pat, p=128))
                fw.cp(b[:], a[:], eng=("dve", "act", "pool")[i % 3])
                fw.dma("pool", dst[e].r(pat, p=128).k(("e", e)), b[:].r("p (a b) -> p a b", a=n0))
                i += 1
        fw.release(m)

    def phase_moe(self, l):
        fw = self.fw
        m = fw.mark()
        ident = self.c(C_IDENT)
        W = self.ln_work()
        last = (l == DEPTH - 1)
        h2t = [fw.sb("e_h2%d" % i, [128, KC, 512], BF16) for i in range(2)]
        l1t = [fw.sb("e_l1%d" % i, [128, KC, 512]) for i in range(2)]
        gTt = [fw.sb("e_gT%d" % i, [16, 512]) for i in range(2)]
        w1r = [fw.sb("e_w1%d" % i, [128, KC, DFF], BF16) for i in range(2)]
        w3r = [fw.sb("e_w3%d" % i, [128, KC, DFF], BF16) for i in range(2)]
        w2r = [fw.sb("e_w2%d" % i, [128, 4, D], BF16) for i in range(2)]
        gb = [fw.sb("e_gb%d" % i, [128, 512]) for i in range(2)]
        s1 = [fw.sb("e_s1%d" % i, [128, 512]) for i in range(2)]
        tq = [fw.sb("e_t%d" % i, [128, 512]) for i in range(2)]
        ab = [fw.sb("e_a%d" % i, [128, 4, 512], BF16) for i in range(2)]
        accs = [fw.sb("e_acc%d" % i, [128, KC, 512]) for i in range(2)]
        otok = [fw.sb("e_ot%d" % i, [128, D]) for i in range(2)]
        resv = self.resT[:].r("(kc p) t -> p kc t", p=128)
        l1v = self.lat1T[:].r("(kc p) t -> p kc t", p=128)
        h2v = self.h2T[:].r("(kc p) t -> p kc t", p=128)
        ei = 0
        pending = None
        loaded = False
        for gi, (tok0, TB) in enumerate(GROUPS):
            if gi == 0 and last:
                continue
            md = self.mod_for(gi)
            h2, l1, gT = h2t[gi % 2], l1t[gi % 2], gTt[gi % 2]
            acc = accs[gi % 2]

            def group_loads(gj):
                t0_, TB_ = GROUPS[gj]
                fw.dma("sp", h2t[gj % 2][:, :, 0:TB_], h2v[:, :, t0_:t0_ + TB_])
                fw.dma("sp", l1t[gj % 2][:, :, 0:TB_], l1v[:, :, t0_:t0_ + TB_])
                fw.dma("sp", gTt[gj % 2][:, 0:TB_], self.gateT[:, t0_:t0_ + TB_])
            if not loaded:
                group_loads(gi)
                loaded = True
            for e in range(NE):
                w1, w3, w2 = w1r[ei % 2], w3r[ei % 2], w2r[ei % 2]
                fw.dma("sp", w1[:], self.we1b[e].r("(kc p) f -> p kc f", p=128))
                fw.dma("pool", w3[:], self.we3b[e].r("(kc p) f -> p kc f", p=128))
                fw.dma("sp", w2[:], self.we2b[e].r("(fc p) d -> p fc d", p=128))
                pgb = self.PS[6]
                fw.mm(pgb[:, 0:TB], self.cst[0:16, C_SEL + e * 128:C_SEL + (e + 1) * 128], gT[:, 0:TB])
                g_ = gb[ei % 2]
                fw.cp(g_[:, 0:TB], pgb[:, 0:TB], eng="act")
                a_ = ab[ei % 2]
                for f in range(4):
                    p1 = self.PS[f % 2]
                    p3 = self.PS[2 + f % 2]
                    for kc in range(KC):
                        fw.mm(p1[:, 0:TB], w1[:, kc, f * 128:(f + 1) * 128], h2[:, kc, 0:TB], start=(kc == 0), stop=(kc == KC - 1))
                    for kc in range(KC):
                        fw.mm(p3[:, 0:TB], w3[:, kc, f * 128:(f + 1) * 128], h2[:, kc, 0:TB], start=(kc == 0), stop=(kc == KC - 1))
                    s_ = s1[f % 2]
                    fw.act(s_[:, 0:TB], p1[:, 0:TB], AF.Silu)
                    t_ = tq[f % 2]
                    fw.tt(t_[:, 0:TB], s_[:, 0:TB], p3[:, 0:TB], ALU.mult)
                    fw.tt(a_[:, f, 0:TB].k(f), t_[:, 0:TB], g_[:, 0:TB], ALU.mult, eng="pool")
                for Dc in range(KC):
                    po = self.PS[4 + Dc % 2]
                    for f in range(4):
                        fw.mm(po[:, 0:TB], w2[:, f, Dc * 128:(Dc + 1) * 128], a_[:, f, 0:TB], start=(f == 0), stop=(f == 3))
                    if e == 0:
                        fw.cp(acc[:, Dc, 0:TB].k(Dc), po[:, 0:TB], eng="act")
                    else:
                        fw.tt(acc[:, Dc, 0:TB].k(Dc), acc[:, Dc, 0:TB].k(Dc), po[:, 0:TB], ALU.add)
                ei += 1
                if e == 1:
                    if pending is not None:
                        pending()
                        pending = None
                    if gi + 1 < len(GROUPS):
                        group_loads(gi + 1)

            def epilogue(gi=gi, tok0=tok0, TB=TB, md=md, acc=acc, l1=l1):
                res2 = acc
                lat2 = acc
                g2c = 40
                for Dc in range(KC):
                    fw.ts(l1[:, Dc, 0:TB].k(Dc), l1[:, Dc, 0:TB].k(Dc), ALPHA, ALU.mult, eng="pool")
                    fw.stt(res2[:, Dc, 0:TB].k(Dc), acc[:, Dc, 0:TB].k(Dc), md[:, g2c + Dc:g2c + Dc + 1], l1[:, Dc, 0:TB].k(Dc),
                           ALU.mult, ALU.add)
                self.layernorm_fm(res2, TB, "ln2", lat2, W)
                if not last:
                    fw.dma("pool", resv[:, :, tok0:tok0 + TB].k(("g", gi)), lat2[:, :, 0:TB])
                if last and gi > 0:
                    for jt in range(TB // 128):
                        o_ = otok[jt % 2]
                        for half in range(2):
                            ps = self.PS[6 + half]
                            for q in range(4):
                                kc = half * 4 + q
                                fw.tr(ps[:, q * 128:(q + 1) * 128], lat2[:, kc, jt * 128:(jt + 1) * 128], ident)
                            fw.cp(o_[:, half * 512:(half + 1) * 512].k(half), ps[:, 0:512], eng=("dve" if half == 0 else "act"))
                        r0 = tok0 - LC + jt * 128
                        fw.dma("sp", self.out_d[r0:r0 + 128, :].k(("o", r0)), o_[:])
            pending = epilogue
        if pending is not None:
            pending()
        fw.release(m)


def _core_inputs(inp, b, shared, consts):
    cc = np.empty((128, KC, 2), np.float32)
    cc[:, :, 0] = np.asarray(inp["c"][b], np.float32).reshape(KC, 128).T
    cc[:, :, 1] = np.asarray(inp["c_ctx"], np.float32).reshape(KC, 128).T
    m = {"x": np.ascontiguousarray(inp["x"][b], np.float32),
         "ctx": np.ascontiguousarray(inp["ctx"][b], np.float32),
         "cc": cc.reshape(128, KC * 2)}
    m.update(consts)
    m.update(shared)
    return m


_PROG = {}


def kernel(**inputs):
    if "p" not in _PROG:
        _PROG["p"] = Prog()
    prog = _PROG["p"]
    consts = _constants()
    shared = _shared_inputs(inputs)
    n = 8
    in_maps = [_core_inputs(inputs, b, shared, consts) for b in range(n)]
    res = run_bass_kernel_spmd(prog.nc, in_maps, core_ids=list(range(n)))
    return np.stack([np.asarray(r["out"], np.float32) for r in res.results], 0)
```
